# Optimizing a Trainium2 kernel written in Bass

```python
import jax, jax.numpy as jnp
from jax import lax
import numpy as np

D_MODEL = 1024
BATCH = 8
SEQ = 4096
DEPTH = 1

ATT_HEADS = 8
ATT_KV_HEADS = 2
ATT_HEAD_DIM = 64
IDX_HEADS = 8
IDX_HEAD_DIM = 64
TOPK_MAX = 256
Q_BLOCK = 128
DN_HEADS = 4
DN_HEAD_DIM = 128
DN_CONV = 4
DN_CHUNK = 64
PEER_HEADS = 8
PEER_N_KEYS = 128
PEER_N_EXPERTS = PEER_N_KEYS * PEER_N_KEYS
PEER_KEY_DIM = 256
PEER_TOPK = 16
PEER_TOKEN_BLOCK = 128
EPS = 1e-6

ATT_Q_W = ATT_HEADS * ATT_HEAD_DIM
ATT_KV_W = ATT_KV_HEADS * ATT_HEAD_DIM
IDX_Q_W = IDX_HEADS * IDX_HEAD_DIM
DN_W = DN_HEADS * DN_HEAD_DIM
IN_SPLITS = (ATT_Q_W, ATT_KV_W, ATT_KV_W,
             IDX_Q_W, IDX_HEAD_DIM, IDX_HEADS,
             DN_W, DN_W, DN_W, DN_W,
             DN_HEADS, DN_HEADS,
             D_MODEL, D_MODEL)
IN_WIDTH = sum(IN_SPLITS)

kernel_name = 'hybrid_dsa_gdn_peer_block'


def rms_norm(x, gain):
    xf = x.astype(jnp.float32)
    y = xf * lax.rsqrt(jnp.mean(xf * xf, axis=-1, keepdims=True) + EPS)
    return (y * gain.astype(jnp.float32)).astype(x.dtype)


def l2_norm(x):
    return x * lax.rsqrt(jnp.sum(x * x, axis=-1, keepdims=True) + EPS)


def causal_depthwise_conv(x, w):
    k_width, chans = w.shape
    return lax.conv_general_dilated(x, w[:, None, :], window_strides=(1,), padding=[(k_width - 1, 0)],
                                    dimension_numbers=('NWC', 'WIO', 'NWC'), feature_group_count=chans)


def dsa_attention(q, k, v, iq, ik, iw, k_sel):
    f32 = jnp.float32
    b, t = q.shape[:2]
    nb = t // Q_BLOCK
    grp = ATT_HEADS // ATT_KV_HEADS
    scale = ATT_HEAD_DIM ** -0.5
    key_pos = jnp.arange(t)
    ik32 = ik.astype(f32)

    def to_blocks(a):
        return jnp.moveaxis(a.reshape((b, nb, Q_BLOCK) + a.shape[2:]), 1, 0)

    def block(args):
        qb, iqb, iwb, start = args
        qpos = start + jnp.arange(Q_BLOCK)
        causal = key_pos[None, :] <= qpos[:, None]
        dots = jnp.einsum('bqhd,bsd->bqhs', iqb.astype(f32), ik32)
        score = jnp.einsum('bqh,bqhs->bqs', iwb.astype(f32), jax.nn.relu(dots))
        score = jnp.where(causal[None], score, -jnp.inf)
        _, sel = lax.top_k(score, k_sel)
        valid = sel <= qpos[None, :, None]
        k_g = jax.vmap(lambda kb, ib: kb[ib])(k, sel)
        v_g = jax.vmap(lambda vb, ib: vb[ib])(v, sel)
        qg = qb.reshape(b, Q_BLOCK, ATT_KV_HEADS, grp, ATT_HEAD_DIM).astype(f32)
        logits = jnp.einsum('bqhgd,bqkhd->bqhgk', qg, k_g.astype(f32)) * scale
        logits = jnp.where(valid[:, :, None, None, :], logits, -1e30)
        p = jax.nn.softmax(logits, axis=-1)
        o = jnp.einsum('bqhgk,bqkhd->bqhgd', p, v_g.astype(f32))
        return o.reshape(b, Q_BLOCK, ATT_Q_W).astype(qb.dtype)

    starts = jnp.arange(nb) * Q_BLOCK
    out = lax.map(block, (to_blocks(q), to_blocks(iq), to_blocks(iw), starts))
    return jnp.moveaxis(out, 0, 1).reshape(b, t, ATT_Q_W)


def gated_deltanet(q, k, v, g, beta):
    f32 = jnp.float32
    out_dtype = q.dtype
    b, t, h, d = q.shape
    c = DN_CHUNK
    n = t // c
    q = l2_norm(q.astype(f32)) * (d ** -0.5)
    k = l2_norm(k.astype(f32))
    v = v.astype(f32)

    def chunk(a):
        a = a.reshape((b, n, c, h) + a.shape[3:])
        return jnp.moveaxis(a, 3, 1)

    q, k, v = chunk(q), chunk(k), chunk(v)
    g = jnp.cumsum(chunk(g.astype(f32)), axis=-1)
    beta = chunk(beta.astype(f32))
    tri = jnp.tril(jnp.ones((c, c), bool))
    strict = jnp.tril(jnp.ones((c, c), bool), -1)
    decay = jnp.exp(jnp.where(tri, g[..., :, None] - g[..., None, :], -jnp.inf))
    k_beta = k * beta[..., None]
    a_mat = jnp.where(strict, jnp.einsum('bhnid,bhnjd->bhnij', k_beta, k) * decay, 0.0)
    rhs = jnp.concatenate([v * beta[..., None], k_beta * jnp.exp(g)[..., None]], axis=-1)
    sol = lax.linalg.triangular_solve(a_mat, rhs, left_side=True, lower=True, unit_diagonal=True)
    u, w = sol[..., :d], sol[..., d:]
    intra = jnp.where(tri, jnp.einsum('bhnid,bhnjd->bhnij', q, k) * decay, 0.0)

    def step(state, inp):
        q_i, k_i, u_i, w_i, g_i, att_i = inp
        v_new = u_i - jnp.einsum('bhcd,bhde->bhce', w_i, state)
        o = (jnp.einsum('bhcd,bhde->bhce', q_i * jnp.exp(g_i)[..., None], state)
             + jnp.einsum('bhij,bhje->bhie', att_i, v_new))
        g_last = g_i[..., -1]
        state = (state * jnp.exp(g_last)[..., None, None]
                 + jnp.einsum('bhcd,bhce->bhde', k_i * jnp.exp(g_last[..., None] - g_i)[..., None], v_new))
        return state, o

    xs = tuple(jnp.moveaxis(a, 2, 0) for a in (q, k, u, w, g, intra))
    state0 = jnp.zeros((b, h, d, d), f32)
    _, o = lax.scan(step, state0, xs)
    o = jnp.moveaxis(o, 0, 2).reshape(b, h, t, d).transpose(0, 2, 1, 3)
    return o.astype(out_dtype)


def peer(hn, w_query, sub_keys, u_tab, v_tab):
    f32 = jnp.float32
    b, t, dm = hn.shape
    ntok = b * t
    hf = hn.reshape(ntok, dm)
    q = (hf @ w_query).reshape(ntok, PEER_HEADS, 2, PEER_KEY_DIM // 2)
    s = jnp.einsum('nhpd,hpkd->nhpk', q.astype(f32), sub_keys.astype(f32))
    top_s, top_i = lax.top_k(s, PEER_TOPK)
    cand_s = (top_s[:, :, 0, :, None] + top_s[:, :, 1, None, :]).reshape(ntok, PEER_HEADS, -1)
    cand_i = (top_i[:, :, 0, :, None] * PEER_N_KEYS + top_i[:, :, 1, None, :]).reshape(ntok, PEER_HEADS, -1)
    best_s, pos = lax.top_k(cand_s, PEER_TOPK)
    expert = jnp.take_along_axis(cand_i, pos, axis=-1)
    gate = jax.nn.softmax(best_s, axis=-1)
    nblk = ntok // PEER_TOKEN_BLOCK

    def block(args):
        hb, eb, gb = args
        act = jax.nn.gelu(jnp.einsum('nhkd,nd->nhk', u_tab[eb], hb).astype(f32), approximate=False)
        coef = (gb * act).astype(hb.dtype)
        return jnp.einsum('nhk,nhkd->nd', coef, v_tab[eb])

    out = lax.map(block, (hf.reshape(nblk, PEER_TOKEN_BLOCK, dm),
                          expert.reshape(nblk, PEER_TOKEN_BLOCK, PEER_HEADS, PEER_TOPK),
                          gate.reshape(nblk, PEER_TOKEN_BLOCK, PEER_HEADS, PEER_TOPK)))
    return out.reshape(b, t, dm)


def setup_inputs(seed: int = 0) -> dict:
    key = jax.random.key(seed)
    ks = jax.random.split(key, 17)
    f32 = jnp.float32
    nl = DEPTH

    def nrm(k, shape, scale):
        return jax.random.normal(k, shape, f32) * scale

    return {
        'x': nrm(ks[0], (BATCH, SEQ, D_MODEL), 1.0),
        'norm1_gain': 1.0 + nrm(ks[1], (nl, D_MODEL), 0.01),
        'w_in': nrm(ks[2], (nl, D_MODEL, IN_WIDTH), D_MODEL ** -0.5),
        'q_norm_gain': 1.0 + nrm(ks[3], (nl, ATT_HEAD_DIM), 0.01),
        'k_norm_gain': 1.0 + nrm(ks[4], (nl, ATT_HEAD_DIM), 0.01),
        'dn_conv_w': nrm(ks[5], (nl, DN_CONV, 3 * DN_W), DN_CONV ** -0.5),
        'dn_a_log': jnp.log(jax.random.uniform(ks[6], (nl, DN_HEADS), f32, 1.0, 16.0)),
        'dn_dt_bias': nrm(ks[7], (nl, DN_HEADS), 0.1),
        'dn_out_norm_gain': 1.0 + nrm(ks[8], (nl, DN_HEAD_DIM), 0.01),
        'w_att_branch': nrm(ks[9], (nl, ATT_Q_W, D_MODEL), ATT_Q_W ** -0.5),
        'w_dn_branch': nrm(ks[10], (nl, DN_W, D_MODEL), DN_W ** -0.5),
        'w_o': nrm(ks[11], (nl, D_MODEL, D_MODEL), D_MODEL ** -0.5),
        'norm2_gain': 1.0 + nrm(ks[12], (nl, D_MODEL), 0.01),
        'peer_w_query': nrm(ks[13], (nl, D_MODEL, PEER_HEADS * PEER_KEY_DIM), D_MODEL ** -0.5),
        'peer_sub_keys': nrm(ks[14], (nl, PEER_HEADS, 2, PEER_N_KEYS, PEER_KEY_DIM // 2), (PEER_KEY_DIM // 2) ** -0.5),
        'peer_u': nrm(ks[15], (nl, PEER_N_EXPERTS, D_MODEL), D_MODEL ** -0.5),
        'peer_v': nrm(ks[16], (nl, PEER_N_EXPERTS, D_MODEL), PEER_HEADS ** -0.5),
    }


def reference(x, norm1_gain, w_in, q_norm_gain, k_norm_gain, dn_conv_w, dn_a_log, dn_dt_bias,
              dn_out_norm_gain, w_att_branch, w_dn_branch, w_o, norm2_gain, peer_w_query,
              peer_sub_keys, peer_u, peer_v):
    b, t, _ = x.shape
    k_sel = min(TOPK_MAX, t // 4)
    cuts = np.cumsum(IN_SPLITS)[:-1].tolist()
    for layer in range(DEPTH):
        h = rms_norm(x, norm1_gain[layer])
        proj = h @ w_in[layer]
        (aq, ak, av, iq, ik, iw, dq, dk, dv, dz, da, db, ga, gb) = jnp.split(proj, cuts, axis=-1)

        aq = rms_norm(aq.reshape(b, t, ATT_HEADS, ATT_HEAD_DIM), q_norm_gain[layer])
        ak = rms_norm(ak.reshape(b, t, ATT_KV_HEADS, ATT_HEAD_DIM), k_norm_gain[layer])
        av = av.reshape(b, t, ATT_KV_HEADS, ATT_HEAD_DIM)
        iq = iq.reshape(b, t, IDX_HEADS, IDX_HEAD_DIM)
        y_att = dsa_attention(aq, ak, av, iq, ik, iw, k_sel)

        qkv = jax.nn.silu(causal_depthwise_conv(jnp.concatenate([dq, dk, dv], axis=-1), dn_conv_w[layer]))
        dq, dk, dv = jnp.split(qkv, 3, axis=-1)
        shp = (b, t, DN_HEADS, DN_HEAD_DIM)
        decay = -jnp.exp(dn_a_log[layer].astype(jnp.float32)) * jax.nn.softplus(
            da.astype(jnp.float32) + dn_dt_bias[layer].astype(jnp.float32))
        beta = jax.nn.sigmoid(db.astype(jnp.float32))
        o_dn = gated_deltanet(dq.reshape(shp), dk.reshape(shp), dv.reshape(shp), decay, beta)
        y_dn = (rms_norm(o_dn, dn_out_norm_gain[layer]) * jax.nn.silu(dz.reshape(shp))).reshape(b, t, DN_W)

        merged = (jax.nn.sigmoid(ga) * (y_att @ w_att_branch[layer])
                  + jax.nn.sigmoid(gb) * (y_dn @ w_dn_branch[layer]))
        x = x + merged @ w_o[layer]

        h2 = rms_norm(x, norm2_gain[layer])
        x = x + peer(h2, peer_w_query[layer], peer_sub_keys[layer], peer_u[layer], peer_v[layer])
    return x
```

```python
import contextlib
import numpy as np
import concourse.bass as bass
import concourse.mybir as mybir
from concourse.bass_utils import run_bass_kernel_spmd

F32 = mybir.dt.float32
BF16 = mybir.dt.bfloat16
I32 = mybir.dt.int32
U32 = mybir.dt.uint32
AF = mybir.ActivationFunctionType
ALU = mybir.AluOpType
AX = mybir.AxisListType

T = 4096
D = 1024
NT = T // 128
IN_W = 5456
EPS = 1e-6
N_CORES = 8

C_AQ, C_AK, C_AV, C_IQ, C_IK, C_IW = 0, 512, 640, 768, 1280, 1344
C_DQ, C_DK, C_DV, C_DZ, C_DA, C_DB, C_GA, C_GB = 1352, 1864, 2376, 2888, 3400, 3404, 3408, 4432


class Res:
    __slots__ = ("name", "writer", "readers")

    def __init__(self, name):
        self.name = name
        self.writer = None
        self.readers = {}


class Tl:
    def __init__(self, t, name):
        self.t = t.ap() if hasattr(t, "ap") and "DRam" in type(t).__name__ else t
        self.r = Res(name)

    def __getitem__(self, idx):
        return self.t[idx]


class Sched:
    CE = ("tensor", "vector", "scalar", "gpsimd")

    def __init__(self, nc, st, n_dma=12):
        self.nc = nc
        self.st = st
        self.eng = {n: getattr(nc, n) for n in self.CE + ("sync",)}
        self.sem = {}
        self.cnt = {}
        for n in self.CE:
            self.sem[n] = st.enter_context(nc.semaphore("s_" + n))
            self.cnt[n] = 0
        self.dq = {}
        for q in ("sync", "gpsimd"):
            sems = [st.enter_context(nc.semaphore("d_%s_%d" % (q, i))) for i in range(n_dma)]
            for i, s in enumerate(sems):
                self.sem[(q, i)] = s
                self.cnt[(q, i)] = 0
            self.dq[q] = [0, n_dma]
        self.seen = {}
        self.ninst = 0

    def _wait(self, E, key, val):
        if val <= 0:
            return
        if E == "tensor" and key == "tensor":
            return
        k = (E, key)
        if self.seen.get(k, 0) >= val:
            return
        self.eng[E].wait_ge(self.sem[key], val)
        self.seen[k] = val
        self.ninst += 1

    def _deps(self, E, reads, writes):
        for r in reads:
            r = r.r if isinstance(r, Tl) else r
            if r.writer is not None:
                self._wait(E, *r.writer)
        for w in writes:
            w = w.r if isinstance(w, Tl) else w
            if w.writer is not None:
                self._wait(E, *w.writer)
            for key, val in w.readers.items():
                self._wait(E, key, val)

    def _mark(self, ev, reads, writes):
        key, val = ev
        for r in reads:
            r = r.r if isinstance(r, Tl) else r
            r.readers[key] = val
        for w in writes:
            w = w.r if isinstance(w, Tl) else w
            w.writer = ev
            w.readers = {}

    def op(self, E, emit, reads=(), writes=()):
        self._deps(E, reads, writes)
        inst = emit(self.eng[E])
        self.cnt[E] += 1
        inst.then_inc(self.sem[E], 1)
        self.ninst += 1
        self._mark((E, self.cnt[E]), reads, writes)

    def dma(self, q, out, in_, reads=(), writes=(), indirect=None, **kw):
        st = self.dq[q]
        i = st[0]
        st[0] = (i + 1) % st[1]
        key = (q, i)
        self._wait(q, key, self.cnt[key])
        self._deps(q, reads, writes)
        if indirect is not None:
            inst = self.eng[q].indirect_dma_start(out=out, out_offset=None, in_=in_, in_offset=indirect, **kw)
        else:
            inst = self.eng[q].dma_start(out=out, in_=in_, **kw)
        self.cnt[key] += 16
        inst.then_inc(self.sem[key], 16)
        self.ninst += 1
        self._mark((key, self.cnt[key]), reads, writes)

    def barrier(self):
        for E in self.CE + ("sync",):
            for key, val in self.cnt.items():
                if key == E:
                    continue
                if E == "tensor" and key == "tensor":
                    continue
                self._wait(E, key, val)

    def finish(self):
        for E in self.CE + ("sync",):
            for key, val in self.cnt.items():
                if key == E:
                    continue
                k = (E, key)
                if val > 0 and self.seen.get(k, 0) < val:
                    self.eng[E].wait_ge(self.sem[key], val)
                    self.seen[k] = val


class Ctx:
    pass


def build_program(debug=False, phases=("A", "B", "C", "P", "D"), **opts):
    nc = bass.Bass("TRN2", target_bir_lowering=False)
    g = Ctx()
    for k_, v_ in opts.items():
        setattr(g, k_, v_)
    g.nc = nc
    g.debug = debug

    def din(name, shape, dt=F32):
        return nc.dram_tensor(name, list(shape), dt, kind="ExternalInput")

    g.x = din("x", [T, D])
    g.norm1_gain = din("norm1_gain", [1, D])
    g.w_in = din("w_in", [D, IN_W])
    g.q_norm_gain = din("q_norm_gain", [1, 64])
    g.k_norm_gain = din("k_norm_gain", [1, 64])
    g.dn_conv_w = din("dn_conv_w", [4, 1536])
    g.dn_a_log = din("dn_a_log", [1, 4])
    g.dn_dt_bias = din("dn_dt_bias", [1, 4])
    g.dn_out_norm_gain = din("dn_out_norm_gain", [1, 128])
    g.w_att_branch = din("w_att_branch", [512, D])
    g.w_dn_branch = din("w_dn_branch", [512, D])
    g.w_o = din("w_o", [D, D])
    g.norm2_gain = din("norm2_gain", [1, D])
    g.peer_w_query = din("peer_w_query", [D, 2048])
    g.peer_sub_keys = din("peer_sub_keys", [16, 128, 128])
    g.peer_u = din("peer_u", [16384, D])
    g.peer_v = din("peer_v", [16384, D])
    g.out = nc.dram_tensor("out", [T, D], F32, kind="ExternalOutput")

    skind = "ExternalOutput" if debug else "Internal"

    def dscr(name, shape, dt):
        return Tl(nc.dram_tensor(name, list(shape), dt, kind=skind), name)

    g.qT_s = dscr("qT_s", [NT, 64, 8, 128], BF16)
    g.iqT_s = dscr("iqT_s", [NT, 64, 8, 128], BF16)
    g.kT_s = dscr("kT_s", [64, 2, T], BF16)
    g.ikT_s = dscr("ikT_s", [64, T], BF16)
    g.v_s = dscr("v_s", [T, 128], BF16)
    g.iw_s = dscr("iw_s", [128, NT, 8], F32)
    g.gb_s = dscr("gb_s", [128, NT, 8], F32)
    g.dnq_s = dscr("dnq_s", [T, 512], F32)
    g.dnk_s = dscr("dnk_s", [T, 512], F32)
    g.dnv_s = dscr("dnv_s", [T, 512], F32)
    g.dz_s = dscr("dz_s", [T, 512], BF16)
    g.gate_s = dscr("gate_s", [T, 2048], BF16)
    g.yatt_s = dscr("yatt_s", [T, 512], BF16)
    g.ydn_s = dscr("ydn_s", [T, 512], BF16)
    g.ub_s = Tl(nc.dram_tensor("ub_s", [16384, D], BF16, kind="Internal"), "ub_s")
    g.vb_s = Tl(nc.dram_tensor("vb_s", [16384, D], BF16, kind="Internal"), "vb_s")

    with contextlib.ExitStack() as st:
        S = Sched(nc, st)
        g.S = S
        g.st = st
        g.banks = [Tl(st.enter_context(nc.psum_tensor("bank%d" % i, [128, 512], F32)), "bank%d" % i)
                   for i in range(8)]
        g.bank_rr = 0
        setup_consts(g)
        if "A" in phases:
            phase_a(g)
        if "B" in phases:
            phase_b(g)
        if "C" in phases:
            phase_c(g)
        if "P" in phases:
            phase_p(g)
        if "D" in phases:
            phase_de(g)
        S.finish()
    return nc


def next_bank(g):
    n = getattr(g, "nrot", 8)
    g.bank_rr = g.bank_rr % n
    b = g.banks[g.bank_rr]
    g.bank_rr = (g.bank_rr + 1) % n
    return b


def sb(g, st, name, shape, dt):
    g.uid = getattr(g, "uid", 0) + 1
    name = "%s_%d" % (name, g.uid)
    return Tl(st.enter_context(g.nc.sbuf_tensor(name, list(shape), dt)), name)


def setup_consts(g):
    nc, S, st = g.nc, g.S, g.st
    g.fill0 = nc.gpsimd.to_reg(0.0)
    g.fillneg = nc.gpsimd.to_reg(-1e30)
    g.ones_f = sb(g, st, "ones_f", [128, 128], F32)
    g.ident_f = sb(g, st, "ident_f", [128, 128], F32)
    g.ident_b = sb(g, st, "ident_b", [128, 128], BF16)
    g.eps_t = sb(g, st, "eps_t", [128, 1], F32)
    S.op("gpsimd", lambda e: e.memset(g.ones_f[:], 1.0), writes=[g.ones_f])
    S.op("gpsimd", lambda e: e.memset(g.eps_t[:], EPS), writes=[g.eps_t])
    S.op("gpsimd", lambda e: e.affine_select(out=g.ident_f[:], in_=g.ones_f[:], pattern=[[-1, 128]],
                                             compare_op=ALU.is_equal, fill=g.fill0, base=0, channel_multiplier=1),
         reads=[g.ones_f], writes=[g.ident_f])
    S.op("vector", lambda e: e.tensor_copy(out=g.ident_b[:], in_=g.ident_f[:]), reads=[g.ident_f], writes=[g.ident_b])


def bcast_row(ap_row, n):
    return ap_row.partition_broadcast(n)


def rstd_op(g, out, ss, scale, n_free, tmp):
    S = g.S
    S.op("scalar", lambda e: e.activation(out=tmp[:, 0:n_free], in_=ss[:, 0:n_free], func=AF.Sqrt,
                                          bias=g.eps_t[:, 0:1], scale=scale),
         reads=[ss, g.eps_t], writes=[tmp])
    S.op("vector", lambda e: e.reciprocal(out=out[:, 0:n_free], in_=tmp[:, 0:n_free]), reads=[tmp], writes=[out])


def phase_a(g):
    nc, S = g.nc, g.S
    with contextlib.ExitStack() as ph:
        def A(name, shape, dt):
            return sb(g, ph, name, shape, dt)

        w_bf = A("w_bf", [128, 8, IN_W], BF16)
        WCH = 1364
        wst = [A("wst%d" % i, [128, WCH], F32) for i in range(2)]
        w_in_v = g.w_in.ap().rearrange("(kc p) n -> p kc n", p=128)
        k = 0
        for kc in range(8):
            for c in range(IN_W // WCH):
                s_ = wst[k % 2]
                S.dma("sync", s_[:], w_in_v[:, kc, c * WCH:(c + 1) * WCH], writes=[s_])
                eng = ("vector", "gpsimd", "scalar")[k % 3]
                dst = w_bf[:, kc, c * WCH:(c + 1) * WCH]
                if eng == "scalar":
                    S.op(eng, lambda e: e.copy(out=dst, in_=s_[:]), reads=[s_], writes=[w_bf])
                else:
                    S.op(eng, lambda e: e.tensor_copy(out=dst, in_=s_[:]), reads=[s_], writes=[w_bf])
                k += 1

        g1_bc = A("g1_bc", [128, D], F32)
        S.dma("sync", g1_bc[:], g.norm1_gain.ap().partition_broadcast(128), writes=[g1_bc])
        gq_bc = A("gq_bc", [128, 64], F32)
        gk_bc = A("gk_bc", [128, 64], F32)
        S.dma("sync", gq_bc[:], g.q_norm_gain.ap().partition_broadcast(128), writes=[gq_bc])
        S.dma("sync", gk_bc[:], g.k_norm_gain.ap().partition_broadcast(128), writes=[gk_bc])
        S.op("vector", lambda e: e.tensor_scalar(out=gq_bc[:], in0=gq_bc[:], scalar1=0.125, scalar2=None, op0=ALU.mult),
             reads=[gq_bc], writes=[gq_bc])
        cw_bc = A("cw_bc", [128, 4, 1536], F32)
        for j in range(4):
            S.dma("sync", cw_bc[:, j, :], g.dn_conv_w.ap()[j:j + 1, :].partition_broadcast(128), writes=[cw_bc])
        dtb_bc = A("dtb_bc", [128, 4], F32)
        nea_bc = A("nea_bc", [128, 4], F32)
        S.dma("sync", dtb_bc[:], g.dn_dt_bias.ap().partition_broadcast(128), writes=[dtb_bc])
        S.dma("sync", nea_bc[:], g.dn_a_log.ap().partition_broadcast(128), writes=[nea_bc])
        S.op("scalar", lambda e: e.activation(out=nea_bc[:], in_=nea_bc[:], func=AF.Exp), reads=[nea_bc], writes=[nea_bc])
        S.op("vector", lambda e: e.tensor_scalar(out=nea_bc[:], in0=nea_bc[:], scalar1=-1.0, scalar2=None, op0=ALU.mult),
             reads=[nea_bc], writes=[nea_bc])

        shf = A("shf", [128, 128], F32)
        Sh = [g.ident_b] + [A("sh%d" % d, [128, 128], BF16) for d in (1, 2, 3)]
        ShP = [None] + [A("shp%d" % d, [128, 128], BF16) for d in (1, 2, 3)]
        for d in (1, 2, 3):
            S.op("gpsimd", lambda e: e.affine_select(out=shf[:], in_=g.ones_f[:], pattern=[[-1, 128]],
                                                     compare_op=ALU.is_equal, fill=g.fill0, base=d, channel_multiplier=1),
                 reads=[g.ones_f], writes=[shf])
            S.op("vector", lambda e: e.tensor_copy(out=Sh[d][:], in_=shf[:]), reads=[shf], writes=[Sh[d]])
            S.op("gpsimd", lambda e: e.affine_select(out=shf[:], in_=g.ones_f[:], pattern=[[-1, 128]],
                                                     compare_op=ALU.is_equal, fill=g.fill0, base=d - 128, channel_multiplier=1),
                 reads=[g.ones_f], writes=[shf])
            S.op("vector", lambda e: e.tensor_copy(out=ShP[d][:], in_=shf[:]), reads=[shf], writes=[ShP[d]])

        xt = [A("xt%d" % i, [128, D], F32) for i in range(2)]
        junk = A("junk", [128, D], BF16)
        h_bf = A("h_bf", [128, D], BF16)
        hT = [A("hT%d" % i, [128, 8, 128], BF16) for i in range(2)]
        ss1 = A("ss1", [128, 1], F32)
        sd1 = A("sd1", [128, 1], F32)
        rs1 = A("rs1", [128, 1], F32)
        sq = A("sq", [128, 512], F32)
        ss8 = A("ss8", [128, 8], F32)
        sd8 = A("sd8", [128, 8], F32)
        rs8 = A("rs8", [128, 8], F32)
        tmpf = A("tmpf", [128, 512], F32)
        qn_bf = A("qn_bf", [128, 512], BF16)
        kn_bf = A("kn_bf", [128, 128], BF16)
        iq_bf = A("iq_bf", [128, 512], BF16)
        ik_bf = A("ik_bf", [128, 64], BF16)
        qT_t = [A("qT_t%d" % i, [64, 8, 128], BF16) for i in range(2)]
        iqT_t = [A("iqT_t%d" % i, [64, 8, 128], BF16) for i in range(2)]
        kT_t = [A("kT_t%d" % i, [64, 2, 128], BF16) for i in range(2)]
        ikT_t = [A("ikT_t%d" % i, [64, 128], BF16) for i in range(2)]
        v_t = [A("v_t%d" % i, [128, 128], BF16) for i in range(2)]
        iw_all = A("iw_all", [128, NT, 8], F32)
        gb_all = A("gb_all", [128, NT, 8], F32)
        ab_tmp = A("ab_tmp", [128, 4], F32)
        xw = [A("xw%d" % i, [128, 4, 1536], BF16) for i in range(2)]
        yc = [A("yc%d" % i, [128, 512], F32) for i in range(3)]
        dz_t = [A("dz_t%d" % i, [128, 512], BF16) for i in range(2)]
        gt_t = [A("gt_t%d" % i, [128, 1024], BF16) for i in range(2)]
        print("phase A sbuf remaining", nc.sbuf_bytes_remaining)

        def proj(cols, lo, hi, hTt):
            b = next_bank(g)
            n = hi - lo
            for kc in range(8):
                S.op("tensor", lambda e: e.matmul(out=b[:, 0:n], lhsT=hTt[:, kc, :], rhs=w_bf[:, kc, lo:hi],
                                                  start=(kc == 0), stop=(kc == 7)),
                     reads=[hTt, w_bf], writes=[b])
            return b

        def headnorm(ps, nh, gain_bc, out_bf):
            n = nh * 64
            S.op("scalar", lambda e: e.activation(out=sq[:, 0:n], in_=ps[:, 0:n], func=AF.Square), reads=[ps], writes=[sq])
            S.op("vector", lambda e: e.tensor_reduce(out=ss8[:, 0:nh], in_=sq[:, 0:n].rearrange("p (h d) -> p h d", d=64),
                                                     axis=AX.X, op=ALU.add), reads=[sq], writes=[ss8])
            rstd_op(g, rs8, ss8, 1.0 / 64, nh, sd8)
            S.op("vector", lambda e: e.tensor_tensor(out=tmpf[:, 0:n].rearrange("p (h d) -> p h d", d=64),
                                                     in0=ps[:, 0:n].rearrange("p (h d) -> p h d", d=64),
                                                     in1=rs8[:, 0:nh].unsqueeze(2).to_broadcast([128, nh, 64]), op=ALU.mult),
                 reads=[ps, rs8], writes=[tmpf])
            S.op("vector", lambda e: e.tensor_tensor(out=out_bf[:, 0:n].rearrange("p (h d) -> p h d", d=64),
                                                     in0=tmpf[:, 0:n].rearrange("p (h d) -> p h d", d=64),
                                                     in1=gain_bc[:, :].unsqueeze(1).to_broadcast([128, nh, 64]), op=ALU.mult),
                 reads=[tmpf, gain_bc], writes=[out_bf])

        def transpose_heads(src_bf, nh, dstT, eng):
            b = next_bank(g)
            bv = b[:, :].bitcast(BF16)
            for h in range(nh):
                S.op("tensor", lambda e: e.transpose(out=bv[0:64, h * 128:(h + 1) * 128], in_=src_bf[:, h * 64:(h + 1) * 64],
                                                     identity=g.ident_b[:]),
                     reads=[src_bf, g.ident_b], writes=[b])
            src = bv[0:64, 0:nh * 128]
            dst = dstT[:, :, :].rearrange("p h t -> p (h t)") if nh > 1 else dstT[:, :]
            if eng == "scalar":
                S.op("scalar", lambda e: e.copy(out=dst, in_=src), reads=[b], writes=[dstT])
            else:
                S.op("vector", lambda e: e.tensor_copy(out=dst, in_=src), reads=[b], writes=[dstT])

        x_v = g.x.ap().rearrange("(n p) d -> n p d", p=128)
        S.dma("sync", xt[0][:], x_v[0], writes=[xt[0]])
        NTR = getattr(g, "ntr", NT)
        SECT = getattr(g, "sect", 99)
        for tt in range(NTR):
            cur = tt % 2
            if tt + 1 < NTR:
                S.dma("sync", xt[1 - cur][:], x_v[tt + 1], writes=[xt[1 - cur]])
            x_t = xt[cur]
            hTt = hT[cur]
            rows = slice(tt * 128, (tt + 1) * 128)
            S.op("scalar", lambda e: e.activation(out=junk[:], in_=x_t[:], func=AF.Square, accum_out=ss1[:, 0:1]),
                 reads=[x_t], writes=[junk, ss1])
            rstd_op(g, rs1, ss1, 1.0 / D, 1, sd1)
            S.op("vector", lambda e: e.scalar_tensor_tensor(out=h_bf[:], in0=x_t[:], scalar=rs1[:, 0:1], in1=g1_bc[:],
                                                            op0=ALU.mult, op1=ALU.mult),
                 reads=[x_t, rs1, g1_bc], writes=[h_bf])
            for half in range(2):
                b = next_bank(g)
                bv = b[:, :].bitcast(BF16)
                for j in range(4):
                    kc = half * 4 + j
                    S.op("tensor", lambda e: e.transpose(out=bv[:, j * 128:(j + 1) * 128], in_=h_bf[:, kc * 128:(kc + 1) * 128],
                                                         identity=g.ident_b[:]),
                         reads=[h_bf, g.ident_b], writes=[b])
                dst = hTt[:, half * 4:half * 4 + 4, :].rearrange("p k t -> p (k t)")
                if half == 0:
                    S.op("scalar", lambda e: e.copy(out=dst, in_=bv[:, 0:512]), reads=[b], writes=[hTt])
                else:
                    S.op("vector", lambda e: e.tensor_copy(out=dst, in_=bv[:, 0:512]), reads=[b], writes=[hTt])

            if SECT < 1:
                continue
            ps = proj("aq", C_AQ, C_AQ + 512, hTt)
            headnorm(ps, 8, gq_bc, qn_bf)
            transpose_heads(qn_bf, 8, qT_t[cur], "scalar")
            S.dma("gpsimd", g.qT_s[tt], qT_t[cur][:], reads=[qT_t[cur]], writes=[g.qT_s])
            if SECT < 2:
                continue
            ps = proj("kv", C_AK, C_AK + 256, hTt)
            headnorm(ps, 2, gk_bc, kn_bf)
            S.op("scalar", lambda e: e.copy(out=v_t[cur][:], in_=ps[:, 128:256]), reads=[ps], writes=[v_t[cur]])
            transpose_heads(kn_bf, 2, kT_t[cur], "vector")
            S.dma("gpsimd", g.kT_s[:, :, rows], kT_t[cur][:], reads=[kT_t[cur]], writes=[g.kT_s])
            S.dma("gpsimd", g.v_s[rows], v_t[cur][:], reads=[v_t[cur]], writes=[g.v_s])
            if SECT < 3:
                continue
            ps = proj("iq", C_IQ, C_IQ + 512, hTt)
            S.op("scalar", lambda e: e.copy(out=iq_bf[:], in_=ps[:, 0:512]), reads=[ps], writes=[iq_bf])
            transpose_heads(iq_bf, 8, iqT_t[cur], "vector")
            S.dma("gpsimd", g.iqT_s[tt], iqT_t[cur][:], reads=[iqT_t[cur]], writes=[g.iqT_s])
            if SECT < 4:
                continue
            ps = proj("ikw", C_IK, C_IK + 72, hTt)
            S.op("vector", lambda e: e.tensor_copy(out=ik_bf[:], in_=ps[:, 0:64]), reads=[ps], writes=[ik_bf])
            S.op("scalar", lambda e: e.copy(out=iw_all[:, tt, :], in_=ps[:, 64:72]), reads=[ps], writes=[iw_all])
            transpose_heads(ik_bf, 1, ikT_t[cur], "scalar")
            if "ikT" not in getattr(g, "skip", ()):
                S.dma("gpsimd", g.ikT_s[:, rows], ikT_t[cur][:], reads=[ikT_t[cur]], writes=[g.ikT_s])
            if SECT < 5:
                continue
            ps = proj("ab", C_DA, C_DA + 8, hTt)
            S.op("vector", lambda e: e.tensor_tensor(out=ab_tmp[:], in0=ps[:, 0:4], in1=dtb_bc[:], op=ALU.add),
                 reads=[ps, dtb_bc], writes=[ab_tmp])
            S.op("scalar", lambda e: e.activation(out=ab_tmp[:], in_=ab_tmp[:], func=AF.Exp), reads=[ab_tmp], writes=[ab_tmp])
            S.op("scalar", lambda e: e.activation(out=ab_tmp[:], in_=ab_tmp[:], func=AF.Ln, bias=g.ones_f[:, 0:1], scale=1.0),
                 reads=[ab_tmp, g.ones_f], writes=[ab_tmp])
            S.op("vector", lambda e: e.tensor_tensor(out=gb_all[:, tt, 0:4], in0=ab_tmp[:], in1=nea_bc[:], op=ALU.mult),
                 reads=[ab_tmp, nea_bc], writes=[gb_all])
            S.op("scalar", lambda e: e.activation(out=gb_all[:, tt, 4:8], in_=ps[:, 4:8], func=AF.Sigmoid),
                 reads=[ps], writes=[gb_all])
            if SECT < 6:
                continue
            for gi, (c0, dst_s) in enumerate(((C_DQ, g.dnq_s), (C_DK, g.dnk_s), (C_DV, g.dnv_s))):
                ps = proj("dn", c0, c0 + 512, hTt)
                cs = slice(gi * 512, (gi + 1) * 512)
                for j in range(4):
                    S.op("vector", lambda e: e.tensor_tensor(out=xw[cur][:, j, cs], in0=ps[:, 0:512], in1=cw_bc[:, j, cs], op=ALU.mult),
                         reads=[ps, cw_bc], writes=[xw[cur]])
                b = next_bank(g)
                mm = []
                for j in range(4):
                    mm.append((Sh[3 - j], xw[cur], j))
                if tt > 0:
                    for j in range(3):
                        mm.append((ShP[3 - j], xw[1 - cur], j))
                for i, (sh, xsrc, j) in enumerate(mm):
                    S.op("tensor", lambda e: e.matmul(out=b[:, 0:512], lhsT=sh[:], rhs=xsrc[:, j, cs],
                                                      start=(i == 0), stop=(i == len(mm) - 1)),
                         reads=[sh, xsrc], writes=[b])
                y = yc[gi]
                S.op("scalar", lambda e: e.activation(out=y[:], in_=b[:, 0:512], func=AF.Silu), reads=[b], writes=[y])
                if gi < 2:
                    S.op("gpsimd", lambda e: e.tensor_tensor(out=sq[:], in0=y[:], in1=y[:], op=ALU.mult), reads=[y], writes=[sq])
                    S.op("vector", lambda e: e.tensor_reduce(out=ss8[:, 0:4], in_=sq[:].rearrange("p (h d) -> p h d", d=128),
                                                             axis=AX.X, op=ALU.add), reads=[sq], writes=[ss8])
                    rstd_op(g, rs8, ss8, 1.0, 4, sd8)
                    if gi == 0:
                        S.op("vector", lambda e: e.tensor_scalar(out=rs8[:, 0:4], in0=rs8[:, 0:4], scalar1=128 ** -0.5, scalar2=None,
                                                                 op0=ALU.mult), reads=[rs8], writes=[rs8])
                    S.op("vector", lambda e: e.tensor_tensor(out=y[:].rearrange("p (h d) -> p h d", d=128),
                                                             in0=y[:].rearrange("p (h d) -> p h d", d=128),
                                                             in1=rs8[:, 0:4].unsqueeze(2).to_broadcast([128, 4, 128]), op=ALU.mult),
                         reads=[y, rs8], writes=[y])
                S.dma("gpsimd", dst_s[rows], y[:], reads=[y], writes=[dst_s])
            if SECT < 7:
                continue
            ps = proj("dz", C_DZ, C_DZ + 512, hTt)
            S.op("scalar", lambda e: e.activation(out=dz_t[cur][:], in_=ps[:, 0:512], func=AF.Silu), reads=[ps], writes=[dz_t[cur]])
            S.dma("gpsimd", g.dz_s[rows], dz_t[cur][:], reads=[dz_t[cur]], writes=[g.dz_s])
            if SECT < 8:
                continue
            for gi, c0 in enumerate((C_GA, C_GB)):
                for hf in range(2):
                    ps = proj("gate", c0 + hf * 512, c0 + (hf + 1) * 512, hTt)
                    S.op("scalar", lambda e: e.activation(out=gt_t[gi][:, hf * 512:(hf + 1) * 512], in_=ps[:, 0:512], func=AF.Sigmoid),
                         reads=[ps], writes=[gt_t[gi]])
                S.dma("gpsimd", g.gate_s[rows, gi * 1024:(gi + 1) * 1024], gt_t[gi][:], reads=[gt_t[gi]], writes=[g.gate_s])
        S.dma("gpsimd", g.iw_s[:, :, :], iw_all[:], reads=[iw_all], writes=[g.iw_s])
        S.dma("gpsimd", g.gb_s[:, :, :], gb_all[:], reads=[gb_all], writes=[g.gb_s])
        S.barrier()


NIT = 15


def phase_b(g):
    nc, S = g.nc, g.S
    g.nrot = 6
    acc = g.banks[6:8]
    with contextlib.ExitStack() as ph:
        def A(name, shape, dt):
            return sb(g, ph, name, shape, dt)

        kT_all = A("kT_all", [64, 2, T], BF16)
        ikT_all = A("ikT_all", [64, T], BF16)
        v_raw = A("v_raw", [128, NT, 128], BF16)
        v_all = A("v_all", [128, NT, 2, 65], BF16)
        iw_all = A("iw_all", [128, NT, 8], F32)
        S.dma("sync", kT_all[:], g.kT_s[:, :, :], reads=[g.kT_s], writes=[kT_all])
        S.dma("sync", ikT_all[:], g.ikT_s[:, :], reads=[g.ikT_s], writes=[ikT_all])
        S.dma("sync", v_raw[:], g.v_s[:, :].rearrange("(n p) c -> p n c", p=128), reads=[g.v_s], writes=[v_raw])
        S.dma("sync", iw_all[:], g.iw_s[:, :, :], reads=[g.iw_s], writes=[iw_all])
        S.op("gpsimd", lambda e: e.memset(v_all[:], 1.0), writes=[v_all])
        S.op("vector", lambda e: e.tensor_copy(out=v_all[:, :, :, 0:64],
                                               in_=v_raw[:].rearrange("p n (g d) -> p n g d", d=64)),
             reads=[v_raw], writes=[v_all])
        thr0 = A("thr0", [128, 1], F32)
        S.op("gpsimd", lambda e: e.memset(thr0[:], -1e29), writes=[thr0])

        sc = [A("sc%d" % i, [128, T], F32) for i in range(2)]
        Rb = [A("Rb%d" % i, [128, 512], F32) for i in range(2)]
        junk = A("junkb", [128, T], BF16)
        mask = A("mask", [128, T], BF16)
        maskT = [A("maskT%d" % i, [128, NT, 128], BF16) for i in range(2)]
        iqT = [A("iqT%d" % i, [64, 8, 128], BF16) for i in range(2)]
        qT = [A("qT%d" % i, [64, 8, 128], BF16) for i in range(2)]
        Eb = [A("Eb%d" % i, [128, 512], BF16) for i in range(2)]
        Pb = [A("Pb%d" % i, [128, 512], BF16) for i in range(2)]
        yat = [A("yat%d" % i, [128, 512], BF16) for i in range(2)]
        rec = A("rec", [128, 4], F32)
        hi = A("hi", [128, 1], F32)
        lo = A("lo", [128, 1], F32)
        rk = A("rk", [128, 1], F32)
        mid = A("mid", [128, 1], F32)
        cnt = A("cnt", [128, 1], F32)
        step = A("step", [128, 1], F32)
        print("phase B sbuf remaining", nc.sbuf_bytes_remaining)

        NQB = getattr(g, "nqb", NT)
        S.dma("sync", iqT[0][:], g.iqT_s[0], reads=[g.iqT_s], writes=[iqT[0]])
        S.dma("sync", qT[0][:], g.qT_s[0], reads=[g.qT_s], writes=[qT[0]])
        ke = 0
        for qb in range(NQB):
            cur = qb % 2
            if qb + 1 < NQB:
                S.dma("sync", iqT[1 - cur][:], g.iqT_s[qb + 1], reads=[g.iqT_s], writes=[iqT[1 - cur]])
                S.dma("sync", qT[1 - cur][:], g.qT_s[qb + 1], reads=[g.qT_s], writes=[qT[1 - cur]])
            NS = qb + 1
            SS = NS * 128
            sct = sc[cur]
            for ci in range((SS + 511) // 512):
                c0 = ci * 512
                n = min(512, SS - c0)
                for h in range(8):
                    b = next_bank(g)
                    S.op("tensor", lambda e: e.matmul(out=b[:, 0:n], lhsT=iqT[cur][:, h, :], rhs=ikT_all[:, c0:c0 + n],
                                                      start=True, stop=True),
                         reads=[iqT[cur], ikT_all], writes=[b])
                    R = Rb[ke % 2]
                    ke += 1
                    S.op("scalar", lambda e: e.activation(out=R[:, 0:n], in_=b[:, 0:n], func=AF.Relu), reads=[b], writes=[R])
                    if h == 0:
                        S.op("vector", lambda e: e.tensor_scalar(out=sct[:, c0:c0 + n], in0=R[:, 0:n], scalar1=iw_all[:, qb, 0:1],
                                                                 scalar2=None, op0=ALU.mult),
                             reads=[R, iw_all], writes=[sct])
                    else:
                        S.op("vector", lambda e: e.scalar_tensor_tensor(out=sct[:, c0:c0 + n], in0=R[:, 0:n],
                                                                        scalar=iw_all[:, qb, h:h + 1], in1=sct[:, c0:c0 + n],
                                                                        op0=ALU.mult, op1=ALU.add),
                             reads=[R, iw_all, sct], writes=[sct])
            dg = sct[:, qb * 128:(qb + 1) * 128]
            S.op("gpsimd", lambda e: e.affine_select(out=dg, in_=dg, pattern=[[-1, 128]], compare_op=ALU.is_ge,
                                                     fill=g.fillneg, base=0, channel_multiplier=1),
                 reads=[sct], writes=[sct])
            if qb >= 2:
                S.op("vector", lambda e: e.tensor_reduce(out=hi[:], in_=sct[:, 0:SS], axis=AX.X, op=ALU.max), reads=[sct], writes=[hi])
                S.op("vector", lambda e: e.tensor_reduce(out=lo[:], in_=sct[:, 0:qb * 128], axis=AX.X, op=ALU.min), reads=[sct], writes=[lo])
                S.op("vector", lambda e: e.tensor_tensor(out=rk[:], in0=hi[:], in1=lo[:], op=ALU.subtract), reads=[hi, lo], writes=[rk])
                for it in range(NIT):
                    S.op("vector", lambda e: e.tensor_scalar(out=rk[:], in0=rk[:], scalar1=0.5, scalar2=None, op0=ALU.mult),
                         reads=[rk], writes=[rk])
                    S.op("vector", lambda e: e.tensor_tensor(out=mid[:], in0=lo[:], in1=rk[:], op=ALU.add), reads=[lo, rk], writes=[mid])
                    S.op("vector", lambda e: e.tensor_scalar(out=junk[:, 0:SS], in0=sct[:, 0:SS], scalar1=mid[:, 0:1], scalar2=None,
                                                             op0=ALU.is_ge, op1=ALU.add, accum_out=cnt[:, 0:1]),
                         reads=[sct, mid], writes=[junk, cnt])
                    S.op("vector", lambda e: e.scalar_tensor_tensor(out=step[:], in0=cnt[:], scalar=255.5, in1=rk[:],
                                                                    op0=ALU.is_ge, op1=ALU.mult), reads=[cnt, rk], writes=[step])
                    S.op("vector", lambda e: e.tensor_tensor(out=lo[:], in0=lo[:], in1=step[:], op=ALU.add), reads=[lo, step], writes=[lo])
                thr = lo
            else:
                thr = thr0
            S.op("vector", lambda e: e.tensor_scalar(out=mask[:, 0:SS], in0=sct[:, 0:SS], scalar1=thr[:, 0:1], scalar2=None,
                                                     op0=ALU.is_ge), reads=[sct, thr], writes=[mask])
            mT = maskT[cur]
            for b0 in range(0, NS, 8):
                nb = min(8, NS - b0)
                b = next_bank(g)
                bv = b[:, :].bitcast(BF16)
                for j in range(nb):
                    S.op("tensor", lambda e: e.transpose(out=bv[:, j * 128:(j + 1) * 128],
                                                         in_=mask[:, (b0 + j) * 128:(b0 + j + 1) * 128], identity=g.ident_b[:]),
                         reads=[mask, g.ident_b], writes=[b])
                dst = mT[:, b0:b0 + nb, :].rearrange("p n t -> p (n t)")
                if (b0 // 8) % 2 == 0:
                    S.op("scalar", lambda e: e.copy(out=dst, in_=bv[:, 0:nb * 128]), reads=[b], writes=[mT])
                else:
                    S.op("gpsimd", lambda e: e.tensor_copy(out=dst, in_=bv[:, 0:nb * 128]), reads=[b], writes=[mT]) if False else \
                        S.op("vector", lambda e: e.tensor_copy(out=dst, in_=bv[:, 0:nb * 128]), reads=[b], writes=[mT])
            yt = yat[cur]
            for gi in range(2):
                po = acc[gi]
                for sbk in range(NS):
                    b = next_bank(g)
                    S.op("tensor", lambda e: e.matmul(out=b[:, 0:512], lhsT=kT_all[:, gi, sbk * 128:(sbk + 1) * 128],
                                                      rhs=qT[cur][:, 4 * gi:4 * gi + 4, :].rearrange("p h t -> p (h t)"),
                                                      start=True, stop=True),
                         reads=[kT_all, qT[cur]], writes=[b])
                    E = Eb[ke % 2]
                    P = Pb[ke % 2]
                    ke += 1
                    S.op("scalar", lambda e: e.activation(out=E[:], in_=b[:, 0:512], func=AF.Exp), reads=[b], writes=[E])
                    S.op("vector", lambda e: e.tensor_tensor(out=P[:].rearrange("p (h t) -> p h t", h=4),
                                                             in0=E[:].rearrange("p (h t) -> p h t", h=4),
                                                             in1=mT[:, sbk, :].unsqueeze(1).to_broadcast([128, 4, 128]), op=ALU.mult),
                         reads=[E, mT], writes=[P])
                    for h in range(4):
                        S.op("tensor", lambda e: e.matmul(out=po[:, h * 65:(h + 1) * 65], lhsT=P[:, h * 128:(h + 1) * 128],
                                                          rhs=v_all[:, sbk, gi, :], start=(sbk == 0 and h == 0),
                                                          stop=(sbk == NS - 1), skip_group_check=True),
                             reads=[P, v_all], writes=[po])
                pov = po[:, 0:260].rearrange("p (h e) -> p h e", e=65)
                S.op("vector", lambda e: e.reciprocal(out=rec[:], in_=pov[:, :, 64]), reads=[po], writes=[rec])
                S.op("vector", lambda e: e.tensor_tensor(out=yt[:, gi * 256:(gi + 1) * 256].rearrange("p (h d) -> p h d", d=64),
                                                         in0=pov[:, :, 0:64],
                                                         in1=rec[:, :].unsqueeze(2).to_broadcast([128, 4, 64]), op=ALU.mult),
                     reads=[po, rec], writes=[yt])
            S.dma("gpsimd", g.yatt_s[qb * 128:(qb + 1) * 128], yt[:], reads=[yt], writes=[g.yatt_s])
        S.barrier()
    g.nrot = 8


def phase_c(g):
    nc, S = g.nc, g.S
    g.nrot = 8
    GS = 2
    with contextlib.ExitStack() as ph:
        def A(name, shape, dt=F32):
            return sb(g, ph, name, shape, dt)

        utri = A("utri", [64, 64])
        sel63 = A("sel63", [64, 128])
        S.op("gpsimd", lambda e: e.affine_select(out=utri[:], in_=g.ones_f[0:64, 0:64], pattern=[[1, 64]], compare_op=ALU.is_ge,
                                                 fill=g.fill0, base=0, channel_multiplier=-1), reads=[g.ones_f], writes=[utri])
        S.op("gpsimd", lambda e: e.affine_select(out=sel63[:], in_=g.ones_f[0:64, :], pattern=[[0, 128]], compare_op=ALU.is_equal,
                                                 fill=g.fill0, base=-63, channel_multiplier=1), reads=[g.ones_f], writes=[sel63])
        gno = A("gno", [128, 128])
        S.dma("sync", gno[:], g.dn_out_norm_gain.ap().partition_broadcast(128), writes=[gno])
        gbc = [A("gbc%d" % h, [64, NT, 8]) for h in range(2)]
        gn = [A("gn%d" % h, [64, NT, 8]) for h in range(2)]
        egc = [A("egc%d" % h, [64, NT, 4]) for h in range(2)]
        bg = [A("bg%d" % h, [64, NT, 4]) for h in range(2)]
        kd = [A("kd%d" % h, [64, NT, 4]) for h in range(2)]
        elast = [A("elast%d" % h, [128, NT, 4]) for h in range(2)]
        for h in range(2):
            S.dma("sync", gbc[h][:], g.gb_s[h * 64:(h + 1) * 64, :, :], reads=[g.gb_s], writes=[gbc[h]])
            b = next_bank(g)
            S.op("tensor", lambda e: e.matmul(out=b[0:64, 0:128], lhsT=utri[:], rhs=gbc[h][:, :, 0:4], start=True, stop=True),
                 reads=[utri, gbc[h]], writes=[b])
            S.op("vector", lambda e: e.tensor_copy(out=gn[h][:, :, 0:4], in_=b[0:64, 0:128].rearrange("p (n f) -> p n f", f=4)),
                 reads=[b], writes=[gn[h]])
            S.op("vector", lambda e: e.tensor_scalar(out=gn[h][:, :, 4:8], in0=gbc[h][:, :, 4:8], scalar1=-1.0, scalar2=None, op0=ALU.mult),
                 reads=[gbc[h]], writes=[gn[h]])
            S.op("scalar", lambda e: e.activation(out=egc[h][:], in_=gn[h][:, :, 0:4], func=AF.Exp), reads=[gn[h]], writes=[egc[h]])
            S.op("vector", lambda e: e.tensor_tensor(out=bg[h][:], in0=egc[h][:], in1=gbc[h][:, :, 4:8], op=ALU.mult),
                 reads=[egc[h], gbc[h]], writes=[bg[h]])
            b2 = next_bank(g)
            S.op("tensor", lambda e: e.matmul(out=b2[:, 0:128], lhsT=sel63[:], rhs=gn[h][:, :, 0:4], start=True, stop=True),
                 reads=[sel63, gn[h]], writes=[b2])
            S.op("scalar", lambda e: e.activation(out=elast[h][:], in_=b2[:, 0:128].rearrange("p (n f) -> p n f", f=4), func=AF.Exp),
                 reads=[b2], writes=[elast[h]])
            S.op("vector", lambda e: e.tensor_tensor(out=kd[h][:], in0=b2[0:64, 0:128].rearrange("p (n f) -> p n f", f=4),
                                                     in1=gn[h][:, :, 0:4], op=ALU.subtract), reads=[b2, gn[h]], writes=[kd[h]])
            S.op("scalar", lambda e: e.activation(out=kd[h][:], in_=kd[h][:], func=AF.Exp), reads=[kd[h]], writes=[kd[h]])

        Sst = A("Sst", [128, 4, 128])
        S.op("gpsimd", lambda e: e.memset(Sst[:], 0.0), writes=[Sst])

        class Slot:
            pass
        slots = []
        for i in range(GS):
            s_ = Slot()
            s_.q = A("cq%d" % i, [64, 512]); s_.k = A("ck%d" % i, [64, 512]); s_.v = A("cv%d" % i, [64, 512])
            s_.dz = A("cdz%d" % i, [64, 512], BF16)
            s_.dgb = A("dgb%d" % i, [64, 512]); s_.G1 = A("G1%d" % i, [64, 256]); s_.G2 = A("G2%d" % i, [64, 256])
            s_.sel3 = A("sel3%d" % i, [64, 768]); s_.E3 = A("E3%d" % i, [64, 768])
            s_.DATn = A("DATn%d" % i, [64, 256]); s_.DAn = A("DAn%d" % i, [64, 256])
            s_.kqT = A("kqT%d" % i, [128, 512])
            s_.MM = [A("MM%d_%d" % (i, j), [64, 512]) for j in range(2)]
            s_.XT = A("XT%d" % i, [64, 256]); s_.inT = A("inT%d" % i, [64, 256])
            s_.vb = A("vb%d" % i, [64, 512]); s_.kbg = A("kbg%d" % i, [64, 512]); s_.kdec = A("kdec%d" % i, [64, 512])
            s_.u = A("u%d" % i, [64, 512]); s_.wT = A("wT%d" % i, [128, 256])
            slots.append(s_)
        vnew = A("vnew", [64, 512])
        otmp = A("otmp", [64, 512])
        osq = A("osq", [64, 512])
        oss = A("oss", [64, 4]); osd = A("osd", [64, 4]); ors = A("ors", [64, 4])
        yout = [A("yout%d" % i, [64, 512], BF16) for i in range(2)]
        print("phase C sbuf remaining", nc.sbuf_bytes_remaining)
        idb = g.ident_f[0:64, 0:64]
        NCH = getattr(g, "nch", 64)

        def bc_h(ap4, n):
            return ap4.unsqueeze(2).to_broadcast([64, 4, n])

        def v3(ap, n):
            return ap.rearrange("p (h f) -> p h f", h=4)

        for c0 in range(0, NCH, GS):
            chunks = list(range(c0, min(NCH, c0 + GS)))
            info = {}
            for c in chunks:
                sl = slots[c % GS]
                tt, half = c // 2, c % 2
                rows = slice(c * 64, (c + 1) * 64)
                S.dma("sync", sl.q[:], g.dnq_s[rows], reads=[g.dnq_s], writes=[sl.q])
                S.dma("sync", sl.k[:], g.dnk_s[rows], reads=[g.dnk_s], writes=[sl.k])
                S.dma("sync", sl.v[:], g.dnv_s[rows], reads=[g.dnv_s], writes=[sl.v])
                S.dma("sync", sl.dz[:], g.dz_s[rows], reads=[g.dz_s], writes=[sl.dz])
                info[c] = (sl, tt, half)
            for c in chunks:
                sl, tt, half = info[c]
                gnc = gn[half][:, tt, :]
                S.op("vector", lambda e: e.tensor_tensor(out=sl.dgb[:].rearrange("p (a f) -> p a f", a=8),
                                                         in0=gnc.unsqueeze(2).to_broadcast([64, 8, 64]),
                                                         in1=idb.unsqueeze(1).to_broadcast([64, 8, 64]), op=ALU.mult),
                     reads=[gn[half], g.ident_f], writes=[sl.dgb])
                bR = next_bank(g)
                S.op("tensor", lambda e: e.matmul(out=bR[0:64, 0:512], lhsT=g.ones_f[0:64, 0:64], rhs=sl.dgb[:], start=True, stop=True),
                     reads=[g.ones_f, sl.dgb], writes=[bR])
                S.op("vector", lambda e: e.tensor_tensor(out=v3(sl.G1[:], 64), in0=v3(bR[0:64, 0:256], 64),
                                                         in1=bc_h(gn[half][:, tt, 0:4], 64), op=ALU.subtract),
                     reads=[bR, gn[half]], writes=[sl.G1])
                S.op("vector", lambda e: e.tensor_scalar(out=sl.G2[:], in0=sl.G1[:], scalar1=-1.0, scalar2=None, op0=ALU.mult),
                     reads=[sl.G1], writes=[sl.G2])
                S.op("gpsimd", lambda e: e.affine_select(out=sl.sel3[:, 0:256], in_=sl.G1[:], pattern=[[0, 4], [1, 64]],
                                                         compare_op=ALU.is_ge, fill=g.fillneg, base=0, channel_multiplier=-1),
                     reads=[sl.G1], writes=[sl.sel3])
                S.op("gpsimd", lambda e: e.affine_select(out=sl.sel3[:, 256:512], in_=sl.G1[:], pattern=[[0, 4], [1, 64]],
                                                         compare_op=ALU.is_ge, fill=g.fillneg, base=-1, channel_multiplier=-1),
                     reads=[sl.G1], writes=[sl.sel3])
                S.op("gpsimd", lambda e: e.affine_select(out=sl.sel3[:, 512:768], in_=sl.G2[:], pattern=[[0, 4], [-1, 64]],
                                                         compare_op=ALU.is_ge, fill=g.fillneg, base=-1, channel_multiplier=1),
                     reads=[sl.G2], writes=[sl.sel3])
                S.op("scalar", lambda e: e.activation(out=sl.E3[:], in_=sl.sel3[:], func=AF.Exp), reads=[sl.sel3], writes=[sl.E3])
                S.op("vector", lambda e: e.tensor_tensor(out=sl.DATn[:], in0=sl.E3[:, 256:512], in1=bR[0:64, 256:512], op=ALU.mult),
                     reads=[sl.E3, bR], writes=[sl.DATn])
                S.op("gpsimd", lambda e: e.tensor_tensor(out=v3(sl.DAn[:], 64), in0=v3(sl.E3[:, 512:768], 64),
                                                         in1=bc_h(gn[half][:, tt, 4:8], 64), op=ALU.mult),
                     reads=[sl.E3, gn[half]], writes=[sl.DAn])
                S.op("vector", lambda e: e.tensor_tensor(out=v3(sl.vb[:], 128), in0=v3(sl.v[:], 128),
                                                         in1=bc_h(gbc[half][:, tt, 4:8], 128), op=ALU.mult),
                     reads=[sl.v, gbc[half]], writes=[sl.vb])
                S.op("gpsimd", lambda e: e.tensor_tensor(out=v3(sl.kbg[:], 128), in0=v3(sl.k[:], 128),
                                                         in1=bc_h(bg[half][:, tt, :], 128), op=ALU.mult),
                     reads=[sl.k, bg[half]], writes=[sl.kbg])
                S.op("gpsimd", lambda e: e.tensor_tensor(out=v3(sl.kdec[:], 128), in0=v3(sl.k[:], 128),
                                                         in1=bc_h(kd[half][:, tt, :], 128), op=ALU.mult),
                     reads=[sl.k, kd[half]], writes=[sl.kdec])
                bT = next_bank(g)
                for hd in range(4):
                    S.op("tensor", lambda e: e.transpose(out=bT[:, hd * 64:(hd + 1) * 64], in_=sl.k[:, hd * 128:(hd + 1) * 128], identity=idb),
                         reads=[sl.k, g.ident_f], writes=[bT])
                for hd in range(4):
                    S.op("tensor", lambda e: e.transpose(out=bT[:, 256 + hd * 64:256 + (hd + 1) * 64], in_=sl.q[:, hd * 128:(hd + 1) * 128],
                                                         identity=idb), reads=[sl.q, g.ident_f], writes=[bT])
                S.op("scalar", lambda e: e.copy(out=sl.kqT[:], in_=bT[:, 0:512]), reads=[bT], writes=[sl.kqT])
                bK = next_bank(g)
                for hd in range(4):
                    kT_h = sl.kqT[:, hd * 64:(hd + 1) * 64]
                    qT_h = sl.kqT[:, 256 + hd * 64:256 + (hd + 1) * 64]
                    S.op("tensor", lambda e: e.matmul(out=bK[0:64, hd * 64:(hd + 1) * 64], lhsT=kT_h, rhs=kT_h, start=(hd == 0), stop=True,
                                                      skip_group_check=True), reads=[sl.kqT], writes=[bK])
                for hd in range(4):
                    kT_h = sl.kqT[:, hd * 64:(hd + 1) * 64]
                    qT_h = sl.kqT[:, 256 + hd * 64:256 + (hd + 1) * 64]
                    S.op("tensor", lambda e: e.matmul(out=bK[0:64, 256 + hd * 64:256 + (hd + 1) * 64], lhsT=kT_h, rhs=qT_h, start=False,
                                                      stop=True, skip_group_check=True), reads=[sl.kqT], writes=[bK])
                MM0 = sl.MM[0]
                S.op("vector", lambda e: e.tensor_tensor(out=MM0[:, 0:256], in0=bK[0:64, 0:256], in1=sl.DAn[:], op=ALU.mult),
                     reads=[bK, sl.DAn], writes=[MM0])
                S.op("vector", lambda e: e.tensor_tensor(out=MM0[:, 256:512], in0=bK[0:64, 0:256], in1=sl.DATn[:], op=ALU.mult),
                     reads=[bK, sl.DATn], writes=[MM0])
                S.op("vector", lambda e: e.tensor_tensor(out=sl.inT[:], in0=bK[0:64, 256:512], in1=sl.E3[:, 0:256], op=ALU.mult),
                     reads=[bK, sl.E3], writes=[sl.inT])
                S.op("gpsimd", lambda e: e.tensor_tensor(out=v3(sl.XT[:], 64), in0=v3(MM0[:, 256:512], 64),
                                                         in1=idb.unsqueeze(1).to_broadcast([64, 4, 64]), op=ALU.add),
                     reads=[MM0, g.ident_f], writes=[sl.XT])
            for lvl in range(1, 6):
                for c in chunks:
                    sl, tt, half = info[c]
                    Mp = sl.MM[(lvl - 1) % 2]
                    Mn = sl.MM[lvl % 2]
                    bM = next_bank(g)
                    for hd in range(4):
                        M_h = Mp[:, hd * 64:(hd + 1) * 64]
                        MT_h = Mp[:, 256 + hd * 64:256 + (hd + 1) * 64]
                        S.op("tensor", lambda e: e.matmul(out=bM[0:64, hd * 64:(hd + 1) * 64], lhsT=MT_h, rhs=M_h, start=(hd == 0), stop=True,
                                                          skip_group_check=True), reads=[Mp], writes=[bM])
                    nw = 256
                    if lvl < 5:
                        nw = 512
                        for hd in range(4):
                            M_h = Mp[:, hd * 64:(hd + 1) * 64]
                            MT_h = Mp[:, 256 + hd * 64:256 + (hd + 1) * 64]
                            S.op("tensor", lambda e: e.matmul(out=bM[0:64, 256 + hd * 64:256 + (hd + 1) * 64], lhsT=M_h, rhs=MT_h, start=False,
                                                              stop=True, skip_group_check=True), reads=[Mp], writes=[bM])
                    S.op("scalar", lambda e: e.copy(out=Mn[:, 0:nw], in_=bM[0:64, 0:nw]), reads=[bM], writes=[Mn])
                    bX = next_bank(g)
                    for hd in range(4):
                        S.op("tensor", lambda e: e.matmul(out=bX[0:64, hd * 64:(hd + 1) * 64], lhsT=Mn[:, hd * 64:(hd + 1) * 64],
                                                          rhs=sl.XT[:, hd * 64:(hd + 1) * 64], start=(hd == 0), stop=True,
                                                          skip_group_check=True), reads=[Mn, sl.XT], writes=[bX])
                    S.op("vector", lambda e: e.tensor_tensor(out=sl.XT[:], in0=bX[0:64, 0:256], in1=sl.XT[:], op=ALU.add),
                         reads=[bX, sl.XT], writes=[sl.XT])
            for c in chunks:
                sl, tt, half = info[c]
                bU = next_bank(g)
                for hd in range(4):
                    S.op("tensor", lambda e: e.matmul(out=bU[0:64, hd * 128:(hd + 1) * 128], lhsT=sl.XT[:, hd * 64:(hd + 1) * 64],
                                                      rhs=sl.vb[:, hd * 128:(hd + 1) * 128], start=(hd == 0), stop=True,
                                                      skip_group_check=True), reads=[sl.XT, sl.vb], writes=[bU])
                S.op("scalar", lambda e: e.copy(out=sl.u[:], in_=bU[0:64, 0:512]), reads=[bU], writes=[sl.u])
                bW = next_bank(g)
                for hd in range(4):
                    S.op("tensor", lambda e: e.matmul(out=bW[:, hd * 64:(hd + 1) * 64], lhsT=sl.kbg[:, hd * 128:(hd + 1) * 128],
                                                      rhs=sl.XT[:, hd * 64:(hd + 1) * 64], start=(hd == 0), stop=True,
                                                      skip_group_check=True), reads=[sl.XT, sl.kbg], writes=[bW])
                S.op("vector", lambda e: e.tensor_copy(out=sl.wT[:], in_=bW[:, 0:256]), reads=[bW], writes=[sl.wT])
            for c in chunks:
                sl, tt, half = info[c]
                rows = slice(c * 64, (c + 1) * 64)
                b1 = next_bank(g)
                for hd in range(4):
                    S.op("tensor", lambda e: e.matmul(out=b1[0:64, hd * 128:(hd + 1) * 128], lhsT=sl.wT[:, hd * 64:(hd + 1) * 64],
                                                      rhs=Sst[:, hd, :], start=(hd == 0), stop=True, skip_group_check=True),
                         reads=[sl.wT, Sst], writes=[b1])
                S.op("vector", lambda e: e.tensor_tensor(out=vnew[:], in0=sl.u[:], in1=b1[0:64, 0:512], op=ALU.subtract),
                     reads=[sl.u, b1], writes=[vnew])
                b2 = next_bank(g)
                for hd in range(4):
                    S.op("tensor", lambda e: e.matmul(out=b2[0:64, hd * 128:(hd + 1) * 128], lhsT=sl.kqT[:, 256 + hd * 64:256 + (hd + 1) * 64],
                                                      rhs=Sst[:, hd, :], start=(hd == 0), stop=True, skip_group_check=True),
                         reads=[sl.kqT, Sst], writes=[b2])
                b3 = next_bank(g)
                for hd in range(4):
                    S.op("tensor", lambda e: e.matmul(out=b3[0:64, hd * 128:(hd + 1) * 128], lhsT=sl.inT[:, hd * 64:(hd + 1) * 64],
                                                      rhs=vnew[:, hd * 128:(hd + 1) * 128], start=(hd == 0), stop=True, skip_group_check=True),
                         reads=[sl.inT, vnew], writes=[b3])
                b4 = next_bank(g)
                for hd in range(4):
                    S.op("tensor", lambda e: e.matmul(out=b4[:, hd * 128:(hd + 1) * 128], lhsT=sl.kdec[:, hd * 128:(hd + 1) * 128],
                                                      rhs=vnew[:, hd * 128:(hd + 1) * 128], start=(hd == 0), stop=True, skip_group_check=True),
                         reads=[sl.kdec, vnew], writes=[b4])
                for hd in range(4):
                    S.op("vector", lambda e: e.scalar_tensor_tensor(out=Sst[:, hd, :], in0=Sst[:, hd, :], scalar=elast[half][:, tt, hd:hd + 1],
                                                                    in1=b4[:, hd * 128:(hd + 1) * 128], op0=ALU.mult, op1=ALU.add),
                         reads=[Sst, elast[half], b4], writes=[Sst])
                S.op("vector", lambda e: e.tensor_tensor(out=v3(otmp[:], 128), in0=v3(b2[0:64, 0:512], 128),
                                                         in1=bc_h(egc[half][:, tt, :], 128), op=ALU.mult),
                     reads=[b2, egc[half]], writes=[otmp])
                S.op("vector", lambda e: e.tensor_tensor(out=otmp[:], in0=otmp[:], in1=b3[0:64, 0:512], op=ALU.add),
                     reads=[otmp, b3], writes=[otmp])
                S.op("scalar", lambda e: e.activation(out=osq[:], in_=otmp[:], func=AF.Square), reads=[otmp], writes=[osq])
                S.op("vector", lambda e: e.tensor_reduce(out=oss[:], in_=v3(osq[:], 128), axis=AX.X, op=ALU.add), reads=[osq], writes=[oss])
                S.op("scalar", lambda e: e.activation(out=osd[:], in_=oss[:], func=AF.Sqrt, bias=g.eps_t[0:64, 0:1], scale=1.0 / 128),
                     reads=[oss, g.eps_t], writes=[osd])
                S.op("vector", lambda e: e.reciprocal(out=ors[:], in_=osd[:]), reads=[osd], writes=[ors])
                S.op("vector", lambda e: e.tensor_tensor(out=v3(otmp[:], 128), in0=v3(otmp[:], 128), in1=bc_h(ors[:, :], 128), op=ALU.mult),
                     reads=[otmp, ors], writes=[otmp])
                S.op("gpsimd", lambda e: e.tensor_tensor(out=v3(otmp[:], 128), in0=v3(otmp[:], 128),
                                                         in1=gno[0:64, :].unsqueeze(1).to_broadcast([64, 4, 128]), op=ALU.mult),
                     reads=[otmp, gno], writes=[otmp])
                yo = yout[c % 2]
                S.op("vector", lambda e: e.tensor_tensor(out=yo[:], in0=otmp[:], in1=sl.dz[:], op=ALU.mult),
                     reads=[otmp, sl.dz], writes=[yo])
                S.dma("gpsimd", g.ydn_s[rows], yo[:], reads=[yo], writes=[g.ydn_s])
        S.barrier()


def phase_p(g):
    nc, S = g.nc, g.S
    with contextlib.ExitStack() as ph:
        stg = [sb(g, ph, "pstg%d" % i, [128, 4096], F32) for i in range(2)]
        cst = [sb(g, ph, "pcst%d" % i, [128, 4096], BF16) for i in range(2)]
        k = 0
        for src, dst in ((g.peer_u, g.ub_s), (g.peer_v, g.vb_s)):
            sv = src.ap().rearrange("(b p j) d -> b p (j d)", p=128, j=4)
            dv = dst.t.rearrange("(b p j) d -> b p (j d)", p=128, j=4)
            for b in range(getattr(g, "npb", 32)):
                s_, c_ = stg[k % 2], cst[k % 2]
                S.dma("sync", s_[:], sv[b], writes=[s_])
                eng = ("vector", "gpsimd", "scalar")[k % 3]
                if eng == "scalar":
                    S.op(eng, lambda e: e.copy(out=c_[:], in_=s_[:]), reads=[s_], writes=[c_])
                else:
                    S.op(eng, lambda e: e.tensor_copy(out=c_[:], in_=s_[:]), reads=[s_], writes=[c_])
                S.dma("sync", dv[b], c_[:], reads=[c_], writes=[dst])
                k += 1
        S.barrier()


def phase_de(g):
    nc, S = g.nc, g.S
    g.nrot = 8
    with contextlib.ExitStack() as ph:
        def A(name, shape, dt=F32):
            return sb(g, ph, name, shape, dt)

        wA = A("wA", [128, 4, D], BF16)
        wB = A("wB", [128, 4, D], BF16)
        wo = A("wo", [128, 8, D], BF16)
        wq = A("wq", [128, 8, 2048], BF16)
        wstg = [A("wstg%d" % i, [128, 2048], F32) for i in range(2)]
        k = 0
        jobs = []
        for (src, dstt, nk, ncol) in ((g.w_att_branch, wA, 4, D), (g.w_dn_branch, wB, 4, D), (g.w_o, wo, 8, D),
                                      (g.peer_w_query, wq, 8, 2048)):
            sv = src.ap().rearrange("(kc p) n -> p kc n", p=128)
            for kc in range(nk):
                s_ = wstg[k % 2]
                S.dma("sync", s_[:, 0:ncol], sv[:, kc, :], writes=[s_])
                eng = ("vector", "gpsimd", "scalar")[k % 3]
                dst = dstt[:, kc, :]
                if eng == "scalar":
                    S.op(eng, lambda e: e.copy(out=dst, in_=s_[:, 0:ncol]), reads=[s_], writes=[dstt])
                else:
                    S.op(eng, lambda e: e.tensor_copy(out=dst, in_=s_[:, 0:ncol]), reads=[s_], writes=[dstt])
                k += 1
        g2_bc = A("g2_bc", [128, D])
        S.dma("sync", g2_bc[:], g.norm2_gain.ap().partition_broadcast(128), writes=[g2_bc])
        skT = A("skT", [128, 16, 128])
        for hp in range(16):
            s_ = wstg[hp % 2]
            S.dma("sync", s_[:, 0:128], g.peer_sub_keys.ap()[hp], writes=[s_])
            b = next_bank(g)
            S.op("tensor", lambda e: e.transpose(out=b[:, 0:128], in_=s_[:, 0:128], identity=g.ident_f[:]),
                 reads=[s_, g.ident_f], writes=[b])
            S.op("vector", lambda e: e.tensor_copy(out=skT[:, hp, :], in_=b[:, 0:128]), reads=[b], writes=[skT])
        iota_i = A("iota_i", [128, 16], I32)
        iota_f = A("iota_f", [128, 16])
        S.op("gpsimd", lambda e: e.iota(out=iota_i[:], pattern=[[1, 16]], base=0, channel_multiplier=0), writes=[iota_i])
        S.op("vector", lambda e: e.tensor_copy(out=iota_f[:], in_=iota_i[:]), reads=[iota_i], writes=[iota_f])

        ya = A("ya", [128, 512], BF16); yd = A("yd", [128, 512], BF16)
        gt = A("gt", [128, 2048], BF16)
        xt = A("xt", [128, D])
        yT = A("yT", [128, 8, 128], BF16)
        t1 = A("t1", [128, D])
        t2 = A("t2", [128, D])
        mg = A("mg", [128, D], BF16)
        mgT = A("mgT", [128, 8, 128], BF16)
        x1 = A("x1", [128, D])
        junk = A("junkd", [128, D], BF16)
        ss1 = A("ss1", [128, 1]); sd1 = A("sd1", [128, 1]); rs1 = A("rs1", [128, 1])
        h2 = A("h2", [128, D], BF16)
        h2T = A("h2T", [128, 8, 128], BF16)
        qTp = A("qTp", [128, 16, 128])
        s_sb = A("s_sb", [128, 16, 128])
        s2 = A("s2", [128, 256])
        m16 = A("m16", [128, 16, 16])
        i16 = A("i16", [128, 16, 16], U32)
        cand = A("cand", [128, 8, 256])
        best = A("best", [128, 8, 16])
        pos = A("pos", [128, 8, 16], U32)
        au = A("au", [128, 128], U32); bu = A("bu", [128, 128], U32)
        af = A("af", [128, 128]); bf = A("bf", [128, 128])
        i16f = A("i16f", [128, 16, 16])
        oh = A("oh", [128, 128, 16])
        e0 = A("e0", [128, 128]); e1 = A("e1", [128, 128])
        eidx = A("eidx", [128, 128], U32)
        gd = A("gd", [128, 8, 16]); gsum = A("gsum", [128, 8]); grec = A("grec", [128, 8])
        gate = A("gate", [128, 128])
        act = A("act", [128, 128]); coef = A("coef", [128, 128])
        NB = 4
        ug = [A("ug%d" % i, [128, D], BF16) for i in range(NB)]
        vg = [A("vg%d" % i, [128, D], BF16) for i in range(NB)]
        acc = A("acc", [128, D])
        print("phase DE sbuf remaining", nc.sbuf_bytes_remaining)
        x_v = g.x.ap().rearrange("(n p) d -> n p d", p=128)
        o_v = g.out.ap().rearrange("(n p) d -> n p d", p=128)
        NTD = getattr(g, "ntd", NT)
        for tt in range(NTD):
            rows = slice(tt * 128, (tt + 1) * 128)
            S.dma("sync", ya[:], g.yatt_s[rows], reads=[g.yatt_s], writes=[ya])
            S.dma("sync", yd[:], g.ydn_s[rows], reads=[g.ydn_s], writes=[yd])
            S.dma("sync", gt[:], g.gate_s[rows], reads=[g.gate_s], writes=[gt])
            S.dma("sync", xt[:], x_v[tt], writes=[xt])
            b = next_bank(g)
            bv = b[:, :].bitcast(BF16)
            for j in range(4):
                S.op("tensor", lambda e: e.transpose(out=bv[:, j * 128:(j + 1) * 128], in_=ya[:, j * 128:(j + 1) * 128], identity=g.ident_b[:]),
                     reads=[ya, g.ident_b], writes=[b])
            for j in range(4):
                S.op("tensor", lambda e: e.transpose(out=bv[:, (4 + j) * 128:(5 + j) * 128], in_=yd[:, j * 128:(j + 1) * 128], identity=g.ident_b[:]),
                     reads=[yd, g.ident_b], writes=[b])
            S.op("scalar", lambda e: e.copy(out=yT[:].rearrange("p k t -> p (k t)"), in_=bv[:, 0:1024]), reads=[b], writes=[yT])
            for hf in range(2):
                cs = slice(hf * 512, (hf + 1) * 512)
                bA = next_bank(g)
                for kc in range(4):
                    S.op("tensor", lambda e: e.matmul(out=bA[:, 0:512], lhsT=yT[:, kc, :], rhs=wA[:, kc, cs], start=(kc == 0), stop=(kc == 3)),
                         reads=[yT, wA], writes=[bA])
                bB = next_bank(g)
                for kc in range(4):
                    S.op("tensor", lambda e: e.matmul(out=bB[:, 0:512], lhsT=yT[:, 4 + kc, :], rhs=wB[:, kc, cs], start=(kc == 0), stop=(kc == 3)),
                         reads=[yT, wB], writes=[bB])
                S.op("vector", lambda e: e.tensor_tensor(out=t1[:, cs], in0=bA[:, 0:512], in1=gt[:, cs], op=ALU.mult),
                     reads=[bA, gt], writes=[t1])
                S.op("vector", lambda e: e.tensor_tensor(out=t2[:, cs], in0=bB[:, 0:512], in1=gt[:, 1024 + hf * 512:1024 + (hf + 1) * 512], op=ALU.mult),
                     reads=[bB, gt], writes=[t2])
            S.op("gpsimd", lambda e: e.tensor_tensor(out=mg[:], in0=t1[:], in1=t2[:], op=ALU.add), reads=[t1, t2], writes=[mg])
            for half in range(2):
                b = next_bank(g)
                bv = b[:, :].bitcast(BF16)
                for j in range(4):
                    kc = half * 4 + j
                    S.op("tensor", lambda e: e.transpose(out=bv[:, j * 128:(j + 1) * 128], in_=mg[:, kc * 128:(kc + 1) * 128], identity=g.ident_b[:]),
                         reads=[mg, g.ident_b], writes=[b])
                S.op("scalar", lambda e: e.copy(out=mgT[:, half * 4:half * 4 + 4, :].rearrange("p k t -> p (k t)"), in_=bv[:, 0:512]),
                     reads=[b], writes=[mgT])
            for hf in range(2):
                cs = slice(hf * 512, (hf + 1) * 512)
                b = next_bank(g)
                for kc in range(8):
                    S.op("tensor", lambda e: e.matmul(out=b[:, 0:512], lhsT=mgT[:, kc, :], rhs=wo[:, kc, cs], start=(kc == 0), stop=(kc == 7)),
                         reads=[mgT, wo], writes=[b])
                S.op("vector", lambda e: e.tensor_tensor(out=x1[:, cs], in0=b[:, 0:512], in1=xt[:, cs], op=ALU.add),
                     reads=[b, xt], writes=[x1])
            S.op("scalar", lambda e: e.activation(out=junk[:], in_=x1[:], func=AF.Square, accum_out=ss1[:, 0:1]),
                 reads=[x1], writes=[junk, ss1])
            rstd_op(g, rs1, ss1, 1.0 / D, 1, sd1)
            S.op("vector", lambda e: e.scalar_tensor_tensor(out=h2[:], in0=x1[:], scalar=rs1[:, 0:1], in1=g2_bc[:], op0=ALU.mult, op1=ALU.mult),
                 reads=[x1, rs1, g2_bc], writes=[h2])
            for half in range(2):
                b = next_bank(g)
                bv = b[:, :].bitcast(BF16)
                for j in range(4):
                    kc = half * 4 + j
                    S.op("tensor", lambda e: e.transpose(out=bv[:, j * 128:(j + 1) * 128], in_=h2[:, kc * 128:(kc + 1) * 128], identity=g.ident_b[:]),
                         reads=[h2, g.ident_b], writes=[b])
                S.op("scalar", lambda e: e.copy(out=h2T[:, half * 4:half * 4 + 4, :].rearrange("p k t -> p (k t)"), in_=bv[:, 0:512]),
                     reads=[b], writes=[h2T])
            for q4 in range(4):
                b = next_bank(g)
                for j in range(4):
                    hp = q4 * 4 + j
                    for kc in range(8):
                        S.op("tensor", lambda e: e.matmul(out=b[:, j * 128:(j + 1) * 128], lhsT=wq[:, kc, hp * 128:(hp + 1) * 128],
                                                          rhs=h2T[:, kc, :], start=(j == 0 and kc == 0), stop=(kc == 7), skip_group_check=True),
                             reads=[wq, h2T], writes=[b])
                dst = qTp[:, q4 * 4:q4 * 4 + 4, :].rearrange("p a t -> p (a t)")
                if q4 % 2 == 0:
                    S.op("scalar", lambda e: e.copy(out=dst, in_=b[:, 0:512]), reads=[b], writes=[qTp])
                else:
                    S.op("vector", lambda e: e.tensor_copy(out=dst, in_=b[:, 0:512]), reads=[b], writes=[qTp])
            for q4 in range(4):
                b = next_bank(g)
                for j in range(4):
                    hp = q4 * 4 + j
                    S.op("tensor", lambda e: e.matmul(out=b[:, j * 128:(j + 1) * 128], lhsT=qTp[:, hp, :], rhs=skT[:, hp, :],
                                                      start=(j == 0), stop=True, skip_group_check=True),
                         reads=[qTp, skT], writes=[b])
                S.op("scalar", lambda e: e.copy(out=s_sb[:, q4 * 4:q4 * 4 + 4, :].rearrange("p a t -> p (a t)"), in_=b[:, 0:512]),
                     reads=[b], writes=[s_sb])
            for hp in range(16):
                sv = s_sb[:, hp, :]
                S.op("vector", lambda e: e.max(out=m16[:, hp, 0:8], in_=sv), reads=[s_sb], writes=[m16])
                S.op("vector", lambda e: e.max_index(out=i16[:, hp, 0:8], in_max=m16[:, hp, 0:8], in_values=sv), reads=[s_sb, m16], writes=[i16])
                S.op("vector", lambda e: e.match_replace(out=s2[:, 0:128], in_to_replace=m16[:, hp, 0:8], in_values=sv, imm_value=-1e30),
                     reads=[s_sb, m16], writes=[s2])
                S.op("vector", lambda e: e.max(out=m16[:, hp, 8:16], in_=s2[:, 0:128]), reads=[s2], writes=[m16])
                S.op("vector", lambda e: e.max_index(out=i16[:, hp, 8:16], in_max=m16[:, hp, 8:16], in_values=s2[:, 0:128]),
                     reads=[s2, m16], writes=[i16])
            m16v = m16[:].rearrange("p (h two) a -> p h two a", two=2)
            S.op("vector", lambda e: e.tensor_tensor(out=cand[:].rearrange("p h (a b) -> p h a b", b=16),
                                                     in0=m16v[:, :, 0, :].unsqueeze(3).to_broadcast([128, 8, 16, 16]),
                                                     in1=m16v[:, :, 1, :].unsqueeze(2).to_broadcast([128, 8, 16, 16]), op=ALU.add),
                 reads=[m16], writes=[cand])
            for h in range(8):
                cv = cand[:, h, :]
                S.op("vector", lambda e: e.max(out=best[:, h, 0:8], in_=cv), reads=[cand], writes=[best])
                S.op("vector", lambda e: e.max_index(out=pos[:, h, 0:8], in_max=best[:, h, 0:8], in_values=cv), reads=[cand, best], writes=[pos])
                S.op("vector", lambda e: e.match_replace(out=s2[:], in_to_replace=best[:, h, 0:8], in_values=cv, imm_value=-1e30),
                     reads=[cand, best], writes=[s2])
                S.op("vector", lambda e: e.max(out=best[:, h, 8:16], in_=s2[:]), reads=[s2], writes=[best])
                S.op("vector", lambda e: e.max_index(out=pos[:, h, 8:16], in_max=best[:, h, 8:16], in_values=s2[:]), reads=[s2, best], writes=[pos])
            posf = pos[:].rearrange("p h k -> p (h k)")
            S.op("vector", lambda e: e.tensor_scalar(out=au[:], in0=posf, scalar1=4, scalar2=None, op0=ALU.logical_shift_right), reads=[pos], writes=[au])
            S.op("vector", lambda e: e.tensor_scalar(out=bu[:], in0=posf, scalar1=15, scalar2=None, op0=ALU.bitwise_and), reads=[pos], writes=[bu])
            S.op("vector", lambda e: e.tensor_copy(out=af[:], in_=au[:]), reads=[au], writes=[af])
            S.op("vector", lambda e: e.tensor_copy(out=bf[:], in_=bu[:]), reads=[bu], writes=[bf])
            S.op("vector", lambda e: e.tensor_copy(out=i16f[:], in_=i16[:]), reads=[i16], writes=[i16f])
            i16fv = i16f[:].rearrange("p (h two) a -> p h two a", two=2)
            for which, (sel, dst) in enumerate(((af, e0), (bf, e1))):
                S.op("vector", lambda e: e.tensor_tensor(out=oh[:], in0=sel[:, :].unsqueeze(2).to_broadcast([128, 128, 16]),
                                                         in1=iota_f[:, :].unsqueeze(1).to_broadcast([128, 128, 16]), op=ALU.is_equal),
                     reads=[sel, iota_f], writes=[oh])
                S.op("vector", lambda e: e.tensor_tensor(out=oh[:].rearrange("p (h k) a -> p h k a", k=16),
                                                         in0=oh[:].rearrange("p (h k) a -> p h k a", k=16),
                                                         in1=i16fv[:, :, which, :].unsqueeze(2).to_broadcast([128, 8, 16, 16]), op=ALU.mult),
                     reads=[oh, i16f], writes=[oh])
                S.op("vector", lambda e: e.tensor_reduce(out=dst[:], in_=oh[:], axis=AX.X, op=ALU.add), reads=[oh], writes=[dst])
            S.op("vector", lambda e: e.scalar_tensor_tensor(out=e0[:], in0=e0[:], scalar=128.0, in1=e1[:], op0=ALU.mult, op1=ALU.add),
                 reads=[e0, e1], writes=[e0])
            S.op("vector", lambda e: e.tensor_copy(out=eidx[:], in_=e0[:]), reads=[e0], writes=[eidx])
            S.op("vector", lambda e: e.tensor_tensor(out=gd[:], in0=best[:], in1=best[:, :, 0:1].to_broadcast([128, 8, 16]), op=ALU.subtract),
                 reads=[best], writes=[gd])
            S.op("scalar", lambda e: e.activation(out=gd[:], in_=gd[:], func=AF.Exp), reads=[gd], writes=[gd])
            S.op("vector", lambda e: e.tensor_reduce(out=gsum[:], in_=gd[:], axis=AX.X, op=ALU.add), reads=[gd], writes=[gsum])
            S.op("vector", lambda e: e.reciprocal(out=grec[:], in_=gsum[:]), reads=[gsum], writes=[grec])
            S.op("vector", lambda e: e.tensor_tensor(out=gate[:].rearrange("p (h k) -> p h k", k=16), in0=gd[:],
                                                     in1=grec[:, :].unsqueeze(2).to_broadcast([128, 8, 16]), op=ALU.mult),
                 reads=[gd, grec], writes=[gate])
            for j in range(128):
                ub = ug[j % NB]
                S.dma("gpsimd", ub[:], g.ub_s[:, :], reads=[eidx, g.ub_s], writes=[ub],
                      indirect=bass.IndirectOffsetOnAxis(ap=eidx[:, j:j + 1], axis=0))
                S.op("vector", lambda e: e.scalar_tensor_tensor(out=junk[:], in0=ub[:], scalar=1.0, in1=h2[:], op0=ALU.mult, op1=ALU.mult,
                                                                accum_out=act[:, j:j + 1]),
                     reads=[ub, h2], writes=[junk, act])
            S.op("scalar", lambda e: e.activation(out=coef[:], in_=act[:], func=AF.Gelu), reads=[act], writes=[coef])
            S.op("vector", lambda e: e.tensor_tensor(out=coef[:], in0=coef[:], in1=gate[:], op=ALU.mult), reads=[coef, gate], writes=[coef])
            for j in range(128):
                vb_ = vg[j % NB]
                S.dma("gpsimd", vb_[:], g.vb_s[:, :], reads=[eidx, g.vb_s], writes=[vb_],
                      indirect=bass.IndirectOffsetOnAxis(ap=eidx[:, j:j + 1], axis=0))
                if j == 0:
                    S.op("vector", lambda e: e.scalar_tensor_tensor(out=acc[:], in0=vb_[:], scalar=coef[:, 0:1], in1=x1[:], op0=ALU.mult, op1=ALU.add),
                         reads=[vb_, coef, x1], writes=[acc])
                else:
                    S.op("vector", lambda e: e.scalar_tensor_tensor(out=acc[:], in0=vb_[:], scalar=coef[:, j:j + 1], in1=acc[:], op0=ALU.mult, op1=ALU.add),
                         reads=[vb_, coef, acc], writes=[acc])
            S.dma("sync", o_v[tt], acc[:], reads=[acc])
        S.barrier()


_CACHE = {}


def kernel(**inputs):
    x = np.asarray(inputs["x"], dtype=np.float32)
    if "nc" not in _CACHE:
        _CACHE["nc"] = build_program()
    nc = _CACHE["nc"]
    shared = {}
    for k in ("norm1_gain", "w_in", "q_norm_gain", "k_norm_gain", "dn_conv_w", "dn_a_log", "dn_dt_bias",
              "dn_out_norm_gain", "w_att_branch", "w_dn_branch", "w_o", "norm2_gain", "peer_w_query",
              "peer_u", "peer_v"):
        a = np.asarray(inputs[k], dtype=np.float32)[0]
        if a.ndim == 1:
            a = a.reshape(1, -1)
        shared[k] = np.ascontiguousarray(a)
    shared["peer_sub_keys"] = np.ascontiguousarray(
        np.asarray(inputs["peer_sub_keys"], dtype=np.float32)[0].reshape(16, 128, 128))
    in_maps = []
    for c in range(N_CORES):
        m = dict(shared)
        m["x"] = np.ascontiguousarray(x[c])
        in_maps.append(m)
    res = run_bass_kernel_spmd(nc, in_maps, core_ids=list(range(N_CORES)))
    out = np.stack([np.asarray(r["out"]) for r in res.results], axis=0)
    return out.astype(np.float32)
```

```python
import contextlib
import numpy as np
import concourse.bass as bass
import concourse.mybir as mybir
from concourse.bass_utils import run_bass_kernel_spmd

F32 = mybir.dt.float32
BF16 = mybir.dt.bfloat16
I32 = mybir.dt.int32
U32 = mybir.dt.uint32
AF = mybir.ActivationFunctionType
ALU = mybir.AluOpType
AX = mybir.AxisListType

T = 4096
D = 1024
NT = T // 128
IN_W = 5456
EPS = 1e-6
N_CORES = 8

C_AQ, C_AK, C_AV, C_IQ, C_IK, C_IW = 0, 512, 640, 768, 1280, 1344
C_DQ, C_DK, C_DV, C_DZ, C_DA, C_DB, C_GA, C_GB = 1352, 1864, 2376, 2888, 3400, 3404, 3408, 4432


class Res:
    __slots__ = ("name", "writer", "readers")

    def __init__(self, name):
        self.name = name
        self.writer = None
        self.readers = {}


class Tl:
    def __init__(self, t, name):
        self.t = t.ap() if hasattr(t, "ap") and "DRam" in type(t).__name__ else t
        self.r = Res(name)

    def __getitem__(self, idx):
        return self.t[idx]


class _RecEng:
    def __getattr__(self, name):
        def f(*args, **kw):
            self.__dict__["name"] = name
            self.__dict__["args"] = args
            self.__dict__["kw"] = kw
            return None
        return f


class Sched:
    CE = ("tensor", "vector", "scalar", "gpsimd")

    def __init__(self, nc, st, n_dma=12):
        self.nc = nc
        self.st = st
        self.eng = {n: getattr(nc, n) for n in self.CE + ("sync",)}
        self.sem = {}
        self.cnt = {}
        for n in self.CE:
            self.sem[n] = st.enter_context(nc.semaphore("s_" + n))
            self.cnt[n] = 0
        self.dq = {}
        for q in ("sync", "gpsimd"):
            sems = [st.enter_context(nc.semaphore("d_%s_%d" % (q, i))) for i in range(n_dma)]
            for i, s in enumerate(sems):
                self.sem[(q, i)] = s
                self.cnt[(q, i)] = 0
            self.dq[q] = [0, n_dma]
        self.seen = {}
        self.ninst = 0
        self.rec = None

    def record(self):
        self.rec = []

    def stop(self):
        r = self.rec
        self.rec = None
        return r

    def replay(self, *lists):
        lists = [l for l in lists if l]
        items = []
        for li, l in enumerate(lists):
            n = len(l)
            for i, it in enumerate(l):
                items.append(((i + 0.5) / n, li, i, it))
        items.sort(key=lambda t: (t[0], t[1], t[2]))
        for _, _, _, it in items:
            if it[0] == "op":
                _, E, name, args, kw, reads, writes = it
                self.op(E, lambda e: getattr(e, name)(*args, **kw), reads, writes)
            else:
                _, q, out, in_, reads, writes, indirect, kw = it
                self.dma(q, out, in_, reads, writes, indirect, **kw)

    def _wait(self, E, key, val):
        if val <= 0:
            return
        if E == "tensor" and key == "tensor":
            return
        k = (E, key)
        if self.seen.get(k, 0) >= val:
            return
        self.eng[E].wait_ge(self.sem[key], val)
        self.seen[k] = val
        self.ninst += 1

    def _deps(self, E, reads, writes):
        for r in reads:
            r = getattr(r, "r", r)
            if r.writer is not None:
                self._wait(E, *r.writer)
        for w in writes:
            w = getattr(w, "r", w)
            if w.writer is not None:
                self._wait(E, *w.writer)
            for key, val in w.readers.items():
                self._wait(E, key, val)

    def _mark(self, ev, reads, writes):
        key, val = ev
        for r in reads:
            r = getattr(r, "r", r)
            r.readers[key] = val
        for w in writes:
            w = getattr(w, "r", w)
            w.writer = ev
            w.readers = {}

    def op(self, E, emit, reads=(), writes=()):
        if self.rec is not None:
            r = _RecEng()
            emit(r)
            self.rec.append(("op", E, r.name, r.args, r.kw, list(reads), list(writes)))
            return
        self._deps(E, reads, writes)
        inst = emit(self.eng[E])
        self.cnt[E] += 1
        inst.then_inc(self.sem[E], 1)
        self.ninst += 1
        self._mark((E, self.cnt[E]), reads, writes)

    def dma(self, q, out, in_, reads=(), writes=(), indirect=None, **kw):
        if self.rec is not None:
            self.rec.append(("dma", q, out, in_, list(reads), list(writes), indirect, kw))
            return
        st = self.dq[q]
        i = st[0]
        st[0] = (i + 1) % st[1]
        key = (q, i)
        self._wait(q, key, self.cnt[key])
        self._deps(q, reads, writes)
        if indirect is not None:
            inst = self.eng[q].indirect_dma_start(out=out, out_offset=None, in_=in_, in_offset=indirect, **kw)
        else:
            inst = self.eng[q].dma_start(out=out, in_=in_, **kw)
        self.cnt[key] += 16
        inst.then_inc(self.sem[key], 16)
        self.ninst += 1
        self._mark((key, self.cnt[key]), reads, writes)

    def barrier(self):
        for E in self.CE + ("sync",):
            for key, val in self.cnt.items():
                if key == E:
                    continue
                if E == "tensor" and key == "tensor":
                    continue
                self._wait(E, key, val)

    def finish(self):
        for E in self.CE + ("sync",):
            for key, val in self.cnt.items():
                if key == E:
                    continue
                k = (E, key)
                if val > 0 and self.seen.get(k, 0) < val:
                    self.eng[E].wait_ge(self.sem[key], val)
                    self.seen[k] = val


class Ctx:
    pass


def build_program(debug=False, phases=("A", "B", "C", "P", "D"), **opts):
    nc = bass.Bass("TRN2", target_bir_lowering=False)
    g = Ctx()
    for k_, v_ in opts.items():
        setattr(g, k_, v_)
    g.nc = nc
    g.debug = debug

    def din(name, shape, dt=F32):
        return nc.dram_tensor(name, list(shape), dt, kind="ExternalInput")

    g.x = din("x", [T, D])
    g.norm1_gain = din("norm1_gain", [1, D])
    g.w_in = din("w_in", [D, IN_W])
    g.q_norm_gain = din("q_norm_gain", [1, 64])
    g.k_norm_gain = din("k_norm_gain", [1, 64])
    g.dn_conv_w = din("dn_conv_w", [4, 1536])
    g.dn_a_log = din("dn_a_log", [1, 4])
    g.dn_dt_bias = din("dn_dt_bias", [1, 4])
    g.dn_out_norm_gain = din("dn_out_norm_gain", [1, 128])
    g.w_att_branch = din("w_att_branch", [512, D])
    g.w_dn_branch = din("w_dn_branch", [512, D])
    g.w_o = din("w_o", [D, D])
    g.norm2_gain = din("norm2_gain", [1, D])
    g.peer_w_query = din("peer_w_query", [D, 2048])
    g.peer_sub_keys = din("peer_sub_keys", [16, 128, 128])
    g.peer_u = din("peer_u", [16384, D])
    g.peer_v = din("peer_v", [16384, D])
    g.out = nc.dram_tensor("out", [T, D], F32, kind="ExternalOutput")

    skind = "ExternalOutput" if debug else "Internal"

    def dscr(name, shape, dt):
        return Tl(nc.dram_tensor(name, list(shape), dt, kind=skind), name)

    g.qT_s = dscr("qT_s", [NT, 64, 8, 128], BF16)
    g.iqT_s = dscr("iqT_s", [NT, 64, 8, 128], BF16)
    g.kT_s = dscr("kT_s", [64, 2, T], BF16)
    g.ikT_s = dscr("ikT_s", [64, T], BF16)
    g.v_s = dscr("v_s", [T, 128], BF16)
    g.iw_s = dscr("iw_s", [128, NT, 8], F32)
    g.gb_s = dscr("gb_s", [128, NT, 8], F32)
    g.dnq_s = dscr("dnq_s", [T, 512], F32)
    g.dnk_s = dscr("dnk_s", [T, 512], F32)
    g.dnv_s = dscr("dnv_s", [T, 512], F32)
    g.dz_s = dscr("dz_s", [T, 512], BF16)
    g.gate_s = dscr("gate_s", [T, 2048], BF16)
    g.yatt_s = dscr("yatt_s", [T, 512], BF16)
    g.ydn_s = dscr("ydn_s", [T, 512], BF16)
    g.uv_s = Tl(nc.dram_tensor("uv_s", [16384, 2 * D], BF16, kind="Internal"), "uv_s")

    with contextlib.ExitStack() as st:
        S = Sched(nc, st)
        g.S = S
        g.st = st
        g.banks = [Tl(st.enter_context(nc.psum_tensor("bank%d" % i, [128, 512], F32)), "bank%d" % i)
                   for i in range(8)]
        g.bank_rr = 0
        setup_consts(g)
        if "A" in phases:
            phase_a(g)
        if "B" in phases:
            phase_b(g)
        if "C" in phases:
            phase_c(g)
        if "P" in phases:
            phase_p(g)
        if "D" in phases:
            phase_de(g)
        S.finish()
    return nc


def next_bank(g):
    n = getattr(g, "nrot", 8)
    g.bank_rr = g.bank_rr % n
    b = g.banks[g.bank_rr]
    g.bank_rr = (g.bank_rr + 1) % n
    return b


def sb(g, st, name, shape, dt):
    g.uid = getattr(g, "uid", 0) + 1
    name = "%s_%d" % (name, g.uid)
    return Tl(st.enter_context(g.nc.sbuf_tensor(name, list(shape), dt)), name)


def setup_consts(g):
    nc, S, st = g.nc, g.S, g.st
    g.fill0 = nc.gpsimd.to_reg(0.0)
    g.fillneg = nc.gpsimd.to_reg(-1e30)
    g.ones_f = sb(g, st, "ones_f", [128, 128], F32)
    g.ident_f = sb(g, st, "ident_f", [128, 128], F32)
    g.ident_b = sb(g, st, "ident_b", [128, 128], BF16)
    g.eps_t = sb(g, st, "eps_t", [128, 1], F32)
    S.op("gpsimd", lambda e: e.memset(g.ones_f[:], 1.0), writes=[g.ones_f])
    S.op("gpsimd", lambda e: e.memset(g.eps_t[:], EPS), writes=[g.eps_t])
    S.op("gpsimd", lambda e: e.affine_select(out=g.ident_f[:], in_=g.ones_f[:], pattern=[[-1, 128]],
                                             compare_op=ALU.is_equal, fill=g.fill0, base=0, channel_multiplier=1),
         reads=[g.ones_f], writes=[g.ident_f])
    S.op("vector", lambda e: e.tensor_copy(out=g.ident_b[:], in_=g.ident_f[:]), reads=[g.ident_f], writes=[g.ident_b])


def bcast_row(ap_row, n):
    return ap_row.partition_broadcast(n)


def rstd_op(g, out, ss, scale, n_free, tmp):
    S = g.S
    S.op("scalar", lambda e: e.activation(out=tmp[:, 0:n_free], in_=ss[:, 0:n_free], func=AF.Sqrt,
                                          bias=g.eps_t[:, 0:1], scale=scale),
         reads=[ss, g.eps_t], writes=[tmp])
    S.op("vector", lambda e: e.reciprocal(out=out[:, 0:n_free], in_=tmp[:, 0:n_free]), reads=[tmp], writes=[out])


def phase_a(g):
    nc, S = g.nc, g.S
    with contextlib.ExitStack() as ph:
        def A(name, shape, dt):
            return sb(g, ph, name, shape, dt)

        w_bf = A("w_bf", [128, 8, IN_W], BF16)
        WCH = 1364
        wst = [A("wst%d" % i, [128, WCH], F32) for i in range(2)]
        w_in_v = g.w_in.ap().rearrange("(kc p) n -> p kc n", p=128)
        k = 0
        for kc in range(8):
            for c in range(IN_W // WCH):
                s_ = wst[k % 2]
                S.dma("sync", s_[:], w_in_v[:, kc, c * WCH:(c + 1) * WCH], writes=[s_])
                eng = ("vector", "gpsimd", "scalar")[k % 3]
                dst = w_bf[:, kc, c * WCH:(c + 1) * WCH]
                if eng == "scalar":
                    S.op(eng, lambda e: e.copy(out=dst, in_=s_[:]), reads=[s_], writes=[w_bf])
                else:
                    S.op(eng, lambda e: e.tensor_copy(out=dst, in_=s_[:]), reads=[s_], writes=[w_bf])
                k += 1

        g1_bc = A("g1_bc", [128, D], F32)
        S.dma("sync", g1_bc[:], g.norm1_gain.ap().partition_broadcast(128), writes=[g1_bc])
        gq_bc = A("gq_bc", [128, 64], F32)
        gk_bc = A("gk_bc", [128, 64], F32)
        S.dma("sync", gq_bc[:], g.q_norm_gain.ap().partition_broadcast(128), writes=[gq_bc])
        S.dma("sync", gk_bc[:], g.k_norm_gain.ap().partition_broadcast(128), writes=[gk_bc])
        S.op("vector", lambda e: e.tensor_scalar(out=gq_bc[:], in0=gq_bc[:], scalar1=0.125, scalar2=None, op0=ALU.mult),
             reads=[gq_bc], writes=[gq_bc])
        cw_bc = A("cw_bc", [128, 4, 1536], F32)
        for j in range(4):
            S.dma("sync", cw_bc[:, j, :], g.dn_conv_w.ap()[j:j + 1, :].partition_broadcast(128), writes=[cw_bc])
        dtb_bc = A("dtb_bc", [128, 4], F32)
        nea_bc = A("nea_bc", [128, 4], F32)
        S.dma("sync", dtb_bc[:], g.dn_dt_bias.ap().partition_broadcast(128), writes=[dtb_bc])
        S.dma("sync", nea_bc[:], g.dn_a_log.ap().partition_broadcast(128), writes=[nea_bc])
        S.op("scalar", lambda e: e.activation(out=nea_bc[:], in_=nea_bc[:], func=AF.Exp), reads=[nea_bc], writes=[nea_bc])
        S.op("vector", lambda e: e.tensor_scalar(out=nea_bc[:], in0=nea_bc[:], scalar1=-1.0, scalar2=None, op0=ALU.mult),
             reads=[nea_bc], writes=[nea_bc])

        shf = A("shf", [128, 128], F32)
        Sh = [g.ident_b] + [A("sh%d" % d, [128, 128], BF16) for d in (1, 2, 3)]
        ShP = [None] + [A("shp%d" % d, [128, 128], BF16) for d in (1, 2, 3)]
        for d in (1, 2, 3):
            S.op("gpsimd", lambda e: e.affine_select(out=shf[:], in_=g.ones_f[:], pattern=[[-1, 128]],
                                                     compare_op=ALU.is_equal, fill=g.fill0, base=d, channel_multiplier=1),
                 reads=[g.ones_f], writes=[shf])
            S.op("vector", lambda e: e.tensor_copy(out=Sh[d][:], in_=shf[:]), reads=[shf], writes=[Sh[d]])
            S.op("gpsimd", lambda e: e.affine_select(out=shf[:], in_=g.ones_f[:], pattern=[[-1, 128]],
                                                     compare_op=ALU.is_equal, fill=g.fill0, base=d - 128, channel_multiplier=1),
                 reads=[g.ones_f], writes=[shf])
            S.op("vector", lambda e: e.tensor_copy(out=ShP[d][:], in_=shf[:]), reads=[shf], writes=[ShP[d]])

        xt = [A("xt%d" % i, [128, D], F32) for i in range(2)]
        junk = A("junk", [128, D], BF16)
        h_bf = A("h_bf", [128, D], BF16)
        hT = [A("hT%d" % i, [128, 8, 128], BF16) for i in range(2)]
        ss1 = A("ss1", [128, 1], F32)
        sd1 = A("sd1", [128, 1], F32)
        rs1 = A("rs1", [128, 1], F32)
        sq = A("sq", [128, 512], F32)
        ss8 = A("ss8", [128, 8], F32)
        sd8 = A("sd8", [128, 8], F32)
        rs8 = A("rs8", [128, 8], F32)
        tmpf = A("tmpf", [128, 512], F32)
        qn_bf = A("qn_bf", [128, 512], BF16)
        kn_bf = A("kn_bf", [128, 128], BF16)
        iq_bf = A("iq_bf", [128, 512], BF16)
        ik_bf = A("ik_bf", [128, 64], BF16)
        qT_t = [A("qT_t%d" % i, [64, 8, 128], BF16) for i in range(2)]
        iqT_t = [A("iqT_t%d" % i, [64, 8, 128], BF16) for i in range(2)]
        kT_t = [A("kT_t%d" % i, [64, 2, 128], BF16) for i in range(2)]
        ikT_t = [A("ikT_t%d" % i, [64, 128], BF16) for i in range(2)]
        v_t = [A("v_t%d" % i, [128, 128], BF16) for i in range(2)]
        iw_all = A("iw_all", [128, NT, 8], F32)
        gb_all = A("gb_all", [128, NT, 8], F32)
        ab_tmp = A("ab_tmp", [128, 4], F32)
        xw = [A("xw%d" % i, [128, 4, 1536], BF16) for i in range(2)]
        yc = [A("yc%d" % i, [128, 512], F32) for i in range(3)]
        dz_t = [A("dz_t%d" % i, [128, 512], BF16) for i in range(2)]
        gt_t = [A("gt_t%d" % i, [128, 1024], BF16) for i in range(2)]
        print("phase A sbuf remaining", nc.sbuf_bytes_remaining)

        def proj(cols, lo, hi, hTt):
            b = next_bank(g)
            n = hi - lo
            for kc in range(8):
                S.op("tensor", lambda e: e.matmul(out=b[:, 0:n], lhsT=hTt[:, kc, :], rhs=w_bf[:, kc, lo:hi],
                                                  start=(kc == 0), stop=(kc == 7)),
                     reads=[hTt, w_bf], writes=[b])
            return b

        def headnorm(ps, nh, gain_bc, out_bf):
            n = nh * 64
            S.op("scalar", lambda e: e.activation(out=sq[:, 0:n], in_=ps[:, 0:n], func=AF.Square), reads=[ps], writes=[sq])
            S.op("vector", lambda e: e.tensor_reduce(out=ss8[:, 0:nh], in_=sq[:, 0:n].rearrange("p (h d) -> p h d", d=64),
                                                     axis=AX.X, op=ALU.add), reads=[sq], writes=[ss8])
            rstd_op(g, rs8, ss8, 1.0 / 64, nh, sd8)
            S.op("vector", lambda e: e.tensor_tensor(out=tmpf[:, 0:n].rearrange("p (h d) -> p h d", d=64),
                                                     in0=ps[:, 0:n].rearrange("p (h d) -> p h d", d=64),
                                                     in1=rs8[:, 0:nh].unsqueeze(2).to_broadcast([128, nh, 64]), op=ALU.mult),
                 reads=[ps, rs8], writes=[tmpf])
            S.op("vector", lambda e: e.tensor_tensor(out=out_bf[:, 0:n].rearrange("p (h d) -> p h d", d=64),
                                                     in0=tmpf[:, 0:n].rearrange("p (h d) -> p h d", d=64),
                                                     in1=gain_bc[:, :].unsqueeze(1).to_broadcast([128, nh, 64]), op=ALU.mult),
                 reads=[tmpf, gain_bc], writes=[out_bf])

        def transpose_heads(src_bf, nh, dstT, eng):
            b = next_bank(g)
            bv = b[:, :].bitcast(BF16)
            for h in range(nh):
                S.op("tensor", lambda e: e.transpose(out=bv[0:64, h * 128:(h + 1) * 128], in_=src_bf[:, h * 64:(h + 1) * 64],
                                                     identity=g.ident_b[:]),
                     reads=[src_bf, g.ident_b], writes=[b])
            src = bv[0:64, 0:nh * 128]
            dst = dstT[:, :, :].rearrange("p h t -> p (h t)") if nh > 1 else dstT[:, :]
            if eng == "scalar":
                S.op("scalar", lambda e: e.copy(out=dst, in_=src), reads=[b], writes=[dstT])
            else:
                S.op("vector", lambda e: e.tensor_copy(out=dst, in_=src), reads=[b], writes=[dstT])

        x_v = g.x.ap().rearrange("(n p) d -> n p d", p=128)
        S.dma("sync", xt[0][:], x_v[0], writes=[xt[0]])
        NTR = getattr(g, "ntr", NT)
        SECT = getattr(g, "sect", 99)
        for tt in range(NTR):
            cur = tt % 2
            if tt + 1 < NTR:
                S.dma("sync", xt[1 - cur][:], x_v[tt + 1], writes=[xt[1 - cur]])
            x_t = xt[cur]
            hTt = hT[cur]
            rows = slice(tt * 128, (tt + 1) * 128)
            S.op("scalar", lambda e: e.activation(out=junk[:], in_=x_t[:], func=AF.Square, accum_out=ss1[:, 0:1]),
                 reads=[x_t], writes=[junk, ss1])
            rstd_op(g, rs1, ss1, 1.0 / D, 1, sd1)
            S.op("vector", lambda e: e.scalar_tensor_tensor(out=h_bf[:], in0=x_t[:], scalar=rs1[:, 0:1], in1=g1_bc[:],
                                                            op0=ALU.mult, op1=ALU.mult),
                 reads=[x_t, rs1, g1_bc], writes=[h_bf])
            for half in range(2):
                b = next_bank(g)
                bv = b[:, :].bitcast(BF16)
                for j in range(4):
                    kc = half * 4 + j
                    S.op("tensor", lambda e: e.transpose(out=bv[:, j * 128:(j + 1) * 128], in_=h_bf[:, kc * 128:(kc + 1) * 128],
                                                         identity=g.ident_b[:]),
                         reads=[h_bf, g.ident_b], writes=[b])
                dst = hTt[:, half * 4:half * 4 + 4, :].rearrange("p k t -> p (k t)")
                if half == 0:
                    S.op("scalar", lambda e: e.copy(out=dst, in_=bv[:, 0:512]), reads=[b], writes=[hTt])
                else:
                    S.op("vector", lambda e: e.tensor_copy(out=dst, in_=bv[:, 0:512]), reads=[b], writes=[hTt])

            if SECT < 1:
                continue
            ps = proj("aq", C_AQ, C_AQ + 512, hTt)
            headnorm(ps, 8, gq_bc, qn_bf)
            transpose_heads(qn_bf, 8, qT_t[cur], "scalar")
            S.dma("gpsimd", g.qT_s[tt], qT_t[cur][:], reads=[qT_t[cur]], writes=[g.qT_s])
            if SECT < 2:
                continue
            ps = proj("kv", C_AK, C_AK + 256, hTt)
            headnorm(ps, 2, gk_bc, kn_bf)
            S.op("scalar", lambda e: e.copy(out=v_t[cur][:], in_=ps[:, 128:256]), reads=[ps], writes=[v_t[cur]])
            transpose_heads(kn_bf, 2, kT_t[cur], "vector")
            S.dma("gpsimd", g.kT_s[:, :, rows], kT_t[cur][:], reads=[kT_t[cur]], writes=[g.kT_s])
            S.dma("gpsimd", g.v_s[rows], v_t[cur][:], reads=[v_t[cur]], writes=[g.v_s])
            if SECT < 3:
                continue
            ps = proj("iq", C_IQ, C_IQ + 512, hTt)
            S.op("scalar", lambda e: e.copy(out=iq_bf[:], in_=ps[:, 0:512]), reads=[ps], writes=[iq_bf])
            transpose_heads(iq_bf, 8, iqT_t[cur], "vector")
            S.dma("gpsimd", g.iqT_s[tt], iqT_t[cur][:], reads=[iqT_t[cur]], writes=[g.iqT_s])
            if SECT < 4:
                continue
            ps = proj("ikw", C_IK, C_IK + 72, hTt)
            S.op("vector", lambda e: e.tensor_copy(out=ik_bf[:], in_=ps[:, 0:64]), reads=[ps], writes=[ik_bf])
            S.op("scalar", lambda e: e.copy(out=iw_all[:, tt, :], in_=ps[:, 64:72]), reads=[ps], writes=[iw_all])
            transpose_heads(ik_bf, 1, ikT_t[cur], "scalar")
            if "ikT" not in getattr(g, "skip", ()):
                S.dma("gpsimd", g.ikT_s[:, rows], ikT_t[cur][:], reads=[ikT_t[cur]], writes=[g.ikT_s])
            if SECT < 5:
                continue
            ps = proj("ab", C_DA, C_DA + 8, hTt)
            S.op("vector", lambda e: e.tensor_tensor(out=ab_tmp[:], in0=ps[:, 0:4], in1=dtb_bc[:], op=ALU.add),
                 reads=[ps, dtb_bc], writes=[ab_tmp])
            S.op("scalar", lambda e: e.activation(out=ab_tmp[:], in_=ab_tmp[:], func=AF.Exp), reads=[ab_tmp], writes=[ab_tmp])
            S.op("scalar", lambda e: e.activation(out=ab_tmp[:], in_=ab_tmp[:], func=AF.Ln, bias=g.ones_f[:, 0:1], scale=1.0),
                 reads=[ab_tmp, g.ones_f], writes=[ab_tmp])
            S.op("vector", lambda e: e.tensor_tensor(out=gb_all[:, tt, 0:4], in0=ab_tmp[:], in1=nea_bc[:], op=ALU.mult),
                 reads=[ab_tmp, nea_bc], writes=[gb_all])
            S.op("scalar", lambda e: e.activation(out=gb_all[:, tt, 4:8], in_=ps[:, 4:8], func=AF.Sigmoid),
                 reads=[ps], writes=[gb_all])
            if SECT < 6:
                continue
            for gi, (c0, dst_s) in enumerate(((C_DQ, g.dnq_s), (C_DK, g.dnk_s), (C_DV, g.dnv_s))):
                ps = proj("dn", c0, c0 + 512, hTt)
                cs = slice(gi * 512, (gi + 1) * 512)
                for j in range(4):
                    S.op("vector", lambda e: e.tensor_tensor(out=xw[cur][:, j, cs], in0=ps[:, 0:512], in1=cw_bc[:, j, cs], op=ALU.mult),
                         reads=[ps, cw_bc], writes=[xw[cur]])
                b = next_bank(g)
                mm = []
                for j in range(4):
                    mm.append((Sh[3 - j], xw[cur], j))
                if tt > 0:
                    for j in range(3):
                        mm.append((ShP[3 - j], xw[1 - cur], j))
                for i, (sh, xsrc, j) in enumerate(mm):
                    S.op("tensor", lambda e: e.matmul(out=b[:, 0:512], lhsT=sh[:], rhs=xsrc[:, j, cs],
                                                      start=(i == 0), stop=(i == len(mm) - 1)),
                         reads=[sh, xsrc], writes=[b])
                y = yc[gi]
                S.op("scalar", lambda e: e.activation(out=y[:], in_=b[:, 0:512], func=AF.Silu), reads=[b], writes=[y])
                if gi < 2:
                    S.op("gpsimd", lambda e: e.tensor_tensor(out=sq[:], in0=y[:], in1=y[:], op=ALU.mult), reads=[y], writes=[sq])
                    S.op("vector", lambda e: e.tensor_reduce(out=ss8[:, 0:4], in_=sq[:].rearrange("p (h d) -> p h d", d=128),
                                                             axis=AX.X, op=ALU.add), reads=[sq], writes=[ss8])
                    rstd_op(g, rs8, ss8, 1.0, 4, sd8)
                    if gi == 0:
                        S.op("vector", lambda e: e.tensor_scalar(out=rs8[:, 0:4], in0=rs8[:, 0:4], scalar1=128 ** -0.5, scalar2=None,
                                                                 op0=ALU.mult), reads=[rs8], writes=[rs8])
                    S.op("vector", lambda e: e.tensor_tensor(out=y[:].rearrange("p (h d) -> p h d", d=128),
                                                             in0=y[:].rearrange("p (h d) -> p h d", d=128),
                                                             in1=rs8[:, 0:4].unsqueeze(2).to_broadcast([128, 4, 128]), op=ALU.mult),
                         reads=[y, rs8], writes=[y])
                S.dma("gpsimd", dst_s[rows], y[:], reads=[y], writes=[dst_s])
            if SECT < 7:
                continue
            ps = proj("dz", C_DZ, C_DZ + 512, hTt)
            S.op("scalar", lambda e: e.activation(out=dz_t[cur][:], in_=ps[:, 0:512], func=AF.Silu), reads=[ps], writes=[dz_t[cur]])
            S.dma("gpsimd", g.dz_s[rows], dz_t[cur][:], reads=[dz_t[cur]], writes=[g.dz_s])
            if SECT < 8:
                continue
            for gi, c0 in enumerate((C_GA, C_GB)):
                for hf in range(2):
                    ps = proj("gate", c0 + hf * 512, c0 + (hf + 1) * 512, hTt)
                    S.op("scalar", lambda e: e.activation(out=gt_t[gi][:, hf * 512:(hf + 1) * 512], in_=ps[:, 0:512], func=AF.Sigmoid),
                         reads=[ps], writes=[gt_t[gi]])
                S.dma("gpsimd", g.gate_s[rows, gi * 1024:(gi + 1) * 1024], gt_t[gi][:], reads=[gt_t[gi]], writes=[g.gate_s])
        S.dma("gpsimd", g.iw_s[:, :, :], iw_all[:], reads=[iw_all], writes=[g.iw_s])
        S.dma("gpsimd", g.gb_s[:, :, :], gb_all[:], reads=[gb_all], writes=[g.gb_s])
        S.barrier()


NIT = 15


def phase_b(g):
    nc, S = g.nc, g.S
    g.nrot = 6
    acc = g.banks[6:8]
    with contextlib.ExitStack() as ph:
        def A(name, shape, dt):
            return sb(g, ph, name, shape, dt)

        kT_all = A("kT_all", [64, 2, T], BF16)
        ikT_all = A("ikT_all", [64, T], BF16)
        v_raw = A("v_raw", [128, NT, 128], BF16)
        v_all = A("v_all", [128, NT, 2, 65], BF16)
        iw_all = A("iw_all", [128, NT, 8], F32)
        S.dma("sync", kT_all[:], g.kT_s[:, :, :], reads=[g.kT_s], writes=[kT_all])
        S.dma("sync", ikT_all[:], g.ikT_s[:, :], reads=[g.ikT_s], writes=[ikT_all])
        S.dma("sync", v_raw[:], g.v_s[:, :].rearrange("(n p) c -> p n c", p=128), reads=[g.v_s], writes=[v_raw])
        S.dma("sync", iw_all[:], g.iw_s[:, :, :], reads=[g.iw_s], writes=[iw_all])
        S.op("gpsimd", lambda e: e.memset(v_all[:], 1.0), writes=[v_all])
        S.op("vector", lambda e: e.tensor_copy(out=v_all[:, :, :, 0:64],
                                               in_=v_raw[:].rearrange("p n (g d) -> p n g d", d=64)),
             reads=[v_raw], writes=[v_all])
        thr0 = A("thr0", [128, 1], F32)
        S.op("gpsimd", lambda e: e.memset(thr0[:], -1e29), writes=[thr0])

        sc = [A("sc%d" % i, [128, T], F32) for i in range(2)]
        Rb = [A("Rb%d" % i, [128, 512], F32) for i in range(2)]
        junk = A("junkb", [128, T], BF16)
        mask = A("mask", [128, T], BF16)
        maskT = [A("maskT%d" % i, [128, NT, 128], BF16) for i in range(2)]
        iqT = [A("iqT%d" % i, [64, 8, 128], BF16) for i in range(2)]
        qT = [A("qT%d" % i, [64, 8, 128], BF16) for i in range(2)]
        Eb = [A("Eb%d" % i, [128, 512], BF16) for i in range(2)]
        Pb = [A("Pb%d" % i, [128, 512], BF16) for i in range(2)]
        yat = [A("yat%d" % i, [128, 512], BF16) for i in range(2)]
        rec = A("rec", [128, 4], F32)
        hi = A("hi", [128, 1], F32)
        lo = A("lo", [128, 1], F32)
        rk = A("rk", [128, 1], F32)
        mid = A("mid", [128, 1], F32)
        cnt = A("cnt", [128, 1], F32)
        step = A("step", [128, 1], F32)
        print("phase B sbuf remaining", nc.sbuf_bytes_remaining)

        NQB = getattr(g, "nqb", NT)
        S.dma("sync", iqT[0][:], g.iqT_s[0], reads=[g.iqT_s], writes=[iqT[0]])
        S.dma("sync", qT[0][:], g.qT_s[0], reads=[g.qT_s], writes=[qT[0]])
        ke = 0
        for qb in range(NQB):
            cur = qb % 2
            if qb + 1 < NQB:
                S.dma("sync", iqT[1 - cur][:], g.iqT_s[qb + 1], reads=[g.iqT_s], writes=[iqT[1 - cur]])
                S.dma("sync", qT[1 - cur][:], g.qT_s[qb + 1], reads=[g.qT_s], writes=[qT[1 - cur]])
            NS = qb + 1
            SS = NS * 128
            sct = sc[cur]
            for ci in range((SS + 511) // 512):
                c0 = ci * 512
                n = min(512, SS - c0)
                for h in range(8):
                    b = next_bank(g)
                    S.op("tensor", lambda e: e.matmul(out=b[:, 0:n], lhsT=iqT[cur][:, h, :], rhs=ikT_all[:, c0:c0 + n],
                                                      start=True, stop=True),
                         reads=[iqT[cur], ikT_all], writes=[b])
                    R = Rb[ke % 2]
                    ke += 1
                    S.op("scalar", lambda e: e.activation(out=R[:, 0:n], in_=b[:, 0:n], func=AF.Relu), reads=[b], writes=[R])
                    if h == 0:
                        S.op("vector", lambda e: e.tensor_scalar(out=sct[:, c0:c0 + n], in0=R[:, 0:n], scalar1=iw_all[:, qb, 0:1],
                                                                 scalar2=None, op0=ALU.mult),
                             reads=[R, iw_all], writes=[sct])
                    else:
                        S.op("vector", lambda e: e.scalar_tensor_tensor(out=sct[:, c0:c0 + n], in0=R[:, 0:n],
                                                                        scalar=iw_all[:, qb, h:h + 1], in1=sct[:, c0:c0 + n],
                                                                        op0=ALU.mult, op1=ALU.add),
                             reads=[R, iw_all, sct], writes=[sct])
            dg = sct[:, qb * 128:(qb + 1) * 128]
            S.op("gpsimd", lambda e: e.affine_select(out=dg, in_=dg, pattern=[[-1, 128]], compare_op=ALU.is_ge,
                                                     fill=g.fillneg, base=0, channel_multiplier=1),
                 reads=[sct], writes=[sct])
            if qb >= 2:
                S.op("vector", lambda e: e.tensor_reduce(out=hi[:], in_=sct[:, 0:SS], axis=AX.X, op=ALU.max), reads=[sct], writes=[hi])
                S.op("vector", lambda e: e.tensor_reduce(out=lo[:], in_=sct[:, 0:qb * 128], axis=AX.X, op=ALU.min), reads=[sct], writes=[lo])
                S.op("vector", lambda e: e.tensor_tensor(out=rk[:], in0=hi[:], in1=lo[:], op=ALU.subtract), reads=[hi, lo], writes=[rk])
                for it in range(NIT):
                    S.op("vector", lambda e: e.tensor_scalar(out=rk[:], in0=rk[:], scalar1=0.5, scalar2=None, op0=ALU.mult),
                         reads=[rk], writes=[rk])
                    S.op("vector", lambda e: e.tensor_tensor(out=mid[:], in0=lo[:], in1=rk[:], op=ALU.add), reads=[lo, rk], writes=[mid])
                    S.op("vector", lambda e: e.tensor_scalar(out=junk[:, 0:SS], in0=sct[:, 0:SS], scalar1=mid[:, 0:1], scalar2=None,
                                                             op0=ALU.is_ge, op1=ALU.add, accum_out=cnt[:, 0:1]),
                         reads=[sct, mid], writes=[junk, cnt])
                    S.op("vector", lambda e: e.scalar_tensor_tensor(out=step[:], in0=cnt[:], scalar=255.5, in1=rk[:],
                                                                    op0=ALU.is_ge, op1=ALU.mult), reads=[cnt, rk], writes=[step])
                    S.op("vector", lambda e: e.tensor_tensor(out=lo[:], in0=lo[:], in1=step[:], op=ALU.add), reads=[lo, step], writes=[lo])
                thr = lo
            else:
                thr = thr0
            S.op("vector", lambda e: e.tensor_scalar(out=mask[:, 0:SS], in0=sct[:, 0:SS], scalar1=thr[:, 0:1], scalar2=None,
                                                     op0=ALU.is_ge), reads=[sct, thr], writes=[mask])
            mT = maskT[cur]
            for b0 in range(0, NS, 8):
                nb = min(8, NS - b0)
                b = next_bank(g)
                bv = b[:, :].bitcast(BF16)
                for j in range(nb):
                    S.op("tensor", lambda e: e.transpose(out=bv[:, j * 128:(j + 1) * 128],
                                                         in_=mask[:, (b0 + j) * 128:(b0 + j + 1) * 128], identity=g.ident_b[:]),
                         reads=[mask, g.ident_b], writes=[b])
                dst = mT[:, b0:b0 + nb, :].rearrange("p n t -> p (n t)")
                if (b0 // 8) % 2 == 0:
                    S.op("scalar", lambda e: e.copy(out=dst, in_=bv[:, 0:nb * 128]), reads=[b], writes=[mT])
                else:
                    S.op("gpsimd", lambda e: e.tensor_copy(out=dst, in_=bv[:, 0:nb * 128]), reads=[b], writes=[mT]) if False else \
                        S.op("vector", lambda e: e.tensor_copy(out=dst, in_=bv[:, 0:nb * 128]), reads=[b], writes=[mT])
            yt = yat[cur]
            for gi in range(2):
                po = acc[gi]
                for sbk in range(NS):
                    b = next_bank(g)
                    S.op("tensor", lambda e: e.matmul(out=b[:, 0:512], lhsT=kT_all[:, gi, sbk * 128:(sbk + 1) * 128],
                                                      rhs=qT[cur][:, 4 * gi:4 * gi + 4, :].rearrange("p h t -> p (h t)"),
                                                      start=True, stop=True),
                         reads=[kT_all, qT[cur]], writes=[b])
                    E = Eb[ke % 2]
                    P = Pb[ke % 2]
                    ke += 1
                    S.op("scalar", lambda e: e.activation(out=E[:], in_=b[:, 0:512], func=AF.Exp), reads=[b], writes=[E])
                    S.op("vector", lambda e: e.tensor_tensor(out=P[:].rearrange("p (h t) -> p h t", h=4),
                                                             in0=E[:].rearrange("p (h t) -> p h t", h=4),
                                                             in1=mT[:, sbk, :].unsqueeze(1).to_broadcast([128, 4, 128]), op=ALU.mult),
                         reads=[E, mT], writes=[P])
                    for h in range(4):
                        S.op("tensor", lambda e: e.matmul(out=po[:, h * 65:(h + 1) * 65], lhsT=P[:, h * 128:(h + 1) * 128],
                                                          rhs=v_all[:, sbk, gi, :], start=(sbk == 0 and h == 0),
                                                          stop=(sbk == NS - 1), skip_group_check=True),
                             reads=[P, v_all], writes=[po])
                pov = po[:, 0:260].rearrange("p (h e) -> p h e", e=65)
                S.op("vector", lambda e: e.reciprocal(out=rec[:], in_=pov[:, :, 64]), reads=[po], writes=[rec])
                S.op("vector", lambda e: e.tensor_tensor(out=yt[:, gi * 256:(gi + 1) * 256].rearrange("p (h d) -> p h d", d=64),
                                                         in0=pov[:, :, 0:64],
                                                         in1=rec[:, :].unsqueeze(2).to_broadcast([128, 4, 64]), op=ALU.mult),
                     reads=[po, rec], writes=[yt])
            S.dma("gpsimd", g.yatt_s[qb * 128:(qb + 1) * 128], yt[:], reads=[yt], writes=[g.yatt_s])
        S.barrier()
    g.nrot = 8


def phase_c(g):
    nc, S = g.nc, g.S
    g.nrot = 8
    GS = 2
    with contextlib.ExitStack() as ph:
        def A(name, shape, dt=F32):
            return sb(g, ph, name, shape, dt)

        utri = A("utri", [64, 64])
        sel63 = A("sel63", [64, 128])
        S.op("gpsimd", lambda e: e.affine_select(out=utri[:], in_=g.ones_f[0:64, 0:64], pattern=[[1, 64]], compare_op=ALU.is_ge,
                                                 fill=g.fill0, base=0, channel_multiplier=-1), reads=[g.ones_f], writes=[utri])
        S.op("gpsimd", lambda e: e.affine_select(out=sel63[:], in_=g.ones_f[0:64, :], pattern=[[0, 128]], compare_op=ALU.is_equal,
                                                 fill=g.fill0, base=-63, channel_multiplier=1), reads=[g.ones_f], writes=[sel63])
        gno = A("gno", [128, 128])
        S.dma("sync", gno[:], g.dn_out_norm_gain.ap().partition_broadcast(128), writes=[gno])
        gbc = [A("gbc%d" % h, [64, NT, 8]) for h in range(2)]
        gn = [A("gn%d" % h, [64, NT, 8]) for h in range(2)]
        egc = [A("egc%d" % h, [64, NT, 4]) for h in range(2)]
        bg = [A("bg%d" % h, [64, NT, 4]) for h in range(2)]
        kd = [A("kd%d" % h, [64, NT, 4]) for h in range(2)]
        elast = [A("elast%d" % h, [128, NT, 4]) for h in range(2)]
        for h in range(2):
            S.dma("sync", gbc[h][:], g.gb_s[h * 64:(h + 1) * 64, :, :], reads=[g.gb_s], writes=[gbc[h]])
            b = next_bank(g)
            S.op("tensor", lambda e: e.matmul(out=b[0:64, 0:128], lhsT=utri[:], rhs=gbc[h][:, :, 0:4], start=True, stop=True),
                 reads=[utri, gbc[h]], writes=[b])
            S.op("vector", lambda e: e.tensor_copy(out=gn[h][:, :, 0:4], in_=b[0:64, 0:128].rearrange("p (n f) -> p n f", f=4)),
                 reads=[b], writes=[gn[h]])
            S.op("vector", lambda e: e.tensor_scalar(out=gn[h][:, :, 4:8], in0=gbc[h][:, :, 4:8], scalar1=-1.0, scalar2=None, op0=ALU.mult),
                 reads=[gbc[h]], writes=[gn[h]])
            S.op("scalar", lambda e: e.activation(out=egc[h][:], in_=gn[h][:, :, 0:4], func=AF.Exp), reads=[gn[h]], writes=[egc[h]])
            S.op("vector", lambda e: e.tensor_tensor(out=bg[h][:], in0=egc[h][:], in1=gbc[h][:, :, 4:8], op=ALU.mult),
                 reads=[egc[h], gbc[h]], writes=[bg[h]])
            b2 = next_bank(g)
            S.op("tensor", lambda e: e.matmul(out=b2[:, 0:128], lhsT=sel63[:], rhs=gn[h][:, :, 0:4], start=True, stop=True),
                 reads=[sel63, gn[h]], writes=[b2])
            S.op("scalar", lambda e: e.activation(out=elast[h][:], in_=b2[:, 0:128].rearrange("p (n f) -> p n f", f=4), func=AF.Exp),
                 reads=[b2], writes=[elast[h]])
            S.op("vector", lambda e: e.tensor_tensor(out=kd[h][:], in0=b2[0:64, 0:128].rearrange("p (n f) -> p n f", f=4),
                                                     in1=gn[h][:, :, 0:4], op=ALU.subtract), reads=[b2, gn[h]], writes=[kd[h]])
            S.op("scalar", lambda e: e.activation(out=kd[h][:], in_=kd[h][:], func=AF.Exp), reads=[kd[h]], writes=[kd[h]])

        Sst = A("Sst", [128, 4, 128])
        S.op("gpsimd", lambda e: e.memset(Sst[:], 0.0), writes=[Sst])

        class Slot:
            pass
        slots = []
        for i in range(GS):
            s_ = Slot()
            s_.q = A("cq%d" % i, [64, 512]); s_.k = A("ck%d" % i, [64, 512]); s_.v = A("cv%d" % i, [64, 512])
            s_.dz = A("cdz%d" % i, [64, 512], BF16)
            s_.dgb = A("dgb%d" % i, [64, 512]); s_.G1 = A("G1%d" % i, [64, 256]); s_.G2 = A("G2%d" % i, [64, 256])
            s_.sel3 = A("sel3%d" % i, [64, 768]); s_.E3 = A("E3%d" % i, [64, 768])
            s_.DATn = A("DATn%d" % i, [64, 256]); s_.DAn = A("DAn%d" % i, [64, 256])
            s_.kqT = A("kqT%d" % i, [128, 512])
            s_.MM = [A("MM%d_%d" % (i, j), [64, 512]) for j in range(2)]
            s_.XT = A("XT%d" % i, [64, 256]); s_.inT = A("inT%d" % i, [64, 256])
            s_.vb = A("vb%d" % i, [64, 512]); s_.kbg = A("kbg%d" % i, [64, 512]); s_.kdec = A("kdec%d" % i, [64, 512])
            s_.u = A("u%d" % i, [64, 512]); s_.wT = A("wT%d" % i, [128, 256])
            slots.append(s_)
        vnew = A("vnew", [64, 512])
        otmp = A("otmp", [64, 512])
        osq = A("osq", [64, 512])
        oss = A("oss", [64, 4]); osd = A("osd", [64, 4]); ors = A("ors", [64, 4])
        yout = [A("yout%d" % i, [64, 512], BF16) for i in range(2)]
        print("phase C sbuf remaining", nc.sbuf_bytes_remaining)
        idb = g.ident_f[0:64, 0:64]
        NCH = getattr(g, "nch", 64)

        def bc_h(ap4, n):
            return ap4.unsqueeze(2).to_broadcast([64, 4, n])

        def v3(ap, n):
            return ap.rearrange("p (h f) -> p h f", h=4)

        for c0 in range(0, NCH, GS):
            chunks = list(range(c0, min(NCH, c0 + GS)))
            info = {}
            for c in chunks:
                sl = slots[c % GS]
                tt, half = c // 2, c % 2
                rows = slice(c * 64, (c + 1) * 64)
                S.dma("sync", sl.q[:], g.dnq_s[rows], reads=[g.dnq_s], writes=[sl.q])
                S.dma("sync", sl.k[:], g.dnk_s[rows], reads=[g.dnk_s], writes=[sl.k])
                S.dma("sync", sl.v[:], g.dnv_s[rows], reads=[g.dnv_s], writes=[sl.v])
                S.dma("sync", sl.dz[:], g.dz_s[rows], reads=[g.dz_s], writes=[sl.dz])
                info[c] = (sl, tt, half)
            for c in chunks:
                sl, tt, half = info[c]
                gnc = gn[half][:, tt, :]
                S.op("vector", lambda e: e.tensor_tensor(out=sl.dgb[:].rearrange("p (a f) -> p a f", a=8),
                                                         in0=gnc.unsqueeze(2).to_broadcast([64, 8, 64]),
                                                         in1=idb.unsqueeze(1).to_broadcast([64, 8, 64]), op=ALU.mult),
                     reads=[gn[half], g.ident_f], writes=[sl.dgb])
                bR = next_bank(g)
                S.op("tensor", lambda e: e.matmul(out=bR[0:64, 0:512], lhsT=g.ones_f[0:64, 0:64], rhs=sl.dgb[:], start=True, stop=True),
                     reads=[g.ones_f, sl.dgb], writes=[bR])
                S.op("vector", lambda e: e.tensor_tensor(out=v3(sl.G1[:], 64), in0=v3(bR[0:64, 0:256], 64),
                                                         in1=bc_h(gn[half][:, tt, 0:4], 64), op=ALU.subtract),
                     reads=[bR, gn[half]], writes=[sl.G1])
                S.op("vector", lambda e: e.tensor_scalar(out=sl.G2[:], in0=sl.G1[:], scalar1=-1.0, scalar2=None, op0=ALU.mult),
                     reads=[sl.G1], writes=[sl.G2])
                S.op("gpsimd", lambda e: e.affine_select(out=sl.sel3[:, 0:256], in_=sl.G1[:], pattern=[[0, 4], [1, 64]],
                                                         compare_op=ALU.is_ge, fill=g.fillneg, base=0, channel_multiplier=-1),
                     reads=[sl.G1], writes=[sl.sel3])
                S.op("gpsimd", lambda e: e.affine_select(out=sl.sel3[:, 256:512], in_=sl.G1[:], pattern=[[0, 4], [1, 64]],
                                                         compare_op=ALU.is_ge, fill=g.fillneg, base=-1, channel_multiplier=-1),
                     reads=[sl.G1], writes=[sl.sel3])
                S.op("gpsimd", lambda e: e.affine_select(out=sl.sel3[:, 512:768], in_=sl.G2[:], pattern=[[0, 4], [-1, 64]],
                                                         compare_op=ALU.is_ge, fill=g.fillneg, base=-1, channel_multiplier=1),
                     reads=[sl.G2], writes=[sl.sel3])
                S.op("scalar", lambda e: e.activation(out=sl.E3[:], in_=sl.sel3[:], func=AF.Exp), reads=[sl.sel3], writes=[sl.E3])
                S.op("vector", lambda e: e.tensor_tensor(out=sl.DATn[:], in0=sl.E3[:, 256:512], in1=bR[0:64, 256:512], op=ALU.mult),
                     reads=[sl.E3, bR], writes=[sl.DATn])
                S.op("gpsimd", lambda e: e.tensor_tensor(out=v3(sl.DAn[:], 64), in0=v3(sl.E3[:, 512:768], 64),
                                                         in1=bc_h(gn[half][:, tt, 4:8], 64), op=ALU.mult),
                     reads=[sl.E3, gn[half]], writes=[sl.DAn])
                S.op("vector", lambda e: e.tensor_tensor(out=v3(sl.vb[:], 128), in0=v3(sl.v[:], 128),
                                                         in1=bc_h(gbc[half][:, tt, 4:8], 128), op=ALU.mult),
                     reads=[sl.v, gbc[half]], writes=[sl.vb])
                S.op("gpsimd", lambda e: e.tensor_tensor(out=v3(sl.kbg[:], 128), in0=v3(sl.k[:], 128),
                                                         in1=bc_h(bg[half][:, tt, :], 128), op=ALU.mult),
                     reads=[sl.k, bg[half]], writes=[sl.kbg])
                S.op("gpsimd", lambda e: e.tensor_tensor(out=v3(sl.kdec[:], 128), in0=v3(sl.k[:], 128),
                                                         in1=bc_h(kd[half][:, tt, :], 128), op=ALU.mult),
                     reads=[sl.k, kd[half]], writes=[sl.kdec])
                bT = next_bank(g)
                for hd in range(4):
                    S.op("tensor", lambda e: e.transpose(out=bT[:, hd * 64:(hd + 1) * 64], in_=sl.k[:, hd * 128:(hd + 1) * 128], identity=idb),
                         reads=[sl.k, g.ident_f], writes=[bT])
                for hd in range(4):
                    S.op("tensor", lambda e: e.transpose(out=bT[:, 256 + hd * 64:256 + (hd + 1) * 64], in_=sl.q[:, hd * 128:(hd + 1) * 128],
                                                         identity=idb), reads=[sl.q, g.ident_f], writes=[bT])
                S.op("scalar", lambda e: e.copy(out=sl.kqT[:], in_=bT[:, 0:512]), reads=[bT], writes=[sl.kqT])
                bK = next_bank(g)
                for hd in range(4):
                    kT_h = sl.kqT[:, hd * 64:(hd + 1) * 64]
                    qT_h = sl.kqT[:, 256 + hd * 64:256 + (hd + 1) * 64]
                    S.op("tensor", lambda e: e.matmul(out=bK[0:64, hd * 64:(hd + 1) * 64], lhsT=kT_h, rhs=kT_h, start=(hd == 0), stop=True,
                                                      skip_group_check=True), reads=[sl.kqT], writes=[bK])
                for hd in range(4):
                    kT_h = sl.kqT[:, hd * 64:(hd + 1) * 64]
                    qT_h = sl.kqT[:, 256 + hd * 64:256 + (hd + 1) * 64]
                    S.op("tensor", lambda e: e.matmul(out=bK[0:64, 256 + hd * 64:256 + (hd + 1) * 64], lhsT=kT_h, rhs=qT_h, start=False,
                                                      stop=True, skip_group_check=True), reads=[sl.kqT], writes=[bK])
                MM0 = sl.MM[0]
                S.op("vector", lambda e: e.tensor_tensor(out=MM0[:, 0:256], in0=bK[0:64, 0:256], in1=sl.DAn[:], op=ALU.mult),
                     reads=[bK, sl.DAn], writes=[MM0])
                S.op("vector", lambda e: e.tensor_tensor(out=MM0[:, 256:512], in0=bK[0:64, 0:256], in1=sl.DATn[:], op=ALU.mult),
                     reads=[bK, sl.DATn], writes=[MM0])
                S.op("vector", lambda e: e.tensor_tensor(out=sl.inT[:], in0=bK[0:64, 256:512], in1=sl.E3[:, 0:256], op=ALU.mult),
                     reads=[bK, sl.E3], writes=[sl.inT])
                S.op("gpsimd", lambda e: e.tensor_tensor(out=v3(sl.XT[:], 64), in0=v3(MM0[:, 256:512], 64),
                                                         in1=idb.unsqueeze(1).to_broadcast([64, 4, 64]), op=ALU.add),
                     reads=[MM0, g.ident_f], writes=[sl.XT])
            for lvl in range(1, 6):
                for c in chunks:
                    sl, tt, half = info[c]
                    Mp = sl.MM[(lvl - 1) % 2]
                    Mn = sl.MM[lvl % 2]
                    bM = next_bank(g)
                    for hd in range(4):
                        M_h = Mp[:, hd * 64:(hd + 1) * 64]
                        MT_h = Mp[:, 256 + hd * 64:256 + (hd + 1) * 64]
                        S.op("tensor", lambda e: e.matmul(out=bM[0:64, hd * 64:(hd + 1) * 64], lhsT=MT_h, rhs=M_h, start=(hd == 0), stop=True,
                                                          skip_group_check=True), reads=[Mp], writes=[bM])
                    nw = 256
                    if lvl < 5:
                        nw = 512
                        for hd in range(4):
                            M_h = Mp[:, hd * 64:(hd + 1) * 64]
                            MT_h = Mp[:, 256 + hd * 64:256 + (hd + 1) * 64]
                            S.op("tensor", lambda e: e.matmul(out=bM[0:64, 256 + hd * 64:256 + (hd + 1) * 64], lhsT=M_h, rhs=MT_h, start=False,
                                                              stop=True, skip_group_check=True), reads=[Mp], writes=[bM])
                    S.op("scalar", lambda e: e.copy(out=Mn[:, 0:nw], in_=bM[0:64, 0:nw]), reads=[bM], writes=[Mn])
                    bX = next_bank(g)
                    for hd in range(4):
                        S.op("tensor", lambda e: e.matmul(out=bX[0:64, hd * 64:(hd + 1) * 64], lhsT=Mn[:, hd * 64:(hd + 1) * 64],
                                                          rhs=sl.XT[:, hd * 64:(hd + 1) * 64], start=(hd == 0), stop=True,
                                                          skip_group_check=True), reads=[Mn, sl.XT], writes=[bX])
                    S.op("vector", lambda e: e.tensor_tensor(out=sl.XT[:], in0=bX[0:64, 0:256], in1=sl.XT[:], op=ALU.add),
                         reads=[bX, sl.XT], writes=[sl.XT])
            for c in chunks:
                sl, tt, half = info[c]
                bU = next_bank(g)
                for hd in range(4):
                    S.op("tensor", lambda e: e.matmul(out=bU[0:64, hd * 128:(hd + 1) * 128], lhsT=sl.XT[:, hd * 64:(hd + 1) * 64],
                                                      rhs=sl.vb[:, hd * 128:(hd + 1) * 128], start=(hd == 0), stop=True,
                                                      skip_group_check=True), reads=[sl.XT, sl.vb], writes=[bU])
                S.op("scalar", lambda e: e.copy(out=sl.u[:], in_=bU[0:64, 0:512]), reads=[bU], writes=[sl.u])
                bW = next_bank(g)
                for hd in range(4):
                    S.op("tensor", lambda e: e.matmul(out=bW[:, hd * 64:(hd + 1) * 64], lhsT=sl.kbg[:, hd * 128:(hd + 1) * 128],
                                                      rhs=sl.XT[:, hd * 64:(hd + 1) * 64], start=(hd == 0), stop=True,
                                                      skip_group_check=True), reads=[sl.XT, sl.kbg], writes=[bW])
                S.op("vector", lambda e: e.tensor_copy(out=sl.wT[:], in_=bW[:, 0:256]), reads=[bW], writes=[sl.wT])
            for c in chunks:
                sl, tt, half = info[c]
                rows = slice(c * 64, (c + 1) * 64)
                b1 = next_bank(g)
                for hd in range(4):
                    S.op("tensor", lambda e: e.matmul(out=b1[0:64, hd * 128:(hd + 1) * 128], lhsT=sl.wT[:, hd * 64:(hd + 1) * 64],
                                                      rhs=Sst[:, hd, :], start=(hd == 0), stop=True, skip_group_check=True),
                         reads=[sl.wT, Sst], writes=[b1])
                S.op("vector", lambda e: e.tensor_tensor(out=vnew[:], in0=sl.u[:], in1=b1[0:64, 0:512], op=ALU.subtract),
                     reads=[sl.u, b1], writes=[vnew])
                b2 = next_bank(g)
                for hd in range(4):
                    S.op("tensor", lambda e: e.matmul(out=b2[0:64, hd * 128:(hd + 1) * 128], lhsT=sl.kqT[:, 256 + hd * 64:256 + (hd + 1) * 64],
                                                      rhs=Sst[:, hd, :], start=(hd == 0), stop=True, skip_group_check=True),
                         reads=[sl.kqT, Sst], writes=[b2])
                b3 = next_bank(g)
                for hd in range(4):
                    S.op("tensor", lambda e: e.matmul(out=b3[0:64, hd * 128:(hd + 1) * 128], lhsT=sl.inT[:, hd * 64:(hd + 1) * 64],
                                                      rhs=vnew[:, hd * 128:(hd + 1) * 128], start=(hd == 0), stop=True, skip_group_check=True),
                         reads=[sl.inT, vnew], writes=[b3])
                b4 = next_bank(g)
                for hd in range(4):
                    S.op("tensor", lambda e: e.matmul(out=b4[:, hd * 128:(hd + 1) * 128], lhsT=sl.kdec[:, hd * 128:(hd + 1) * 128],
                                                      rhs=vnew[:, hd * 128:(hd + 1) * 128], start=(hd == 0), stop=True, skip_group_check=True),
                         reads=[sl.kdec, vnew], writes=[b4])
                for hd in range(4):
                    S.op("vector", lambda e: e.scalar_tensor_tensor(out=Sst[:, hd, :], in0=Sst[:, hd, :], scalar=elast[half][:, tt, hd:hd + 1],
                                                                    in1=b4[:, hd * 128:(hd + 1) * 128], op0=ALU.mult, op1=ALU.add),
                         reads=[Sst, elast[half], b4], writes=[Sst])
                S.op("vector", lambda e: e.tensor_tensor(out=v3(otmp[:], 128), in0=v3(b2[0:64, 0:512], 128),
                                                         in1=bc_h(egc[half][:, tt, :], 128), op=ALU.mult),
                     reads=[b2, egc[half]], writes=[otmp])
                S.op("vector", lambda e: e.tensor_tensor(out=otmp[:], in0=otmp[:], in1=b3[0:64, 0:512], op=ALU.add),
                     reads=[otmp, b3], writes=[otmp])
                S.op("scalar", lambda e: e.activation(out=osq[:], in_=otmp[:], func=AF.Square), reads=[otmp], writes=[osq])
                S.op("vector", lambda e: e.tensor_reduce(out=oss[:], in_=v3(osq[:], 128), axis=AX.X, op=ALU.add), reads=[osq], writes=[oss])
                S.op("scalar", lambda e: e.activation(out=osd[:], in_=oss[:], func=AF.Sqrt, bias=g.eps_t[0:64, 0:1], scale=1.0 / 128),
                     reads=[oss, g.eps_t], writes=[osd])
                S.op("vector", lambda e: e.reciprocal(out=ors[:], in_=osd[:]), reads=[osd], writes=[ors])
                S.op("vector", lambda e: e.tensor_tensor(out=v3(otmp[:], 128), in0=v3(otmp[:], 128), in1=bc_h(ors[:, :], 128), op=ALU.mult),
                     reads=[otmp, ors], writes=[otmp])
                S.op("gpsimd", lambda e: e.tensor_tensor(out=v3(otmp[:], 128), in0=v3(otmp[:], 128),
                                                         in1=gno[0:64, :].unsqueeze(1).to_broadcast([64, 4, 128]), op=ALU.mult),
                     reads=[otmp, gno], writes=[otmp])
                yo = yout[c % 2]
                S.op("vector", lambda e: e.tensor_tensor(out=yo[:], in0=otmp[:], in1=sl.dz[:], op=ALU.mult),
                     reads=[otmp, sl.dz], writes=[yo])
                S.dma("gpsimd", g.ydn_s[rows], yo[:], reads=[yo], writes=[g.ydn_s])
        S.barrier()


def phase_p(g):
    nc, S = g.nc, g.S
    with contextlib.ExitStack() as ph:
        stg = [sb(g, ph, "pstg%d" % i, [128, 4096], F32) for i in range(2)]
        cst = [sb(g, ph, "pcst%d" % i, [128, 4096], BF16) for i in range(2)]
        k = 0
        for ti, src in enumerate((g.peer_u, g.peer_v)):
            dst = g.uv_s
            sv = src.ap().rearrange("(b p j) d -> b p (j d)", p=128, j=4)
            dv = dst.t.rearrange("(b p j) d -> b p j d", p=128, j=4)
            for b in range(getattr(g, "npb", 32)):
                s_, c_ = stg[k % 2], cst[k % 2]
                S.dma("sync", s_[:], sv[b], writes=[s_])
                eng = ("vector", "gpsimd", "scalar")[k % 3]
                if eng == "scalar":
                    S.op(eng, lambda e: e.copy(out=c_[:], in_=s_[:]), reads=[s_], writes=[c_])
                else:
                    S.op(eng, lambda e: e.tensor_copy(out=c_[:], in_=s_[:]), reads=[s_], writes=[c_])
                S.dma("sync", dv[b][:, :, ti * D:(ti + 1) * D], c_[:].rearrange("p (j d) -> p j d", j=4), reads=[c_], writes=[dst])
                k += 1
        S.barrier()


def phase_de(g):
    nc, S = g.nc, g.S
    g.nrot = 8
    with contextlib.ExitStack() as ph:
        def A(name, shape, dt=F32):
            return sb(g, ph, name, shape, dt)

        wA = A("wA", [128, 4, D], BF16)
        wB = A("wB", [128, 4, D], BF16)
        wo = A("wo", [128, 8, D], BF16)
        wq = A("wq", [128, 8, 2048], BF16)
        cand = A("cand", [128, 8, 256])
        candf = cand[:].rearrange("p a b -> p (a b)")

        class _V:
            def __init__(self, ap, tl):
                self.ap, self.r = ap, tl.r

            def __getitem__(self, idx):
                return self.ap[idx]
        wstg = [_V(candf[:, i * 512:(i + 1) * 512], cand) for i in range(2)]
        k = 0
        for (src, dstt, nk, ncol) in ((g.w_att_branch, wA, 4, D), (g.w_dn_branch, wB, 4, D), (g.w_o, wo, 8, D),
                                      (g.peer_w_query, wq, 8, 2048)):
            sv = src.ap().rearrange("(kc p) n -> p kc n", p=128)
            for kc in range(nk):
                for c0 in range(0, ncol, 512):
                    s_ = wstg[k % 2]
                    S.dma("sync", s_[:], sv[:, kc, c0:c0 + 512], writes=[s_])
                    eng = ("vector", "gpsimd", "scalar")[k % 3]
                    dst = dstt[:, kc, c0:c0 + 512]
                    if eng == "scalar":
                        S.op(eng, lambda e: e.copy(out=dst, in_=s_[:]), reads=[s_], writes=[dstt])
                    else:
                        S.op(eng, lambda e: e.tensor_copy(out=dst, in_=s_[:]), reads=[s_], writes=[dstt])
                    k += 1
        g2_bc = A("g2_bc", [128, D])
        S.dma("sync", g2_bc[:], g.norm2_gain.ap().partition_broadcast(128), writes=[g2_bc])
        skT = A("skT", [128, 16, 128])
        for hp in range(16):
            s_ = wstg[hp % 2]
            S.dma("sync", s_[:, 0:128], g.peer_sub_keys.ap()[hp], writes=[s_])
            b = next_bank(g)
            S.op("tensor", lambda e: e.transpose(out=b[:, 0:128], in_=s_[:, 0:128], identity=g.ident_f[:]),
                 reads=[s_, g.ident_f], writes=[b])
            S.op("vector", lambda e: e.tensor_copy(out=skT[:, hp, :], in_=b[:, 0:128]), reads=[b], writes=[skT])
        iota_i = A("iota_i", [128, 16], I32)
        iota_f = A("iota_f", [128, 16])
        S.op("gpsimd", lambda e: e.iota(out=iota_i[:], pattern=[[1, 16]], base=0, channel_multiplier=0), writes=[iota_i])
        S.op("vector", lambda e: e.tensor_copy(out=iota_f[:], in_=iota_i[:]), reads=[iota_i], writes=[iota_f])

        ya = A("ya", [128, 512], BF16); yd = A("yd", [128, 512], BF16)
        gt = A("gt", [128, 2048], BF16)
        xt = A("xt", [128, D])
        yT = A("yT", [128, 8, 128], BF16)
        mg = A("mg", [128, D], BF16)
        mgT = A("mgT", [128, 8, 128], BF16)
        x1s = [A("x1_%d" % i, [128, D]) for i in range(2)]
        junk = A("junkd", [128, D], BF16)
        ss1 = A("ss1", [128, 1]); sd1 = A("sd1", [128, 1]); rs1 = A("rs1", [128, 1])
        h2s = [A("h2_%d" % i, [128, D], BF16) for i in range(2)]
        junk2 = A("junk2", [128, D], BF16)
        h2T = A("h2T", [128, 8, 128], BF16)
        qTp = A("qTp", [128, 16, 128])
        s_sb = A("s_sb", [128, 16, 128])
        s2 = A("s2", [128, 256])
        m16 = A("m16", [128, 16, 16])
        i16 = A("i16", [128, 16, 16], U32)
        best = A("best", [128, 8, 16])
        pos = A("pos", [128, 8, 16], U32)
        au = A("au", [128, 128], U32); bu = A("bu", [128, 128], U32)
        af = A("af", [128, 128]); bf = A("bf", [128, 128])
        i16f = A("i16f", [128, 16, 16])
        oh = A("oh", [128, 128, 16])
        ohf = oh[:].rearrange("p a b -> p (a b)")
        e0 = A("e0", [128, 128]); e1 = A("e1", [128, 128])
        eidxs = [A("eidx%d" % i, [128, 128], U32) for i in range(2)]
        gd = A("gd", [128, 8, 16]); gsum = A("gsum", [128, 8]); grec = A("grec", [128, 8])
        gates = [A("gate%d" % i, [128, 128]) for i in range(2)]
        act = A("act", [128, 128]); coef = A("coef", [128, 128])
        NBc = 10
        uv = [A("uv%d" % i, [128, 2 * D], BF16) for i in range(NBc)]
        prod = [A("prod%d" % i, [128, D], BF16) for i in range(2)]
        dgt = [A("dgt%d" % i, [128, 8, 128], BF16) for i in range(2)]
        ag = A("ag", [128, 128])
        acc = A("acc", [128, D])
        g.nrot = 6
        pacc = g.banks[6:8]
        print("phase DE sbuf remaining", nc.sbuf_bytes_remaining)
        x_v = g.x.ap().rearrange("(n p) d -> n p d", p=128)
        o_v = g.out.ap().rearrange("(n p) d -> n p d", p=128)
        NTD = getattr(g, "ntd", NT)
        def stage_x(tt):
            rows = slice(tt * 128, (tt + 1) * 128)
            x1, h2, eidx, gate = x1s[tt % 2], h2s[tt % 2], eidxs[tt % 2], gates[tt % 2]
            S.dma("sync", ya[:], g.yatt_s[rows], reads=[g.yatt_s], writes=[ya])
            S.dma("sync", yd[:], g.ydn_s[rows], reads=[g.ydn_s], writes=[yd])
            S.dma("sync", gt[:], g.gate_s[rows], reads=[g.gate_s], writes=[gt])
            S.dma("sync", xt[:], x_v[tt], writes=[xt])
            b = next_bank(g)
            bv = b[:, :].bitcast(BF16)
            for j in range(4):
                S.op("tensor", lambda e: e.transpose(out=bv[:, j * 128:(j + 1) * 128], in_=ya[:, j * 128:(j + 1) * 128], identity=g.ident_b[:]),
                     reads=[ya, g.ident_b], writes=[b])
            for j in range(4):
                S.op("tensor", lambda e: e.transpose(out=bv[:, (4 + j) * 128:(5 + j) * 128], in_=yd[:, j * 128:(j + 1) * 128], identity=g.ident_b[:]),
                     reads=[yd, g.ident_b], writes=[b])
            S.op("scalar", lambda e: e.copy(out=yT[:].rearrange("p k t -> p (k t)"), in_=bv[:, 0:1024]), reads=[b], writes=[yT])
            for hf in range(2):
                cs = slice(hf * 512, (hf + 1) * 512)
                bA = next_bank(g)
                for kc in range(4):
                    S.op("tensor", lambda e: e.matmul(out=bA[:, 0:512], lhsT=yT[:, kc, :], rhs=wA[:, kc, cs], start=(kc == 0), stop=(kc == 3)),
                         reads=[yT, wA], writes=[bA])
                bB = next_bank(g)
                for kc in range(4):
                    S.op("tensor", lambda e: e.matmul(out=bB[:, 0:512], lhsT=yT[:, 4 + kc, :], rhs=wB[:, kc, cs], start=(kc == 0), stop=(kc == 3)),
                         reads=[yT, wB], writes=[bB])
                S.op("vector", lambda e: e.tensor_tensor(out=ohf[:, cs], in0=bA[:, 0:512], in1=gt[:, cs], op=ALU.mult),
                     reads=[bA, gt], writes=[oh])
                S.op("vector", lambda e: e.tensor_tensor(out=ohf[:, 1024 + hf * 512:1024 + (hf + 1) * 512], in0=bB[:, 0:512], in1=gt[:, 1024 + hf * 512:1024 + (hf + 1) * 512], op=ALU.mult),
                     reads=[bB, gt], writes=[oh])
            S.op("vector", lambda e: e.tensor_tensor(out=mg[:], in0=ohf[:, 0:1024], in1=ohf[:, 1024:2048], op=ALU.add), reads=[oh], writes=[mg])
            for half in range(2):
                b = next_bank(g)
                bv = b[:, :].bitcast(BF16)
                for j in range(4):
                    kc = half * 4 + j
                    S.op("tensor", lambda e: e.transpose(out=bv[:, j * 128:(j + 1) * 128], in_=mg[:, kc * 128:(kc + 1) * 128], identity=g.ident_b[:]),
                         reads=[mg, g.ident_b], writes=[b])
                S.op("scalar", lambda e: e.copy(out=mgT[:, half * 4:half * 4 + 4, :].rearrange("p k t -> p (k t)"), in_=bv[:, 0:512]),
                     reads=[b], writes=[mgT])
            for hf in range(2):
                cs = slice(hf * 512, (hf + 1) * 512)
                b = next_bank(g)
                for kc in range(8):
                    S.op("tensor", lambda e: e.matmul(out=b[:, 0:512], lhsT=mgT[:, kc, :], rhs=wo[:, kc, cs], start=(kc == 0), stop=(kc == 7)),
                         reads=[mgT, wo], writes=[b])
                S.op("vector", lambda e: e.tensor_tensor(out=x1[:, cs], in0=b[:, 0:512], in1=xt[:, cs], op=ALU.add),
                     reads=[b, xt], writes=[x1])
            S.op("scalar", lambda e: e.activation(out=junk[:], in_=x1[:], func=AF.Square, accum_out=ss1[:, 0:1]),
                 reads=[x1], writes=[junk, ss1])
            rstd_op(g, rs1, ss1, 1.0 / D, 1, sd1)
            S.op("vector", lambda e: e.scalar_tensor_tensor(out=h2[:], in0=x1[:], scalar=rs1[:, 0:1], in1=g2_bc[:], op0=ALU.mult, op1=ALU.mult),
                 reads=[x1, rs1, g2_bc], writes=[h2])
            for half in range(2):
                b = next_bank(g)
                bv = b[:, :].bitcast(BF16)
                for j in range(4):
                    kc = half * 4 + j
                    S.op("tensor", lambda e: e.transpose(out=bv[:, j * 128:(j + 1) * 128], in_=h2[:, kc * 128:(kc + 1) * 128], identity=g.ident_b[:]),
                         reads=[h2, g.ident_b], writes=[b])
                S.op("scalar", lambda e: e.copy(out=h2T[:, half * 4:half * 4 + 4, :].rearrange("p k t -> p (k t)"), in_=bv[:, 0:512]),
                     reads=[b], writes=[h2T])
            for q4 in range(4):
                b = next_bank(g)
                for j in range(4):
                    hp = q4 * 4 + j
                    for kc in range(8):
                        S.op("tensor", lambda e: e.matmul(out=b[:, j * 128:(j + 1) * 128], lhsT=wq[:, kc, hp * 128:(hp + 1) * 128],
                                                          rhs=h2T[:, kc, :], start=(j == 0 and kc == 0), stop=(kc == 7), skip_group_check=True),
                             reads=[wq, h2T], writes=[b])
                dst = qTp[:, q4 * 4:q4 * 4 + 4, :].rearrange("p a t -> p (a t)")
                if q4 % 2 == 0:
                    S.op("scalar", lambda e: e.copy(out=dst, in_=b[:, 0:512]), reads=[b], writes=[qTp])
                else:
                    S.op("vector", lambda e: e.tensor_copy(out=dst, in_=b[:, 0:512]), reads=[b], writes=[qTp])
            for q4 in range(4):
                b = next_bank(g)
                for j in range(4):
                    hp = q4 * 4 + j
                    S.op("tensor", lambda e: e.matmul(out=b[:, j * 128:(j + 1) * 128], lhsT=qTp[:, hp, :], rhs=skT[:, hp, :],
                                                      start=(j == 0), stop=True, skip_group_check=True),
                         reads=[qTp, skT], writes=[b])
                S.op("scalar", lambda e: e.copy(out=s_sb[:, q4 * 4:q4 * 4 + 4, :].rearrange("p a t -> p (a t)"), in_=b[:, 0:512]),
                     reads=[b], writes=[s_sb])
            for hp in range(16):
                sv = s_sb[:, hp, :]
                S.op("vector", lambda e: e.max(out=m16[:, hp, 0:8], in_=sv), reads=[s_sb], writes=[m16])
                S.op("vector", lambda e: e.max_index(out=i16[:, hp, 0:8], in_max=m16[:, hp, 0:8], in_values=sv), reads=[s_sb, m16], writes=[i16])
                S.op("vector", lambda e: e.match_replace(out=s2[:, 0:128], in_to_replace=m16[:, hp, 0:8], in_values=sv, imm_value=-1e30),
                     reads=[s_sb, m16], writes=[s2])
                S.op("vector", lambda e: e.max(out=m16[:, hp, 8:16], in_=s2[:, 0:128]), reads=[s2], writes=[m16])
                S.op("vector", lambda e: e.max_index(out=i16[:, hp, 8:16], in_max=m16[:, hp, 8:16], in_values=s2[:, 0:128]),
                     reads=[s2, m16], writes=[i16])
            m16v = m16[:].rearrange("p (h two) a -> p h two a", two=2)
            S.op("vector", lambda e: e.tensor_tensor(out=cand[:].rearrange("p h (a b) -> p h a b", b=16),
                                                     in0=m16v[:, :, 0, :].unsqueeze(3).to_broadcast([128, 8, 16, 16]),
                                                     in1=m16v[:, :, 1, :].unsqueeze(2).to_broadcast([128, 8, 16, 16]), op=ALU.add),
                 reads=[m16], writes=[cand])
            for h in range(8):
                cv = cand[:, h, :]
                S.op("vector", lambda e: e.max(out=best[:, h, 0:8], in_=cv), reads=[cand], writes=[best])
                S.op("vector", lambda e: e.max_index(out=pos[:, h, 0:8], in_max=best[:, h, 0:8], in_values=cv), reads=[cand, best], writes=[pos])
                S.op("vector", lambda e: e.match_replace(out=s2[:], in_to_replace=best[:, h, 0:8], in_values=cv, imm_value=-1e30),
                     reads=[cand, best], writes=[s2])
                S.op("vector", lambda e: e.max(out=best[:, h, 8:16], in_=s2[:]), reads=[s2], writes=[best])
                S.op("vector", lambda e: e.max_index(out=pos[:, h, 8:16], in_max=best[:, h, 8:16], in_values=s2[:]), reads=[s2, best], writes=[pos])
            posf = pos[:].rearrange("p h k -> p (h k)")
            S.op("vector", lambda e: e.tensor_scalar(out=au[:], in0=posf, scalar1=4, scalar2=None, op0=ALU.logical_shift_right), reads=[pos], writes=[au])
            S.op("vector", lambda e: e.tensor_scalar(out=bu[:], in0=posf, scalar1=15, scalar2=None, op0=ALU.bitwise_and), reads=[pos], writes=[bu])
            S.op("vector", lambda e: e.tensor_copy(out=af[:], in_=au[:]), reads=[au], writes=[af])
            S.op("vector", lambda e: e.tensor_copy(out=bf[:], in_=bu[:]), reads=[bu], writes=[bf])
            S.op("vector", lambda e: e.tensor_copy(out=i16f[:], in_=i16[:]), reads=[i16], writes=[i16f])
            i16fv = i16f[:].rearrange("p (h two) a -> p h two a", two=2)
            for which, (sel, dst) in enumerate(((af, e0), (bf, e1))):
                S.op("vector", lambda e: e.tensor_tensor(out=oh[:], in0=sel[:, :].unsqueeze(2).to_broadcast([128, 128, 16]),
                                                         in1=iota_f[:, :].unsqueeze(1).to_broadcast([128, 128, 16]), op=ALU.is_equal),
                     reads=[sel, iota_f], writes=[oh])
                S.op("vector", lambda e: e.tensor_tensor(out=oh[:].rearrange("p (h k) a -> p h k a", k=16),
                                                         in0=oh[:].rearrange("p (h k) a -> p h k a", k=16),
                                                         in1=i16fv[:, :, which, :].unsqueeze(2).to_broadcast([128, 8, 16, 16]), op=ALU.mult),
                     reads=[oh, i16f], writes=[oh])
                S.op("vector", lambda e: e.tensor_reduce(out=dst[:], in_=oh[:], axis=AX.X, op=ALU.add), reads=[oh], writes=[dst])
            S.op("vector", lambda e: e.scalar_tensor_tensor(out=e0[:], in0=e0[:], scalar=128.0, in1=e1[:], op0=ALU.mult, op1=ALU.add),
                 reads=[e0, e1], writes=[e0])
            S.op("vector", lambda e: e.tensor_copy(out=eidx[:], in_=e0[:]), reads=[e0], writes=[eidx])
            S.op("vector", lambda e: e.tensor_tensor(out=gd[:], in0=best[:], in1=best[:, :, 0:1].to_broadcast([128, 8, 16]), op=ALU.subtract),
                 reads=[best], writes=[gd])
            S.op("scalar", lambda e: e.activation(out=gd[:], in_=gd[:], func=AF.Exp), reads=[gd], writes=[gd])
            S.op("vector", lambda e: e.tensor_reduce(out=gsum[:], in_=gd[:], axis=AX.X, op=ALU.add), reads=[gd], writes=[gsum])
            S.op("vector", lambda e: e.reciprocal(out=grec[:], in_=gsum[:]), reads=[gsum], writes=[grec])
            S.op("vector", lambda e: e.tensor_tensor(out=gate[:].rearrange("p (h k) -> p h k", k=16), in0=gd[:],
                                                     in1=grec[:, :].unsqueeze(2).to_broadcast([128, 8, 16]), op=ALU.mult),
                 reads=[gd, grec], writes=[gate])

        def stage_y(tt):
            x1, h2, eidx, gate = x1s[tt % 2], h2s[tt % 2], eidxs[tt % 2], gates[tt % 2]
            for grp in range(getattr(g, "ngrp", 16)):
                gs = slice(grp * 8, (grp + 1) * 8)
                for jj in range(8):
                    j = grp * 8 + jj
                    ub = uv[j % NBc]
                    S.dma("gpsimd", ub[:], g.uv_s[:, :], reads=[eidx, g.uv_s], writes=[ub],
                          indirect=bass.IndirectOffsetOnAxis(ap=eidx[:, j:j + 1], axis=0))
                    pr = prod[j % 2]
                    S.op("vector", lambda e: e.tensor_tensor(out=pr[:], in0=ub[:, 0:D], in1=h2[:], op=ALU.mult), reads=[ub, h2], writes=[pr])
                    S.op("scalar", lambda e: e.activation(out=junk2[:], in_=pr[:], func=AF.Identity, accum_out=act[:, j:j + 1]),
                         reads=[pr], writes=[junk2, act])
                S.op("scalar", lambda e: e.activation(out=ag[:, gs], in_=act[:, gs], func=AF.Gelu), reads=[act], writes=[ag])
                S.op("vector", lambda e: e.tensor_tensor(out=coef[:, gs], in0=ag[:, gs], in1=gate[:, gs], op=ALU.mult), reads=[ag, gate], writes=[coef])
                dg = dgt[grp % 2]
                S.op("vector", lambda e: e.tensor_tensor(out=dg[:], in0=g.ident_b[:, :].unsqueeze(1).to_broadcast([128, 8, 128]),
                                                         in1=coef[:, gs].unsqueeze(2).to_broadcast([128, 8, 128]), op=ALU.mult),
                     reads=[g.ident_b, coef], writes=[dg])
                for jj in range(8):
                    j = grp * 8 + jj
                    ub = uv[j % NBc]
                    for hf in range(2):
                        S.op("tensor", lambda e: e.matmul(out=pacc[hf][:, 0:512], lhsT=dg[:, jj, :], rhs=ub[:, D + hf * 512:D + (hf + 1) * 512],
                                                          start=(j == 0), stop=(j == 127 or j == getattr(g, 'ngrp', 16) * 8 - 1)),
                             reads=[dg, ub], writes=[pacc[hf]])
            for hf in range(2):
                cs = slice(hf * 512, (hf + 1) * 512)
                S.op("vector", lambda e: e.tensor_tensor(out=acc[:, cs], in0=pacc[hf][:, 0:512], in1=x1[:, cs], op=ALU.add),
                     reads=[pacc[hf], x1], writes=[acc])
            S.dma("sync", o_v[tt], acc[:], reads=[acc])

        S.record()
        stage_x(0)
        S.replay(S.stop())
        for tt in range(NTD):
            S.record()
            stage_y(tt)
            ry = S.stop()
            rx = None
            if tt + 1 < NTD:
                S.record()
                stage_x(tt + 1)
                rx = S.stop()
            S.replay(ry, rx)
        S.barrier()


_CACHE = {}


def kernel(**inputs):
    x = np.asarray(inputs["x"], dtype=np.float32)
    if "nc" not in _CACHE:
        _CACHE["nc"] = build_program()
    nc = _CACHE["nc"]
    shared = {}
    for k in ("norm1_gain", "w_in", "q_norm_gain", "k_norm_gain", "dn_conv_w", "dn_a_log", "dn_dt_bias",
              "dn_out_norm_gain", "w_att_branch", "w_dn_branch", "w_o", "norm2_gain", "peer_w_query",
              "peer_u", "peer_v"):
        a = np.asarray(inputs[k], dtype=np.float32)[0]
        if a.ndim == 1:
            a = a.reshape(1, -1)
        shared[k] = np.ascontiguousarray(a)
    shared["peer_sub_keys"] = np.ascontiguousarray(
        np.asarray(inputs["peer_sub_keys"], dtype=np.float32)[0].reshape(16, 128, 128))
    in_maps = []
    for c in range(N_CORES):
        m = dict(shared)
        m["x"] = np.ascontiguousarray(x[c])
        in_maps.append(m)
    res = run_bass_kernel_spmd(nc, in_maps, core_ids=list(range(N_CORES)))
    out = np.stack([np.asarray(r["out"]) for r in res.results], axis=0)
    return out.astype(np.float32)
```

```python
import contextlib
import numpy as np
import concourse.bass as bass
import concourse.mybir as mybir
from concourse.bass_utils import run_bass_kernel_spmd

F32 = mybir.dt.float32
BF16 = mybir.dt.bfloat16
I32 = mybir.dt.int32
U32 = mybir.dt.uint32
AF = mybir.ActivationFunctionType
ALU = mybir.AluOpType
AX = mybir.AxisListType

T = 4096
D = 1024
NT = T // 128
IN_W = 5456
EPS = 1e-6
N_CORES = 8

C_AQ, C_AK, C_AV, C_IQ, C_IK, C_IW = 0, 512, 640, 768, 1280, 1344
C_DQ, C_DK, C_DV, C_DZ, C_DA, C_DB, C_GA, C_GB = 1352, 1864, 2376, 2888, 3400, 3404, 3408, 4432


class Res:
    __slots__ = ("name", "writer", "readers")

    def __init__(self, name):
        self.name = name
        self.writer = None
        self.readers = {}


class Tl:
    def __init__(self, t, name):
        self.t = t.ap() if hasattr(t, "ap") and "DRam" in type(t).__name__ else t
        self.r = Res(name)

    def __getitem__(self, idx):
        return self.t[idx]


class _RecEng:
    def __getattr__(self, name):
        def f(*args, **kw):
            self.__dict__["name"] = name
            self.__dict__["args"] = args
            self.__dict__["kw"] = kw
            return None
        return f


class Sched:
    CE = ("tensor", "vector", "scalar", "gpsimd")

    def __init__(self, nc, st, n_dma=12):
        self.nc = nc
        self.st = st
        self.eng = {n: getattr(nc, n) for n in self.CE + ("sync",)}
        self.sem = {}
        self.cnt = {}
        for n in self.CE:
            self.sem[n] = st.enter_context(nc.semaphore("s_" + n))
            self.cnt[n] = 0
        self.dq = {}
        for q in ("sync", "gpsimd"):
            sems = [st.enter_context(nc.semaphore("d_%s_%d" % (q, i))) for i in range(n_dma)]
            for i, s in enumerate(sems):
                self.sem[(q, i)] = s
                self.cnt[(q, i)] = 0
            self.dq[q] = [0, n_dma]
        self.seen = {}
        self.ninst = 0
        self.rec = None

    def record(self):
        self.rec = []

    def stop(self):
        r = self.rec
        self.rec = None
        return r

    def replay(self, *lists):
        lists = [l for l in lists if l]
        items = []
        for li, l in enumerate(lists):
            n = len(l)
            for i, it in enumerate(l):
                items.append(((i + 0.5) / n, li, i, it))
        items.sort(key=lambda t: (t[0], t[1], t[2]))
        for _, _, _, it in items:
            if it[0] == "op":
                _, E, name, args, kw, reads, writes = it
                self.op(E, lambda e: getattr(e, name)(*args, **kw), reads, writes)
            else:
                _, q, out, in_, reads, writes, indirect, kw = it
                self.dma(q, out, in_, reads, writes, indirect, **kw)

    def _wait(self, E, key, val):
        if val <= 0:
            return
        if E == "tensor" and key == "tensor":
            return
        k = (E, key)
        if self.seen.get(k, 0) >= val:
            return
        self.eng[E].wait_ge(self.sem[key], val)
        self.seen[k] = val
        self.ninst += 1

    def _deps(self, E, reads, writes):
        for r in reads:
            r = getattr(r, "r", r)
            if r.writer is not None:
                self._wait(E, *r.writer)
        for w in writes:
            w = getattr(w, "r", w)
            if w.writer is not None:
                self._wait(E, *w.writer)
            for key, val in w.readers.items():
                self._wait(E, key, val)

    def _mark(self, ev, reads, writes):
        key, val = ev
        for r in reads:
            r = getattr(r, "r", r)
            r.readers[key] = val
        for w in writes:
            w = getattr(w, "r", w)
            w.writer = ev
            w.readers = {}

    def op(self, E, emit, reads=(), writes=()):
        if self.rec is not None:
            r = _RecEng()
            emit(r)
            self.rec.append(("op", E, r.name, r.args, r.kw, list(reads), list(writes)))
            return
        self._deps(E, reads, writes)
        inst = emit(self.eng[E])
        self.cnt[E] += 1
        inst.then_inc(self.sem[E], 1)
        self.ninst += 1
        self._mark((E, self.cnt[E]), reads, writes)

    def dma(self, q, out, in_, reads=(), writes=(), indirect=None, **kw):
        if self.rec is not None:
            self.rec.append(("dma", q, out, in_, list(reads), list(writes), indirect, kw))
            return
        st = self.dq[q]
        i = st[0]
        st[0] = (i + 1) % st[1]
        key = (q, i)
        self._wait(q, key, self.cnt[key])
        self._deps(q, reads, writes)
        if indirect is not None:
            inst = self.eng[q].indirect_dma_start(out=out, out_offset=None, in_=in_, in_offset=indirect, **kw)
        else:
            inst = self.eng[q].dma_start(out=out, in_=in_, **kw)
        self.cnt[key] += 16
        inst.then_inc(self.sem[key], 16)
        self.ninst += 1
        self._mark((key, self.cnt[key]), reads, writes)

    def barrier(self):
        for E in self.CE + ("sync",):
            for key, val in self.cnt.items():
                if key == E:
                    continue
                if E == "tensor" and key == "tensor":
                    continue
                self._wait(E, key, val)

    def finish(self):
        for E in self.CE + ("sync",):
            for key, val in self.cnt.items():
                if key == E:
                    continue
                k = (E, key)
                if val > 0 and self.seen.get(k, 0) < val:
                    self.eng[E].wait_ge(self.sem[key], val)
                    self.seen[k] = val


class Ctx:
    pass


def build_program(debug=False, phases=("A", "B", "C", "P", "D"), **opts):
    nc = bass.Bass("TRN2", target_bir_lowering=False)
    g = Ctx()
    for k_, v_ in opts.items():
        setattr(g, k_, v_)
    g.nc = nc
    g.debug = debug

    def din(name, shape, dt=F32):
        return nc.dram_tensor(name, list(shape), dt, kind="ExternalInput")

    g.x = din("x", [T, D])
    g.norm1_gain = din("norm1_gain", [1, D])
    g.w_in = din("w_in", [D, IN_W])
    g.q_norm_gain = din("q_norm_gain", [1, 64])
    g.k_norm_gain = din("k_norm_gain", [1, 64])
    g.dn_conv_w = din("dn_conv_w", [4, 1536])
    g.dn_a_log = din("dn_a_log", [1, 4])
    g.dn_dt_bias = din("dn_dt_bias", [1, 4])
    g.dn_out_norm_gain = din("dn_out_norm_gain", [1, 128])
    g.w_att_branch = din("w_att_branch", [512, D])
    g.w_dn_branch = din("w_dn_branch", [512, D])
    g.w_o = din("w_o", [D, D])
    g.norm2_gain = din("norm2_gain", [1, D])
    g.peer_w_query = din("peer_w_query", [D, 2048])
    g.peer_sub_keys = din("peer_sub_keys", [16, 128, 128])
    g.peer_u = din("peer_u", [16384, D])
    g.peer_v = din("peer_v", [16384, D])
    g.out = nc.dram_tensor("out", [T, D], F32, kind="ExternalOutput")

    skind = "ExternalOutput" if debug else "Internal"

    def dscr(name, shape, dt):
        return Tl(nc.dram_tensor(name, list(shape), dt, kind=skind), name)

    g.qT_s = dscr("qT_s", [NT, 64, 8, 128], BF16)
    g.iqT_s = dscr("iqT_s", [NT, 64, 8, 128], BF16)
    g.kT_s = dscr("kT_s", [64, 2, T], BF16)
    g.ikT_s = dscr("ikT_s", [64, T], BF16)
    g.v_s = dscr("v_s", [T, 128], BF16)
    g.iw_s = dscr("iw_s", [128, NT, 8], F32)
    g.gb_s = dscr("gb_s", [128, NT, 8], F32)
    g.dnq_s = dscr("dnq_s", [T, 512], F32)
    g.dnk_s = dscr("dnk_s", [T, 512], F32)
    g.dnv_s = dscr("dnv_s", [T, 512], F32)
    g.dz_s = dscr("dz_s", [T, 512], BF16)
    g.gate_s = dscr("gate_s", [T, 2048], BF16)
    g.yatt_s = dscr("yatt_s", [T, 512], BF16)
    g.ydn_s = dscr("ydn_s", [T, 512], BF16)
    g.uv_s = Tl(nc.dram_tensor("uv_s", [16384, 2 * D], BF16, kind="Internal"), "uv_s")

    with contextlib.ExitStack() as st:
        S = Sched(nc, st)
        g.S = S
        g.st = st
        g.banks = [Tl(st.enter_context(nc.psum_tensor("bank%d" % i, [128, 512], F32)), "bank%d" % i)
                   for i in range(8)]
        g.bank_rr = 0
        g.pool_rr = {}
        setup_consts(g)
        if "A" in phases:
            phase_a(g)
        if "B" in phases:
            phase_b(g)
        if "C" in phases:
            phase_c(g)
        if "P" in phases:
            phase_p(g)
        if "D" in phases:
            phase_de(g)
        S.finish()
    return nc


def next_bank(g, pool=None):
    if pool is not None:
        rr = g.pool_rr.get(pool, 0)
        g.pool_rr[pool] = rr + 1
        return g.banks[pool[rr % len(pool)]]
    n = getattr(g, "nrot", 8)
    g.bank_rr = g.bank_rr % n
    b = g.banks[g.bank_rr]
    g.bank_rr = (g.bank_rr + 1) % n
    return b


def sb(g, st, name, shape, dt):
    g.uid = getattr(g, "uid", 0) + 1
    name = "%s_%d" % (name, g.uid)
    return Tl(st.enter_context(g.nc.sbuf_tensor(name, list(shape), dt)), name)


def setup_consts(g):
    nc, S, st = g.nc, g.S, g.st
    g.fill0 = nc.gpsimd.to_reg(0.0)
    g.fillneg = nc.gpsimd.to_reg(-1e30)
    g.ones_f = sb(g, st, "ones_f", [128, 128], F32)
    g.ident_f = sb(g, st, "ident_f", [128, 128], F32)
    g.ident_b = sb(g, st, "ident_b", [128, 128], BF16)
    g.eps_t = sb(g, st, "eps_t", [128, 1], F32)
    S.op("gpsimd", lambda e: e.memset(g.ones_f[:], 1.0), writes=[g.ones_f])
    S.op("gpsimd", lambda e: e.memset(g.eps_t[:], EPS), writes=[g.eps_t])
    S.op("gpsimd", lambda e: e.affine_select(out=g.ident_f[:], in_=g.ones_f[:], pattern=[[-1, 128]],
                                             compare_op=ALU.is_equal, fill=g.fill0, base=0, channel_multiplier=1),
         reads=[g.ones_f], writes=[g.ident_f])
    S.op("vector", lambda e: e.tensor_copy(out=g.ident_b[:], in_=g.ident_f[:]), reads=[g.ident_f], writes=[g.ident_b])


def bcast_row(ap_row, n):
    return ap_row.partition_broadcast(n)


def rstd_op(g, out, ss, scale, n_free, tmp):
    S = g.S
    S.op("scalar", lambda e: e.activation(out=tmp[:, 0:n_free], in_=ss[:, 0:n_free], func=AF.Sqrt,
                                          bias=g.eps_t[:, 0:1], scale=scale),
         reads=[ss, g.eps_t], writes=[tmp])
    S.op("vector", lambda e: e.reciprocal(out=out[:, 0:n_free], in_=tmp[:, 0:n_free]), reads=[tmp], writes=[out])


def phase_a(g):
    nc, S = g.nc, g.S
    with contextlib.ExitStack() as ph:
        def A(name, shape, dt):
            return sb(g, ph, name, shape, dt)

        w_bf = A("w_bf", [128, 8, IN_W], BF16)
        WCH = 1364
        wst = [A("wst%d" % i, [128, WCH], F32) for i in range(2)]
        w_in_v = g.w_in.ap().rearrange("(kc p) n -> p kc n", p=128)
        k = 0
        for kc in range(8):
            for c in range(IN_W // WCH):
                s_ = wst[k % 2]
                S.dma("sync", s_[:], w_in_v[:, kc, c * WCH:(c + 1) * WCH], writes=[s_])
                eng = ("vector", "gpsimd", "scalar")[k % 3]
                dst = w_bf[:, kc, c * WCH:(c + 1) * WCH]
                if eng == "scalar":
                    S.op(eng, lambda e: e.copy(out=dst, in_=s_[:]), reads=[s_], writes=[w_bf])
                else:
                    S.op(eng, lambda e: e.tensor_copy(out=dst, in_=s_[:]), reads=[s_], writes=[w_bf])
                k += 1

        g1_bc = A("g1_bc", [128, D], F32)
        S.dma("sync", g1_bc[:], g.norm1_gain.ap().partition_broadcast(128), writes=[g1_bc])
        gq_bc = A("gq_bc", [128, 64], F32)
        gk_bc = A("gk_bc", [128, 64], F32)
        S.dma("sync", gq_bc[:], g.q_norm_gain.ap().partition_broadcast(128), writes=[gq_bc])
        S.dma("sync", gk_bc[:], g.k_norm_gain.ap().partition_broadcast(128), writes=[gk_bc])
        S.op("vector", lambda e: e.tensor_scalar(out=gq_bc[:], in0=gq_bc[:], scalar1=0.125, scalar2=None, op0=ALU.mult),
             reads=[gq_bc], writes=[gq_bc])
        cw_bc = A("cw_bc", [128, 4, 1536], F32)
        for j in range(4):
            S.dma("sync", cw_bc[:, j, :], g.dn_conv_w.ap()[j:j + 1, :].partition_broadcast(128), writes=[cw_bc])
        dtb_bc = A("dtb_bc", [128, 4], F32)
        nea_bc = A("nea_bc", [128, 4], F32)
        S.dma("sync", dtb_bc[:], g.dn_dt_bias.ap().partition_broadcast(128), writes=[dtb_bc])
        S.dma("sync", nea_bc[:], g.dn_a_log.ap().partition_broadcast(128), writes=[nea_bc])
        S.op("scalar", lambda e: e.activation(out=nea_bc[:], in_=nea_bc[:], func=AF.Exp), reads=[nea_bc], writes=[nea_bc])
        S.op("vector", lambda e: e.tensor_scalar(out=nea_bc[:], in0=nea_bc[:], scalar1=-1.0, scalar2=None, op0=ALU.mult),
             reads=[nea_bc], writes=[nea_bc])

        shf = A("shf", [128, 128], F32)
        Sh = [g.ident_b] + [A("sh%d" % d, [128, 128], BF16) for d in (1, 2, 3)]
        ShP = [None] + [A("shp%d" % d, [128, 128], BF16) for d in (1, 2, 3)]
        for d in (1, 2, 3):
            S.op("gpsimd", lambda e: e.affine_select(out=shf[:], in_=g.ones_f[:], pattern=[[-1, 128]],
                                                     compare_op=ALU.is_equal, fill=g.fill0, base=d, channel_multiplier=1),
                 reads=[g.ones_f], writes=[shf])
            S.op("vector", lambda e: e.tensor_copy(out=Sh[d][:], in_=shf[:]), reads=[shf], writes=[Sh[d]])
            S.op("gpsimd", lambda e: e.affine_select(out=shf[:], in_=g.ones_f[:], pattern=[[-1, 128]],
                                                     compare_op=ALU.is_equal, fill=g.fill0, base=d - 128, channel_multiplier=1),
                 reads=[g.ones_f], writes=[shf])
            S.op("vector", lambda e: e.tensor_copy(out=ShP[d][:], in_=shf[:]), reads=[shf], writes=[ShP[d]])

        xt = [A("xt%d" % i, [128, D], F32) for i in range(2)]
        junk = A("junk", [128, D], BF16)
        h_bf = A("h_bf", [128, D], BF16)
        hT = [A("hT%d" % i, [128, 8, 128], BF16) for i in range(2)]
        ss1 = A("ss1", [128, 1], F32)
        sd1 = A("sd1", [128, 1], F32)
        rs1 = A("rs1", [128, 1], F32)
        sq = A("sq", [128, 512], F32)
        ss8 = A("ss8", [128, 8], F32)
        sd8 = A("sd8", [128, 8], F32)
        rs8 = A("rs8", [128, 8], F32)
        tmpf = A("tmpf", [128, 512], F32)
        qn_bf = A("qn_bf", [128, 512], BF16)
        kn_bf = A("kn_bf", [128, 128], BF16)
        iq_bf = A("iq_bf", [128, 512], BF16)
        ik_bf = A("ik_bf", [128, 64], BF16)
        qT_t = [A("qT_t%d" % i, [64, 8, 128], BF16) for i in range(2)]
        iqT_t = [A("iqT_t%d" % i, [64, 8, 128], BF16) for i in range(2)]
        kT_t = [A("kT_t%d" % i, [64, 2, 128], BF16) for i in range(2)]
        ikT_t = [A("ikT_t%d" % i, [64, 128], BF16) for i in range(2)]
        v_t = [A("v_t%d" % i, [128, 128], BF16) for i in range(2)]
        iw_all = A("iw_all", [128, NT, 8], F32)
        gb_all = A("gb_all", [128, NT, 8], F32)
        ab_tmp = A("ab_tmp", [128, 4], F32)
        xw = [A("xw%d" % i, [128, 4, 1536], BF16) for i in range(2)]
        yc = [A("yc%d" % i, [128, 512], F32) for i in range(3)]
        dz_t = [A("dz_t%d" % i, [128, 512], BF16) for i in range(2)]
        gt_t = [A("gt_t%d" % i, [128, 1024], BF16) for i in range(2)]
        print("phase A sbuf remaining", nc.sbuf_bytes_remaining)

        def proj(cols, lo, hi, hTt):
            b = next_bank(g)
            n = hi - lo
            for kc in range(8):
                S.op("tensor", lambda e: e.matmul(out=b[:, 0:n], lhsT=hTt[:, kc, :], rhs=w_bf[:, kc, lo:hi],
                                                  start=(kc == 0), stop=(kc == 7)),
                     reads=[hTt, w_bf], writes=[b])
            return b

        def headnorm(ps, nh, gain_bc, out_bf):
            n = nh * 64
            S.op("scalar", lambda e: e.activation(out=sq[:, 0:n], in_=ps[:, 0:n], func=AF.Square), reads=[ps], writes=[sq])
            S.op("vector", lambda e: e.tensor_reduce(out=ss8[:, 0:nh], in_=sq[:, 0:n].rearrange("p (h d) -> p h d", d=64),
                                                     axis=AX.X, op=ALU.add), reads=[sq], writes=[ss8])
            rstd_op(g, rs8, ss8, 1.0 / 64, nh, sd8)
            S.op("vector", lambda e: e.tensor_tensor(out=tmpf[:, 0:n].rearrange("p (h d) -> p h d", d=64),
                                                     in0=ps[:, 0:n].rearrange("p (h d) -> p h d", d=64),
                                                     in1=rs8[:, 0:nh].unsqueeze(2).to_broadcast([128, nh, 64]), op=ALU.mult),
                 reads=[ps, rs8], writes=[tmpf])
            S.op("vector", lambda e: e.tensor_tensor(out=out_bf[:, 0:n].rearrange("p (h d) -> p h d", d=64),
                                                     in0=tmpf[:, 0:n].rearrange("p (h d) -> p h d", d=64),
                                                     in1=gain_bc[:, :].unsqueeze(1).to_broadcast([128, nh, 64]), op=ALU.mult),
                 reads=[tmpf, gain_bc], writes=[out_bf])

        def transpose_heads(src_bf, nh, dstT, eng):
            b = next_bank(g)
            bv = b[:, :].bitcast(BF16)
            for h in range(nh):
                S.op("tensor", lambda e: e.transpose(out=bv[0:64, h * 128:(h + 1) * 128], in_=src_bf[:, h * 64:(h + 1) * 64],
                                                     identity=g.ident_b[:]),
                     reads=[src_bf, g.ident_b], writes=[b])
            src = bv[0:64, 0:nh * 128]
            dst = dstT[:, :, :].rearrange("p h t -> p (h t)") if nh > 1 else dstT[:, :]
            if eng == "scalar":
                S.op("scalar", lambda e: e.copy(out=dst, in_=src), reads=[b], writes=[dstT])
            else:
                S.op("vector", lambda e: e.tensor_copy(out=dst, in_=src), reads=[b], writes=[dstT])

        x_v = g.x.ap().rearrange("(n p) d -> n p d", p=128)
        S.dma("sync", xt[0][:], x_v[0], writes=[xt[0]])
        NTR = getattr(g, "ntr", NT)
        SECT = getattr(g, "sect", 99)
        for tt in range(NTR):
            cur = tt % 2
            if tt + 1 < NTR:
                S.dma("sync", xt[1 - cur][:], x_v[tt + 1], writes=[xt[1 - cur]])
            x_t = xt[cur]
            hTt = hT[cur]
            rows = slice(tt * 128, (tt + 1) * 128)
            S.op("scalar", lambda e: e.activation(out=junk[:], in_=x_t[:], func=AF.Square, accum_out=ss1[:, 0:1]),
                 reads=[x_t], writes=[junk, ss1])
            rstd_op(g, rs1, ss1, 1.0 / D, 1, sd1)
            S.op("vector", lambda e: e.scalar_tensor_tensor(out=h_bf[:], in0=x_t[:], scalar=rs1[:, 0:1], in1=g1_bc[:],
                                                            op0=ALU.mult, op1=ALU.mult),
                 reads=[x_t, rs1, g1_bc], writes=[h_bf])
            for half in range(2):
                b = next_bank(g)
                bv = b[:, :].bitcast(BF16)
                for j in range(4):
                    kc = half * 4 + j
                    S.op("tensor", lambda e: e.transpose(out=bv[:, j * 128:(j + 1) * 128], in_=h_bf[:, kc * 128:(kc + 1) * 128],
                                                         identity=g.ident_b[:]),
                         reads=[h_bf, g.ident_b], writes=[b])
                dst = hTt[:, half * 4:half * 4 + 4, :].rearrange("p k t -> p (k t)")
                if half == 0:
                    S.op("scalar", lambda e: e.copy(out=dst, in_=bv[:, 0:512]), reads=[b], writes=[hTt])
                else:
                    S.op("vector", lambda e: e.tensor_copy(out=dst, in_=bv[:, 0:512]), reads=[b], writes=[hTt])

            if SECT < 1:
                continue
            ps = proj("aq", C_AQ, C_AQ + 512, hTt)
            headnorm(ps, 8, gq_bc, qn_bf)
            transpose_heads(qn_bf, 8, qT_t[cur], "scalar")
            S.dma("gpsimd", g.qT_s[tt], qT_t[cur][:], reads=[qT_t[cur]], writes=[g.qT_s])
            if SECT < 2:
                continue
            ps = proj("kv", C_AK, C_AK + 256, hTt)
            headnorm(ps, 2, gk_bc, kn_bf)
            S.op("scalar", lambda e: e.copy(out=v_t[cur][:], in_=ps[:, 128:256]), reads=[ps], writes=[v_t[cur]])
            transpose_heads(kn_bf, 2, kT_t[cur], "vector")
            S.dma("gpsimd", g.kT_s[:, :, rows], kT_t[cur][:], reads=[kT_t[cur]], writes=[g.kT_s])
            S.dma("gpsimd", g.v_s[rows], v_t[cur][:], reads=[v_t[cur]], writes=[g.v_s])
            if SECT < 3:
                continue
            ps = proj("iq", C_IQ, C_IQ + 512, hTt)
            S.op("scalar", lambda e: e.copy(out=iq_bf[:], in_=ps[:, 0:512]), reads=[ps], writes=[iq_bf])
            transpose_heads(iq_bf, 8, iqT_t[cur], "vector")
            S.dma("gpsimd", g.iqT_s[tt], iqT_t[cur][:], reads=[iqT_t[cur]], writes=[g.iqT_s])
            if SECT < 4:
                continue
            ps = proj("ikw", C_IK, C_IK + 72, hTt)
            S.op("vector", lambda e: e.tensor_copy(out=ik_bf[:], in_=ps[:, 0:64]), reads=[ps], writes=[ik_bf])
            S.op("scalar", lambda e: e.copy(out=iw_all[:, tt, :], in_=ps[:, 64:72]), reads=[ps], writes=[iw_all])
            transpose_heads(ik_bf, 1, ikT_t[cur], "scalar")
            if "ikT" not in getattr(g, "skip", ()):
                S.dma("gpsimd", g.ikT_s[:, rows], ikT_t[cur][:], reads=[ikT_t[cur]], writes=[g.ikT_s])
            if SECT < 5:
                continue
            ps = proj("ab", C_DA, C_DA + 8, hTt)
            S.op("vector", lambda e: e.tensor_tensor(out=ab_tmp[:], in0=ps[:, 0:4], in1=dtb_bc[:], op=ALU.add),
                 reads=[ps, dtb_bc], writes=[ab_tmp])
            S.op("scalar", lambda e: e.activation(out=ab_tmp[:], in_=ab_tmp[:], func=AF.Exp), reads=[ab_tmp], writes=[ab_tmp])
            S.op("scalar", lambda e: e.activation(out=ab_tmp[:], in_=ab_tmp[:], func=AF.Ln, bias=g.ones_f[:, 0:1], scale=1.0),
                 reads=[ab_tmp, g.ones_f], writes=[ab_tmp])
            S.op("vector", lambda e: e.tensor_tensor(out=gb_all[:, tt, 0:4], in0=ab_tmp[:], in1=nea_bc[:], op=ALU.mult),
                 reads=[ab_tmp, nea_bc], writes=[gb_all])
            S.op("scalar", lambda e: e.activation(out=gb_all[:, tt, 4:8], in_=ps[:, 4:8], func=AF.Sigmoid),
                 reads=[ps], writes=[gb_all])
            if SECT < 6:
                continue
            for gi, (c0, dst_s) in enumerate(((C_DQ, g.dnq_s), (C_DK, g.dnk_s), (C_DV, g.dnv_s))):
                ps = proj("dn", c0, c0 + 512, hTt)
                cs = slice(gi * 512, (gi + 1) * 512)
                for j in range(4):
                    S.op("vector", lambda e: e.tensor_tensor(out=xw[cur][:, j, cs], in0=ps[:, 0:512], in1=cw_bc[:, j, cs], op=ALU.mult),
                         reads=[ps, cw_bc], writes=[xw[cur]])
                b = next_bank(g)
                mm = []
                for j in range(4):
                    mm.append((Sh[3 - j], xw[cur], j))
                if tt > 0:
                    for j in range(3):
                        mm.append((ShP[3 - j], xw[1 - cur], j))
                for i, (sh, xsrc, j) in enumerate(mm):
                    S.op("tensor", lambda e: e.matmul(out=b[:, 0:512], lhsT=sh[:], rhs=xsrc[:, j, cs],
                                                      start=(i == 0), stop=(i == len(mm) - 1)),
                         reads=[sh, xsrc], writes=[b])
                y = yc[gi]
                S.op("scalar", lambda e: e.activation(out=y[:], in_=b[:, 0:512], func=AF.Silu), reads=[b], writes=[y])
                if gi < 2:
                    S.op("gpsimd", lambda e: e.tensor_tensor(out=sq[:], in0=y[:], in1=y[:], op=ALU.mult), reads=[y], writes=[sq])
                    S.op("vector", lambda e: e.tensor_reduce(out=ss8[:, 0:4], in_=sq[:].rearrange("p (h d) -> p h d", d=128),
                                                             axis=AX.X, op=ALU.add), reads=[sq], writes=[ss8])
                    rstd_op(g, rs8, ss8, 1.0, 4, sd8)
                    if gi == 0:
                        S.op("vector", lambda e: e.tensor_scalar(out=rs8[:, 0:4], in0=rs8[:, 0:4], scalar1=128 ** -0.5, scalar2=None,
                                                                 op0=ALU.mult), reads=[rs8], writes=[rs8])
                    S.op("vector", lambda e: e.tensor_tensor(out=y[:].rearrange("p (h d) -> p h d", d=128),
                                                             in0=y[:].rearrange("p (h d) -> p h d", d=128),
                                                             in1=rs8[:, 0:4].unsqueeze(2).to_broadcast([128, 4, 128]), op=ALU.mult),
                         reads=[y, rs8], writes=[y])
                S.dma("gpsimd", dst_s[rows], y[:], reads=[y], writes=[dst_s])
            if SECT < 7:
                continue
            ps = proj("dz", C_DZ, C_DZ + 512, hTt)
            S.op("scalar", lambda e: e.activation(out=dz_t[cur][:], in_=ps[:, 0:512], func=AF.Silu), reads=[ps], writes=[dz_t[cur]])
            S.dma("gpsimd", g.dz_s[rows], dz_t[cur][:], reads=[dz_t[cur]], writes=[g.dz_s])
            if SECT < 8:
                continue
            for gi, c0 in enumerate((C_GA, C_GB)):
                for hf in range(2):
                    ps = proj("gate", c0 + hf * 512, c0 + (hf + 1) * 512, hTt)
                    S.op("scalar", lambda e: e.activation(out=gt_t[gi][:, hf * 512:(hf + 1) * 512], in_=ps[:, 0:512], func=AF.Sigmoid),
                         reads=[ps], writes=[gt_t[gi]])
                S.dma("gpsimd", g.gate_s[rows, gi * 1024:(gi + 1) * 1024], gt_t[gi][:], reads=[gt_t[gi]], writes=[g.gate_s])
        S.dma("gpsimd", g.iw_s[:, :, :], iw_all[:], reads=[iw_all], writes=[g.iw_s])
        S.dma("gpsimd", g.gb_s[:, :, :], gb_all[:], reads=[gb_all], writes=[g.gb_s])
        S.barrier()


NIT = 15


def phase_b(g):
    nc, S = g.nc, g.S
    g.nrot = 6
    acc = g.banks[6:8]
    with contextlib.ExitStack() as ph:
        def A(name, shape, dt):
            return sb(g, ph, name, shape, dt)

        kT_all = A("kT_all", [64, 2, T], BF16)
        ikT_all = A("ikT_all", [64, T], BF16)
        v_raw = A("v_raw", [128, NT, 128], BF16)
        v_all = A("v_all", [128, NT, 2, 65], BF16)
        iw_all = A("iw_all", [128, NT, 8], F32)
        S.dma("sync", kT_all[:], g.kT_s[:, :, :], reads=[g.kT_s], writes=[kT_all])
        S.dma("sync", ikT_all[:], g.ikT_s[:, :], reads=[g.ikT_s], writes=[ikT_all])
        S.dma("sync", v_raw[:], g.v_s[:, :].rearrange("(n p) c -> p n c", p=128), reads=[g.v_s], writes=[v_raw])
        S.dma("sync", iw_all[:], g.iw_s[:, :, :], reads=[g.iw_s], writes=[iw_all])
        S.op("gpsimd", lambda e: e.memset(v_all[:], 1.0), writes=[v_all])
        S.op("vector", lambda e: e.tensor_copy(out=v_all[:, :, :, 0:64],
                                               in_=v_raw[:].rearrange("p n (g d) -> p n g d", d=64)),
             reads=[v_raw], writes=[v_all])
        thr0 = A("thr0", [128, 1], F32)
        S.op("gpsimd", lambda e: e.memset(thr0[:], -1e29), writes=[thr0])

        sc = [A("sc%d" % i, [128, T], F32) for i in range(2)]
        Rb = [A("Rb%d" % i, [128, 512], F32) for i in range(2)]
        junk = A("junkb", [128, T], BF16)
        mask = A("mask", [128, T], BF16)
        maskT = [A("maskT%d" % i, [128, NT, 128], BF16) for i in range(2)]
        iqT = [A("iqT%d" % i, [64, 8, 128], BF16) for i in range(2)]
        qT = [A("qT%d" % i, [64, 8, 128], BF16) for i in range(2)]
        Eb = [A("Eb%d" % i, [128, 512], BF16) for i in range(2)]
        Pb = [A("Pb%d" % i, [128, 512], BF16) for i in range(2)]
        yat = [A("yat%d" % i, [128, 512], BF16) for i in range(2)]
        rec = A("rec", [128, 4], F32)
        hi = A("hi", [128, 1], F32)
        lo = A("lo", [128, 1], F32)
        rk = A("rk", [128, 1], F32)
        mid = A("mid", [128, 1], F32)
        cnt = A("cnt", [128, 1], F32)
        step = A("step", [128, 1], F32)
        print("phase B sbuf remaining", nc.sbuf_bytes_remaining)

        NQB = getattr(g, "nqb", NT)
        pw = A("pw", [128, NIT + 1], F32)
        for it in range(NIT + 1):
            S.op("gpsimd", lambda e: e.memset(pw[:, it:it + 1], 2.0 ** -(it + 1)), writes=[pw])
        rkall = A("rkall", [128, NIT + 1], F32)
        nmid = A("nmid", [128, 1], F32)
        tq = A("tq", [128, 1], F32)
        cnt2 = A("cnt2", [128, 1], F32)
        thr_t = A("thr_t", [128, 1], F32)
        ctr = {"ke": 0}

        def stage1(qb):
            cur = qb % 2
            S.dma("sync", iqT[cur][:], g.iqT_s[qb], reads=[g.iqT_s], writes=[iqT[cur]])
            S.dma("sync", qT[cur][:], g.qT_s[qb], reads=[g.qT_s], writes=[qT[cur]])
            NS = qb + 1
            SS = NS * 128
            sct = sc[cur]
            for ci in range((SS + 511) // 512):
                c0 = ci * 512
                n = min(512, SS - c0)
                for h in range(8):
                    b = next_bank(g, (0, 1, 2))
                    S.op("tensor", lambda e: e.matmul(out=b[:, 0:n], lhsT=iqT[cur][:, h, :], rhs=ikT_all[:, c0:c0 + n],
                                                      start=True, stop=True),
                         reads=[iqT[cur], ikT_all], writes=[b])
                    R = Rb[ctr["ke"] % 2]
                    ctr["ke"] += 1
                    S.op("scalar", lambda e: e.activation(out=R[:, 0:n], in_=b[:, 0:n], func=AF.Relu), reads=[b], writes=[R])
                    if h == 0:
                        S.op("vector", lambda e: e.tensor_scalar(out=sct[:, c0:c0 + n], in0=R[:, 0:n], scalar1=iw_all[:, qb, 0:1],
                                                                 scalar2=None, op0=ALU.mult),
                             reads=[R, iw_all], writes=[sct])
                    else:
                        S.op("vector", lambda e: e.scalar_tensor_tensor(out=sct[:, c0:c0 + n], in0=R[:, 0:n],
                                                                        scalar=iw_all[:, qb, h:h + 1], in1=sct[:, c0:c0 + n],
                                                                        op0=ALU.mult, op1=ALU.add),
                             reads=[R, iw_all, sct], writes=[sct])
            dg = sct[:, qb * 128:(qb + 1) * 128]
            S.op("gpsimd", lambda e: e.affine_select(out=dg, in_=dg, pattern=[[-1, 128]], compare_op=ALU.is_ge,
                                                     fill=g.fillneg, base=0, channel_multiplier=1),
                 reads=[sct], writes=[sct])
            ctr["split"] = len(S.rec)
            if qb >= 2:
                S.op("vector", lambda e: e.tensor_reduce(out=hi[:], in_=sct[:, 0:SS], axis=AX.X, op=ALU.max), reads=[sct], writes=[hi])
                S.op("vector", lambda e: e.tensor_reduce(out=lo[:], in_=sct[:, 0:qb * 128], axis=AX.X, op=ALU.min), reads=[sct], writes=[lo])
                S.op("vector", lambda e: e.tensor_tensor(out=rk[:], in0=hi[:], in1=lo[:], op=ALU.subtract), reads=[hi, lo], writes=[rk])
                S.op("vector", lambda e: e.tensor_tensor(out=rkall[:], in0=pw[:], in1=rk[:, 0:1].to_broadcast([128, NIT + 1]), op=ALU.mult),
                     reads=[pw, rk], writes=[rkall])
                S.op("vector", lambda e: e.tensor_scalar(out=nmid[:], in0=lo[:], scalar1=rkall[:, 0:1], scalar2=-1.0, op0=ALU.add, op1=ALU.mult),
                     reads=[lo, rkall], writes=[nmid])
                for it in range(NIT):
                    if getattr(g, "dvecnt", 0):
                        S.op("vector", lambda e: e.tensor_scalar(out=mid[:], in0=nmid[:], scalar1=-1.0, scalar2=None, op0=ALU.mult),
                             reads=[nmid], writes=[mid])
                        S.op("vector", lambda e: e.tensor_scalar(out=junk[:, 0:SS], in0=sct[:, 0:SS], scalar1=mid[:, 0:1], scalar2=None,
                                                                 op0=ALU.is_gt, op1=ALU.add, accum_out=cnt[:, 0:1]),
                             reads=[sct, mid], writes=[junk, cnt])
                        S.op("vector", lambda e: e.tensor_scalar(out=cnt2[:], in0=cnt[:], scalar1=2.0, scalar2=float(SS), op0=ALU.mult, op1=ALU.subtract),
                             reads=[cnt], writes=[cnt2])
                    else:
                        S.op("scalar", lambda e: e.activation(out=junk[:, 0:SS], in_=sct[:, 0:SS], func=AF.Sign, bias=nmid[:, 0:1], scale=1.0,
                                                              accum_out=cnt[:, 0:1]),
                             reads=[sct, nmid], writes=[junk, cnt])
                        S.op("scalar", lambda e: e.copy(out=cnt2[:], in_=cnt[:]), reads=[cnt], writes=[cnt2])
                    S.op("vector", lambda e: e.tensor_scalar(out=tq[:], in0=cnt2[:], scalar1=511.5 - SS, scalar2=0.5, op0=ALU.is_lt, op1=ALU.subtract),
                         reads=[cnt2], writes=[tq])
                    S.op("vector", lambda e: e.scalar_tensor_tensor(out=nmid[:], in0=tq[:], scalar=rkall[:, it:it + 1], in1=nmid[:],
                                                                    op0=ALU.mult, op1=ALU.add), reads=[tq, rkall, nmid], writes=[nmid])
                S.op("vector", lambda e: e.tensor_scalar(out=thr_t[:], in0=nmid[:], scalar1=-1.0, scalar2=rkall[:, NIT:NIT + 1],
                                                         op0=ALU.mult, op1=ALU.subtract), reads=[nmid, rkall], writes=[thr_t])
                thr = thr_t
            else:
                thr = thr0
            S.op("vector", lambda e: e.tensor_scalar(out=mask[:, 0:SS], in0=sct[:, 0:SS], scalar1=thr[:, 0:1], scalar2=None,
                                                     op0=ALU.is_ge), reads=[sct, thr], writes=[mask])
            mT = maskT[cur]
            for b0 in range(0, NS, 8):
                nb = min(8, NS - b0)
                b = next_bank(g, (0, 1, 2))
                bv = b[:, :].bitcast(BF16)
                for j in range(nb):
                    S.op("tensor", lambda e: e.transpose(out=bv[:, j * 128:(j + 1) * 128],
                                                         in_=mask[:, (b0 + j) * 128:(b0 + j + 1) * 128], identity=g.ident_b[:]),
                         reads=[mask, g.ident_b], writes=[b])
                dst = mT[:, b0:b0 + nb, :].rearrange("p n t -> p (n t)")
                S.op("gpsimd", lambda e: e.tensor_copy(out=dst, in_=bv[:, 0:nb * 128]), reads=[b], writes=[mT]) if False else \
                    S.op("vector", lambda e: e.tensor_copy(out=dst, in_=bv[:, 0:nb * 128]), reads=[b], writes=[mT])

        def stage2(qb):
            cur = qb % 2
            NS = qb + 1
            mT = maskT[cur]
            yt = yat[cur]
            for gi in range(2):
                po = acc[gi]
                for sbk in range(NS):
                    b = next_bank(g, (3, 4, 5))
                    S.op("tensor", lambda e: e.matmul(out=b[:, 0:512], lhsT=kT_all[:, gi, sbk * 128:(sbk + 1) * 128],
                                                      rhs=qT[cur][:, 4 * gi:4 * gi + 4, :].rearrange("p h t -> p (h t)"),
                                                      start=True, stop=True),
                         reads=[kT_all, qT[cur]], writes=[b])
                    E = Eb[ctr["ke"] % 2]
                    P = Pb[ctr["ke"] % 2]
                    ctr["ke"] += 1
                    S.op("scalar", lambda e: e.activation(out=E[:], in_=b[:, 0:512], func=AF.Exp), reads=[b], writes=[E])
                    S.op("vector", lambda e: e.tensor_tensor(out=P[:].rearrange("p (h t) -> p h t", h=4),
                                                             in0=E[:].rearrange("p (h t) -> p h t", h=4),
                                                             in1=mT[:, sbk, :].unsqueeze(1).to_broadcast([128, 4, 128]), op=ALU.mult),
                         reads=[E, mT], writes=[P])
                    for h in range(4):
                        S.op("tensor", lambda e: e.matmul(out=po[:, h * 65:(h + 1) * 65], lhsT=P[:, h * 128:(h + 1) * 128],
                                                          rhs=v_all[:, sbk, gi, :], start=(sbk == 0 and h == 0),
                                                          stop=(sbk == NS - 1), skip_group_check=True),
                             reads=[P, v_all], writes=[po])
                pov = po[:, 0:260].rearrange("p (h e) -> p h e", e=65)
                S.op("vector", lambda e: e.reciprocal(out=rec[:], in_=pov[:, :, 64]), reads=[po], writes=[rec])
                S.op("vector", lambda e: e.tensor_tensor(out=yt[:, gi * 256:(gi + 1) * 256].rearrange("p (h d) -> p h d", d=64),
                                                         in0=pov[:, :, 0:64],
                                                         in1=rec[:, :].unsqueeze(2).to_broadcast([128, 4, 64]), op=ALU.mult),
                     reads=[po, rec], writes=[yt])
            S.dma("gpsimd", g.yatt_s[qb * 128:(qb + 1) * 128], yt[:], reads=[yt], writes=[g.yatt_s])

        S.record()
        stage1(0)
        S.replay(S.stop())
        for qb in range(NQB):
            S.record()
            stage2(qb)
            r2 = S.stop()
            r1 = None
            if qb + 1 < NQB:
                S.record()
                stage1(qb + 1)
                r1 = S.stop()
            noil = getattr(g, "noil", 0)
            if noil == 1 or r1 is None:
                S.replay(r2)
                S.replay(r1)
            elif noil == 2:
                S.replay(r2, r1[:ctr["split"]])
                S.replay(r1[ctr["split"]:])
            elif noil == 4:
                S.replay(r1[:2])
                S.replay(r2, r1[2:ctr["split"]])
                S.replay(r1[ctr["split"]:])
            elif noil == 5:
                S.replay(r2, r1[:2])
                S.replay(r1[2:])
            elif noil == 3:
                S.replay(r1[:ctr["split"]])
                S.replay(r2, r1[ctr["split"]:])
            else:
                S.replay(r2, r1)
        S.barrier()
    g.nrot = 8


def phase_c(g):
    nc, S = g.nc, g.S
    g.nrot = 8
    GS = 2
    with contextlib.ExitStack() as ph:
        def A(name, shape, dt=F32):
            return sb(g, ph, name, shape, dt)

        utri = A("utri", [64, 64])
        sel63 = A("sel63", [64, 128])
        S.op("gpsimd", lambda e: e.affine_select(out=utri[:], in_=g.ones_f[0:64, 0:64], pattern=[[1, 64]], compare_op=ALU.is_ge,
                                                 fill=g.fill0, base=0, channel_multiplier=-1), reads=[g.ones_f], writes=[utri])
        S.op("gpsimd", lambda e: e.affine_select(out=sel63[:], in_=g.ones_f[0:64, :], pattern=[[0, 128]], compare_op=ALU.is_equal,
                                                 fill=g.fill0, base=-63, channel_multiplier=1), reads=[g.ones_f], writes=[sel63])
        gno = A("gno", [128, 128])
        S.dma("sync", gno[:], g.dn_out_norm_gain.ap().partition_broadcast(128), writes=[gno])
        gbc = [A("gbc%d" % h, [64, NT, 8]) for h in range(2)]
        gn = [A("gn%d" % h, [64, NT, 8]) for h in range(2)]
        egc = [A("egc%d" % h, [64, NT, 4]) for h in range(2)]
        bg = [A("bg%d" % h, [64, NT, 4]) for h in range(2)]
        kd = [A("kd%d" % h, [64, NT, 4]) for h in range(2)]
        elast = [A("elast%d" % h, [128, NT, 4]) for h in range(2)]
        for h in range(2):
            S.dma("sync", gbc[h][:], g.gb_s[h * 64:(h + 1) * 64, :, :], reads=[g.gb_s], writes=[gbc[h]])
            b = next_bank(g)
            S.op("tensor", lambda e: e.matmul(out=b[0:64, 0:128], lhsT=utri[:], rhs=gbc[h][:, :, 0:4], start=True, stop=True),
                 reads=[utri, gbc[h]], writes=[b])
            S.op("vector", lambda e: e.tensor_copy(out=gn[h][:, :, 0:4], in_=b[0:64, 0:128].rearrange("p (n f) -> p n f", f=4)),
                 reads=[b], writes=[gn[h]])
            S.op("vector", lambda e: e.tensor_scalar(out=gn[h][:, :, 4:8], in0=gbc[h][:, :, 4:8], scalar1=-1.0, scalar2=None, op0=ALU.mult),
                 reads=[gbc[h]], writes=[gn[h]])
            S.op("scalar", lambda e: e.activation(out=egc[h][:], in_=gn[h][:, :, 0:4], func=AF.Exp), reads=[gn[h]], writes=[egc[h]])
            S.op("vector", lambda e: e.tensor_tensor(out=bg[h][:], in0=egc[h][:], in1=gbc[h][:, :, 4:8], op=ALU.mult),
                 reads=[egc[h], gbc[h]], writes=[bg[h]])
            b2 = next_bank(g)
            S.op("tensor", lambda e: e.matmul(out=b2[:, 0:128], lhsT=sel63[:], rhs=gn[h][:, :, 0:4], start=True, stop=True),
                 reads=[sel63, gn[h]], writes=[b2])
            S.op("scalar", lambda e: e.activation(out=elast[h][:], in_=b2[:, 0:128].rearrange("p (n f) -> p n f", f=4), func=AF.Exp),
                 reads=[b2], writes=[elast[h]])
            S.op("vector", lambda e: e.tensor_tensor(out=kd[h][:], in0=b2[0:64, 0:128].rearrange("p (n f) -> p n f", f=4),
                                                     in1=gn[h][:, :, 0:4], op=ALU.subtract), reads=[b2, gn[h]], writes=[kd[h]])
            S.op("scalar", lambda e: e.activation(out=kd[h][:], in_=kd[h][:], func=AF.Exp), reads=[kd[h]], writes=[kd[h]])

        Sst = A("Sst", [128, 4, 128])
        S.op("gpsimd", lambda e: e.memset(Sst[:], 0.0), writes=[Sst])

        class Slot:
            pass
        slots = []
        for i in range(GS):
            s_ = Slot()
            s_.q = A("cq%d" % i, [64, 512]); s_.k = A("ck%d" % i, [64, 512]); s_.v = A("cv%d" % i, [64, 512])
            s_.dz = A("cdz%d" % i, [64, 512], BF16)
            s_.dgb = A("dgb%d" % i, [64, 512]); s_.G1 = A("G1%d" % i, [64, 256]); s_.G2 = A("G2%d" % i, [64, 256])
            s_.sel3 = A("sel3%d" % i, [64, 768]); s_.E3 = A("E3%d" % i, [64, 768])
            s_.DATn = A("DATn%d" % i, [64, 256]); s_.DAn = A("DAn%d" % i, [64, 256])
            s_.kqT = A("kqT%d" % i, [128, 512])
            s_.MM = [A("MM%d_%d" % (i, j), [64, 512]) for j in range(2)]
            s_.XT = A("XT%d" % i, [64, 256]); s_.inT = A("inT%d" % i, [64, 256])
            s_.vb = A("vb%d" % i, [64, 512]); s_.kbg = A("kbg%d" % i, [64, 512]); s_.kdec = A("kdec%d" % i, [64, 512])
            s_.u = A("u%d" % i, [64, 512]); s_.wT = A("wT%d" % i, [128, 256])
            slots.append(s_)
        vnew = A("vnew", [64, 512])
        otmp = A("otmp", [64, 512])
        osq = A("osq", [64, 512])
        oss = A("oss", [64, 4]); osd = A("osd", [64, 4]); ors = A("ors", [64, 4])
        yout = [A("yout%d" % i, [64, 512], BF16) for i in range(2)]
        print("phase C sbuf remaining", nc.sbuf_bytes_remaining)
        idb = g.ident_f[0:64, 0:64]
        NCH = getattr(g, "nch", 64)

        def bc_h(ap4, n):
            return ap4.unsqueeze(2).to_broadcast([64, 4, n])

        def v3(ap, n):
            return ap.rearrange("p (h f) -> p h f", h=4)

        for c0 in range(0, NCH, GS):
            chunks = list(range(c0, min(NCH, c0 + GS)))
            info = {}
            for c in chunks:
                sl = slots[c % GS]
                tt, half = c // 2, c % 2
                rows = slice(c * 64, (c + 1) * 64)
                S.dma("sync", sl.q[:], g.dnq_s[rows], reads=[g.dnq_s], writes=[sl.q])
                S.dma("sync", sl.k[:], g.dnk_s[rows], reads=[g.dnk_s], writes=[sl.k])
                S.dma("sync", sl.v[:], g.dnv_s[rows], reads=[g.dnv_s], writes=[sl.v])
                S.dma("sync", sl.dz[:], g.dz_s[rows], reads=[g.dz_s], writes=[sl.dz])
                info[c] = (sl, tt, half)
            for c in chunks:
                sl, tt, half = info[c]
                gnc = gn[half][:, tt, :]
                S.op("vector", lambda e: e.tensor_tensor(out=sl.dgb[:].rearrange("p (a f) -> p a f", a=8),
                                                         in0=gnc.unsqueeze(2).to_broadcast([64, 8, 64]),
                                                         in1=idb.unsqueeze(1).to_broadcast([64, 8, 64]), op=ALU.mult),
                     reads=[gn[half], g.ident_f], writes=[sl.dgb])
                bR = next_bank(g)
                S.op("tensor", lambda e: e.matmul(out=bR[0:64, 0:512], lhsT=g.ones_f[0:64, 0:64], rhs=sl.dgb[:], start=True, stop=True),
                     reads=[g.ones_f, sl.dgb], writes=[bR])
                S.op("vector", lambda e: e.tensor_tensor(out=v3(sl.G1[:], 64), in0=v3(bR[0:64, 0:256], 64),
                                                         in1=bc_h(gn[half][:, tt, 0:4], 64), op=ALU.subtract),
                     reads=[bR, gn[half]], writes=[sl.G1])
                S.op("vector", lambda e: e.tensor_scalar(out=sl.G2[:], in0=sl.G1[:], scalar1=-1.0, scalar2=None, op0=ALU.mult),
                     reads=[sl.G1], writes=[sl.G2])
                S.op("gpsimd", lambda e: e.affine_select(out=sl.sel3[:, 0:256], in_=sl.G1[:], pattern=[[0, 4], [1, 64]],
                                                         compare_op=ALU.is_ge, fill=g.fillneg, base=0, channel_multiplier=-1),
                     reads=[sl.G1], writes=[sl.sel3])
                S.op("gpsimd", lambda e: e.affine_select(out=sl.sel3[:, 256:512], in_=sl.G1[:], pattern=[[0, 4], [1, 64]],
                                                         compare_op=ALU.is_ge, fill=g.fillneg, base=-1, channel_multiplier=-1),
                     reads=[sl.G1], writes=[sl.sel3])
                S.op("gpsimd", lambda e: e.affine_select(out=sl.sel3[:, 512:768], in_=sl.G2[:], pattern=[[0, 4], [-1, 64]],
                                                         compare_op=ALU.is_ge, fill=g.fillneg, base=-1, channel_multiplier=1),
                     reads=[sl.G2], writes=[sl.sel3])
                S.op("scalar", lambda e: e.activation(out=sl.E3[:], in_=sl.sel3[:], func=AF.Exp), reads=[sl.sel3], writes=[sl.E3])
                S.op("vector", lambda e: e.tensor_tensor(out=sl.DATn[:], in0=sl.E3[:, 256:512], in1=bR[0:64, 256:512], op=ALU.mult),
                     reads=[sl.E3, bR], writes=[sl.DATn])
                S.op("gpsimd", lambda e: e.tensor_tensor(out=v3(sl.DAn[:], 64), in0=v3(sl.E3[:, 512:768], 64),
                                                         in1=bc_h(gn[half][:, tt, 4:8], 64), op=ALU.mult),
                     reads=[sl.E3, gn[half]], writes=[sl.DAn])
                S.op("vector", lambda e: e.tensor_tensor(out=v3(sl.vb[:], 128), in0=v3(sl.v[:], 128),
                                                         in1=bc_h(gbc[half][:, tt, 4:8], 128), op=ALU.mult),
                     reads=[sl.v, gbc[half]], writes=[sl.vb])
                S.op("gpsimd", lambda e: e.tensor_tensor(out=v3(sl.kbg[:], 128), in0=v3(sl.k[:], 128),
                                                         in1=bc_h(bg[half][:, tt, :], 128), op=ALU.mult),
                     reads=[sl.k, bg[half]], writes=[sl.kbg])
                S.op("gpsimd", lambda e: e.tensor_tensor(out=v3(sl.kdec[:], 128), in0=v3(sl.k[:], 128),
                                                         in1=bc_h(kd[half][:, tt, :], 128), op=ALU.mult),
                     reads=[sl.k, kd[half]], writes=[sl.kdec])
                bT = next_bank(g)
                for hd in range(4):
                    S.op("tensor", lambda e: e.transpose(out=bT[:, hd * 64:(hd + 1) * 64], in_=sl.k[:, hd * 128:(hd + 1) * 128], identity=idb),
                         reads=[sl.k, g.ident_f], writes=[bT])
                for hd in range(4):
                    S.op("tensor", lambda e: e.transpose(out=bT[:, 256 + hd * 64:256 + (hd + 1) * 64], in_=sl.q[:, hd * 128:(hd + 1) * 128],
                                                         identity=idb), reads=[sl.q, g.ident_f], writes=[bT])
                S.op("scalar", lambda e: e.copy(out=sl.kqT[:], in_=bT[:, 0:512]), reads=[bT], writes=[sl.kqT])
                bK = next_bank(g)
                for hd in range(4):
                    kT_h = sl.kqT[:, hd * 64:(hd + 1) * 64]
                    qT_h = sl.kqT[:, 256 + hd * 64:256 + (hd + 1) * 64]
                    S.op("tensor", lambda e: e.matmul(out=bK[0:64, hd * 64:(hd + 1) * 64], lhsT=kT_h, rhs=kT_h, start=(hd == 0), stop=True,
                                                      skip_group_check=True), reads=[sl.kqT], writes=[bK])
                for hd in range(4):
                    kT_h = sl.kqT[:, hd * 64:(hd + 1) * 64]
                    qT_h = sl.kqT[:, 256 + hd * 64:256 + (hd + 1) * 64]
                    S.op("tensor", lambda e: e.matmul(out=bK[0:64, 256 + hd * 64:256 + (hd + 1) * 64], lhsT=kT_h, rhs=qT_h, start=False,
                                                      stop=True, skip_group_check=True), reads=[sl.kqT], writes=[bK])
                MM0 = sl.MM[0]
                S.op("vector", lambda e: e.tensor_tensor(out=MM0[:, 0:256], in0=bK[0:64, 0:256], in1=sl.DAn[:], op=ALU.mult),
                     reads=[bK, sl.DAn], writes=[MM0])
                S.op("vector", lambda e: e.tensor_tensor(out=MM0[:, 256:512], in0=bK[0:64, 0:256], in1=sl.DATn[:], op=ALU.mult),
                     reads=[bK, sl.DATn], writes=[MM0])
                S.op("vector", lambda e: e.tensor_tensor(out=sl.inT[:], in0=bK[0:64, 256:512], in1=sl.E3[:, 0:256], op=ALU.mult),
                     reads=[bK, sl.E3], writes=[sl.inT])
                S.op("gpsimd", lambda e: e.tensor_tensor(out=v3(sl.XT[:], 64), in0=v3(MM0[:, 256:512], 64),
                                                         in1=idb.unsqueeze(1).to_broadcast([64, 4, 64]), op=ALU.add),
                     reads=[MM0, g.ident_f], writes=[sl.XT])
            for lvl in range(1, 6):
                for c in chunks:
                    sl, tt, half = info[c]
                    Mp = sl.MM[(lvl - 1) % 2]
                    Mn = sl.MM[lvl % 2]
                    bM = next_bank(g)
                    for hd in range(4):
                        M_h = Mp[:, hd * 64:(hd + 1) * 64]
                        MT_h = Mp[:, 256 + hd * 64:256 + (hd + 1) * 64]
                        S.op("tensor", lambda e: e.matmul(out=bM[0:64, hd * 64:(hd + 1) * 64], lhsT=MT_h, rhs=M_h, start=(hd == 0), stop=True,
                                                          skip_group_check=True), reads=[Mp], writes=[bM])
                    nw = 256
                    if lvl < 5:
                        nw = 512
                        for hd in range(4):
                            M_h = Mp[:, hd * 64:(hd + 1) * 64]
                            MT_h = Mp[:, 256 + hd * 64:256 + (hd + 1) * 64]
                            S.op("tensor", lambda e: e.matmul(out=bM[0:64, 256 + hd * 64:256 + (hd + 1) * 64], lhsT=M_h, rhs=MT_h, start=False,
                                                              stop=True, skip_group_check=True), reads=[Mp], writes=[bM])
                    S.op("scalar", lambda e: e.copy(out=Mn[:, 0:nw], in_=bM[0:64, 0:nw]), reads=[bM], writes=[Mn])
                    bX = next_bank(g)
                    for hd in range(4):
                        S.op("tensor", lambda e: e.matmul(out=bX[0:64, hd * 64:(hd + 1) * 64], lhsT=Mn[:, hd * 64:(hd + 1) * 64],
                                                          rhs=sl.XT[:, hd * 64:(hd + 1) * 64], start=(hd == 0), stop=True,
                                                          skip_group_check=True), reads=[Mn, sl.XT], writes=[bX])
                    S.op("vector", lambda e: e.tensor_tensor(out=sl.XT[:], in0=bX[0:64, 0:256], in1=sl.XT[:], op=ALU.add),
                         reads=[bX, sl.XT], writes=[sl.XT])
            for c in chunks:
                sl, tt, half = info[c]
                bU = next_bank(g)
                for hd in range(4):
                    S.op("tensor", lambda e: e.matmul(out=bU[0:64, hd * 128:(hd + 1) * 128], lhsT=sl.XT[:, hd * 64:(hd + 1) * 64],
                                                      rhs=sl.vb[:, hd * 128:(hd + 1) * 128], start=(hd == 0), stop=True,
                                                      skip_group_check=True), reads=[sl.XT, sl.vb], writes=[bU])
                S.op("scalar", lambda e: e.copy(out=sl.u[:], in_=bU[0:64, 0:512]), reads=[bU], writes=[sl.u])
                bW = next_bank(g)
                for hd in range(4):
                    S.op("tensor", lambda e: e.matmul(out=bW[:, hd * 64:(hd + 1) * 64], lhsT=sl.kbg[:, hd * 128:(hd + 1) * 128],
                                                      rhs=sl.XT[:, hd * 64:(hd + 1) * 64], start=(hd == 0), stop=True,
                                                      skip_group_check=True), reads=[sl.XT, sl.kbg], writes=[bW])
                S.op("vector", lambda e: e.tensor_copy(out=sl.wT[:], in_=bW[:, 0:256]), reads=[bW], writes=[sl.wT])
            for c in chunks:
                sl, tt, half = info[c]
                rows = slice(c * 64, (c + 1) * 64)
                b1 = next_bank(g)
                for hd in range(4):
                    S.op("tensor", lambda e: e.matmul(out=b1[0:64, hd * 128:(hd + 1) * 128], lhsT=sl.wT[:, hd * 64:(hd + 1) * 64],
                                                      rhs=Sst[:, hd, :], start=(hd == 0), stop=True, skip_group_check=True),
                         reads=[sl.wT, Sst], writes=[b1])
                S.op("vector", lambda e: e.tensor_tensor(out=vnew[:], in0=sl.u[:], in1=b1[0:64, 0:512], op=ALU.subtract),
                     reads=[sl.u, b1], writes=[vnew])
                b2 = next_bank(g)
                for hd in range(4):
                    S.op("tensor", lambda e: e.matmul(out=b2[0:64, hd * 128:(hd + 1) * 128], lhsT=sl.kqT[:, 256 + hd * 64:256 + (hd + 1) * 64],
                                                      rhs=Sst[:, hd, :], start=(hd == 0), stop=True, skip_group_check=True),
                         reads=[sl.kqT, Sst], writes=[b2])
                b3 = next_bank(g)
                for hd in range(4):
                    S.op("tensor", lambda e: e.matmul(out=b3[0:64, hd * 128:(hd + 1) * 128], lhsT=sl.inT[:, hd * 64:(hd + 1) * 64],
                                                      rhs=vnew[:, hd * 128:(hd + 1) * 128], start=(hd == 0), stop=True, skip_group_check=True),
                         reads=[sl.inT, vnew], writes=[b3])
                b4 = next_bank(g)
                for hd in range(4):
                    S.op("tensor", lambda e: e.matmul(out=b4[:, hd * 128:(hd + 1) * 128], lhsT=sl.kdec[:, hd * 128:(hd + 1) * 128],
                                                      rhs=vnew[:, hd * 128:(hd + 1) * 128], start=(hd == 0), stop=True, skip_group_check=True),
                         reads=[sl.kdec, vnew], writes=[b4])
                for hd in range(4):
                    S.op("vector", lambda e: e.scalar_tensor_tensor(out=Sst[:, hd, :], in0=Sst[:, hd, :], scalar=elast[half][:, tt, hd:hd + 1],
                                                                    in1=b4[:, hd * 128:(hd + 1) * 128], op0=ALU.mult, op1=ALU.add),
                         reads=[Sst, elast[half], b4], writes=[Sst])
                S.op("vector", lambda e: e.tensor_tensor(out=v3(otmp[:], 128), in0=v3(b2[0:64, 0:512], 128),
                                                         in1=bc_h(egc[half][:, tt, :], 128), op=ALU.mult),
                     reads=[b2, egc[half]], writes=[otmp])
                S.op("vector", lambda e: e.tensor_tensor(out=otmp[:], in0=otmp[:], in1=b3[0:64, 0:512], op=ALU.add),
                     reads=[otmp, b3], writes=[otmp])
                S.op("scalar", lambda e: e.activation(out=osq[:], in_=otmp[:], func=AF.Square), reads=[otmp], writes=[osq])
                S.op("vector", lambda e: e.tensor_reduce(out=oss[:], in_=v3(osq[:], 128), axis=AX.X, op=ALU.add), reads=[osq], writes=[oss])
                S.op("scalar", lambda e: e.activation(out=osd[:], in_=oss[:], func=AF.Sqrt, bias=g.eps_t[0:64, 0:1], scale=1.0 / 128),
                     reads=[oss, g.eps_t], writes=[osd])
                S.op("vector", lambda e: e.reciprocal(out=ors[:], in_=osd[:]), reads=[osd], writes=[ors])
                S.op("vector", lambda e: e.tensor_tensor(out=v3(otmp[:], 128), in0=v3(otmp[:], 128), in1=bc_h(ors[:, :], 128), op=ALU.mult),
                     reads=[otmp, ors], writes=[otmp])
                S.op("gpsimd", lambda e: e.tensor_tensor(out=v3(otmp[:], 128), in0=v3(otmp[:], 128),
                                                         in1=gno[0:64, :].unsqueeze(1).to_broadcast([64, 4, 128]), op=ALU.mult),
                     reads=[otmp, gno], writes=[otmp])
                yo = yout[c % 2]
                S.op("vector", lambda e: e.tensor_tensor(out=yo[:], in0=otmp[:], in1=sl.dz[:], op=ALU.mult),
                     reads=[otmp, sl.dz], writes=[yo])
                S.dma("gpsimd", g.ydn_s[rows], yo[:], reads=[yo], writes=[g.ydn_s])
        S.barrier()


def phase_p(g):
    nc, S = g.nc, g.S
    with contextlib.ExitStack() as ph:
        stg = [sb(g, ph, "pstg%d" % i, [128, 4096], F32) for i in range(2)]
        cst = [sb(g, ph, "pcst%d" % i, [128, 4096], BF16) for i in range(2)]
        k = 0
        for ti, src in enumerate((g.peer_u, g.peer_v)):
            dst = g.uv_s
            sv = src.ap().rearrange("(b p j) d -> b p (j d)", p=128, j=4)
            dv = dst.t.rearrange("(b p j) d -> b p j d", p=128, j=4)
            for b in range(getattr(g, "npb", 32)):
                s_, c_ = stg[k % 2], cst[k % 2]
                S.dma("sync", s_[:], sv[b], writes=[s_])
                eng = ("vector", "gpsimd", "scalar")[k % 3]
                if eng == "scalar":
                    S.op(eng, lambda e: e.copy(out=c_[:], in_=s_[:]), reads=[s_], writes=[c_])
                else:
                    S.op(eng, lambda e: e.tensor_copy(out=c_[:], in_=s_[:]), reads=[s_], writes=[c_])
                S.dma("sync", dv[b][:, :, ti * D:(ti + 1) * D], c_[:].rearrange("p (j d) -> p j d", j=4), reads=[c_], writes=[dst])
                k += 1
        S.barrier()


def phase_de(g):
    nc, S = g.nc, g.S
    g.nrot = 8
    with contextlib.ExitStack() as ph:
        def A(name, shape, dt=F32):
            return sb(g, ph, name, shape, dt)

        wA = A("wA", [128, 4, D], BF16)
        wB = A("wB", [128, 4, D], BF16)
        wo = A("wo", [128, 8, D], BF16)
        wq = A("wq", [128, 8, 2048], BF16)
        cand = A("cand", [128, 8, 256])
        candf = cand[:].rearrange("p a b -> p (a b)")

        class _V:
            def __init__(self, ap, tl):
                self.ap, self.r = ap, tl.r

            def __getitem__(self, idx):
                return self.ap[idx]
        wstg = [_V(candf[:, i * 512:(i + 1) * 512], cand) for i in range(2)]
        k = 0
        for (src, dstt, nk, ncol) in ((g.w_att_branch, wA, 4, D), (g.w_dn_branch, wB, 4, D), (g.w_o, wo, 8, D),
                                      (g.peer_w_query, wq, 8, 2048)):
            sv = src.ap().rearrange("(kc p) n -> p kc n", p=128)
            for kc in range(nk):
                for c0 in range(0, ncol, 512):
                    s_ = wstg[k % 2]
                    S.dma("sync", s_[:], sv[:, kc, c0:c0 + 512], writes=[s_])
                    eng = ("vector", "gpsimd", "scalar")[k % 3]
                    dst = dstt[:, kc, c0:c0 + 512]
                    if eng == "scalar":
                        S.op(eng, lambda e: e.copy(out=dst, in_=s_[:]), reads=[s_], writes=[dstt])
                    else:
                        S.op(eng, lambda e: e.tensor_copy(out=dst, in_=s_[:]), reads=[s_], writes=[dstt])
                    k += 1
        g2_bc = A("g2_bc", [128, D])
        S.dma("sync", g2_bc[:], g.norm2_gain.ap().partition_broadcast(128), writes=[g2_bc])
        skT = A("skT", [128, 16, 128])
        for hp in range(16):
            s_ = wstg[hp % 2]
            S.dma("sync", s_[:, 0:128], g.peer_sub_keys.ap()[hp], writes=[s_])
            b = next_bank(g)
            S.op("tensor", lambda e: e.transpose(out=b[:, 0:128], in_=s_[:, 0:128], identity=g.ident_f[:]),
                 reads=[s_, g.ident_f], writes=[b])
            S.op("vector", lambda e: e.tensor_copy(out=skT[:, hp, :], in_=b[:, 0:128]), reads=[b], writes=[skT])
        iota_i = A("iota_i", [128, 16], I32)
        iota_f = A("iota_f", [128, 16])
        S.op("gpsimd", lambda e: e.iota(out=iota_i[:], pattern=[[1, 16]], base=0, channel_multiplier=0), writes=[iota_i])
        S.op("vector", lambda e: e.tensor_copy(out=iota_f[:], in_=iota_i[:]), reads=[iota_i], writes=[iota_f])

        ya = A("ya", [128, 512], BF16); yd = A("yd", [128, 512], BF16)
        gt = A("gt", [128, 2048], BF16)
        xt = A("xt", [128, D])
        yT = A("yT", [128, 8, 128], BF16)
        mg = A("mg", [128, D], BF16)
        mgT = A("mgT", [128, 8, 128], BF16)
        x1s = [A("x1_%d" % i, [128, D]) for i in range(2)]
        junk = A("junkd", [128, D], BF16)
        ss1 = A("ss1", [128, 1]); sd1 = A("sd1", [128, 1]); rs1 = A("rs1", [128, 1])
        h2s = [A("h2_%d" % i, [128, D], BF16) for i in range(2)]
        junk2 = A("junk2", [128, D], BF16)
        h2T = A("h2T", [128, 8, 128], BF16)
        qTp = A("qTp", [128, 16, 128])
        s_sb = A("s_sb", [128, 16, 128])
        s2 = A("s2", [128, 256])
        m16 = A("m16", [128, 16, 16])
        i16 = A("i16", [128, 16, 16], U32)
        best = A("best", [128, 8, 16])
        pos = A("pos", [128, 8, 16], U32)
        au = A("au", [128, 128], U32); bu = A("bu", [128, 128], U32)
        af = A("af", [128, 128]); bf = A("bf", [128, 128])
        i16f = A("i16f", [128, 16, 16])
        oh = A("oh", [128, 128, 16])
        ohf = oh[:].rearrange("p a b -> p (a b)")
        e0 = A("e0", [128, 128]); e1 = A("e1", [128, 128])
        eidxs = [A("eidx%d" % i, [128, 128], U32) for i in range(2)]
        gd = A("gd", [128, 8, 16]); gsum = A("gsum", [128, 8]); grec = A("grec", [128, 8])
        gates = [A("gate%d" % i, [128, 128]) for i in range(2)]
        act = A("act", [128, 128]); coef = A("coef", [128, 128])
        NBc = 10
        uv = [A("uv%d" % i, [128, 2 * D], BF16) for i in range(NBc)]
        prod = [A("prod%d" % i, [128, D], BF16) for i in range(2)]
        dgs = [A("dgs%d" % i, [128, 128], BF16) for i in range(4)]
        act1 = [A("act1_%d" % i, [128, 1]) for i in range(8)]
        ag1 = [A("ag1_%d" % i, [128, 1]) for i in range(8)]
        acc = A("acc", [128, D])
        g.nrot = 6
        pacc = g.banks[6:8]
        print("phase DE sbuf remaining", nc.sbuf_bytes_remaining)
        x_v = g.x.ap().rearrange("(n p) d -> n p d", p=128)
        o_v = g.out.ap().rearrange("(n p) d -> n p d", p=128)
        NTD = getattr(g, "ntd", NT)
        def stage_x(tt):
            rows = slice(tt * 128, (tt + 1) * 128)
            x1, h2, eidx, gate = x1s[tt % 2], h2s[tt % 2], eidxs[tt % 2], gates[tt % 2]
            S.dma("sync", ya[:], g.yatt_s[rows], reads=[g.yatt_s], writes=[ya])
            S.dma("sync", yd[:], g.ydn_s[rows], reads=[g.ydn_s], writes=[yd])
            S.dma("sync", gt[:], g.gate_s[rows], reads=[g.gate_s], writes=[gt])
            S.dma("sync", xt[:], x_v[tt], writes=[xt])
            b = next_bank(g)
            bv = b[:, :].bitcast(BF16)
            for j in range(4):
                S.op("tensor", lambda e: e.transpose(out=bv[:, j * 128:(j + 1) * 128], in_=ya[:, j * 128:(j + 1) * 128], identity=g.ident_b[:]),
                     reads=[ya, g.ident_b], writes=[b])
            for j in range(4):
                S.op("tensor", lambda e: e.transpose(out=bv[:, (4 + j) * 128:(5 + j) * 128], in_=yd[:, j * 128:(j + 1) * 128], identity=g.ident_b[:]),
                     reads=[yd, g.ident_b], writes=[b])
            S.op("scalar", lambda e: e.copy(out=yT[:].rearrange("p k t -> p (k t)"), in_=bv[:, 0:1024]), reads=[b], writes=[yT])
            for hf in range(2):
                cs = slice(hf * 512, (hf + 1) * 512)
                bA = next_bank(g)
                for kc in range(4):
                    S.op("tensor", lambda e: e.matmul(out=bA[:, 0:512], lhsT=yT[:, kc, :], rhs=wA[:, kc, cs], start=(kc == 0), stop=(kc == 3)),
                         reads=[yT, wA], writes=[bA])
                bB = next_bank(g)
                for kc in range(4):
                    S.op("tensor", lambda e: e.matmul(out=bB[:, 0:512], lhsT=yT[:, 4 + kc, :], rhs=wB[:, kc, cs], start=(kc == 0), stop=(kc == 3)),
                         reads=[yT, wB], writes=[bB])
                S.op("vector", lambda e: e.tensor_tensor(out=ohf[:, cs], in0=bA[:, 0:512], in1=gt[:, cs], op=ALU.mult),
                     reads=[bA, gt], writes=[oh])
                S.op("vector", lambda e: e.tensor_tensor(out=ohf[:, 1024 + hf * 512:1024 + (hf + 1) * 512], in0=bB[:, 0:512], in1=gt[:, 1024 + hf * 512:1024 + (hf + 1) * 512], op=ALU.mult),
                     reads=[bB, gt], writes=[oh])
            S.op("vector", lambda e: e.tensor_tensor(out=mg[:], in0=ohf[:, 0:1024], in1=ohf[:, 1024:2048], op=ALU.add), reads=[oh], writes=[mg])
            for half in range(2):
                b = next_bank(g)
                bv = b[:, :].bitcast(BF16)
                for j in range(4):
                    kc = half * 4 + j
                    S.op("tensor", lambda e: e.transpose(out=bv[:, j * 128:(j + 1) * 128], in_=mg[:, kc * 128:(kc + 1) * 128], identity=g.ident_b[:]),
                         reads=[mg, g.ident_b], writes=[b])
                S.op("scalar", lambda e: e.copy(out=mgT[:, half * 4:half * 4 + 4, :].rearrange("p k t -> p (k t)"), in_=bv[:, 0:512]),
                     reads=[b], writes=[mgT])
            for hf in range(2):
                cs = slice(hf * 512, (hf + 1) * 512)
                b = next_bank(g)
                for kc in range(8):
                    S.op("tensor", lambda e: e.matmul(out=b[:, 0:512], lhsT=mgT[:, kc, :], rhs=wo[:, kc, cs], start=(kc == 0), stop=(kc == 7)),
                         reads=[mgT, wo], writes=[b])
                S.op("vector", lambda e: e.tensor_tensor(out=x1[:, cs], in0=b[:, 0:512], in1=xt[:, cs], op=ALU.add),
                     reads=[b, xt], writes=[x1])
            S.op("scalar", lambda e: e.activation(out=junk[:], in_=x1[:], func=AF.Square, accum_out=ss1[:, 0:1]),
                 reads=[x1], writes=[junk, ss1])
            rstd_op(g, rs1, ss1, 1.0 / D, 1, sd1)
            S.op("vector", lambda e: e.scalar_tensor_tensor(out=h2[:], in0=x1[:], scalar=rs1[:, 0:1], in1=g2_bc[:], op0=ALU.mult, op1=ALU.mult),
                 reads=[x1, rs1, g2_bc], writes=[h2])
            for half in range(2):
                b = next_bank(g)
                bv = b[:, :].bitcast(BF16)
                for j in range(4):
                    kc = half * 4 + j
                    S.op("tensor", lambda e: e.transpose(out=bv[:, j * 128:(j + 1) * 128], in_=h2[:, kc * 128:(kc + 1) * 128], identity=g.ident_b[:]),
                         reads=[h2, g.ident_b], writes=[b])
                S.op("scalar", lambda e: e.copy(out=h2T[:, half * 4:half * 4 + 4, :].rearrange("p k t -> p (k t)"), in_=bv[:, 0:512]),
                     reads=[b], writes=[h2T])
            for q4 in range(4):
                b = next_bank(g)
                for j in range(4):
                    hp = q4 * 4 + j
                    for kc in range(8):
                        S.op("tensor", lambda e: e.matmul(out=b[:, j * 128:(j + 1) * 128], lhsT=wq[:, kc, hp * 128:(hp + 1) * 128],
                                                          rhs=h2T[:, kc, :], start=(j == 0 and kc == 0), stop=(kc == 7), skip_group_check=True),
                             reads=[wq, h2T], writes=[b])
                dst = qTp[:, q4 * 4:q4 * 4 + 4, :].rearrange("p a t -> p (a t)")
                if q4 % 2 == 0:
                    S.op("scalar", lambda e: e.copy(out=dst, in_=b[:, 0:512]), reads=[b], writes=[qTp])
                else:
                    S.op("vector", lambda e: e.tensor_copy(out=dst, in_=b[:, 0:512]), reads=[b], writes=[qTp])
            for q4 in range(4):
                b = next_bank(g)
                for j in range(4):
                    hp = q4 * 4 + j
                    S.op("tensor", lambda e: e.matmul(out=b[:, j * 128:(j + 1) * 128], lhsT=qTp[:, hp, :], rhs=skT[:, hp, :],
                                                      start=(j == 0), stop=True, skip_group_check=True),
                         reads=[qTp, skT], writes=[b])
                S.op("scalar", lambda e: e.copy(out=s_sb[:, q4 * 4:q4 * 4 + 4, :].rearrange("p a t -> p (a t)"), in_=b[:, 0:512]),
                     reads=[b], writes=[s_sb])
            for hp in range(16):
                sv = s_sb[:, hp, :]
                S.op("vector", lambda e: e.max(out=m16[:, hp, 0:8], in_=sv), reads=[s_sb], writes=[m16])
                S.op("vector", lambda e: e.max_index(out=i16[:, hp, 0:8], in_max=m16[:, hp, 0:8], in_values=sv), reads=[s_sb, m16], writes=[i16])
                S.op("vector", lambda e: e.match_replace(out=s2[:, 0:128], in_to_replace=m16[:, hp, 0:8], in_values=sv, imm_value=-1e30),
                     reads=[s_sb, m16], writes=[s2])
                S.op("vector", lambda e: e.max(out=m16[:, hp, 8:16], in_=s2[:, 0:128]), reads=[s2], writes=[m16])
                S.op("vector", lambda e: e.max_index(out=i16[:, hp, 8:16], in_max=m16[:, hp, 8:16], in_values=s2[:, 0:128]),
                     reads=[s2, m16], writes=[i16])
            m16v = m16[:].rearrange("p (h two) a -> p h two a", two=2)
            S.op("vector", lambda e: e.tensor_tensor(out=cand[:].rearrange("p h (a b) -> p h a b", b=16),
                                                     in0=m16v[:, :, 0, :].unsqueeze(3).to_broadcast([128, 8, 16, 16]),
                                                     in1=m16v[:, :, 1, :].unsqueeze(2).to_broadcast([128, 8, 16, 16]), op=ALU.add),
                 reads=[m16], writes=[cand])
            for h in range(8):
                cv = cand[:, h, :]
                S.op("vector", lambda e: e.max(out=best[:, h, 0:8], in_=cv), reads=[cand], writes=[best])
                S.op("vector", lambda e: e.max_index(out=pos[:, h, 0:8], in_max=best[:, h, 0:8], in_values=cv), reads=[cand, best], writes=[pos])
                S.op("vector", lambda e: e.match_replace(out=s2[:], in_to_replace=best[:, h, 0:8], in_values=cv, imm_value=-1e30),
                     reads=[cand, best], writes=[s2])
                S.op("vector", lambda e: e.max(out=best[:, h, 8:16], in_=s2[:]), reads=[s2], writes=[best])
                S.op("vector", lambda e: e.max_index(out=pos[:, h, 8:16], in_max=best[:, h, 8:16], in_values=s2[:]), reads=[s2, best], writes=[pos])
            posf = pos[:].rearrange("p h k -> p (h k)")
            S.op("vector", lambda e: e.tensor_scalar(out=au[:], in0=posf, scalar1=4, scalar2=None, op0=ALU.logical_shift_right), reads=[pos], writes=[au])
            S.op("vector", lambda e: e.tensor_scalar(out=bu[:], in0=posf, scalar1=15, scalar2=None, op0=ALU.bitwise_and), reads=[pos], writes=[bu])
            S.op("vector", lambda e: e.tensor_copy(out=af[:], in_=au[:]), reads=[au], writes=[af])
            S.op("vector", lambda e: e.tensor_copy(out=bf[:], in_=bu[:]), reads=[bu], writes=[bf])
            S.op("vector", lambda e: e.tensor_copy(out=i16f[:], in_=i16[:]), reads=[i16], writes=[i16f])
            i16fv = i16f[:].rearrange("p (h two) a -> p h two a", two=2)
            for which, (sel, dst) in enumerate(((af, e0), (bf, e1))):
                S.op("vector", lambda e: e.tensor_tensor(out=oh[:], in0=sel[:, :].unsqueeze(2).to_broadcast([128, 128, 16]),
                                                         in1=iota_f[:, :].unsqueeze(1).to_broadcast([128, 128, 16]), op=ALU.is_equal),
                     reads=[sel, iota_f], writes=[oh])
                S.op("vector", lambda e: e.tensor_tensor(out=oh[:].rearrange("p (h k) a -> p h k a", k=16),
                                                         in0=oh[:].rearrange("p (h k) a -> p h k a", k=16),
                                                         in1=i16fv[:, :, which, :].unsqueeze(2).to_broadcast([128, 8, 16, 16]), op=ALU.mult),
                     reads=[oh, i16f], writes=[oh])
                S.op("vector", lambda e: e.tensor_reduce(out=dst[:], in_=oh[:], axis=AX.X, op=ALU.add), reads=[oh], writes=[dst])
            S.op("vector", lambda e: e.scalar_tensor_tensor(out=e0[:], in0=e0[:], scalar=128.0, in1=e1[:], op0=ALU.mult, op1=ALU.add),
                 reads=[e0, e1], writes=[e0])
            S.op("vector", lambda e: e.tensor_copy(out=eidx[:], in_=e0[:]), reads=[e0], writes=[eidx])
            S.op("vector", lambda e: e.tensor_tensor(out=gd[:], in0=best[:], in1=best[:, :, 0:1].to_broadcast([128, 8, 16]), op=ALU.subtract),
                 reads=[best], writes=[gd])
            S.op("scalar", lambda e: e.activation(out=gd[:], in_=gd[:], func=AF.Exp), reads=[gd], writes=[gd])
            S.op("vector", lambda e: e.tensor_reduce(out=gsum[:], in_=gd[:], axis=AX.X, op=ALU.add), reads=[gd], writes=[gsum])
            S.op("vector", lambda e: e.reciprocal(out=grec[:], in_=gsum[:]), reads=[gsum], writes=[grec])
            S.op("vector", lambda e: e.tensor_tensor(out=gate[:].rearrange("p (h k) -> p h k", k=16), in0=gd[:],
                                                     in1=grec[:, :].unsqueeze(2).to_broadcast([128, 8, 16]), op=ALU.mult),
                 reads=[gd, grec], writes=[gate])

        def stage_y(tt):
            x1, h2, eidx, gate = x1s[tt % 2], h2s[tt % 2], eidxs[tt % 2], gates[tt % 2]
            NSL = getattr(g, "ngrp", 16) * 8
            LAG = 3

            def front(j):
                ub = uv[j % NBc]
                S.dma("gpsimd", ub[:], g.uv_s[:, :], reads=[eidx, g.uv_s], writes=[ub],
                      indirect=bass.IndirectOffsetOnAxis(ap=eidx[:, j:j + 1], axis=0))
                pr = prod[j % 2]
                a1 = act1[j % 8]
                a2 = ag1[j % 8]
                S.op("vector", lambda e: e.tensor_tensor(out=pr[:], in0=ub[:, 0:D], in1=h2[:], op=ALU.mult), reads=[ub, h2], writes=[pr])
                S.op("scalar", lambda e: e.activation(out=junk2[:], in_=pr[:], func=AF.Identity, accum_out=a1[:, 0:1]),
                     reads=[pr], writes=[junk2, a1])
                S.op("scalar", lambda e: e.activation(out=a2[:], in_=a1[:], func=AF.Gelu), reads=[a1], writes=[a2])

            def back(j):
                ub = uv[j % NBc]
                a2 = ag1[j % 8]
                dg = dgs[j % 4]
                S.op("vector", lambda e: e.scalar_tensor_tensor(out=dg[:], in0=g.ident_b[:], scalar=a2[:, 0:1],
                                                                in1=gate[:, j:j + 1].to_broadcast([128, 128]), op0=ALU.mult, op1=ALU.mult),
                     reads=[g.ident_b, a2, gate], writes=[dg])
                for hf in range(2):
                    S.op("tensor", lambda e: e.matmul(out=pacc[hf][:, 0:512], lhsT=dg[:], rhs=ub[:, D + hf * 512:D + (hf + 1) * 512],
                                                      start=(j == 0), stop=(j == NSL - 1)),
                         reads=[dg, ub], writes=[pacc[hf]])

            for j in range(NSL + LAG):
                if j < NSL:
                    front(j)
                if j >= LAG:
                    back(j - LAG)
            for hf in range(2):
                cs = slice(hf * 512, (hf + 1) * 512)
                S.op("vector", lambda e: e.tensor_tensor(out=acc[:, cs], in0=pacc[hf][:, 0:512], in1=x1[:, cs], op=ALU.add),
                     reads=[pacc[hf], x1], writes=[acc])
            S.dma("sync", o_v[tt], acc[:], reads=[acc])

        S.record()
        stage_x(0)
        S.replay(S.stop())
        for tt in range(NTD):
            S.record()
            stage_y(tt)
            ry = S.stop()
            rx = None
            if tt + 1 < NTD:
                S.record()
                stage_x(tt + 1)
                rx = S.stop()
            S.replay(ry, rx)
        S.barrier()


_CACHE = {}


def kernel(**inputs):
    x = np.asarray(inputs["x"], dtype=np.float32)
    if "nc" not in _CACHE:
        _CACHE["nc"] = build_program()
    nc = _CACHE["nc"]
    shared = {}
    for k in ("norm1_gain", "w_in", "q_norm_gain", "k_norm_gain", "dn_conv_w", "dn_a_log", "dn_dt_bias",
              "dn_out_norm_gain", "w_att_branch", "w_dn_branch", "w_o", "norm2_gain", "peer_w_query",
              "peer_u", "peer_v"):
        a = np.asarray(inputs[k], dtype=np.float32)[0]
        if a.ndim == 1:
            a = a.reshape(1, -1)
        shared[k] = np.ascontiguousarray(a)
    shared["peer_sub_keys"] = np.ascontiguousarray(
        np.asarray(inputs["peer_sub_keys"], dtype=np.float32)[0].reshape(16, 128, 128))
    in_maps = []
    for c in range(N_CORES):
        m = dict(shared)
        m["x"] = np.ascontiguousarray(x[c])
        in_maps.append(m)
    res = run_bass_kernel_spmd(nc, in_maps, core_ids=list(range(N_CORES)))
    out = np.stack([np.asarray(r["out"]) for r in res.results], axis=0)
    return out.astype(np.float32)
```

```python
import contextlib
import numpy as np
import concourse.bass as bass
import concourse.mybir as mybir
from concourse.bass_utils import run_bass_kernel_spmd

F32 = mybir.dt.float32
BF16 = mybir.dt.bfloat16
I32 = mybir.dt.int32
U32 = mybir.dt.uint32
AF = mybir.ActivationFunctionType
ALU = mybir.AluOpType
AX = mybir.AxisListType

T = 4096
D = 1024
NT = T // 128
IN_W = 5456
EPS = 1e-6
N_CORES = 8

C_AQ, C_AK, C_AV, C_IQ, C_IK, C_IW = 0, 512, 640, 768, 1280, 1344
C_DQ, C_DK, C_DV, C_DZ, C_DA, C_DB, C_GA, C_GB = 1352, 1864, 2376, 2888, 3400, 3404, 3408, 4432


class Res:
    __slots__ = ("name", "writer", "readers")

    def __init__(self, name):
        self.name = name
        self.writer = None
        self.readers = {}


class Tl:
    def __init__(self, t, name):
        self.t = t.ap() if hasattr(t, "ap") and "DRam" in type(t).__name__ else t
        self.r = Res(name)

    def __getitem__(self, idx):
        return self.t[idx]


class _RecEng:
    def __getattr__(self, name):
        def f(*args, **kw):
            self.__dict__["name"] = name
            self.__dict__["args"] = args
            self.__dict__["kw"] = kw
            return None
        return f


class Sched:
    CE = ("tensor", "vector", "scalar", "gpsimd")

    def __init__(self, nc, st, n_dma=12):
        self.nc = nc
        self.st = st
        self.eng = {n: getattr(nc, n) for n in self.CE + ("sync",)}
        self.sem = {}
        self.cnt = {}
        for n in self.CE:
            self.sem[n] = st.enter_context(nc.semaphore("s_" + n))
            self.cnt[n] = 0
        self.dq = {}
        for q in ("sync", "gpsimd"):
            sems = [st.enter_context(nc.semaphore("d_%s_%d" % (q, i))) for i in range(n_dma)]
            for i, s in enumerate(sems):
                self.sem[(q, i)] = s
                self.cnt[(q, i)] = 0
            self.dq[q] = [0, n_dma]
        self.seen = {}
        self.ninst = 0
        self.rec = None

    def record(self):
        self.rec = []

    def stop(self):
        r = self.rec
        self.rec = None
        return r

    def replay(self, *lists):
        lists = [l for l in lists if l]
        items = []
        for li, l in enumerate(lists):
            n = len(l)
            for i, it in enumerate(l):
                items.append(((i + 0.5) / n, li, i, it))
        items.sort(key=lambda t: (t[0], t[1], t[2]))
        for _, _, _, it in items:
            if it[0] == "op":
                _, E, name, args, kw, reads, writes = it
                self.op(E, lambda e: getattr(e, name)(*args, **kw), reads, writes)
            else:
                _, q, out, in_, reads, writes, indirect, kw = it
                self.dma(q, out, in_, reads, writes, indirect, **kw)

    def _wait(self, E, key, val):
        if val <= 0:
            return
        if E == "tensor" and key == "tensor":
            return
        k = (E, key)
        if self.seen.get(k, 0) >= val:
            return
        self.eng[E].wait_ge(self.sem[key], val)
        self.seen[k] = val
        self.ninst += 1

    def _deps(self, E, reads, writes):
        for r in reads:
            r = getattr(r, "r", r)
            if r.writer is not None:
                self._wait(E, *r.writer)
        for w in writes:
            w = getattr(w, "r", w)
            if w.writer is not None:
                self._wait(E, *w.writer)
            for key, val in w.readers.items():
                self._wait(E, key, val)

    def _mark(self, ev, reads, writes):
        key, val = ev
        for r in reads:
            r = getattr(r, "r", r)
            r.readers[key] = val
        for w in writes:
            w = getattr(w, "r", w)
            w.writer = ev
            w.readers = {}

    def op(self, E, emit, reads=(), writes=()):
        if self.rec is not None:
            r = _RecEng()
            emit(r)
            self.rec.append(("op", E, r.name, r.args, r.kw, list(reads), list(writes)))
            return
        self._deps(E, reads, writes)
        inst = emit(self.eng[E])
        self.cnt[E] += 1
        inst.then_inc(self.sem[E], 1)
        self.ninst += 1
        self._mark((E, self.cnt[E]), reads, writes)

    def dma(self, q, out, in_, reads=(), writes=(), indirect=None, **kw):
        if self.rec is not None:
            self.rec.append(("dma", q, out, in_, list(reads), list(writes), indirect, kw))
            return
        st = self.dq[q]
        i = st[0]
        st[0] = (i + 1) % st[1]
        key = (q, i)
        self._wait(q, key, self.cnt[key])
        self._deps(q, reads, writes)
        if indirect is not None:
            inst = self.eng[q].indirect_dma_start(out=out, out_offset=None, in_=in_, in_offset=indirect, **kw)
        else:
            inst = self.eng[q].dma_start(out=out, in_=in_, **kw)
        self.cnt[key] += 16
        inst.then_inc(self.sem[key], 16)
        self.ninst += 1
        self._mark((key, self.cnt[key]), reads, writes)

    def barrier(self):
        for E in self.CE + ("sync",):
            for key, val in self.cnt.items():
                if key == E:
                    continue
                if E == "tensor" and key == "tensor":
                    continue
                self._wait(E, key, val)

    def finish(self):
        for E in self.CE + ("sync",):
            for key, val in self.cnt.items():
                if key == E:
                    continue
                k = (E, key)
                if val > 0 and self.seen.get(k, 0) < val:
                    self.eng[E].wait_ge(self.sem[key], val)
                    self.seen[k] = val


class Ctx:
    pass


def build_program(debug=False, phases=("A", "B", "C", "P", "D"), **opts):
    nc = bass.Bass("TRN2", target_bir_lowering=False)
    g = Ctx()
    for k_, v_ in opts.items():
        setattr(g, k_, v_)
    g.nc = nc
    g.debug = debug

    def din(name, shape, dt=F32):
        return nc.dram_tensor(name, list(shape), dt, kind="ExternalInput")

    g.x = din("x", [T, D])
    g.norm1_gain = din("norm1_gain", [1, D])
    g.w_in = din("w_in", [D, IN_W])
    g.q_norm_gain = din("q_norm_gain", [1, 64])
    g.k_norm_gain = din("k_norm_gain", [1, 64])
    g.dn_conv_w = din("dn_conv_w", [4, 1536])
    g.dn_a_log = din("dn_a_log", [1, 4])
    g.dn_dt_bias = din("dn_dt_bias", [1, 4])
    g.dn_out_norm_gain = din("dn_out_norm_gain", [1, 128])
    g.w_att_branch = din("w_att_branch", [512, D])
    g.w_dn_branch = din("w_dn_branch", [512, D])
    g.w_o = din("w_o", [D, D])
    g.norm2_gain = din("norm2_gain", [1, D])
    g.peer_w_query = din("peer_w_query", [D, 2048])
    g.peer_sub_keys = din("peer_sub_keys", [16, 128, 128])
    g.peer_u = din("peer_u", [16384, D])
    g.peer_v = din("peer_v", [16384, D])
    g.out = nc.dram_tensor("out", [T, D], F32, kind="ExternalOutput")

    skind = "ExternalOutput" if debug else "Internal"

    def dscr(name, shape, dt):
        return Tl(nc.dram_tensor(name, list(shape), dt, kind=skind), name)

    g.qT_s = dscr("qT_s", [NT, 64, 8, 128], BF16)
    g.iqT_s = dscr("iqT_s", [NT, 64, 8, 128], BF16)
    g.kT_s = dscr("kT_s", [64, 2, T], BF16)
    g.ikT_s = dscr("ikT_s", [64, T], BF16)
    g.v_s = dscr("v_s", [T, 128], BF16)
    g.iw_s = dscr("iw_s", [128, NT, 8], F32)
    g.gb_s = dscr("gb_s", [128, NT, 8], F32)
    g.dnq_s = dscr("dnq_s", [T, 512], F32)
    g.dnk_s = dscr("dnk_s", [T, 512], F32)
    g.dnv_s = dscr("dnv_s", [T, 512], F32)
    g.dz_s = dscr("dz_s", [T, 512], BF16)
    g.gate_s = dscr("gate_s", [T, 2048], BF16)
    g.yatt_s = dscr("yatt_s", [T, 512], BF16)
    g.ydn_s = dscr("ydn_s", [T, 512], BF16)
    g.uv_s = Tl(nc.dram_tensor("uv_s", [16384, 2 * D], BF16, kind="Internal"), "uv_s")

    with contextlib.ExitStack() as st:
        S = Sched(nc, st)
        g.S = S
        g.st = st
        g.banks = [Tl(st.enter_context(nc.psum_tensor("bank%d" % i, [128, 512], F32)), "bank%d" % i)
                   for i in range(8)]
        g.bank_rr = 0
        g.pool_rr = {}
        setup_consts(g)
        if "A" in phases:
            phase_a(g)
        g.p_in_b = ("P" in phases and "B" in phases and not getattr(g, "p_sep", 0))
        if "B" in phases:
            phase_b(g)
        if "C" in phases:
            phase_c(g)
        if "P" in phases and not g.p_in_b:
            phase_p(g)
        if "D" in phases:
            phase_de(g)
        S.finish()
    return nc


def next_bank(g, pool=None):
    if pool is not None:
        rr = g.pool_rr.get(pool, 0)
        g.pool_rr[pool] = rr + 1
        return g.banks[pool[rr % len(pool)]]
    n = getattr(g, "nrot", 8)
    g.bank_rr = g.bank_rr % n
    b = g.banks[g.bank_rr]
    g.bank_rr = (g.bank_rr + 1) % n
    return b


def sb(g, st, name, shape, dt):
    g.uid = getattr(g, "uid", 0) + 1
    name = "%s_%d" % (name, g.uid)
    return Tl(st.enter_context(g.nc.sbuf_tensor(name, list(shape), dt)), name)


def setup_consts(g):
    nc, S, st = g.nc, g.S, g.st
    g.fill0 = nc.gpsimd.to_reg(0.0)
    g.fillneg = nc.gpsimd.to_reg(-1e30)
    g.ones_f = sb(g, st, "ones_f", [128, 128], F32)
    g.ident_f = sb(g, st, "ident_f", [128, 128], F32)
    g.ident_b = sb(g, st, "ident_b", [128, 128], BF16)
    g.eps_t = sb(g, st, "eps_t", [128, 1], F32)
    S.op("gpsimd", lambda e: e.memset(g.ones_f[:], 1.0), writes=[g.ones_f])
    S.op("gpsimd", lambda e: e.memset(g.eps_t[:], EPS), writes=[g.eps_t])
    S.op("gpsimd", lambda e: e.affine_select(out=g.ident_f[:], in_=g.ones_f[:], pattern=[[-1, 128]],
                                             compare_op=ALU.is_equal, fill=g.fill0, base=0, channel_multiplier=1),
         reads=[g.ones_f], writes=[g.ident_f])
    S.op("vector", lambda e: e.tensor_copy(out=g.ident_b[:], in_=g.ident_f[:]), reads=[g.ident_f], writes=[g.ident_b])


def bcast_row(ap_row, n):
    return ap_row.partition_broadcast(n)


def rstd_op(g, out, ss, scale, n_free, tmp):
    S = g.S
    S.op("scalar", lambda e: e.activation(out=tmp[:, 0:n_free], in_=ss[:, 0:n_free], func=AF.Sqrt,
                                          bias=g.eps_t[:, 0:1], scale=scale),
         reads=[ss, g.eps_t], writes=[tmp])
    S.op("vector", lambda e: e.reciprocal(out=out[:, 0:n_free], in_=tmp[:, 0:n_free]), reads=[tmp], writes=[out])


def phase_a(g):
    nc, S = g.nc, g.S
    with contextlib.ExitStack() as ph:
        def A(name, shape, dt):
            return sb(g, ph, name, shape, dt)

        w_bf = A("w_bf", [128, 8, IN_W], BF16)
        WCH = 1364
        wst = [A("wst%d" % i, [128, WCH], F32) for i in range(2)]
        w_in_v = g.w_in.ap().rearrange("(kc p) n -> p kc n", p=128)
        k = 0
        for kc in range(8):
            for c in range(IN_W // WCH):
                s_ = wst[k % 2]
                S.dma("sync", s_[:], w_in_v[:, kc, c * WCH:(c + 1) * WCH], writes=[s_])
                eng = ("vector", "gpsimd", "scalar")[k % 3]
                dst = w_bf[:, kc, c * WCH:(c + 1) * WCH]
                if eng == "scalar":
                    S.op(eng, lambda e: e.copy(out=dst, in_=s_[:]), reads=[s_], writes=[w_bf])
                else:
                    S.op(eng, lambda e: e.tensor_copy(out=dst, in_=s_[:]), reads=[s_], writes=[w_bf])
                k += 1

        g1_bc = A("g1_bc", [128, D], F32)
        S.dma("sync", g1_bc[:], g.norm1_gain.ap().partition_broadcast(128), writes=[g1_bc])
        gq_bc = A("gq_bc", [128, 64], F32)
        gk_bc = A("gk_bc", [128, 64], F32)
        S.dma("sync", gq_bc[:], g.q_norm_gain.ap().partition_broadcast(128), writes=[gq_bc])
        S.dma("sync", gk_bc[:], g.k_norm_gain.ap().partition_broadcast(128), writes=[gk_bc])
        S.op("vector", lambda e: e.tensor_scalar(out=gq_bc[:], in0=gq_bc[:], scalar1=0.125, scalar2=None, op0=ALU.mult),
             reads=[gq_bc], writes=[gq_bc])
        cw_bc = A("cw_bc", [128, 4, 1536], F32)
        for j in range(4):
            S.dma("sync", cw_bc[:, j, :], g.dn_conv_w.ap()[j:j + 1, :].partition_broadcast(128), writes=[cw_bc])
        dtb_bc = A("dtb_bc", [128, 4], F32)
        nea_bc = A("nea_bc", [128, 4], F32)
        S.dma("sync", dtb_bc[:], g.dn_dt_bias.ap().partition_broadcast(128), writes=[dtb_bc])
        S.dma("sync", nea_bc[:], g.dn_a_log.ap().partition_broadcast(128), writes=[nea_bc])
        S.op("scalar", lambda e: e.activation(out=nea_bc[:], in_=nea_bc[:], func=AF.Exp), reads=[nea_bc], writes=[nea_bc])
        S.op("vector", lambda e: e.tensor_scalar(out=nea_bc[:], in0=nea_bc[:], scalar1=-1.0, scalar2=None, op0=ALU.mult),
             reads=[nea_bc], writes=[nea_bc])

        shf = A("shf", [128, 128], F32)
        Sh = [g.ident_b] + [A("sh%d" % d, [128, 128], BF16) for d in (1, 2, 3)]
        ShP = [None] + [A("shp%d" % d, [128, 128], BF16) for d in (1, 2, 3)]
        for d in (1, 2, 3):
            S.op("gpsimd", lambda e: e.affine_select(out=shf[:], in_=g.ones_f[:], pattern=[[-1, 128]],
                                                     compare_op=ALU.is_equal, fill=g.fill0, base=d, channel_multiplier=1),
                 reads=[g.ones_f], writes=[shf])
            S.op("vector", lambda e: e.tensor_copy(out=Sh[d][:], in_=shf[:]), reads=[shf], writes=[Sh[d]])
            S.op("gpsimd", lambda e: e.affine_select(out=shf[:], in_=g.ones_f[:], pattern=[[-1, 128]],
                                                     compare_op=ALU.is_equal, fill=g.fill0, base=d - 128, channel_multiplier=1),
                 reads=[g.ones_f], writes=[shf])
            S.op("vector", lambda e: e.tensor_copy(out=ShP[d][:], in_=shf[:]), reads=[shf], writes=[ShP[d]])

        xt = [A("xt%d" % i, [128, D], F32) for i in range(2)]
        junk = A("junk", [128, D], BF16)
        h_bf = A("h_bf", [128, D], BF16)
        hT = [A("hT%d" % i, [128, 8, 128], BF16) for i in range(2)]
        ss1 = A("ss1", [128, 1], F32)
        sd1 = A("sd1", [128, 1], F32)
        rs1 = A("rs1", [128, 1], F32)
        sq = A("sq", [128, 512], F32)
        ss8 = A("ss8", [128, 8], F32)
        sd8 = A("sd8", [128, 8], F32)
        rs8 = A("rs8", [128, 8], F32)
        tmpf = A("tmpf", [128, 512], F32)
        qn_bf = A("qn_bf", [128, 512], BF16)
        kn_bf = A("kn_bf", [128, 128], BF16)
        iq_bf = A("iq_bf", [128, 512], BF16)
        ik_bf = A("ik_bf", [128, 64], BF16)
        qT_t = [A("qT_t%d" % i, [64, 8, 128], BF16) for i in range(2)]
        iqT_t = [A("iqT_t%d" % i, [64, 8, 128], BF16) for i in range(2)]
        kT_t = [A("kT_t%d" % i, [64, 2, 128], BF16) for i in range(2)]
        ikT_t = [A("ikT_t%d" % i, [64, 128], BF16) for i in range(2)]
        v_t = [A("v_t%d" % i, [128, 128], BF16) for i in range(2)]
        iw_all = A("iw_all", [128, NT, 8], F32)
        gb_all = A("gb_all", [128, NT, 8], F32)
        ab_tmp = A("ab_tmp", [128, 4], F32)
        xw = [A("xw%d" % i, [128, 4, 1536], BF16) for i in range(2)]
        yc = [A("yc%d" % i, [128, 512], F32) for i in range(3)]
        dz_t = [A("dz_t%d" % i, [128, 512], BF16) for i in range(2)]
        gt_t = [A("gt_t%d" % i, [128, 1024], BF16) for i in range(2)]
        print("phase A sbuf remaining", nc.sbuf_bytes_remaining)

        def proj(cols, lo, hi, hTt):
            b = next_bank(g)
            n = hi - lo
            for kc in range(8):
                S.op("tensor", lambda e: e.matmul(out=b[:, 0:n], lhsT=hTt[:, kc, :], rhs=w_bf[:, kc, lo:hi],
                                                  start=(kc == 0), stop=(kc == 7)),
                     reads=[hTt, w_bf], writes=[b])
            return b

        def headnorm(ps, nh, gain_bc, out_bf):
            n = nh * 64
            S.op("scalar", lambda e: e.activation(out=sq[:, 0:n], in_=ps[:, 0:n], func=AF.Square), reads=[ps], writes=[sq])
            S.op("vector", lambda e: e.tensor_reduce(out=ss8[:, 0:nh], in_=sq[:, 0:n].rearrange("p (h d) -> p h d", d=64),
                                                     axis=AX.X, op=ALU.add), reads=[sq], writes=[ss8])
            rstd_op(g, rs8, ss8, 1.0 / 64, nh, sd8)
            S.op("vector", lambda e: e.tensor_tensor(out=tmpf[:, 0:n].rearrange("p (h d) -> p h d", d=64),
                                                     in0=ps[:, 0:n].rearrange("p (h d) -> p h d", d=64),
                                                     in1=rs8[:, 0:nh].unsqueeze(2).to_broadcast([128, nh, 64]), op=ALU.mult),
                 reads=[ps, rs8], writes=[tmpf])
            S.op("vector", lambda e: e.tensor_tensor(out=out_bf[:, 0:n].rearrange("p (h d) -> p h d", d=64),
                                                     in0=tmpf[:, 0:n].rearrange("p (h d) -> p h d", d=64),
                                                     in1=gain_bc[:, :].unsqueeze(1).to_broadcast([128, nh, 64]), op=ALU.mult),
                 reads=[tmpf, gain_bc], writes=[out_bf])

        def transpose_heads(src_bf, nh, dstT, eng):
            b = next_bank(g)
            bv = b[:, :].bitcast(BF16)
            for h in range(nh):
                S.op("tensor", lambda e: e.transpose(out=bv[0:64, h * 128:(h + 1) * 128], in_=src_bf[:, h * 64:(h + 1) * 64],
                                                     identity=g.ident_b[:]),
                     reads=[src_bf, g.ident_b], writes=[b])
            src = bv[0:64, 0:nh * 128]
            dst = dstT[:, :, :].rearrange("p h t -> p (h t)") if nh > 1 else dstT[:, :]
            if eng == "scalar":
                S.op("scalar", lambda e: e.copy(out=dst, in_=src), reads=[b], writes=[dstT])
            else:
                S.op("vector", lambda e: e.tensor_copy(out=dst, in_=src), reads=[b], writes=[dstT])

        x_v = g.x.ap().rearrange("(n p) d -> n p d", p=128)
        S.dma("sync", xt[0][:], x_v[0], writes=[xt[0]])
        NTR = getattr(g, "ntr", NT)
        SECT = getattr(g, "sect", 99)
        for tt in range(NTR):
            cur = tt % 2
            if tt + 1 < NTR:
                S.dma("sync", xt[1 - cur][:], x_v[tt + 1], writes=[xt[1 - cur]])
            x_t = xt[cur]
            hTt = hT[cur]
            rows = slice(tt * 128, (tt + 1) * 128)
            S.op("scalar", lambda e: e.activation(out=junk[:], in_=x_t[:], func=AF.Square, accum_out=ss1[:, 0:1]),
                 reads=[x_t], writes=[junk, ss1])
            rstd_op(g, rs1, ss1, 1.0 / D, 1, sd1)
            S.op("vector", lambda e: e.scalar_tensor_tensor(out=h_bf[:], in0=x_t[:], scalar=rs1[:, 0:1], in1=g1_bc[:],
                                                            op0=ALU.mult, op1=ALU.mult),
                 reads=[x_t, rs1, g1_bc], writes=[h_bf])
            for half in range(2):
                b = next_bank(g)
                bv = b[:, :].bitcast(BF16)
                for j in range(4):
                    kc = half * 4 + j
                    S.op("tensor", lambda e: e.transpose(out=bv[:, j * 128:(j + 1) * 128], in_=h_bf[:, kc * 128:(kc + 1) * 128],
                                                         identity=g.ident_b[:]),
                         reads=[h_bf, g.ident_b], writes=[b])
                dst = hTt[:, half * 4:half * 4 + 4, :].rearrange("p k t -> p (k t)")
                if half == 0:
                    S.op("scalar", lambda e: e.copy(out=dst, in_=bv[:, 0:512]), reads=[b], writes=[hTt])
                else:
                    S.op("vector", lambda e: e.tensor_copy(out=dst, in_=bv[:, 0:512]), reads=[b], writes=[hTt])

            if SECT < 1:
                continue
            ps = proj("aq", C_AQ, C_AQ + 512, hTt)
            headnorm(ps, 8, gq_bc, qn_bf)
            transpose_heads(qn_bf, 8, qT_t[cur], "scalar")
            S.dma("gpsimd", g.qT_s[tt], qT_t[cur][:], reads=[qT_t[cur]], writes=[g.qT_s])
            if SECT < 2:
                continue
            ps = proj("kv", C_AK, C_AK + 256, hTt)
            headnorm(ps, 2, gk_bc, kn_bf)
            S.op("scalar", lambda e: e.copy(out=v_t[cur][:], in_=ps[:, 128:256]), reads=[ps], writes=[v_t[cur]])
            transpose_heads(kn_bf, 2, kT_t[cur], "vector")
            S.dma("gpsimd", g.kT_s[:, :, rows], kT_t[cur][:], reads=[kT_t[cur]], writes=[g.kT_s])
            S.dma("gpsimd", g.v_s[rows], v_t[cur][:], reads=[v_t[cur]], writes=[g.v_s])
            if SECT < 3:
                continue
            ps = proj("iq", C_IQ, C_IQ + 512, hTt)
            S.op("scalar", lambda e: e.copy(out=iq_bf[:], in_=ps[:, 0:512]), reads=[ps], writes=[iq_bf])
            transpose_heads(iq_bf, 8, iqT_t[cur], "vector")
            S.dma("gpsimd", g.iqT_s[tt], iqT_t[cur][:], reads=[iqT_t[cur]], writes=[g.iqT_s])
            if SECT < 4:
                continue
            ps = proj("ikw", C_IK, C_IK + 72, hTt)
            S.op("vector", lambda e: e.tensor_copy(out=ik_bf[:], in_=ps[:, 0:64]), reads=[ps], writes=[ik_bf])
            S.op("scalar", lambda e: e.copy(out=iw_all[:, tt, :], in_=ps[:, 64:72]), reads=[ps], writes=[iw_all])
            transpose_heads(ik_bf, 1, ikT_t[cur], "scalar")
            if "ikT" not in getattr(g, "skip", ()):
                S.dma("gpsimd", g.ikT_s[:, rows], ikT_t[cur][:], reads=[ikT_t[cur]], writes=[g.ikT_s])
            if SECT < 5:
                continue
            ps = proj("ab", C_DA, C_DA + 8, hTt)
            S.op("vector", lambda e: e.tensor_tensor(out=ab_tmp[:], in0=ps[:, 0:4], in1=dtb_bc[:], op=ALU.add),
                 reads=[ps, dtb_bc], writes=[ab_tmp])
            S.op("scalar", lambda e: e.activation(out=ab_tmp[:], in_=ab_tmp[:], func=AF.Exp), reads=[ab_tmp], writes=[ab_tmp])
            S.op("scalar", lambda e: e.activation(out=ab_tmp[:], in_=ab_tmp[:], func=AF.Ln, bias=g.ones_f[:, 0:1], scale=1.0),
                 reads=[ab_tmp, g.ones_f], writes=[ab_tmp])
            S.op("vector", lambda e: e.tensor_tensor(out=gb_all[:, tt, 0:4], in0=ab_tmp[:], in1=nea_bc[:], op=ALU.mult),
                 reads=[ab_tmp, nea_bc], writes=[gb_all])
            S.op("scalar", lambda e: e.activation(out=gb_all[:, tt, 4:8], in_=ps[:, 4:8], func=AF.Sigmoid),
                 reads=[ps], writes=[gb_all])
            if SECT < 6:
                continue
            for gi, (c0, dst_s) in enumerate(((C_DQ, g.dnq_s), (C_DK, g.dnk_s), (C_DV, g.dnv_s))):
                ps = proj("dn", c0, c0 + 512, hTt)
                cs = slice(gi * 512, (gi + 1) * 512)
                for j in range(4):
                    S.op("vector", lambda e: e.tensor_tensor(out=xw[cur][:, j, cs], in0=ps[:, 0:512], in1=cw_bc[:, j, cs], op=ALU.mult),
                         reads=[ps, cw_bc], writes=[xw[cur]])
                b = next_bank(g)
                mm = []
                for j in range(4):
                    mm.append((Sh[3 - j], xw[cur], j))
                if tt > 0:
                    for j in range(3):
                        mm.append((ShP[3 - j], xw[1 - cur], j))
                for i, (sh, xsrc, j) in enumerate(mm):
                    S.op("tensor", lambda e: e.matmul(out=b[:, 0:512], lhsT=sh[:], rhs=xsrc[:, j, cs],
                                                      start=(i == 0), stop=(i == len(mm) - 1)),
                         reads=[sh, xsrc], writes=[b])
                y = yc[gi]
                S.op("scalar", lambda e: e.activation(out=y[:], in_=b[:, 0:512], func=AF.Silu), reads=[b], writes=[y])
                if gi < 2:
                    S.op("gpsimd", lambda e: e.tensor_tensor(out=sq[:], in0=y[:], in1=y[:], op=ALU.mult), reads=[y], writes=[sq])
                    S.op("vector", lambda e: e.tensor_reduce(out=ss8[:, 0:4], in_=sq[:].rearrange("p (h d) -> p h d", d=128),
                                                             axis=AX.X, op=ALU.add), reads=[sq], writes=[ss8])
                    rstd_op(g, rs8, ss8, 1.0, 4, sd8)
                    if gi == 0:
                        S.op("vector", lambda e: e.tensor_scalar(out=rs8[:, 0:4], in0=rs8[:, 0:4], scalar1=128 ** -0.5, scalar2=None,
                                                                 op0=ALU.mult), reads=[rs8], writes=[rs8])
                    S.op("vector", lambda e: e.tensor_tensor(out=y[:].rearrange("p (h d) -> p h d", d=128),
                                                             in0=y[:].rearrange("p (h d) -> p h d", d=128),
                                                             in1=rs8[:, 0:4].unsqueeze(2).to_broadcast([128, 4, 128]), op=ALU.mult),
                         reads=[y, rs8], writes=[y])
                S.dma("gpsimd", dst_s[rows], y[:], reads=[y], writes=[dst_s])
            if SECT < 7:
                continue
            ps = proj("dz", C_DZ, C_DZ + 512, hTt)
            S.op("scalar", lambda e: e.activation(out=dz_t[cur][:], in_=ps[:, 0:512], func=AF.Silu), reads=[ps], writes=[dz_t[cur]])
            S.dma("gpsimd", g.dz_s[rows], dz_t[cur][:], reads=[dz_t[cur]], writes=[g.dz_s])
            if SECT < 8:
                continue
            for gi, c0 in enumerate((C_GA, C_GB)):
                for hf in range(2):
                    ps = proj("gate", c0 + hf * 512, c0 + (hf + 1) * 512, hTt)
                    S.op("scalar", lambda e: e.activation(out=gt_t[gi][:, hf * 512:(hf + 1) * 512], in_=ps[:, 0:512], func=AF.Sigmoid),
                         reads=[ps], writes=[gt_t[gi]])
                S.dma("gpsimd", g.gate_s[rows, gi * 1024:(gi + 1) * 1024], gt_t[gi][:], reads=[gt_t[gi]], writes=[g.gate_s])
        S.dma("gpsimd", g.iw_s[:, :, :], iw_all[:], reads=[iw_all], writes=[g.iw_s])
        S.dma("gpsimd", g.gb_s[:, :, :], gb_all[:], reads=[gb_all], writes=[g.gb_s])
        S.barrier()


NIT = 15


def phase_b(g):
    nc, S = g.nc, g.S
    g.nrot = 6
    acc = g.banks[6:8]
    with contextlib.ExitStack() as ph:
        def A(name, shape, dt):
            return sb(g, ph, name, shape, dt)

        kT_all = A("kT_all", [64, 2, T], BF16)
        ikT_all = A("ikT_all", [64, T], BF16)
        v_raw = A("v_raw", [128, NT, 128], BF16)
        v_all = A("v_all", [128, NT, 2, 65], BF16)
        iw_all = A("iw_all", [128, NT, 8], F32)
        S.dma("sync", kT_all[:], g.kT_s[:, :, :], reads=[g.kT_s], writes=[kT_all])
        S.dma("sync", ikT_all[:], g.ikT_s[:, :], reads=[g.ikT_s], writes=[ikT_all])
        S.dma("sync", v_raw[:], g.v_s[:, :].rearrange("(n p) c -> p n c", p=128), reads=[g.v_s], writes=[v_raw])
        S.dma("sync", iw_all[:], g.iw_s[:, :, :], reads=[g.iw_s], writes=[iw_all])
        S.op("gpsimd", lambda e: e.memset(v_all[:], 1.0), writes=[v_all])
        S.op("vector", lambda e: e.tensor_copy(out=v_all[:, :, :, 0:64],
                                               in_=v_raw[:].rearrange("p n (g d) -> p n g d", d=64)),
             reads=[v_raw], writes=[v_all])
        thr0 = A("thr0", [128, 1], F32)
        S.op("gpsimd", lambda e: e.memset(thr0[:], -1e29), writes=[thr0])
        cmask = A("cmask", [128, 128], F32)
        S.op("gpsimd", lambda e: e.memset(cmask[:], 0.0), writes=[cmask])
        S.op("gpsimd", lambda e: e.affine_select(out=cmask[:], in_=cmask[:], pattern=[[-1, 128]], compare_op=ALU.is_ge,
                                                 fill=g.fillneg, base=0, channel_multiplier=1), reads=[cmask], writes=[cmask])

        sc = [A("sc%d" % i, [128, T], F32) for i in range(2)]
        Rb = [A("Rb%d" % i, [128, 512], F32) for i in range(2)]
        junk = A("junkb", [128, T], BF16)
        mask = A("mask", [128, T], BF16)
        maskT = [A("maskT%d" % i, [128, NT, 128], BF16) for i in range(2)]
        iqT = [A("iqT%d" % i, [64, 8, 128], BF16) for i in range(2)]
        qT = [A("qT%d" % i, [64, 8, 128], BF16) for i in range(3)]
        Eb = [A("Eb%d" % i, [128, 512], BF16) for i in range(2)]
        Pb = [A("Pb%d" % i, [128, 512], BF16) for i in range(2)]
        yat = [A("yat%d" % i, [128, 512], BF16) for i in range(2)]
        rec = A("rec", [128, 4], F32)
        hi = A("hi", [128, 1], F32)
        lo = A("lo", [128, 1], F32)
        rk = A("rk", [128, 1], F32)
        mid = A("mid", [128, 1], F32)
        cnt = A("cnt", [128, 1], F32)
        step = A("step", [128, 1], F32)
        print("phase B sbuf remaining", nc.sbuf_bytes_remaining)

        NQB = getattr(g, "nqb", NT)
        pw = A("pw", [128, NIT + 1], F32)
        for it in range(NIT + 1):
            S.op("gpsimd", lambda e: e.memset(pw[:, it:it + 1], 2.0 ** -(it + 1)), writes=[pw])
        rkall = A("rkall", [128, NIT + 1], F32)
        nmid = A("nmid", [128, 1], F32)
        tq = A("tq", [128, 1], F32)
        cnt2 = A("cnt2", [128, 1], F32)
        thr_t = A("thr_t", [128, 1], F32)
        ctr = {"ke": 0}

        def stage1a(qb):
            cur = qb % 2
            S.dma("sync", iqT[cur][:], g.iqT_s[qb], reads=[g.iqT_s], writes=[iqT[cur]])
            S.dma("sync", qT[qb % 3][:], g.qT_s[qb], reads=[g.qT_s], writes=[qT[qb % 3]])
            NS = qb + 1
            SS = NS * 128
            sct = sc[cur]
            for ci in range((SS + 511) // 512):
                c0 = ci * 512
                n = min(512, SS - c0)
                for h in range(8):
                    b = next_bank(g, (0, 1))
                    S.op("tensor", lambda e: e.matmul(out=b[:, 0:n], lhsT=iqT[cur][:, h, :], rhs=ikT_all[:, c0:c0 + n],
                                                      start=True, stop=True),
                         reads=[iqT[cur], ikT_all], writes=[b])
                    R = Rb[ctr["ke"] % 2]
                    ctr["ke"] += 1
                    S.op("scalar", lambda e: e.activation(out=R[:, 0:n], in_=b[:, 0:n], func=AF.Relu), reads=[b], writes=[R])
                    if h == 0:
                        S.op("vector", lambda e: e.tensor_scalar(out=sct[:, c0:c0 + n], in0=R[:, 0:n], scalar1=iw_all[:, qb, 0:1],
                                                                 scalar2=None, op0=ALU.mult),
                             reads=[R, iw_all], writes=[sct])
                    else:
                        S.op("vector", lambda e: e.scalar_tensor_tensor(out=sct[:, c0:c0 + n], in0=R[:, 0:n],
                                                                        scalar=iw_all[:, qb, h:h + 1], in1=sct[:, c0:c0 + n],
                                                                        op0=ALU.mult, op1=ALU.add),
                             reads=[R, iw_all, sct], writes=[sct])
            dg = sct[:, qb * 128:(qb + 1) * 128]
            S.op("vector", lambda e: e.tensor_tensor(out=dg, in0=dg, in1=cmask[:], op=ALU.add), reads=[sct, cmask], writes=[sct])
        def stage1b(qb):
            cur = qb % 2
            NS = qb + 1
            SS = NS * 128
            sct = sc[cur]
            if qb >= 2:
                S.op("vector", lambda e: e.tensor_reduce(out=hi[:], in_=sct[:, 0:SS], axis=AX.X, op=ALU.max), reads=[sct], writes=[hi])
                S.op("vector", lambda e: e.tensor_reduce(out=lo[:], in_=sct[:, 0:qb * 128], axis=AX.X, op=ALU.min), reads=[sct], writes=[lo])
                S.op("vector", lambda e: e.tensor_tensor(out=rk[:], in0=hi[:], in1=lo[:], op=ALU.subtract), reads=[hi, lo], writes=[rk])
                S.op("vector", lambda e: e.tensor_tensor(out=rkall[:], in0=pw[:], in1=rk[:, 0:1].to_broadcast([128, NIT + 1]), op=ALU.mult),
                     reads=[pw, rk], writes=[rkall])
                S.op("vector", lambda e: e.tensor_scalar(out=nmid[:], in0=lo[:], scalar1=rkall[:, 0:1], scalar2=-1.0, op0=ALU.add, op1=ALU.mult),
                     reads=[lo, rkall], writes=[nmid])
                for it in range(NIT):
                    if getattr(g, "dvecnt", 0):
                        S.op("vector", lambda e: e.tensor_scalar(out=mid[:], in0=nmid[:], scalar1=-1.0, scalar2=None, op0=ALU.mult),
                             reads=[nmid], writes=[mid])
                        S.op("vector", lambda e: e.tensor_scalar(out=junk[:, 0:SS], in0=sct[:, 0:SS], scalar1=mid[:, 0:1], scalar2=None,
                                                                 op0=ALU.is_gt, op1=ALU.add, accum_out=cnt[:, 0:1]),
                             reads=[sct, mid], writes=[junk, cnt])
                        S.op("vector", lambda e: e.tensor_scalar(out=cnt2[:], in0=cnt[:], scalar1=2.0, scalar2=float(SS), op0=ALU.mult, op1=ALU.subtract),
                             reads=[cnt], writes=[cnt2])
                    else:
                        S.op("scalar", lambda e: e.activation(out=junk[:, 0:SS], in_=sct[:, 0:SS], func=AF.Sign, bias=nmid[:, 0:1], scale=1.0,
                                                              accum_out=cnt[:, 0:1]),
                             reads=[sct, nmid], writes=[junk, cnt])
                    cx = cnt2 if getattr(g, "dvecnt", 0) else cnt
                    S.op("vector", lambda e: e.tensor_scalar(out=tq[:], in0=cx[:], scalar1=511.5 - SS, scalar2=0.5, op0=ALU.is_lt, op1=ALU.subtract),
                         reads=[cx], writes=[tq])
                    S.op("vector", lambda e: e.scalar_tensor_tensor(out=nmid[:], in0=tq[:], scalar=rkall[:, it:it + 1], in1=nmid[:],
                                                                    op0=ALU.mult, op1=ALU.add), reads=[tq, rkall, nmid], writes=[nmid])
                S.op("vector", lambda e: e.tensor_scalar(out=thr_t[:], in0=nmid[:], scalar1=-1.0, scalar2=rkall[:, NIT:NIT + 1],
                                                         op0=ALU.mult, op1=ALU.subtract), reads=[nmid, rkall], writes=[thr_t])
                thr = thr_t
            else:
                thr = thr0
            S.op("vector", lambda e: e.tensor_scalar(out=mask[:, 0:SS], in0=sct[:, 0:SS], scalar1=thr[:, 0:1], scalar2=None,
                                                     op0=ALU.is_ge), reads=[sct, thr], writes=[mask])
            mT = maskT[cur]
            for b0 in range(0, NS, 8):
                nb = min(8, NS - b0)
                b = next_bank(g, (2,))
                bv = b[:, :].bitcast(BF16)
                for j in range(nb):
                    S.op("tensor", lambda e: e.transpose(out=bv[:, j * 128:(j + 1) * 128],
                                                         in_=mask[:, (b0 + j) * 128:(b0 + j + 1) * 128], identity=g.ident_b[:]),
                         reads=[mask, g.ident_b], writes=[b])
                dst = mT[:, b0:b0 + nb, :].rearrange("p n t -> p (n t)")
                S.op("gpsimd", lambda e: e.tensor_copy(out=dst, in_=bv[:, 0:nb * 128]), reads=[b], writes=[mT]) if False else \
                    S.op("vector", lambda e: e.tensor_copy(out=dst, in_=bv[:, 0:nb * 128]), reads=[b], writes=[mT])

        def stage2(qb):
            cur = qb % 2
            NS = qb + 1
            mT = maskT[cur]
            yt = yat[cur]
            for gi in range(2):
                po = acc[gi]
                for sbk in range(NS):
                    b = next_bank(g, (3, 4, 5))
                    S.op("tensor", lambda e: e.matmul(out=b[:, 0:512], lhsT=kT_all[:, gi, sbk * 128:(sbk + 1) * 128],
                                                      rhs=qT[qb % 3][:, 4 * gi:4 * gi + 4, :].rearrange("p h t -> p (h t)"),
                                                      start=True, stop=True),
                         reads=[kT_all, qT[qb % 3]], writes=[b])
                    E = Eb[ctr["ke"] % 2]
                    P = Pb[ctr["ke"] % 2]
                    ctr["ke"] += 1
                    S.op("scalar", lambda e: e.activation(out=E[:], in_=b[:, 0:512], func=AF.Exp), reads=[b], writes=[E])
                    S.op("vector", lambda e: e.tensor_tensor(out=P[:].rearrange("p (h t) -> p h t", h=4),
                                                             in0=E[:].rearrange("p (h t) -> p h t", h=4),
                                                             in1=mT[:, sbk, :].unsqueeze(1).to_broadcast([128, 4, 128]), op=ALU.mult),
                         reads=[E, mT], writes=[P])
                    for h in range(4):
                        S.op("tensor", lambda e: e.matmul(out=po[:, h * 65:(h + 1) * 65], lhsT=P[:, h * 128:(h + 1) * 128],
                                                          rhs=v_all[:, sbk, gi, :], start=(sbk == 0 and h == 0),
                                                          stop=(sbk == NS - 1), skip_group_check=True),
                             reads=[P, v_all], writes=[po])
                pov = po[:, 0:260].rearrange("p (h e) -> p h e", e=65)
                S.op("vector", lambda e: e.reciprocal(out=rec[:], in_=pov[:, :, 64]), reads=[po], writes=[rec])
                S.op("vector", lambda e: e.tensor_tensor(out=yt[:, gi * 256:(gi + 1) * 256].rearrange("p (h d) -> p h d", d=64),
                                                         in0=pov[:, :, 0:64],
                                                         in1=rec[:, :].unsqueeze(2).to_broadcast([128, 4, 64]), op=ALU.mult),
                     reads=[po, rec], writes=[yt])
            S.dma("sync", g.yatt_s[qb * 128:(qb + 1) * 128], yt[:], reads=[yt], writes=[g.yatt_s])

        plist = phase_p_ops(g, ph) if getattr(g, "p_in_b", False) else []
        def recd(fn, qb):
            if qb >= NQB:
                return None
            S.record()
            fn(qb)
            return S.stop()
        S.replay(recd(stage1a, 0))
        S.replay(recd(stage1b, 0), recd(stage1a, 1))
        for qb in range(NQB):
            pchunk = plist[(qb * len(plist)) // NQB:((qb + 1) * len(plist)) // NQB]
            r2 = recd(stage2, qb)
            r1b = recd(stage1b, qb + 1)
            r1a = recd(stage1a, qb + 2)
            if getattr(g, "noil", 0):
                for r in (r2, pchunk, r1b, r1a):
                    S.replay(r)
            else:
                S.replay(r2, r1b, r1a, pchunk)
        S.barrier()
    g.nrot = 8


def phase_c(g):
    nc, S = g.nc, g.S
    g.nrot = 8
    GS = 2
    with contextlib.ExitStack() as ph:
        def A(name, shape, dt=F32):
            return sb(g, ph, name, shape, dt)

        utri = A("utri", [64, 64])
        sel63 = A("sel63", [64, 128])
        S.op("gpsimd", lambda e: e.affine_select(out=utri[:], in_=g.ones_f[0:64, 0:64], pattern=[[1, 64]], compare_op=ALU.is_ge,
                                                 fill=g.fill0, base=0, channel_multiplier=-1), reads=[g.ones_f], writes=[utri])
        S.op("gpsimd", lambda e: e.affine_select(out=sel63[:], in_=g.ones_f[0:64, :], pattern=[[0, 128]], compare_op=ALU.is_equal,
                                                 fill=g.fill0, base=-63, channel_multiplier=1), reads=[g.ones_f], writes=[sel63])
        gno = A("gno", [128, 128])
        S.dma("sync", gno[:], g.dn_out_norm_gain.ap().partition_broadcast(128), writes=[gno])
        gbc = [A("gbc%d" % h, [64, NT, 8]) for h in range(2)]
        gn = [A("gn%d" % h, [64, NT, 8]) for h in range(2)]
        egc = [A("egc%d" % h, [64, NT, 4]) for h in range(2)]
        bg = [A("bg%d" % h, [64, NT, 4]) for h in range(2)]
        kd = [A("kd%d" % h, [64, NT, 4]) for h in range(2)]
        elast = [A("elast%d" % h, [128, NT, 4]) for h in range(2)]
        for h in range(2):
            S.dma("sync", gbc[h][:], g.gb_s[h * 64:(h + 1) * 64, :, :], reads=[g.gb_s], writes=[gbc[h]])
            b = next_bank(g)
            S.op("tensor", lambda e: e.matmul(out=b[0:64, 0:128], lhsT=utri[:], rhs=gbc[h][:, :, 0:4], start=True, stop=True),
                 reads=[utri, gbc[h]], writes=[b])
            S.op("vector", lambda e: e.tensor_copy(out=gn[h][:, :, 0:4], in_=b[0:64, 0:128].rearrange("p (n f) -> p n f", f=4)),
                 reads=[b], writes=[gn[h]])
            S.op("vector", lambda e: e.tensor_scalar(out=gn[h][:, :, 4:8], in0=gbc[h][:, :, 4:8], scalar1=-1.0, scalar2=None, op0=ALU.mult),
                 reads=[gbc[h]], writes=[gn[h]])
            S.op("scalar", lambda e: e.activation(out=egc[h][:], in_=gn[h][:, :, 0:4], func=AF.Exp), reads=[gn[h]], writes=[egc[h]])
            S.op("vector", lambda e: e.tensor_tensor(out=bg[h][:], in0=egc[h][:], in1=gbc[h][:, :, 4:8], op=ALU.mult),
                 reads=[egc[h], gbc[h]], writes=[bg[h]])
            b2 = next_bank(g)
            S.op("tensor", lambda e: e.matmul(out=b2[:, 0:128], lhsT=sel63[:], rhs=gn[h][:, :, 0:4], start=True, stop=True),
                 reads=[sel63, gn[h]], writes=[b2])
            S.op("scalar", lambda e: e.activation(out=elast[h][:], in_=b2[:, 0:128].rearrange("p (n f) -> p n f", f=4), func=AF.Exp),
                 reads=[b2], writes=[elast[h]])
            S.op("vector", lambda e: e.tensor_tensor(out=kd[h][:], in0=b2[0:64, 0:128].rearrange("p (n f) -> p n f", f=4),
                                                     in1=gn[h][:, :, 0:4], op=ALU.subtract), reads=[b2, gn[h]], writes=[kd[h]])
            S.op("scalar", lambda e: e.activation(out=kd[h][:], in_=kd[h][:], func=AF.Exp), reads=[kd[h]], writes=[kd[h]])

        Sst = A("Sst", [128, 4, 128])
        S.op("gpsimd", lambda e: e.memset(Sst[:], 0.0), writes=[Sst])

        class Slot:
            pass
        slots = []
        for i in range(2 * GS):
            s_ = Slot()
            s_.q = A("cq%d" % i, [64, 512]); s_.k = A("ck%d" % i, [64, 512]); s_.v = A("cv%d" % i, [64, 512])
            s_.dz = A("cdz%d" % i, [64, 512], BF16)
            s_.dgb = A("dgb%d" % i, [64, 512]); s_.G1 = A("G1%d" % i, [64, 256]); s_.G2 = A("G2%d" % i, [64, 256])
            s_.sel3 = A("sel3%d" % i, [64, 768]); s_.E3 = A("E3%d" % i, [64, 768])
            s_.DATn = A("DATn%d" % i, [64, 256]); s_.DAn = A("DAn%d" % i, [64, 256])
            s_.kqT = A("kqT%d" % i, [128, 512])
            s_.MM = [A("MM%d_%d" % (i, j), [64, 512]) for j in range(2)]
            s_.XT = A("XT%d" % i, [64, 256]); s_.inT = A("inT%d" % i, [64, 256])
            s_.vb = A("vb%d" % i, [64, 512]); s_.kbg = A("kbg%d" % i, [64, 512]); s_.kdec = A("kdec%d" % i, [64, 512])
            s_.u = A("u%d" % i, [64, 512]); s_.wT = A("wT%d" % i, [128, 256])
            slots.append(s_)
        vnew = A("vnew", [64, 512])
        otmp = A("otmp", [64, 512])
        osq = A("osq", [64, 512])
        oss = A("oss", [64, 4]); osd = A("osd", [64, 4]); ors = A("ors", [64, 4])
        yout = [A("yout%d" % i, [64, 512], BF16) for i in range(2)]
        print("phase C sbuf remaining", nc.sbuf_bytes_remaining)
        idb = g.ident_f[0:64, 0:64]
        NCH = getattr(g, "nch", 64)

        def bc_h(ap4, n):
            return ap4.unsqueeze(2).to_broadcast([64, 4, n])

        def v3(ap, n):
            return ap.rearrange("p (h f) -> p h f", h=4)

        def par(chunks):
            info = {}
            for c in chunks:
                sl = slots[c % (2 * GS)]
                tt, half = c // 2, c % 2
                rows = slice(c * 64, (c + 1) * 64)
                S.dma("sync", sl.q[:], g.dnq_s[rows], reads=[g.dnq_s], writes=[sl.q])
                S.dma("sync", sl.k[:], g.dnk_s[rows], reads=[g.dnk_s], writes=[sl.k])
                S.dma("sync", sl.v[:], g.dnv_s[rows], reads=[g.dnv_s], writes=[sl.v])
                S.dma("sync", sl.dz[:], g.dz_s[rows], reads=[g.dz_s], writes=[sl.dz])
                info[c] = (sl, tt, half)
            for c in chunks:
                sl, tt, half = info[c]
                gnc = gn[half][:, tt, :]
                S.op("vector", lambda e: e.tensor_tensor(out=sl.dgb[:].rearrange("p (a f) -> p a f", a=8),
                                                         in0=gnc.unsqueeze(2).to_broadcast([64, 8, 64]),
                                                         in1=idb.unsqueeze(1).to_broadcast([64, 8, 64]), op=ALU.mult),
                     reads=[gn[half], g.ident_f], writes=[sl.dgb])
                bR = next_bank(g, (0, 1, 2, 3))
                S.op("tensor", lambda e: e.matmul(out=bR[0:64, 0:512], lhsT=g.ones_f[0:64, 0:64], rhs=sl.dgb[:], start=True, stop=True),
                     reads=[g.ones_f, sl.dgb], writes=[bR])
                S.op("vector", lambda e: e.tensor_tensor(out=v3(sl.G1[:], 64), in0=v3(bR[0:64, 0:256], 64),
                                                         in1=bc_h(gn[half][:, tt, 0:4], 64), op=ALU.subtract),
                     reads=[bR, gn[half]], writes=[sl.G1])
                S.op("vector", lambda e: e.tensor_scalar(out=sl.G2[:], in0=sl.G1[:], scalar1=-1.0, scalar2=None, op0=ALU.mult),
                     reads=[sl.G1], writes=[sl.G2])
                S.op("gpsimd", lambda e: e.affine_select(out=sl.sel3[:, 0:256], in_=sl.G1[:], pattern=[[0, 4], [1, 64]],
                                                         compare_op=ALU.is_ge, fill=g.fillneg, base=0, channel_multiplier=-1),
                     reads=[sl.G1], writes=[sl.sel3])
                S.op("gpsimd", lambda e: e.affine_select(out=sl.sel3[:, 256:512], in_=sl.G1[:], pattern=[[0, 4], [1, 64]],
                                                         compare_op=ALU.is_ge, fill=g.fillneg, base=-1, channel_multiplier=-1),
                     reads=[sl.G1], writes=[sl.sel3])
                S.op("gpsimd", lambda e: e.affine_select(out=sl.sel3[:, 512:768], in_=sl.G2[:], pattern=[[0, 4], [-1, 64]],
                                                         compare_op=ALU.is_ge, fill=g.fillneg, base=-1, channel_multiplier=1),
                     reads=[sl.G2], writes=[sl.sel3])
                S.op("scalar", lambda e: e.activation(out=sl.E3[:], in_=sl.sel3[:], func=AF.Exp), reads=[sl.sel3], writes=[sl.E3])
                S.op("vector", lambda e: e.tensor_tensor(out=sl.DATn[:], in0=sl.E3[:, 256:512], in1=bR[0:64, 256:512], op=ALU.mult),
                     reads=[sl.E3, bR], writes=[sl.DATn])
                S.op("gpsimd", lambda e: e.tensor_tensor(out=v3(sl.DAn[:], 64), in0=v3(sl.E3[:, 512:768], 64),
                                                         in1=bc_h(gn[half][:, tt, 4:8], 64), op=ALU.mult),
                     reads=[sl.E3, gn[half]], writes=[sl.DAn])
                S.op("vector", lambda e: e.tensor_tensor(out=v3(sl.vb[:], 128), in0=v3(sl.v[:], 128),
                                                         in1=bc_h(gbc[half][:, tt, 4:8], 128), op=ALU.mult),
                     reads=[sl.v, gbc[half]], writes=[sl.vb])
                S.op("gpsimd", lambda e: e.tensor_tensor(out=v3(sl.kbg[:], 128), in0=v3(sl.k[:], 128),
                                                         in1=bc_h(bg[half][:, tt, :], 128), op=ALU.mult),
                     reads=[sl.k, bg[half]], writes=[sl.kbg])
                S.op("gpsimd", lambda e: e.tensor_tensor(out=v3(sl.kdec[:], 128), in0=v3(sl.k[:], 128),
                                                         in1=bc_h(kd[half][:, tt, :], 128), op=ALU.mult),
                     reads=[sl.k, kd[half]], writes=[sl.kdec])
                bT = next_bank(g, (0, 1, 2, 3))
                for hd in range(4):
                    S.op("tensor", lambda e: e.transpose(out=bT[:, hd * 64:(hd + 1) * 64], in_=sl.k[:, hd * 128:(hd + 1) * 128], identity=idb),
                         reads=[sl.k, g.ident_f], writes=[bT])
                for hd in range(4):
                    S.op("tensor", lambda e: e.transpose(out=bT[:, 256 + hd * 64:256 + (hd + 1) * 64], in_=sl.q[:, hd * 128:(hd + 1) * 128],
                                                         identity=idb), reads=[sl.q, g.ident_f], writes=[bT])
                S.op("scalar", lambda e: e.copy(out=sl.kqT[:], in_=bT[:, 0:512]), reads=[bT], writes=[sl.kqT])
                bK = next_bank(g, (0, 1, 2, 3))
                for hd in range(4):
                    kT_h = sl.kqT[:, hd * 64:(hd + 1) * 64]
                    qT_h = sl.kqT[:, 256 + hd * 64:256 + (hd + 1) * 64]
                    S.op("tensor", lambda e: e.matmul(out=bK[0:64, hd * 64:(hd + 1) * 64], lhsT=kT_h, rhs=kT_h, start=(hd == 0), stop=True,
                                                      skip_group_check=True), reads=[sl.kqT], writes=[bK])
                for hd in range(4):
                    kT_h = sl.kqT[:, hd * 64:(hd + 1) * 64]
                    qT_h = sl.kqT[:, 256 + hd * 64:256 + (hd + 1) * 64]
                    S.op("tensor", lambda e: e.matmul(out=bK[0:64, 256 + hd * 64:256 + (hd + 1) * 64], lhsT=kT_h, rhs=qT_h, start=False,
                                                      stop=True, skip_group_check=True), reads=[sl.kqT], writes=[bK])
                MM0 = sl.MM[0]
                S.op("vector", lambda e: e.tensor_tensor(out=MM0[:, 0:256], in0=bK[0:64, 0:256], in1=sl.DAn[:], op=ALU.mult),
                     reads=[bK, sl.DAn], writes=[MM0])
                S.op("vector", lambda e: e.tensor_tensor(out=MM0[:, 256:512], in0=bK[0:64, 0:256], in1=sl.DATn[:], op=ALU.mult),
                     reads=[bK, sl.DATn], writes=[MM0])
                S.op("vector", lambda e: e.tensor_tensor(out=sl.inT[:], in0=bK[0:64, 256:512], in1=sl.E3[:, 0:256], op=ALU.mult),
                     reads=[bK, sl.E3], writes=[sl.inT])
                S.op("gpsimd", lambda e: e.tensor_tensor(out=v3(sl.XT[:], 64), in0=v3(MM0[:, 256:512], 64),
                                                         in1=idb.unsqueeze(1).to_broadcast([64, 4, 64]), op=ALU.add),
                     reads=[MM0, g.ident_f], writes=[sl.XT])
            for lvl in range(1, 6):
                for c in chunks:
                    sl, tt, half = info[c]
                    Mp = sl.MM[(lvl - 1) % 2]
                    Mn = sl.MM[lvl % 2]
                    bM = next_bank(g, (0, 1, 2, 3))
                    for hd in range(4):
                        M_h = Mp[:, hd * 64:(hd + 1) * 64]
                        MT_h = Mp[:, 256 + hd * 64:256 + (hd + 1) * 64]
                        S.op("tensor", lambda e: e.matmul(out=bM[0:64, hd * 64:(hd + 1) * 64], lhsT=MT_h, rhs=M_h, start=(hd == 0), stop=True,
                                                          skip_group_check=True), reads=[Mp], writes=[bM])
                    nw = 256
                    if lvl < 5:
                        nw = 512
                        for hd in range(4):
                            M_h = Mp[:, hd * 64:(hd + 1) * 64]
                            MT_h = Mp[:, 256 + hd * 64:256 + (hd + 1) * 64]
                            S.op("tensor", lambda e: e.matmul(out=bM[0:64, 256 + hd * 64:256 + (hd + 1) * 64], lhsT=M_h, rhs=MT_h, start=False,
                                                              stop=True, skip_group_check=True), reads=[Mp], writes=[bM])
                    S.op("scalar", lambda e: e.copy(out=Mn[:, 0:nw], in_=bM[0:64, 0:nw]), reads=[bM], writes=[Mn])
                    bX = next_bank(g, (0, 1, 2, 3))
                    for hd in range(4):
                        S.op("tensor", lambda e: e.matmul(out=bX[0:64, hd * 64:(hd + 1) * 64], lhsT=Mn[:, hd * 64:(hd + 1) * 64],
                                                          rhs=sl.XT[:, hd * 64:(hd + 1) * 64], start=(hd == 0), stop=True,
                                                          skip_group_check=True), reads=[Mn, sl.XT], writes=[bX])
                    S.op("vector", lambda e: e.tensor_tensor(out=sl.XT[:], in0=bX[0:64, 0:256], in1=sl.XT[:], op=ALU.add),
                         reads=[bX, sl.XT], writes=[sl.XT])
            for c in chunks:
                sl, tt, half = info[c]
                bU = next_bank(g, (0, 1, 2, 3))
                for hd in range(4):
                    S.op("tensor", lambda e: e.matmul(out=bU[0:64, hd * 128:(hd + 1) * 128], lhsT=sl.XT[:, hd * 64:(hd + 1) * 64],
                                                      rhs=sl.vb[:, hd * 128:(hd + 1) * 128], start=(hd == 0), stop=True,
                                                      skip_group_check=True), reads=[sl.XT, sl.vb], writes=[bU])
                S.op("scalar", lambda e: e.copy(out=sl.u[:], in_=bU[0:64, 0:512]), reads=[bU], writes=[sl.u])
                bW = next_bank(g, (0, 1, 2, 3))
                for hd in range(4):
                    S.op("tensor", lambda e: e.matmul(out=bW[:, hd * 64:(hd + 1) * 64], lhsT=sl.kbg[:, hd * 128:(hd + 1) * 128],
                                                      rhs=sl.XT[:, hd * 64:(hd + 1) * 64], start=(hd == 0), stop=True,
                                                      skip_group_check=True), reads=[sl.XT, sl.kbg], writes=[bW])
                S.op("vector", lambda e: e.tensor_copy(out=sl.wT[:], in_=bW[:, 0:256]), reads=[bW], writes=[sl.wT])
            return info

        def rec(chunks, info):
            for c in chunks:
                sl, tt, half = info[c]
                rows = slice(c * 64, (c + 1) * 64)
                b1 = next_bank(g, (4, 5, 6, 7))
                for hd in range(4):
                    S.op("tensor", lambda e: e.matmul(out=b1[0:64, hd * 128:(hd + 1) * 128], lhsT=sl.wT[:, hd * 64:(hd + 1) * 64],
                                                      rhs=Sst[:, hd, :], start=(hd == 0), stop=True, skip_group_check=True),
                         reads=[sl.wT, Sst], writes=[b1])
                S.op("vector", lambda e: e.tensor_tensor(out=vnew[:], in0=sl.u[:], in1=b1[0:64, 0:512], op=ALU.subtract),
                     reads=[sl.u, b1], writes=[vnew])
                b2 = next_bank(g, (4, 5, 6, 7))
                for hd in range(4):
                    S.op("tensor", lambda e: e.matmul(out=b2[0:64, hd * 128:(hd + 1) * 128], lhsT=sl.kqT[:, 256 + hd * 64:256 + (hd + 1) * 64],
                                                      rhs=Sst[:, hd, :], start=(hd == 0), stop=True, skip_group_check=True),
                         reads=[sl.kqT, Sst], writes=[b2])
                b3 = next_bank(g, (4, 5, 6, 7))
                for hd in range(4):
                    S.op("tensor", lambda e: e.matmul(out=b3[0:64, hd * 128:(hd + 1) * 128], lhsT=sl.inT[:, hd * 64:(hd + 1) * 64],
                                                      rhs=vnew[:, hd * 128:(hd + 1) * 128], start=(hd == 0), stop=True, skip_group_check=True),
                         reads=[sl.inT, vnew], writes=[b3])
                b4 = next_bank(g, (4, 5, 6, 7))
                for hd in range(4):
                    S.op("tensor", lambda e: e.matmul(out=b4[:, hd * 128:(hd + 1) * 128], lhsT=sl.kdec[:, hd * 128:(hd + 1) * 128],
                                                      rhs=vnew[:, hd * 128:(hd + 1) * 128], start=(hd == 0), stop=True, skip_group_check=True),
                         reads=[sl.kdec, vnew], writes=[b4])
                for hd in range(4):
                    S.op("vector", lambda e: e.scalar_tensor_tensor(out=Sst[:, hd, :], in0=Sst[:, hd, :], scalar=elast[half][:, tt, hd:hd + 1],
                                                                    in1=b4[:, hd * 128:(hd + 1) * 128], op0=ALU.mult, op1=ALU.add),
                         reads=[Sst, elast[half], b4], writes=[Sst])
                S.op("vector", lambda e: e.tensor_tensor(out=v3(otmp[:], 128), in0=v3(b2[0:64, 0:512], 128),
                                                         in1=bc_h(egc[half][:, tt, :], 128), op=ALU.mult),
                     reads=[b2, egc[half]], writes=[otmp])
                S.op("vector", lambda e: e.tensor_tensor(out=otmp[:], in0=otmp[:], in1=b3[0:64, 0:512], op=ALU.add),
                     reads=[otmp, b3], writes=[otmp])
                S.op("scalar", lambda e: e.activation(out=osq[:], in_=otmp[:], func=AF.Square), reads=[otmp], writes=[osq])
                S.op("vector", lambda e: e.tensor_reduce(out=oss[:], in_=v3(osq[:], 128), axis=AX.X, op=ALU.add), reads=[osq], writes=[oss])
                S.op("scalar", lambda e: e.activation(out=osd[:], in_=oss[:], func=AF.Sqrt, bias=g.eps_t[0:64, 0:1], scale=1.0 / 128),
                     reads=[oss, g.eps_t], writes=[osd])
                S.op("vector", lambda e: e.reciprocal(out=ors[:], in_=osd[:]), reads=[osd], writes=[ors])
                S.op("vector", lambda e: e.tensor_tensor(out=v3(otmp[:], 128), in0=v3(otmp[:], 128), in1=bc_h(ors[:, :], 128), op=ALU.mult),
                     reads=[otmp, ors], writes=[otmp])
                S.op("gpsimd", lambda e: e.tensor_tensor(out=v3(otmp[:], 128), in0=v3(otmp[:], 128),
                                                         in1=gno[0:64, :].unsqueeze(1).to_broadcast([64, 4, 128]), op=ALU.mult),
                     reads=[otmp, gno], writes=[otmp])
                yo = yout[c % 2]
                S.op("vector", lambda e: e.tensor_tensor(out=yo[:], in0=otmp[:], in1=sl.dz[:], op=ALU.mult),
                     reads=[otmp, sl.dz], writes=[yo])
                S.dma("gpsimd", g.ydn_s[rows], yo[:], reads=[yo], writes=[g.ydn_s])

        groups = [list(range(c0, min(NCH, c0 + GS))) for c0 in range(0, NCH, GS)]
        S.record()
        inf = par(groups[0])
        S.replay(S.stop())
        for gi_, grp in enumerate(groups):
            S.record()
            rec(grp, inf)
            rr = S.stop()
            rp = None
            if gi_ + 1 < len(groups):
                S.record()
                inf = par(groups[gi_ + 1])
                rp = S.stop()
            S.replay(rr, rp)
        S.barrier()


def phase_p_ops(g, ph):
    nc, S = g.nc, g.S
    S.record()
    if getattr(g, "p_dmacast", 1):
        for ti, src in enumerate((g.peer_u, g.peer_v)):
            dst = g.uv_s
            sv = src.ap().rearrange("(b p j) d -> b p j d", p=128, j=4)
            dv = dst.t.rearrange("(b p j) d -> b p j d", p=128, j=4)
            for b in range(getattr(g, "npb", 32)):
                S.dma("gpsimd", dv[b][:, :, ti * D:(ti + 1) * D], sv[b], writes=[dst])
        return S.stop()
    stg = [sb(g, ph, "pstg%d" % i, [128, 4096], F32) for i in range(2)]
    cst = [sb(g, ph, "pcst%d" % i, [128, 4096], BF16) for i in range(2)]
    k = 0
    for ti, src in enumerate((g.peer_u, g.peer_v)):
        dst = g.uv_s
        sv = src.ap().rearrange("(b p j) d -> b p (j d)", p=128, j=4)
        dv = dst.t.rearrange("(b p j) d -> b p j d", p=128, j=4)
        for b in range(getattr(g, "npb", 32)):
            s_, c_ = stg[k % 2], cst[k % 2]
            pq = "gpsimd" if getattr(g, "p_in_b", False) else "sync"
            S.dma(pq, s_[:], sv[b], writes=[s_])
            S.op("gpsimd", lambda e: e.tensor_copy(out=c_[:], in_=s_[:]), reads=[s_], writes=[c_])
            S.dma(pq, dv[b][:, :, ti * D:(ti + 1) * D], c_[:].rearrange("p (j d) -> p j d", j=4), reads=[c_], writes=[dst])
            k += 1
    return S.stop()


def phase_p(g):
    nc, S = g.nc, g.S
    with contextlib.ExitStack() as ph:
        S.replay(phase_p_ops(g, ph))
        S.barrier()


def phase_de(g):
    nc, S = g.nc, g.S
    g.nrot = 8
    with contextlib.ExitStack() as ph:
        def A(name, shape, dt=F32):
            return sb(g, ph, name, shape, dt)

        wA = A("wA", [128, 4, D], BF16)
        wB = A("wB", [128, 4, D], BF16)
        wo = A("wo", [128, 8, D], BF16)
        wq = A("wq", [128, 8, 2048], BF16)
        cand = A("cand", [128, 8, 256])
        candf = cand[:].rearrange("p a b -> p (a b)")

        class _V:
            def __init__(self, ap, tl):
                self.ap, self.r = ap, tl.r

            def __getitem__(self, idx):
                return self.ap[idx]
        wstg = [_V(candf[:, i * 512:(i + 1) * 512], cand) for i in range(2)]
        k = 0
        for (src, dstt, nk, ncol) in ((g.w_att_branch, wA, 4, D), (g.w_dn_branch, wB, 4, D), (g.w_o, wo, 8, D),
                                      (g.peer_w_query, wq, 8, 2048)):
            sv = src.ap().rearrange("(kc p) n -> p kc n", p=128)
            for kc in range(nk):
                for c0 in range(0, ncol, 512):
                    s_ = wstg[k % 2]
                    S.dma("sync", s_[:], sv[:, kc, c0:c0 + 512], writes=[s_])
                    eng = ("vector", "gpsimd", "scalar")[k % 3]
                    dst = dstt[:, kc, c0:c0 + 512]
                    if eng == "scalar":
                        S.op(eng, lambda e: e.copy(out=dst, in_=s_[:]), reads=[s_], writes=[dstt])
                    else:
                        S.op(eng, lambda e: e.tensor_copy(out=dst, in_=s_[:]), reads=[s_], writes=[dstt])
                    k += 1
        g2_bc = A("g2_bc", [128, D])
        S.dma("sync", g2_bc[:], g.norm2_gain.ap().partition_broadcast(128), writes=[g2_bc])
        skT = A("skT", [128, 16, 128])
        for hp in range(16):
            s_ = wstg[hp % 2]
            S.dma("sync", s_[:, 0:128], g.peer_sub_keys.ap()[hp], writes=[s_])
            b = next_bank(g)
            S.op("tensor", lambda e: e.transpose(out=b[:, 0:128], in_=s_[:, 0:128], identity=g.ident_f[:]),
                 reads=[s_, g.ident_f], writes=[b])
            S.op("vector", lambda e: e.tensor_copy(out=skT[:, hp, :], in_=b[:, 0:128]), reads=[b], writes=[skT])
        iota_i = A("iota_i", [128, 16], I32)
        iota_f = A("iota_f", [128, 16])
        S.op("gpsimd", lambda e: e.iota(out=iota_i[:], pattern=[[1, 16]], base=0, channel_multiplier=0), writes=[iota_i])
        S.op("vector", lambda e: e.tensor_copy(out=iota_f[:], in_=iota_i[:]), reads=[iota_i], writes=[iota_f])

        ya = A("ya", [128, 512], BF16); yd = A("yd", [128, 512], BF16)
        gt = A("gt", [128, 2048], BF16)
        xt = A("xt", [128, D])
        yT = A("yT", [128, 8, 128], BF16)
        mg = A("mg", [128, D], BF16)
        mgT = A("mgT", [128, 8, 128], BF16)
        x1s = [A("x1_%d" % i, [128, D]) for i in range(2)]
        junk = A("junkd", [128, D], BF16)
        ss1 = A("ss1", [128, 1]); sd1 = A("sd1", [128, 1]); rs1 = A("rs1", [128, 1])
        h2s = [A("h2_%d" % i, [128, D], BF16) for i in range(2)]
        junk2 = A("junk2", [128, D], BF16)
        h2T = A("h2T", [128, 8, 128], BF16)
        qTp = A("qTp", [128, 16, 128])
        s_sb = A("s_sb", [128, 16, 128])
        s2 = A("s2", [128, 256])
        m16 = A("m16", [128, 16, 16])
        i16 = A("i16", [128, 16, 16], U32)
        best = A("best", [128, 8, 16])
        pos = A("pos", [128, 8, 16], U32)
        au = A("au", [128, 128], U32); bu = A("bu", [128, 128], U32)
        af = A("af", [128, 128]); bf = A("bf", [128, 128])
        i16f = A("i16f", [128, 16, 16])
        oh = A("oh", [128, 128, 16])
        ohf = oh[:].rearrange("p a b -> p (a b)")
        e0 = A("e0", [128, 128]); e1 = A("e1", [128, 128])
        eidxs = [A("eidx%d" % i, [128, 128], U32) for i in range(2)]
        gd = A("gd", [128, 8, 16]); gsum = A("gsum", [128, 8]); grec = A("grec", [128, 8])
        gates = [A("gate%d" % i, [128, 128]) for i in range(2)]
        act = A("act", [128, 128]); coef = A("coef", [128, 128])
        NBc = 10
        uv = [A("uv%d" % i, [128, 2 * D], BF16) for i in range(NBc)]
        prod = [A("prod%d" % i, [128, D], BF16) for i in range(2)]
        dgs = [A("dgs%d" % i, [128, 128], BF16) for i in range(4)]
        act1 = [A("act1_%d" % i, [128, 1]) for i in range(8)]
        ag1 = [A("ag1_%d" % i, [128, 1]) for i in range(8)]
        acc = A("acc", [128, D])
        g.nrot = 6
        pacc = g.banks[6:8]
        print("phase DE sbuf remaining", nc.sbuf_bytes_remaining)
        x_v = g.x.ap().rearrange("(n p) d -> n p d", p=128)
        o_v = g.out.ap().rearrange("(n p) d -> n p d", p=128)
        NTD = getattr(g, "ntd", NT)
        def stage_x(tt):
            rows = slice(tt * 128, (tt + 1) * 128)
            x1, h2, eidx, gate = x1s[tt % 2], h2s[tt % 2], eidxs[tt % 2], gates[tt % 2]
            S.dma("sync", ya[:], g.yatt_s[rows], reads=[g.yatt_s], writes=[ya])
            S.dma("sync", yd[:], g.ydn_s[rows], reads=[g.ydn_s], writes=[yd])
            S.dma("sync", gt[:], g.gate_s[rows], reads=[g.gate_s], writes=[gt])
            S.dma("sync", xt[:], x_v[tt], writes=[xt])
            b = next_bank(g)
            bv = b[:, :].bitcast(BF16)
            for j in range(4):
                S.op("tensor", lambda e: e.transpose(out=bv[:, j * 128:(j + 1) * 128], in_=ya[:, j * 128:(j + 1) * 128], identity=g.ident_b[:]),
                     reads=[ya, g.ident_b], writes=[b])
            for j in range(4):
                S.op("tensor", lambda e: e.transpose(out=bv[:, (4 + j) * 128:(5 + j) * 128], in_=yd[:, j * 128:(j + 1) * 128], identity=g.ident_b[:]),
                     reads=[yd, g.ident_b], writes=[b])
            S.op("scalar", lambda e: e.copy(out=yT[:].rearrange("p k t -> p (k t)"), in_=bv[:, 0:1024]), reads=[b], writes=[yT])
            for hf in range(2):
                cs = slice(hf * 512, (hf + 1) * 512)
                bA = next_bank(g)
                for kc in range(4):
                    S.op("tensor", lambda e: e.matmul(out=bA[:, 0:512], lhsT=yT[:, kc, :], rhs=wA[:, kc, cs], start=(kc == 0), stop=(kc == 3)),
                         reads=[yT, wA], writes=[bA])
                bB = next_bank(g)
                for kc in range(4):
                    S.op("tensor", lambda e: e.matmul(out=bB[:, 0:512], lhsT=yT[:, 4 + kc, :], rhs=wB[:, kc, cs], start=(kc == 0), stop=(kc == 3)),
                         reads=[yT, wB], writes=[bB])
                S.op("vector", lambda e: e.tensor_tensor(out=ohf[:, cs], in0=bA[:, 0:512], in1=gt[:, cs], op=ALU.mult),
                     reads=[bA, gt], writes=[oh])
                S.op("vector", lambda e: e.tensor_tensor(out=ohf[:, 1024 + hf * 512:1024 + (hf + 1) * 512], in0=bB[:, 0:512], in1=gt[:, 1024 + hf * 512:1024 + (hf + 1) * 512], op=ALU.mult),
                     reads=[bB, gt], writes=[oh])
            S.op("vector", lambda e: e.tensor_tensor(out=mg[:], in0=ohf[:, 0:1024], in1=ohf[:, 1024:2048], op=ALU.add), reads=[oh], writes=[mg])
            for half in range(2):
                b = next_bank(g)
                bv = b[:, :].bitcast(BF16)
                for j in range(4):
                    kc = half * 4 + j
                    S.op("tensor", lambda e: e.transpose(out=bv[:, j * 128:(j + 1) * 128], in_=mg[:, kc * 128:(kc + 1) * 128], identity=g.ident_b[:]),
                         reads=[mg, g.ident_b], writes=[b])
                S.op("scalar", lambda e: e.copy(out=mgT[:, half * 4:half * 4 + 4, :].rearrange("p k t -> p (k t)"), in_=bv[:, 0:512]),
                     reads=[b], writes=[mgT])
            for hf in range(2):
                cs = slice(hf * 512, (hf + 1) * 512)
                b = next_bank(g)
                for kc in range(8):
                    S.op("tensor", lambda e: e.matmul(out=b[:, 0:512], lhsT=mgT[:, kc, :], rhs=wo[:, kc, cs], start=(kc == 0), stop=(kc == 7)),
                         reads=[mgT, wo], writes=[b])
                S.op("vector", lambda e: e.tensor_tensor(out=x1[:, cs], in0=b[:, 0:512], in1=xt[:, cs], op=ALU.add),
                     reads=[b, xt], writes=[x1])
            S.op("scalar", lambda e: e.activation(out=junk[:], in_=x1[:], func=AF.Square, accum_out=ss1[:, 0:1]),
                 reads=[x1], writes=[junk, ss1])
            rstd_op(g, rs1, ss1, 1.0 / D, 1, sd1)
            S.op("vector", lambda e: e.scalar_tensor_tensor(out=h2[:], in0=x1[:], scalar=rs1[:, 0:1], in1=g2_bc[:], op0=ALU.mult, op1=ALU.mult),
                 reads=[x1, rs1, g2_bc], writes=[h2])
            for half in range(2):
                b = next_bank(g)
                bv = b[:, :].bitcast(BF16)
                for j in range(4):
                    kc = half * 4 + j
                    S.op("tensor", lambda e: e.transpose(out=bv[:, j * 128:(j + 1) * 128], in_=h2[:, kc * 128:(kc + 1) * 128], identity=g.ident_b[:]),
                         reads=[h2, g.ident_b], writes=[b])
                S.op("scalar", lambda e: e.copy(out=h2T[:, half * 4:half * 4 + 4, :].rearrange("p k t -> p (k t)"), in_=bv[:, 0:512]),
                     reads=[b], writes=[h2T])
            for q4 in range(4):
                b = next_bank(g)
                for j in range(4):
                    hp = q4 * 4 + j
                    for kc in range(8):
                        S.op("tensor", lambda e: e.matmul(out=b[:, j * 128:(j + 1) * 128], lhsT=wq[:, kc, hp * 128:(hp + 1) * 128],
                                                          rhs=h2T[:, kc, :], start=(j == 0 and kc == 0), stop=(kc == 7), skip_group_check=True),
                             reads=[wq, h2T], writes=[b])
                dst = qTp[:, q4 * 4:q4 * 4 + 4, :].rearrange("p a t -> p (a t)")
                if q4 % 2 == 0:
                    S.op("scalar", lambda e: e.copy(out=dst, in_=b[:, 0:512]), reads=[b], writes=[qTp])
                else:
                    S.op("vector", lambda e: e.tensor_copy(out=dst, in_=b[:, 0:512]), reads=[b], writes=[qTp])
            for q4 in range(4):
                b = next_bank(g)
                for j in range(4):
                    hp = q4 * 4 + j
                    S.op("tensor", lambda e: e.matmul(out=b[:, j * 128:(j + 1) * 128], lhsT=qTp[:, hp, :], rhs=skT[:, hp, :],
                                                      start=(j == 0), stop=True, skip_group_check=True),
                         reads=[qTp, skT], writes=[b])
                S.op("scalar", lambda e: e.copy(out=s_sb[:, q4 * 4:q4 * 4 + 4, :].rearrange("p a t -> p (a t)"), in_=b[:, 0:512]),
                     reads=[b], writes=[s_sb])
            for hp in range(16):
                sv = s_sb[:, hp, :]
                S.op("vector", lambda e: e.max(out=m16[:, hp, 0:8], in_=sv), reads=[s_sb], writes=[m16])
                S.op("vector", lambda e: e.max_index(out=i16[:, hp, 0:8], in_max=m16[:, hp, 0:8], in_values=sv), reads=[s_sb, m16], writes=[i16])
                S.op("vector", lambda e: e.match_replace(out=s2[:, 0:128], in_to_replace=m16[:, hp, 0:8], in_values=sv, imm_value=-1e30),
                     reads=[s_sb, m16], writes=[s2])
                S.op("vector", lambda e: e.max(out=m16[:, hp, 8:16], in_=s2[:, 0:128]), reads=[s2], writes=[m16])
                S.op("vector", lambda e: e.max_index(out=i16[:, hp, 8:16], in_max=m16[:, hp, 8:16], in_values=s2[:, 0:128]),
                     reads=[s2, m16], writes=[i16])
            m16v = m16[:].rearrange("p (h two) a -> p h two a", two=2)
            S.op("vector", lambda e: e.tensor_tensor(out=cand[:].rearrange("p h (a b) -> p h a b", b=16),
                                                     in0=m16v[:, :, 0, :].unsqueeze(3).to_broadcast([128, 8, 16, 16]),
                                                     in1=m16v[:, :, 1, :].unsqueeze(2).to_broadcast([128, 8, 16, 16]), op=ALU.add),
                 reads=[m16], writes=[cand])
            for h in range(8):
                cv = cand[:, h, :]
                S.op("vector", lambda e: e.max(out=best[:, h, 0:8], in_=cv), reads=[cand], writes=[best])
                S.op("vector", lambda e: e.max_index(out=pos[:, h, 0:8], in_max=best[:, h, 0:8], in_values=cv), reads=[cand, best], writes=[pos])
                S.op("vector", lambda e: e.match_replace(out=s2[:], in_to_replace=best[:, h, 0:8], in_values=cv, imm_value=-1e30),
                     reads=[cand, best], writes=[s2])
                S.op("vector", lambda e: e.max(out=best[:, h, 8:16], in_=s2[:]), reads=[s2], writes=[best])
                S.op("vector", lambda e: e.max_index(out=pos[:, h, 8:16], in_max=best[:, h, 8:16], in_values=s2[:]), reads=[s2, best], writes=[pos])
            posf = pos[:].rearrange("p h k -> p (h k)")
            S.op("vector", lambda e: e.tensor_scalar(out=au[:], in0=posf, scalar1=4, scalar2=None, op0=ALU.logical_shift_right), reads=[pos], writes=[au])
            S.op("vector", lambda e: e.tensor_scalar(out=bu[:], in0=posf, scalar1=15, scalar2=None, op0=ALU.bitwise_and), reads=[pos], writes=[bu])
            S.op("vector", lambda e: e.tensor_copy(out=af[:], in_=au[:]), reads=[au], writes=[af])
            S.op("vector", lambda e: e.tensor_copy(out=bf[:], in_=bu[:]), reads=[bu], writes=[bf])
            S.op("vector", lambda e: e.tensor_copy(out=i16f[:], in_=i16[:]), reads=[i16], writes=[i16f])
            i16fv = i16f[:].rearrange("p (h two) a -> p h two a", two=2)
            for which, (sel, dst) in enumerate(((af, e0), (bf, e1))):
                S.op("vector", lambda e: e.tensor_tensor(out=oh[:], in0=sel[:, :].unsqueeze(2).to_broadcast([128, 128, 16]),
                                                         in1=iota_f[:, :].unsqueeze(1).to_broadcast([128, 128, 16]), op=ALU.is_equal),
                     reads=[sel, iota_f], writes=[oh])
                S.op("vector", lambda e: e.tensor_tensor(out=oh[:].rearrange("p (h k) a -> p h k a", k=16),
                                                         in0=oh[:].rearrange("p (h k) a -> p h k a", k=16),
                                                         in1=i16fv[:, :, which, :].unsqueeze(2).to_broadcast([128, 8, 16, 16]), op=ALU.mult),
                     reads=[oh, i16f], writes=[oh])
                S.op("vector", lambda e: e.tensor_reduce(out=dst[:], in_=oh[:], axis=AX.X, op=ALU.add), reads=[oh], writes=[dst])
            S.op("vector", lambda e: e.scalar_tensor_tensor(out=e0[:], in0=e0[:], scalar=128.0, in1=e1[:], op0=ALU.mult, op1=ALU.add),
                 reads=[e0, e1], writes=[e0])
            S.op("vector", lambda e: e.tensor_copy(out=eidx[:], in_=e0[:]), reads=[e0], writes=[eidx])
            S.op("vector", lambda e: e.tensor_tensor(out=gd[:], in0=best[:], in1=best[:, :, 0:1].to_broadcast([128, 8, 16]), op=ALU.subtract),
                 reads=[best], writes=[gd])
            S.op("scalar", lambda e: e.activation(out=gd[:], in_=gd[:], func=AF.Exp), reads=[gd], writes=[gd])
            S.op("vector", lambda e: e.tensor_reduce(out=gsum[:], in_=gd[:], axis=AX.X, op=ALU.add), reads=[gd], writes=[gsum])
            S.op("vector", lambda e: e.reciprocal(out=grec[:], in_=gsum[:]), reads=[gsum], writes=[grec])
            S.op("vector", lambda e: e.tensor_tensor(out=gate[:].rearrange("p (h k) -> p h k", k=16), in0=gd[:],
                                                     in1=grec[:, :].unsqueeze(2).to_broadcast([128, 8, 16]), op=ALU.mult),
                 reads=[gd, grec], writes=[gate])

        def stage_y(tt):
            x1, h2, eidx, gate = x1s[tt % 2], h2s[tt % 2], eidxs[tt % 2], gates[tt % 2]
            NSL = getattr(g, "ngrp", 16) * 8
            LAG = 3

            def front(j):
                ub = uv[j % NBc]
                S.dma("gpsimd", ub[:], g.uv_s[:, :], reads=[eidx, g.uv_s], writes=[ub],
                      indirect=bass.IndirectOffsetOnAxis(ap=eidx[:, j:j + 1], axis=0))
                pr = prod[j % 2]
                a1 = act1[j % 8]
                a2 = ag1[j % 8]
                S.op("vector", lambda e: e.tensor_tensor(out=pr[:], in0=ub[:, 0:D], in1=h2[:], op=ALU.mult), reads=[ub, h2], writes=[pr])
                S.op("scalar", lambda e: e.activation(out=junk2[:], in_=pr[:], func=AF.Identity, accum_out=a1[:, 0:1]),
                     reads=[pr], writes=[junk2, a1])
                S.op("scalar", lambda e: e.activation(out=a2[:], in_=a1[:], func=AF.Gelu), reads=[a1], writes=[a2])

            def back(j):
                ub = uv[j % NBc]
                a2 = ag1[j % 8]
                dg = dgs[j % 4]
                S.op("vector", lambda e: e.scalar_tensor_tensor(out=dg[:], in0=g.ident_b[:], scalar=a2[:, 0:1],
                                                                in1=gate[:, j:j + 1].to_broadcast([128, 128]), op0=ALU.mult, op1=ALU.mult),
                     reads=[g.ident_b, a2, gate], writes=[dg])
                for hf in range(2):
                    S.op("tensor", lambda e: e.matmul(out=pacc[hf][:, 0:512], lhsT=dg[:], rhs=ub[:, D + hf * 512:D + (hf + 1) * 512],
                                                      start=(j == 0), stop=(j == NSL - 1)),
                         reads=[dg, ub], writes=[pacc[hf]])

            for j in range(NSL + LAG):
                if j < NSL:
                    front(j)
                if j >= LAG:
                    back(j - LAG)
            for hf in range(2):
                cs = slice(hf * 512, (hf + 1) * 512)
                S.op("vector", lambda e: e.tensor_tensor(out=acc[:, cs], in0=pacc[hf][:, 0:512], in1=x1[:, cs], op=ALU.add),
                     reads=[pacc[hf], x1], writes=[acc])
            S.dma("sync", o_v[tt], acc[:], reads=[acc])

        S.record()
        stage_x(0)
        S.replay(S.stop())
        for tt in range(NTD):
            S.record()
            stage_y(tt)
            ry = S.stop()
            rx = None
            if tt + 1 < NTD:
                S.record()
                stage_x(tt + 1)
                rx = S.stop()
            S.replay(ry, rx)
        S.barrier()


_CACHE = {}


def kernel(**inputs):
    x = np.asarray(inputs["x"], dtype=np.float32)
    if "nc" not in _CACHE:
        _CACHE["nc"] = build_program()
    nc = _CACHE["nc"]
    shared = {}
    for k in ("norm1_gain", "w_in", "q_norm_gain", "k_norm_gain", "dn_conv_w", "dn_a_log", "dn_dt_bias",
              "dn_out_norm_gain", "w_att_branch", "w_dn_branch", "w_o", "norm2_gain", "peer_w_query",
              "peer_u", "peer_v"):
        a = np.asarray(inputs[k], dtype=np.float32)[0]
        if a.ndim == 1:
            a = a.reshape(1, -1)
        shared[k] = np.ascontiguousarray(a)
    shared["peer_sub_keys"] = np.ascontiguousarray(
        np.asarray(inputs["peer_sub_keys"], dtype=np.float32)[0].reshape(16, 128, 128))
    in_maps = []
    for c in range(N_CORES):
        m = dict(shared)
        m["x"] = np.ascontiguousarray(x[c])
        in_maps.append(m)
    res = run_bass_kernel_spmd(nc, in_maps, core_ids=list(range(N_CORES)))
    out = np.stack([np.asarray(r["out"]) for r in res.results], axis=0)
    return out.astype(np.float32)
```

```python
import contextlib
import numpy as np
import concourse.bass as bass
import concourse.mybir as mybir
from concourse.bass_utils import run_bass_kernel_spmd

F32 = mybir.dt.float32
BF16 = mybir.dt.bfloat16
I32 = mybir.dt.int32
U32 = mybir.dt.uint32
AF = mybir.ActivationFunctionType
ALU = mybir.AluOpType
AX = mybir.AxisListType

T = 4096
D = 1024
NT = T // 128
IN_W = 5456
EPS = 1e-6
N_CORES = 8

C_AQ, C_AK, C_AV, C_IQ, C_IK, C_IW = 0, 512, 640, 768, 1280, 1344
C_DQ, C_DK, C_DV, C_DZ, C_DA, C_DB, C_GA, C_GB = 1352, 1864, 2376, 2888, 3400, 3404, 3408, 4432


class Res:
    __slots__ = ("name", "writer", "readers")

    def __init__(self, name):
        self.name = name
        self.writer = None
        self.readers = {}


class Tl:
    def __init__(self, t, name):
        self.t = t.ap() if hasattr(t, "ap") and "DRam" in type(t).__name__ else t
        self.r = Res(name)

    def __getitem__(self, idx):
        return self.t[idx]


class _RecEng:
    def __getattr__(self, name):
        def f(*args, **kw):
            self.__dict__["name"] = name
            self.__dict__["args"] = args
            self.__dict__["kw"] = kw
            return None
        return f


class Sched:
    CE = ("tensor", "vector", "scalar", "gpsimd")

    def __init__(self, nc, st, n_dma=12):
        self.nc = nc
        self.st = st
        self.eng = {n: getattr(nc, n) for n in self.CE + ("sync",)}
        self.sem = {}
        self.cnt = {}
        for n in self.CE:
            self.sem[n] = st.enter_context(nc.semaphore("s_" + n))
            self.cnt[n] = 0
        self.dq = {}
        for q in ("sync", "gpsimd"):
            sems = [st.enter_context(nc.semaphore("d_%s_%d" % (q, i))) for i in range(n_dma)]
            for i, s in enumerate(sems):
                self.sem[(q, i)] = s
                self.cnt[(q, i)] = 0
            self.dq[q] = [0, n_dma]
        self.seen = {}
        self.ninst = 0
        self.rec = None

    def record(self):
        self.rec = []

    def stop(self):
        r = self.rec
        self.rec = None
        return r

    def replay(self, *lists):
        lists = [l for l in lists if l]
        items = []
        for li, l in enumerate(lists):
            n = len(l)
            for i, it in enumerate(l):
                items.append(((i + 0.5) / n, li, i, it))
        items.sort(key=lambda t: (t[0], t[1], t[2]))
        for _, _, _, it in items:
            if it[0] == "op":
                _, E, name, args, kw, reads, writes = it
                self.op(E, lambda e: getattr(e, name)(*args, **kw), reads, writes)
            else:
                _, q, out, in_, reads, writes, indirect, kw = it
                self.dma(q, out, in_, reads, writes, indirect, **kw)

    def _wait(self, E, key, val):
        if val <= 0:
            return
        if E == "tensor" and key == "tensor":
            return
        k = (E, key)
        if self.seen.get(k, 0) >= val:
            return
        self.eng[E].wait_ge(self.sem[key], val)
        self.seen[k] = val
        self.ninst += 1

    def _deps(self, E, reads, writes):
        for r in reads:
            r = getattr(r, "r", r)
            if r.writer is not None:
                self._wait(E, *r.writer)
        for w in writes:
            w = getattr(w, "r", w)
            if w.writer is not None:
                self._wait(E, *w.writer)
            for key, val in w.readers.items():
                self._wait(E, key, val)

    def _mark(self, ev, reads, writes):
        key, val = ev
        for r in reads:
            r = getattr(r, "r", r)
            r.readers[key] = val
        for w in writes:
            w = getattr(w, "r", w)
            w.writer = ev
            w.readers = {}

    def op(self, E, emit, reads=(), writes=()):
        if self.rec is not None:
            r = _RecEng()
            emit(r)
            self.rec.append(("op", E, r.name, r.args, r.kw, list(reads), list(writes)))
            return
        self._deps(E, reads, writes)
        inst = emit(self.eng[E])
        self.cnt[E] += 1
        inst.then_inc(self.sem[E], 1)
        self.ninst += 1
        self._mark((E, self.cnt[E]), reads, writes)

    def dma(self, q, out, in_, reads=(), writes=(), indirect=None, **kw):
        if self.rec is not None:
            self.rec.append(("dma", q, out, in_, list(reads), list(writes), indirect, kw))
            return
        st = self.dq[q]
        i = st[0]
        st[0] = (i + 1) % st[1]
        key = (q, i)
        self._wait(q, key, self.cnt[key])
        self._deps(q, reads, writes)
        if indirect is not None:
            inst = self.eng[q].indirect_dma_start(out=out, out_offset=None, in_=in_, in_offset=indirect, **kw)
        else:
            inst = self.eng[q].dma_start(out=out, in_=in_, **kw)
        self.cnt[key] += 16
        inst.then_inc(self.sem[key], 16)
        self.ninst += 1
        self._mark((key, self.cnt[key]), reads, writes)

    def barrier(self):
        for E in self.CE + ("sync",):
            for key, val in self.cnt.items():
                if key == E:
                    continue
                if E == "tensor" and key == "tensor":
                    continue
                self._wait(E, key, val)

    def finish(self):
        for E in self.CE + ("sync",):
            for key, val in self.cnt.items():
                if key == E:
                    continue
                k = (E, key)
                if val > 0 and self.seen.get(k, 0) < val:
                    self.eng[E].wait_ge(self.sem[key], val)
                    self.seen[k] = val


class Ctx:
    pass


def build_program(debug=False, phases=("A", "B", "C", "P", "D"), **opts):
    nc = bass.Bass("TRN2", target_bir_lowering=False)
    g = Ctx()
    for k_, v_ in opts.items():
        setattr(g, k_, v_)
    g.nc = nc
    g.debug = debug

    def din(name, shape, dt=F32):
        return nc.dram_tensor(name, list(shape), dt, kind="ExternalInput")

    g.x = din("x", [T, D])
    g.norm1_gain = din("norm1_gain", [1, D])
    g.w_in = din("w_in", [D, IN_W])
    g.q_norm_gain = din("q_norm_gain", [1, 64])
    g.k_norm_gain = din("k_norm_gain", [1, 64])
    g.dn_conv_w = din("dn_conv_w", [4, 1536])
    g.dn_a_log = din("dn_a_log", [1, 4])
    g.dn_dt_bias = din("dn_dt_bias", [1, 4])
    g.dn_out_norm_gain = din("dn_out_norm_gain", [1, 128])
    g.w_att_branch = din("w_att_branch", [512, D])
    g.w_dn_branch = din("w_dn_branch", [512, D])
    g.w_o = din("w_o", [D, D])
    g.norm2_gain = din("norm2_gain", [1, D])
    g.peer_w_query = din("peer_w_query", [D, 2048])
    g.peer_sub_keys = din("peer_sub_keys", [16, 128, 128])
    g.peer_u = din("peer_u", [16384, D])
    g.peer_v = din("peer_v", [16384, D])
    g.out = nc.dram_tensor("out", [T, D], F32, kind="ExternalOutput")

    skind = "ExternalOutput" if debug else "Internal"

    def dscr(name, shape, dt):
        return Tl(nc.dram_tensor(name, list(shape), dt, kind=skind), name)

    g.qT_s = dscr("qT_s", [NT, 64, 8, 128], BF16)
    g.iqT_s = dscr("iqT_s", [NT, 64, 8, 128], BF16)
    g.kT_s = dscr("kT_s", [64, 2, T], BF16)
    g.ikT_s = dscr("ikT_s", [64, T], BF16)
    g.v_s = dscr("v_s", [T, 128], BF16)
    g.iw_s = dscr("iw_s", [128, NT, 8], F32)
    g.gb_s = dscr("gb_s", [128, NT, 8], F32)
    g.dnq_s = dscr("dnq_s", [T, 512], F32)
    g.dnk_s = dscr("dnk_s", [T, 512], F32)
    g.dnv_s = dscr("dnv_s", [T, 512], F32)
    g.dz_s = dscr("dz_s", [T, 512], BF16)
    g.gate_s = dscr("gate_s", [T, 2048], BF16)
    g.yatt_s = dscr("yatt_s", [T, 512], BF16)
    g.ydn_s = dscr("ydn_s", [T, 512], BF16)
    g.uv_s = Tl(nc.dram_tensor("uv_s", [16384, 2 * D], BF16, kind="Internal"), "uv_s")

    with contextlib.ExitStack() as st:
        S = Sched(nc, st)
        g.S = S
        g.st = st
        g.banks = [Tl(st.enter_context(nc.psum_tensor("bank%d" % i, [128, 512], F32)), "bank%d" % i)
                   for i in range(8)]
        g.bank_rr = 0
        g.pool_rr = {}
        setup_consts(g)
        if "A" in phases:
            phase_a(g)
        g.p_in_b = ("P" in phases and "B" in phases and not getattr(g, "p_sep", 0))
        if "B" in phases:
            phase_b(g)
        if "C" in phases:
            phase_c(g)
        if "P" in phases and not g.p_in_b:
            phase_p(g)
        if "D" in phases:
            phase_de(g)
        S.finish()
    return nc


def next_bank(g, pool=None):
    if pool is not None:
        rr = g.pool_rr.get(pool, 0)
        g.pool_rr[pool] = rr + 1
        return g.banks[pool[rr % len(pool)]]
    n = getattr(g, "nrot", 8)
    g.bank_rr = g.bank_rr % n
    b = g.banks[g.bank_rr]
    g.bank_rr = (g.bank_rr + 1) % n
    return b


def sb(g, st, name, shape, dt):
    g.uid = getattr(g, "uid", 0) + 1
    name = "%s_%d" % (name, g.uid)
    return Tl(st.enter_context(g.nc.sbuf_tensor(name, list(shape), dt)), name)


def setup_consts(g):
    nc, S, st = g.nc, g.S, g.st
    g.fill0 = nc.gpsimd.to_reg(0.0)
    g.fillneg = nc.gpsimd.to_reg(-1e30)
    g.ones_f = sb(g, st, "ones_f", [128, 128], F32)
    g.ident_f = sb(g, st, "ident_f", [128, 128], F32)
    g.ident_b = sb(g, st, "ident_b", [128, 128], BF16)
    g.eps_t = sb(g, st, "eps_t", [128, 1], F32)
    S.op("gpsimd", lambda e: e.memset(g.ones_f[:], 1.0), writes=[g.ones_f])
    S.op("gpsimd", lambda e: e.memset(g.eps_t[:], EPS), writes=[g.eps_t])
    S.op("gpsimd", lambda e: e.affine_select(out=g.ident_f[:], in_=g.ones_f[:], pattern=[[-1, 128]],
                                             compare_op=ALU.is_equal, fill=g.fill0, base=0, channel_multiplier=1),
         reads=[g.ones_f], writes=[g.ident_f])
    S.op("vector", lambda e: e.tensor_copy(out=g.ident_b[:], in_=g.ident_f[:]), reads=[g.ident_f], writes=[g.ident_b])


def bcast_row(ap_row, n):
    return ap_row.partition_broadcast(n)


def rstd_op(g, out, ss, scale, n_free, tmp):
    S = g.S
    S.op("scalar", lambda e: e.activation(out=tmp[:, 0:n_free], in_=ss[:, 0:n_free], func=AF.Sqrt,
                                          bias=g.eps_t[:, 0:1], scale=scale),
         reads=[ss, g.eps_t], writes=[tmp])
    S.op("vector", lambda e: e.reciprocal(out=out[:, 0:n_free], in_=tmp[:, 0:n_free]), reads=[tmp], writes=[out])


def phase_a(g):
    nc, S = g.nc, g.S
    with contextlib.ExitStack() as ph:
        def A(name, shape, dt):
            return sb(g, ph, name, shape, dt)

        w_bf = A("w_bf", [128, 8, IN_W], BF16)
        WCH = 1364
        wst = [A("wst%d" % i, [128, WCH], F32) for i in range(2)]
        w_in_v = g.w_in.ap().rearrange("(kc p) n -> p kc n", p=128)
        k = 0
        for kc in range(8):
            for c in range(IN_W // WCH):
                s_ = wst[k % 2]
                S.dma("sync", s_[:], w_in_v[:, kc, c * WCH:(c + 1) * WCH], writes=[s_])
                eng = ("vector", "gpsimd", "scalar")[k % 3]
                dst = w_bf[:, kc, c * WCH:(c + 1) * WCH]
                if eng == "scalar":
                    S.op(eng, lambda e: e.copy(out=dst, in_=s_[:]), reads=[s_], writes=[w_bf])
                else:
                    S.op(eng, lambda e: e.tensor_copy(out=dst, in_=s_[:]), reads=[s_], writes=[w_bf])
                k += 1

        g1_bc = A("g1_bc", [128, D], F32)
        S.dma("sync", g1_bc[:], g.norm1_gain.ap().partition_broadcast(128), writes=[g1_bc])
        gq_bc = A("gq_bc", [128, 64], F32)
        gk_bc = A("gk_bc", [128, 64], F32)
        S.dma("sync", gq_bc[:], g.q_norm_gain.ap().partition_broadcast(128), writes=[gq_bc])
        S.dma("sync", gk_bc[:], g.k_norm_gain.ap().partition_broadcast(128), writes=[gk_bc])
        S.op("vector", lambda e: e.tensor_scalar(out=gq_bc[:], in0=gq_bc[:], scalar1=0.125, scalar2=None, op0=ALU.mult),
             reads=[gq_bc], writes=[gq_bc])
        cw_bc = A("cw_bc", [128, 4, 1536], F32)
        for j in range(4):
            S.dma("sync", cw_bc[:, j, :], g.dn_conv_w.ap()[j:j + 1, :].partition_broadcast(128), writes=[cw_bc])
        dtb_bc = A("dtb_bc", [128, 4], F32)
        nea_bc = A("nea_bc", [128, 4], F32)
        S.dma("sync", dtb_bc[:], g.dn_dt_bias.ap().partition_broadcast(128), writes=[dtb_bc])
        S.dma("sync", nea_bc[:], g.dn_a_log.ap().partition_broadcast(128), writes=[nea_bc])
        S.op("scalar", lambda e: e.activation(out=nea_bc[:], in_=nea_bc[:], func=AF.Exp), reads=[nea_bc], writes=[nea_bc])
        S.op("vector", lambda e: e.tensor_scalar(out=nea_bc[:], in0=nea_bc[:], scalar1=-1.0, scalar2=None, op0=ALU.mult),
             reads=[nea_bc], writes=[nea_bc])

        shf = A("shf", [128, 128], F32)
        Sh = [g.ident_b] + [A("sh%d" % d, [128, 128], BF16) for d in (1, 2, 3)]
        ShP = [None] + [A("shp%d" % d, [128, 128], BF16) for d in (1, 2, 3)]
        for d in (1, 2, 3):
            S.op("gpsimd", lambda e: e.affine_select(out=shf[:], in_=g.ones_f[:], pattern=[[-1, 128]],
                                                     compare_op=ALU.is_equal, fill=g.fill0, base=d, channel_multiplier=1),
                 reads=[g.ones_f], writes=[shf])
            S.op("vector", lambda e: e.tensor_copy(out=Sh[d][:], in_=shf[:]), reads=[shf], writes=[Sh[d]])
            S.op("gpsimd", lambda e: e.affine_select(out=shf[:], in_=g.ones_f[:], pattern=[[-1, 128]],
                                                     compare_op=ALU.is_equal, fill=g.fill0, base=d - 128, channel_multiplier=1),
                 reads=[g.ones_f], writes=[shf])
            S.op("vector", lambda e: e.tensor_copy(out=ShP[d][:], in_=shf[:]), reads=[shf], writes=[ShP[d]])

        xt = [A("xt%d" % i, [128, D], F32) for i in range(2)]
        junk = A("junk", [128, D], BF16)
        h_bf = A("h_bf", [128, D], BF16)
        hT = [A("hT%d" % i, [128, 8, 128], BF16) for i in range(2)]
        ss1 = A("ss1", [128, 1], F32)
        sd1 = A("sd1", [128, 1], F32)
        rs1 = A("rs1", [128, 1], F32)
        sq = A("sq", [128, 512], F32)
        ss8 = A("ss8", [128, 8], F32)
        sd8 = A("sd8", [128, 8], F32)
        rs8 = A("rs8", [128, 8], F32)
        tmpf = A("tmpf", [128, 512], F32)
        qn_bf = A("qn_bf", [128, 512], BF16)
        kn_bf = A("kn_bf", [128, 128], BF16)
        iq_bf = A("iq_bf", [128, 512], BF16)
        ik_bf = A("ik_bf", [128, 64], BF16)
        qT_t = [A("qT_t%d" % i, [64, 8, 128], BF16) for i in range(2)]
        iqT_t = [A("iqT_t%d" % i, [64, 8, 128], BF16) for i in range(2)]
        kT_t = [A("kT_t%d" % i, [64, 2, 128], BF16) for i in range(2)]
        ikT_t = [A("ikT_t%d" % i, [64, 128], BF16) for i in range(2)]
        v_t = [A("v_t%d" % i, [128, 128], BF16) for i in range(2)]
        iw_all = A("iw_all", [128, NT, 8], F32)
        gb_all = A("gb_all", [128, NT, 8], F32)
        ab_tmp = A("ab_tmp", [128, 4], F32)
        xw = [A("xw%d" % i, [128, 4, 1536], BF16) for i in range(2)]
        yc = [A("yc%d" % i, [128, 512], F32) for i in range(3)]
        dz_t = [A("dz_t%d" % i, [128, 512], BF16) for i in range(2)]
        gt_t = [A("gt_t%d" % i, [128, 1024], BF16) for i in range(2)]
        print("phase A sbuf remaining", nc.sbuf_bytes_remaining)

        def proj(cols, lo, hi, hTt):
            b = next_bank(g)
            n = hi - lo
            for kc in range(8):
                S.op("tensor", lambda e: e.matmul(out=b[:, 0:n], lhsT=hTt[:, kc, :], rhs=w_bf[:, kc, lo:hi],
                                                  start=(kc == 0), stop=(kc == 7)),
                     reads=[hTt, w_bf], writes=[b])
            return b

        def headnorm(ps, nh, gain_bc, out_bf):
            n = nh * 64
            S.op("scalar", lambda e: e.activation(out=sq[:, 0:n], in_=ps[:, 0:n], func=AF.Square), reads=[ps], writes=[sq])
            S.op("vector", lambda e: e.tensor_reduce(out=ss8[:, 0:nh], in_=sq[:, 0:n].rearrange("p (h d) -> p h d", d=64),
                                                     axis=AX.X, op=ALU.add), reads=[sq], writes=[ss8])
            rstd_op(g, rs8, ss8, 1.0 / 64, nh, sd8)
            S.op("vector", lambda e: e.tensor_tensor(out=tmpf[:, 0:n].rearrange("p (h d) -> p h d", d=64),
                                                     in0=ps[:, 0:n].rearrange("p (h d) -> p h d", d=64),
                                                     in1=rs8[:, 0:nh].unsqueeze(2).to_broadcast([128, nh, 64]), op=ALU.mult),
                 reads=[ps, rs8], writes=[tmpf])
            S.op("vector", lambda e: e.tensor_tensor(out=out_bf[:, 0:n].rearrange("p (h d) -> p h d", d=64),
                                                     in0=tmpf[:, 0:n].rearrange("p (h d) -> p h d", d=64),
                                                     in1=gain_bc[:, :].unsqueeze(1).to_broadcast([128, nh, 64]), op=ALU.mult),
                 reads=[tmpf, gain_bc], writes=[out_bf])

        def transpose_heads(src_bf, nh, dstT, eng):
            b = next_bank(g)
            bv = b[:, :].bitcast(BF16)
            for h in range(nh):
                S.op("tensor", lambda e: e.transpose(out=bv[0:64, h * 128:(h + 1) * 128], in_=src_bf[:, h * 64:(h + 1) * 64],
                                                     identity=g.ident_b[:]),
                     reads=[src_bf, g.ident_b], writes=[b])
            src = bv[0:64, 0:nh * 128]
            dst = dstT[:, :, :].rearrange("p h t -> p (h t)") if nh > 1 else dstT[:, :]
            if eng == "scalar":
                S.op("scalar", lambda e: e.copy(out=dst, in_=src), reads=[b], writes=[dstT])
            else:
                S.op("vector", lambda e: e.tensor_copy(out=dst, in_=src), reads=[b], writes=[dstT])

        x_v = g.x.ap().rearrange("(n p) d -> n p d", p=128)
        S.dma("sync", xt[0][:], x_v[0], writes=[xt[0]])
        NTR = getattr(g, "ntr", NT)
        SECT = getattr(g, "sect", 99)
        for tt in range(NTR):
            cur = tt % 2
            if tt + 1 < NTR:
                S.dma("sync", xt[1 - cur][:], x_v[tt + 1], writes=[xt[1 - cur]])
            x_t = xt[cur]
            hTt = hT[cur]
            rows = slice(tt * 128, (tt + 1) * 128)
            S.op("scalar", lambda e: e.activation(out=junk[:], in_=x_t[:], func=AF.Square, accum_out=ss1[:, 0:1]),
                 reads=[x_t], writes=[junk, ss1])
            rstd_op(g, rs1, ss1, 1.0 / D, 1, sd1)
            S.op("vector", lambda e: e.scalar_tensor_tensor(out=h_bf[:], in0=x_t[:], scalar=rs1[:, 0:1], in1=g1_bc[:],
                                                            op0=ALU.mult, op1=ALU.mult),
                 reads=[x_t, rs1, g1_bc], writes=[h_bf])
            for half in range(2):
                b = next_bank(g)
                bv = b[:, :].bitcast(BF16)
                for j in range(4):
                    kc = half * 4 + j
                    S.op("tensor", lambda e: e.transpose(out=bv[:, j * 128:(j + 1) * 128], in_=h_bf[:, kc * 128:(kc + 1) * 128],
                                                         identity=g.ident_b[:]),
                         reads=[h_bf, g.ident_b], writes=[b])
                dst = hTt[:, half * 4:half * 4 + 4, :].rearrange("p k t -> p (k t)")
                if half == 0:
                    S.op("scalar", lambda e: e.copy(out=dst, in_=bv[:, 0:512]), reads=[b], writes=[hTt])
                else:
                    S.op("vector", lambda e: e.tensor_copy(out=dst, in_=bv[:, 0:512]), reads=[b], writes=[hTt])

            if SECT < 1:
                continue
            ps = proj("aq", C_AQ, C_AQ + 512, hTt)
            headnorm(ps, 8, gq_bc, qn_bf)
            transpose_heads(qn_bf, 8, qT_t[cur], "scalar")
            S.dma("gpsimd", g.qT_s[tt], qT_t[cur][:], reads=[qT_t[cur]], writes=[g.qT_s])
            if SECT < 2:
                continue
            ps = proj("kv", C_AK, C_AK + 256, hTt)
            headnorm(ps, 2, gk_bc, kn_bf)
            S.op("scalar", lambda e: e.copy(out=v_t[cur][:], in_=ps[:, 128:256]), reads=[ps], writes=[v_t[cur]])
            transpose_heads(kn_bf, 2, kT_t[cur], "vector")
            S.dma("gpsimd", g.kT_s[:, :, rows], kT_t[cur][:], reads=[kT_t[cur]], writes=[g.kT_s])
            S.dma("gpsimd", g.v_s[rows], v_t[cur][:], reads=[v_t[cur]], writes=[g.v_s])
            if SECT < 3:
                continue
            ps = proj("iq", C_IQ, C_IQ + 512, hTt)
            S.op("scalar", lambda e: e.copy(out=iq_bf[:], in_=ps[:, 0:512]), reads=[ps], writes=[iq_bf])
            transpose_heads(iq_bf, 8, iqT_t[cur], "vector")
            S.dma("gpsimd", g.iqT_s[tt], iqT_t[cur][:], reads=[iqT_t[cur]], writes=[g.iqT_s])
            if SECT < 4:
                continue
            ps = proj("ikw", C_IK, C_IK + 72, hTt)
            S.op("vector", lambda e: e.tensor_copy(out=ik_bf[:], in_=ps[:, 0:64]), reads=[ps], writes=[ik_bf])
            S.op("scalar", lambda e: e.copy(out=iw_all[:, tt, :], in_=ps[:, 64:72]), reads=[ps], writes=[iw_all])
            transpose_heads(ik_bf, 1, ikT_t[cur], "scalar")
            if "ikT" not in getattr(g, "skip", ()):
                S.dma("gpsimd", g.ikT_s[:, rows], ikT_t[cur][:], reads=[ikT_t[cur]], writes=[g.ikT_s])
            if SECT < 5:
                continue
            ps = proj("ab", C_DA, C_DA + 8, hTt)
            S.op("vector", lambda e: e.tensor_tensor(out=ab_tmp[:], in0=ps[:, 0:4], in1=dtb_bc[:], op=ALU.add),
                 reads=[ps, dtb_bc], writes=[ab_tmp])
            S.op("scalar", lambda e: e.activation(out=ab_tmp[:], in_=ab_tmp[:], func=AF.Exp), reads=[ab_tmp], writes=[ab_tmp])
            S.op("scalar", lambda e: e.activation(out=ab_tmp[:], in_=ab_tmp[:], func=AF.Ln, bias=g.ones_f[:, 0:1], scale=1.0),
                 reads=[ab_tmp, g.ones_f], writes=[ab_tmp])
            S.op("vector", lambda e: e.tensor_tensor(out=gb_all[:, tt, 0:4], in0=ab_tmp[:], in1=nea_bc[:], op=ALU.mult),
                 reads=[ab_tmp, nea_bc], writes=[gb_all])
            S.op("scalar", lambda e: e.activation(out=gb_all[:, tt, 4:8], in_=ps[:, 4:8], func=AF.Sigmoid),
                 reads=[ps], writes=[gb_all])
            if SECT < 6:
                continue
            for gi, (c0, dst_s) in enumerate(((C_DQ, g.dnq_s), (C_DK, g.dnk_s), (C_DV, g.dnv_s))):
                ps = proj("dn", c0, c0 + 512, hTt)
                cs = slice(gi * 512, (gi + 1) * 512)
                for j in range(4):
                    S.op("vector", lambda e: e.tensor_tensor(out=xw[cur][:, j, cs], in0=ps[:, 0:512], in1=cw_bc[:, j, cs], op=ALU.mult),
                         reads=[ps, cw_bc], writes=[xw[cur]])
                b = next_bank(g)
                mm = []
                for j in range(4):
                    mm.append((Sh[3 - j], xw[cur], j))
                if tt > 0:
                    for j in range(3):
                        mm.append((ShP[3 - j], xw[1 - cur], j))
                for i, (sh, xsrc, j) in enumerate(mm):
                    S.op("tensor", lambda e: e.matmul(out=b[:, 0:512], lhsT=sh[:], rhs=xsrc[:, j, cs],
                                                      start=(i == 0), stop=(i == len(mm) - 1)),
                         reads=[sh, xsrc], writes=[b])
                y = yc[gi]
                S.op("scalar", lambda e: e.activation(out=y[:], in_=b[:, 0:512], func=AF.Silu), reads=[b], writes=[y])
                if gi < 2:
                    S.op("gpsimd", lambda e: e.tensor_tensor(out=sq[:], in0=y[:], in1=y[:], op=ALU.mult), reads=[y], writes=[sq])
                    S.op("vector", lambda e: e.tensor_reduce(out=ss8[:, 0:4], in_=sq[:].rearrange("p (h d) -> p h d", d=128),
                                                             axis=AX.X, op=ALU.add), reads=[sq], writes=[ss8])
                    rstd_op(g, rs8, ss8, 1.0, 4, sd8)
                    if gi == 0:
                        S.op("vector", lambda e: e.tensor_scalar(out=rs8[:, 0:4], in0=rs8[:, 0:4], scalar1=128 ** -0.5, scalar2=None,
                                                                 op0=ALU.mult), reads=[rs8], writes=[rs8])
                    S.op("vector", lambda e: e.tensor_tensor(out=y[:].rearrange("p (h d) -> p h d", d=128),
                                                             in0=y[:].rearrange("p (h d) -> p h d", d=128),
                                                             in1=rs8[:, 0:4].unsqueeze(2).to_broadcast([128, 4, 128]), op=ALU.mult),
                         reads=[y, rs8], writes=[y])
                S.dma("gpsimd", dst_s[rows], y[:], reads=[y], writes=[dst_s])
            if SECT < 7:
                continue
            ps = proj("dz", C_DZ, C_DZ + 512, hTt)
            S.op("scalar", lambda e: e.activation(out=dz_t[cur][:], in_=ps[:, 0:512], func=AF.Silu), reads=[ps], writes=[dz_t[cur]])
            S.dma("gpsimd", g.dz_s[rows], dz_t[cur][:], reads=[dz_t[cur]], writes=[g.dz_s])
            if SECT < 8:
                continue
            for gi, c0 in enumerate((C_GA, C_GB)):
                for hf in range(2):
                    ps = proj("gate", c0 + hf * 512, c0 + (hf + 1) * 512, hTt)
                    S.op("scalar", lambda e: e.activation(out=gt_t[gi][:, hf * 512:(hf + 1) * 512], in_=ps[:, 0:512], func=AF.Sigmoid),
                         reads=[ps], writes=[gt_t[gi]])
                S.dma("gpsimd", g.gate_s[rows, gi * 1024:(gi + 1) * 1024], gt_t[gi][:], reads=[gt_t[gi]], writes=[g.gate_s])
        S.dma("gpsimd", g.iw_s[:, :, :], iw_all[:], reads=[iw_all], writes=[g.iw_s])
        S.dma("gpsimd", g.gb_s[:, :, :], gb_all[:], reads=[gb_all], writes=[g.gb_s])
        S.barrier()


NIT = 15


def phase_b(g):
    nc, S = g.nc, g.S
    g.nrot = 6
    acc = g.banks[6:8]
    with contextlib.ExitStack() as ph:
        def A(name, shape, dt):
            return sb(g, ph, name, shape, dt)

        kT_all = A("kT_all", [64, 2, T], BF16)
        ikT_all = A("ikT_all", [64, T], BF16)
        v_raw = A("v_raw", [128, NT, 128], BF16)
        v_all = A("v_all", [128, NT, 2, 65], BF16)
        iw_all = A("iw_all", [128, NT, 8], F32)
        S.dma("sync", kT_all[:], g.kT_s[:, :, :], reads=[g.kT_s], writes=[kT_all])
        S.dma("sync", ikT_all[:], g.ikT_s[:, :], reads=[g.ikT_s], writes=[ikT_all])
        S.dma("sync", v_raw[:], g.v_s[:, :].rearrange("(n p) c -> p n c", p=128), reads=[g.v_s], writes=[v_raw])
        S.dma("sync", iw_all[:], g.iw_s[:, :, :], reads=[g.iw_s], writes=[iw_all])
        S.op("gpsimd", lambda e: e.memset(v_all[:], 1.0), writes=[v_all])
        S.op("vector", lambda e: e.tensor_copy(out=v_all[:, :, :, 0:64],
                                               in_=v_raw[:].rearrange("p n (g d) -> p n g d", d=64)),
             reads=[v_raw], writes=[v_all])
        thr0 = A("thr0", [128, 1], F32)
        S.op("gpsimd", lambda e: e.memset(thr0[:], -1e29), writes=[thr0])
        cmask = A("cmask", [128, 128], F32)
        S.op("gpsimd", lambda e: e.memset(cmask[:], 0.0), writes=[cmask])
        S.op("gpsimd", lambda e: e.affine_select(out=cmask[:], in_=cmask[:], pattern=[[-1, 128]], compare_op=ALU.is_ge,
                                                 fill=g.fillneg, base=0, channel_multiplier=1), reads=[cmask], writes=[cmask])

        sc = [A("sc%d" % i, [128, T], F32) for i in range(2)]
        Rb = [A("Rb%d" % i, [128, 512], F32) for i in range(2)]
        junk = A("junkb", [128, T], BF16)
        mask = A("mask", [128, T], BF16)
        maskT = [A("maskT%d" % i, [128, NT, 128], BF16) for i in range(2)]
        iqT = [A("iqT%d" % i, [64, 8, 128], BF16) for i in range(2)]
        qT = [A("qT%d" % i, [64, 8, 128], BF16) for i in range(3)]
        Eb = [A("Eb%d" % i, [128, 512], BF16) for i in range(2)]
        Pb = [A("Pb%d" % i, [128, 512], BF16) for i in range(2)]
        yat = [A("yat%d" % i, [128, 512], BF16) for i in range(2)]
        rec = A("rec", [128, 4], F32)
        hi = A("hi", [128, 1], F32)
        lo = A("lo", [128, 1], F32)
        rk = A("rk", [128, 1], F32)
        mid = A("mid", [128, 1], F32)
        cnt = A("cnt", [128, 1], F32)
        step = A("step", [128, 1], F32)
        print("phase B sbuf remaining", nc.sbuf_bytes_remaining)

        NQB = getattr(g, "nqb", NT)
        pw = A("pw", [128, NIT + 1], F32)
        for it in range(NIT + 1):
            S.op("gpsimd", lambda e: e.memset(pw[:, it:it + 1], 2.0 ** -(it + 1)), writes=[pw])
        rkall = A("rkall", [128, NIT + 1], F32)
        nmid = A("nmid", [128, 1], F32)
        tq = A("tq", [128, 1], F32)
        cnt2 = A("cnt2", [128, 1], F32)
        thr_t = A("thr_t", [128, 1], F32)
        ctr = {"ke": 0}

        def stage1a(qb):
            cur = qb % 2
            S.dma("sync", iqT[cur][:], g.iqT_s[qb], reads=[g.iqT_s], writes=[iqT[cur]])
            S.dma("sync", qT[qb % 3][:], g.qT_s[qb], reads=[g.qT_s], writes=[qT[qb % 3]])
            NS = qb + 1
            SS = NS * 128
            sct = sc[cur]
            for ci in range((SS + 511) // 512):
                c0 = ci * 512
                n = min(512, SS - c0)
                for h in range(8):
                    b = next_bank(g, (0, 1))
                    S.op("tensor", lambda e: e.matmul(out=b[:, 0:n], lhsT=iqT[cur][:, h, :], rhs=ikT_all[:, c0:c0 + n],
                                                      start=True, stop=True),
                         reads=[iqT[cur], ikT_all], writes=[b])
                    R = Rb[ctr["ke"] % 2]
                    ctr["ke"] += 1
                    S.op("scalar", lambda e: e.activation(out=R[:, 0:n], in_=b[:, 0:n], func=AF.Relu), reads=[b], writes=[R])
                    if h == 0:
                        S.op("vector", lambda e: e.tensor_scalar(out=sct[:, c0:c0 + n], in0=R[:, 0:n], scalar1=iw_all[:, qb, 0:1],
                                                                 scalar2=None, op0=ALU.mult),
                             reads=[R, iw_all], writes=[sct])
                    else:
                        S.op("vector", lambda e: e.scalar_tensor_tensor(out=sct[:, c0:c0 + n], in0=R[:, 0:n],
                                                                        scalar=iw_all[:, qb, h:h + 1], in1=sct[:, c0:c0 + n],
                                                                        op0=ALU.mult, op1=ALU.add),
                             reads=[R, iw_all, sct], writes=[sct])
            dg = sct[:, qb * 128:(qb + 1) * 128]
            S.op("vector", lambda e: e.tensor_tensor(out=dg, in0=dg, in1=cmask[:], op=ALU.add), reads=[sct, cmask], writes=[sct])
        def stage1b(qb):
            cur = qb % 2
            NS = qb + 1
            SS = NS * 128
            sct = sc[cur]
            if qb >= 2:
                S.op("vector", lambda e: e.tensor_reduce(out=hi[:], in_=sct[:, 0:SS], axis=AX.X, op=ALU.max), reads=[sct], writes=[hi])
                S.op("vector", lambda e: e.tensor_reduce(out=lo[:], in_=sct[:, 0:qb * 128], axis=AX.X, op=ALU.min), reads=[sct], writes=[lo])
                S.op("vector", lambda e: e.tensor_tensor(out=rk[:], in0=hi[:], in1=lo[:], op=ALU.subtract), reads=[hi, lo], writes=[rk])
                S.op("vector", lambda e: e.tensor_tensor(out=rkall[:], in0=pw[:], in1=rk[:, 0:1].to_broadcast([128, NIT + 1]), op=ALU.mult),
                     reads=[pw, rk], writes=[rkall])
                S.op("vector", lambda e: e.tensor_scalar(out=nmid[:], in0=lo[:], scalar1=rkall[:, 0:1], scalar2=-1.0, op0=ALU.add, op1=ALU.mult),
                     reads=[lo, rkall], writes=[nmid])
                for it in range(NIT):
                    if getattr(g, "dvecnt", 0):
                        S.op("vector", lambda e: e.tensor_scalar(out=mid[:], in0=nmid[:], scalar1=-1.0, scalar2=None, op0=ALU.mult),
                             reads=[nmid], writes=[mid])
                        S.op("vector", lambda e: e.tensor_scalar(out=junk[:, 0:SS], in0=sct[:, 0:SS], scalar1=mid[:, 0:1], scalar2=None,
                                                                 op0=ALU.is_gt, op1=ALU.add, accum_out=cnt[:, 0:1]),
                             reads=[sct, mid], writes=[junk, cnt])
                        S.op("vector", lambda e: e.tensor_scalar(out=cnt2[:], in0=cnt[:], scalar1=2.0, scalar2=float(SS), op0=ALU.mult, op1=ALU.subtract),
                             reads=[cnt], writes=[cnt2])
                    else:
                        S.op("scalar", lambda e: e.activation(out=junk[:, 0:SS], in_=sct[:, 0:SS], func=AF.Sign, bias=nmid[:, 0:1], scale=1.0,
                                                              accum_out=cnt[:, 0:1]),
                             reads=[sct, nmid], writes=[junk, cnt])
                    cx = cnt2 if getattr(g, "dvecnt", 0) else cnt
                    S.op("vector", lambda e: e.tensor_scalar(out=tq[:], in0=cx[:], scalar1=511.5 - SS, scalar2=0.5, op0=ALU.is_lt, op1=ALU.subtract),
                         reads=[cx], writes=[tq])
                    S.op("vector", lambda e: e.scalar_tensor_tensor(out=nmid[:], in0=tq[:], scalar=rkall[:, it:it + 1], in1=nmid[:],
                                                                    op0=ALU.mult, op1=ALU.add), reads=[tq, rkall, nmid], writes=[nmid])
                S.op("vector", lambda e: e.tensor_scalar(out=thr_t[:], in0=nmid[:], scalar1=-1.0, scalar2=rkall[:, NIT:NIT + 1],
                                                         op0=ALU.mult, op1=ALU.subtract), reads=[nmid, rkall], writes=[thr_t])
                thr = thr_t
            else:
                thr = thr0
            S.op("vector", lambda e: e.tensor_scalar(out=mask[:, 0:SS], in0=sct[:, 0:SS], scalar1=thr[:, 0:1], scalar2=None,
                                                     op0=ALU.is_ge), reads=[sct, thr], writes=[mask])
            mT = maskT[cur]
            for b0 in range(0, NS, 8):
                nb = min(8, NS - b0)
                b = next_bank(g, (2,))
                bv = b[:, :].bitcast(BF16)
                for j in range(nb):
                    S.op("tensor", lambda e: e.transpose(out=bv[:, j * 128:(j + 1) * 128],
                                                         in_=mask[:, (b0 + j) * 128:(b0 + j + 1) * 128], identity=g.ident_b[:]),
                         reads=[mask, g.ident_b], writes=[b])
                dst = mT[:, b0:b0 + nb, :].rearrange("p n t -> p (n t)")
                S.op("gpsimd", lambda e: e.tensor_copy(out=dst, in_=bv[:, 0:nb * 128]), reads=[b], writes=[mT]) if False else \
                    S.op("vector", lambda e: e.tensor_copy(out=dst, in_=bv[:, 0:nb * 128]), reads=[b], writes=[mT])

        def stage2(qb):
            cur = qb % 2
            NS = qb + 1
            mT = maskT[cur]
            yt = yat[cur]
            for gi in range(2):
                po = acc[gi]
                for sbk in range(NS):
                    b = next_bank(g, (3, 4, 5))
                    S.op("tensor", lambda e: e.matmul(out=b[:, 0:512], lhsT=kT_all[:, gi, sbk * 128:(sbk + 1) * 128],
                                                      rhs=qT[qb % 3][:, 4 * gi:4 * gi + 4, :].rearrange("p h t -> p (h t)"),
                                                      start=True, stop=True),
                         reads=[kT_all, qT[qb % 3]], writes=[b])
                    E = Eb[ctr["ke"] % 2]
                    P = Pb[ctr["ke"] % 2]
                    ctr["ke"] += 1
                    S.op("scalar", lambda e: e.activation(out=E[:], in_=b[:, 0:512], func=AF.Exp), reads=[b], writes=[E])
                    S.op("vector", lambda e: e.tensor_tensor(out=P[:].rearrange("p (h t) -> p h t", h=4),
                                                             in0=E[:].rearrange("p (h t) -> p h t", h=4),
                                                             in1=mT[:, sbk, :].unsqueeze(1).to_broadcast([128, 4, 128]), op=ALU.mult),
                         reads=[E, mT], writes=[P])
                    for h in range(4):
                        S.op("tensor", lambda e: e.matmul(out=po[:, h * 65:(h + 1) * 65], lhsT=P[:, h * 128:(h + 1) * 128],
                                                          rhs=v_all[:, sbk, gi, :], start=(sbk == 0 and h == 0),
                                                          stop=(sbk == NS - 1), skip_group_check=True),
                             reads=[P, v_all], writes=[po])
                pov = po[:, 0:260].rearrange("p (h e) -> p h e", e=65)
                S.op("vector", lambda e: e.reciprocal(out=rec[:], in_=pov[:, :, 64]), reads=[po], writes=[rec])
                S.op("vector", lambda e: e.tensor_tensor(out=yt[:, gi * 256:(gi + 1) * 256].rearrange("p (h d) -> p h d", d=64),
                                                         in0=pov[:, :, 0:64],
                                                         in1=rec[:, :].unsqueeze(2).to_broadcast([128, 4, 64]), op=ALU.mult),
                     reads=[po, rec], writes=[yt])
            S.dma("sync", g.yatt_s[qb * 128:(qb + 1) * 128], yt[:], reads=[yt], writes=[g.yatt_s])

        plist = phase_p_ops(g, ph) if getattr(g, "p_in_b", False) else []
        def recd(fn, qb):
            if qb >= NQB:
                return None
            S.record()
            fn(qb)
            return S.stop()
        S.replay(recd(stage1a, 0))
        S.replay(recd(stage1b, 0), recd(stage1a, 1))
        for qb in range(NQB):
            pchunk = plist[(qb * len(plist)) // NQB:((qb + 1) * len(plist)) // NQB]
            r2 = recd(stage2, qb)
            r1b = recd(stage1b, qb + 1)
            r1a = recd(stage1a, qb + 2)
            if getattr(g, "noil", 0):
                for r in (r2, pchunk, r1b, r1a):
                    S.replay(r)
            else:
                S.replay(r2, r1b, r1a, pchunk)
        S.barrier()
    g.nrot = 8


def phase_c(g):
    nc, S = g.nc, g.S
    g.nrot = 8
    GS = 2
    with contextlib.ExitStack() as ph:
        def A(name, shape, dt=F32):
            return sb(g, ph, name, shape, dt)

        utri = A("utri", [64, 64])
        sel63 = A("sel63", [64, 128])
        S.op("gpsimd", lambda e: e.affine_select(out=utri[:], in_=g.ones_f[0:64, 0:64], pattern=[[1, 64]], compare_op=ALU.is_ge,
                                                 fill=g.fill0, base=0, channel_multiplier=-1), reads=[g.ones_f], writes=[utri])
        S.op("gpsimd", lambda e: e.affine_select(out=sel63[:], in_=g.ones_f[0:64, :], pattern=[[0, 128]], compare_op=ALU.is_equal,
                                                 fill=g.fill0, base=-63, channel_multiplier=1), reads=[g.ones_f], writes=[sel63])
        gno = A("gno", [128, 128])
        S.dma("sync", gno[:], g.dn_out_norm_gain.ap().partition_broadcast(128), writes=[gno])
        gbc = [A("gbc%d" % h, [64, NT, 8]) for h in range(2)]
        gn = [A("gn%d" % h, [64, NT, 8]) for h in range(2)]
        egc = [A("egc%d" % h, [64, NT, 4]) for h in range(2)]
        bg = [A("bg%d" % h, [64, NT, 4]) for h in range(2)]
        kd = [A("kd%d" % h, [64, NT, 4]) for h in range(2)]
        elast = [A("elast%d" % h, [128, NT, 4]) for h in range(2)]
        for h in range(2):
            S.dma("sync", gbc[h][:], g.gb_s[h * 64:(h + 1) * 64, :, :], reads=[g.gb_s], writes=[gbc[h]])
            b = next_bank(g)
            S.op("tensor", lambda e: e.matmul(out=b[0:64, 0:128], lhsT=utri[:], rhs=gbc[h][:, :, 0:4], start=True, stop=True),
                 reads=[utri, gbc[h]], writes=[b])
            S.op("vector", lambda e: e.tensor_copy(out=gn[h][:, :, 0:4], in_=b[0:64, 0:128].rearrange("p (n f) -> p n f", f=4)),
                 reads=[b], writes=[gn[h]])
            S.op("vector", lambda e: e.tensor_scalar(out=gn[h][:, :, 4:8], in0=gbc[h][:, :, 4:8], scalar1=-1.0, scalar2=None, op0=ALU.mult),
                 reads=[gbc[h]], writes=[gn[h]])
            S.op("scalar", lambda e: e.activation(out=egc[h][:], in_=gn[h][:, :, 0:4], func=AF.Exp), reads=[gn[h]], writes=[egc[h]])
            S.op("vector", lambda e: e.tensor_tensor(out=bg[h][:], in0=egc[h][:], in1=gbc[h][:, :, 4:8], op=ALU.mult),
                 reads=[egc[h], gbc[h]], writes=[bg[h]])
            b2 = next_bank(g)
            S.op("tensor", lambda e: e.matmul(out=b2[:, 0:128], lhsT=sel63[:], rhs=gn[h][:, :, 0:4], start=True, stop=True),
                 reads=[sel63, gn[h]], writes=[b2])
            S.op("scalar", lambda e: e.activation(out=elast[h][:], in_=b2[:, 0:128].rearrange("p (n f) -> p n f", f=4), func=AF.Exp),
                 reads=[b2], writes=[elast[h]])
            S.op("vector", lambda e: e.tensor_tensor(out=kd[h][:], in0=b2[0:64, 0:128].rearrange("p (n f) -> p n f", f=4),
                                                     in1=gn[h][:, :, 0:4], op=ALU.subtract), reads=[b2, gn[h]], writes=[kd[h]])
            S.op("scalar", lambda e: e.activation(out=kd[h][:], in_=kd[h][:], func=AF.Exp), reads=[kd[h]], writes=[kd[h]])

        Sst = A("Sst", [128, 4, 128])
        S.op("gpsimd", lambda e: e.memset(Sst[:], 0.0), writes=[Sst])

        class Slot:
            pass
        slots = []
        for i in range(2 * GS):
            s_ = Slot()
            s_.q = A("cq%d" % i, [64, 512]); s_.k = A("ck%d" % i, [64, 512]); s_.v = A("cv%d" % i, [64, 512])
            s_.dz = A("cdz%d" % i, [64, 512], BF16)
            s_.dgb = A("dgb%d" % i, [64, 512]); s_.G1 = A("G1%d" % i, [64, 256]); s_.G2 = A("G2%d" % i, [64, 256])
            s_.sel3 = A("sel3%d" % i, [64, 768]); s_.E3 = A("E3%d" % i, [64, 768])
            s_.DATn = A("DATn%d" % i, [64, 256]); s_.DAn = A("DAn%d" % i, [64, 256])
            s_.kqT = A("kqT%d" % i, [128, 512])
            s_.MM = [A("MM%d_%d" % (i, j), [64, 512]) for j in range(2)]
            s_.XT = A("XT%d" % i, [64, 256]); s_.inT = A("inT%d" % i, [64, 256])
            s_.vb = A("vb%d" % i, [64, 512]); s_.kbg = A("kbg%d" % i, [64, 512]); s_.kdec = A("kdec%d" % i, [64, 512])
            s_.u = A("u%d" % i, [64, 512]); s_.wT = A("wT%d" % i, [128, 256])
            slots.append(s_)
        vnew = A("vnew", [64, 512])
        otmp = A("otmp", [64, 512])
        osq = A("osq", [64, 512])
        oss = A("oss", [64, 4]); osd = A("osd", [64, 4]); ors = A("ors", [64, 4])
        yout = [A("yout%d" % i, [64, 512], BF16) for i in range(2)]
        print("phase C sbuf remaining", nc.sbuf_bytes_remaining)
        idb = g.ident_f[0:64, 0:64]
        NCH = getattr(g, "nch", 64)

        def bc_h(ap4, n):
            return ap4.unsqueeze(2).to_broadcast([64, 4, n])

        def v3(ap, n):
            return ap.rearrange("p (h f) -> p h f", h=4)

        def par(chunks):
            info = {}
            for c in chunks:
                sl = slots[c % (2 * GS)]
                tt, half = c // 2, c % 2
                rows = slice(c * 64, (c + 1) * 64)
                S.dma("sync", sl.q[:], g.dnq_s[rows], reads=[g.dnq_s], writes=[sl.q])
                S.dma("sync", sl.k[:], g.dnk_s[rows], reads=[g.dnk_s], writes=[sl.k])
                S.dma("sync", sl.v[:], g.dnv_s[rows], reads=[g.dnv_s], writes=[sl.v])
                S.dma("sync", sl.dz[:], g.dz_s[rows], reads=[g.dz_s], writes=[sl.dz])
                info[c] = (sl, tt, half)
            for c in chunks:
                sl, tt, half = info[c]
                gnc = gn[half][:, tt, :]
                S.op("vector", lambda e: e.tensor_tensor(out=sl.dgb[:].rearrange("p (a f) -> p a f", a=8),
                                                         in0=gnc.unsqueeze(2).to_broadcast([64, 8, 64]),
                                                         in1=idb.unsqueeze(1).to_broadcast([64, 8, 64]), op=ALU.mult),
                     reads=[gn[half], g.ident_f], writes=[sl.dgb])
                bR = next_bank(g, (0, 1, 2, 3))
                S.op("tensor", lambda e: e.matmul(out=bR[0:64, 0:512], lhsT=g.ones_f[0:64, 0:64], rhs=sl.dgb[:], start=True, stop=True),
                     reads=[g.ones_f, sl.dgb], writes=[bR])
                S.op("vector", lambda e: e.tensor_tensor(out=v3(sl.G1[:], 64), in0=v3(bR[0:64, 0:256], 64),
                                                         in1=bc_h(gn[half][:, tt, 0:4], 64), op=ALU.subtract),
                     reads=[bR, gn[half]], writes=[sl.G1])
                S.op("vector", lambda e: e.tensor_scalar(out=sl.G2[:], in0=sl.G1[:], scalar1=-1.0, scalar2=None, op0=ALU.mult),
                     reads=[sl.G1], writes=[sl.G2])
                S.op("gpsimd", lambda e: e.affine_select(out=sl.sel3[:, 0:256], in_=sl.G1[:], pattern=[[0, 4], [1, 64]],
                                                         compare_op=ALU.is_ge, fill=g.fillneg, base=0, channel_multiplier=-1),
                     reads=[sl.G1], writes=[sl.sel3])
                S.op("gpsimd", lambda e: e.affine_select(out=sl.sel3[:, 256:512], in_=sl.G1[:], pattern=[[0, 4], [1, 64]],
                                                         compare_op=ALU.is_ge, fill=g.fillneg, base=-1, channel_multiplier=-1),
                     reads=[sl.G1], writes=[sl.sel3])
                S.op("gpsimd", lambda e: e.affine_select(out=sl.sel3[:, 512:768], in_=sl.G2[:], pattern=[[0, 4], [-1, 64]],
                                                         compare_op=ALU.is_ge, fill=g.fillneg, base=-1, channel_multiplier=1),
                     reads=[sl.G2], writes=[sl.sel3])
                S.op("scalar", lambda e: e.activation(out=sl.E3[:], in_=sl.sel3[:], func=AF.Exp), reads=[sl.sel3], writes=[sl.E3])
                S.op("vector", lambda e: e.tensor_tensor(out=sl.DATn[:], in0=sl.E3[:, 256:512], in1=bR[0:64, 256:512], op=ALU.mult),
                     reads=[sl.E3, bR], writes=[sl.DATn])
                S.op("gpsimd", lambda e: e.tensor_tensor(out=v3(sl.DAn[:], 64), in0=v3(sl.E3[:, 512:768], 64),
                                                         in1=bc_h(gn[half][:, tt, 4:8], 64), op=ALU.mult),
                     reads=[sl.E3, gn[half]], writes=[sl.DAn])
                S.op("vector", lambda e: e.tensor_tensor(out=v3(sl.vb[:], 128), in0=v3(sl.v[:], 128),
                                                         in1=bc_h(gbc[half][:, tt, 4:8], 128), op=ALU.mult),
                     reads=[sl.v, gbc[half]], writes=[sl.vb])
                S.op("gpsimd", lambda e: e.tensor_tensor(out=v3(sl.kbg[:], 128), in0=v3(sl.k[:], 128),
                                                         in1=bc_h(bg[half][:, tt, :], 128), op=ALU.mult),
                     reads=[sl.k, bg[half]], writes=[sl.kbg])
                S.op("gpsimd", lambda e: e.tensor_tensor(out=v3(sl.kdec[:], 128), in0=v3(sl.k[:], 128),
                                                         in1=bc_h(kd[half][:, tt, :], 128), op=ALU.mult),
                     reads=[sl.k, kd[half]], writes=[sl.kdec])
                bT = next_bank(g, (0, 1, 2, 3))
                for hd in range(4):
                    S.op("tensor", lambda e: e.transpose(out=bT[:, hd * 64:(hd + 1) * 64], in_=sl.k[:, hd * 128:(hd + 1) * 128], identity=idb),
                         reads=[sl.k, g.ident_f], writes=[bT])
                for hd in range(4):
                    S.op("tensor", lambda e: e.transpose(out=bT[:, 256 + hd * 64:256 + (hd + 1) * 64], in_=sl.q[:, hd * 128:(hd + 1) * 128],
                                                         identity=idb), reads=[sl.q, g.ident_f], writes=[bT])
                S.op("scalar", lambda e: e.copy(out=sl.kqT[:], in_=bT[:, 0:512]), reads=[bT], writes=[sl.kqT])
                bK = next_bank(g, (0, 1, 2, 3))
                for hd in range(4):
                    kT_h = sl.kqT[:, hd * 64:(hd + 1) * 64]
                    qT_h = sl.kqT[:, 256 + hd * 64:256 + (hd + 1) * 64]
                    S.op("tensor", lambda e: e.matmul(out=bK[0:64, hd * 64:(hd + 1) * 64], lhsT=kT_h, rhs=kT_h, start=(hd == 0), stop=True,
                                                      skip_group_check=True), reads=[sl.kqT], writes=[bK])
                for hd in range(4):
                    kT_h = sl.kqT[:, hd * 64:(hd + 1) * 64]
                    qT_h = sl.kqT[:, 256 + hd * 64:256 + (hd + 1) * 64]
                    S.op("tensor", lambda e: e.matmul(out=bK[0:64, 256 + hd * 64:256 + (hd + 1) * 64], lhsT=kT_h, rhs=qT_h, start=False,
                                                      stop=True, skip_group_check=True), reads=[sl.kqT], writes=[bK])
                MM0 = sl.MM[0]
                S.op("vector", lambda e: e.tensor_tensor(out=MM0[:, 0:256], in0=bK[0:64, 0:256], in1=sl.DAn[:], op=ALU.mult),
                     reads=[bK, sl.DAn], writes=[MM0])
                S.op("vector", lambda e: e.tensor_tensor(out=MM0[:, 256:512], in0=bK[0:64, 0:256], in1=sl.DATn[:], op=ALU.mult),
                     reads=[bK, sl.DATn], writes=[MM0])
                S.op("vector", lambda e: e.tensor_tensor(out=sl.inT[:], in0=bK[0:64, 256:512], in1=sl.E3[:, 0:256], op=ALU.mult),
                     reads=[bK, sl.E3], writes=[sl.inT])
                S.op("gpsimd", lambda e: e.tensor_tensor(out=v3(sl.XT[:], 64), in0=v3(MM0[:, 256:512], 64),
                                                         in1=idb.unsqueeze(1).to_broadcast([64, 4, 64]), op=ALU.add),
                     reads=[MM0, g.ident_f], writes=[sl.XT])
            for lvl in range(1, 6):
                for c in chunks:
                    sl, tt, half = info[c]
                    Mp = sl.MM[(lvl - 1) % 2]
                    Mn = sl.MM[lvl % 2]
                    bM = next_bank(g, (0, 1, 2, 3))
                    for hd in range(4):
                        M_h = Mp[:, hd * 64:(hd + 1) * 64]
                        MT_h = Mp[:, 256 + hd * 64:256 + (hd + 1) * 64]
                        S.op("tensor", lambda e: e.matmul(out=bM[0:64, hd * 64:(hd + 1) * 64], lhsT=MT_h, rhs=M_h, start=(hd == 0), stop=True,
                                                          skip_group_check=True), reads=[Mp], writes=[bM])
                    nw = 256
                    if lvl < 5:
                        nw = 512
                        for hd in range(4):
                            M_h = Mp[:, hd * 64:(hd + 1) * 64]
                            MT_h = Mp[:, 256 + hd * 64:256 + (hd + 1) * 64]
                            S.op("tensor", lambda e: e.matmul(out=bM[0:64, 256 + hd * 64:256 + (hd + 1) * 64], lhsT=M_h, rhs=MT_h, start=False,
                                                              stop=True, skip_group_check=True), reads=[Mp], writes=[bM])
                    S.op("scalar", lambda e: e.copy(out=Mn[:, 0:nw], in_=bM[0:64, 0:nw]), reads=[bM], writes=[Mn])
                    bX = next_bank(g, (0, 1, 2, 3))
                    for hd in range(4):
                        S.op("tensor", lambda e: e.matmul(out=bX[0:64, hd * 64:(hd + 1) * 64], lhsT=Mn[:, hd * 64:(hd + 1) * 64],
                                                          rhs=sl.XT[:, hd * 64:(hd + 1) * 64], start=(hd == 0), stop=True,
                                                          skip_group_check=True), reads=[Mn, sl.XT], writes=[bX])
                    S.op("vector", lambda e: e.tensor_tensor(out=sl.XT[:], in0=bX[0:64, 0:256], in1=sl.XT[:], op=ALU.add),
                         reads=[bX, sl.XT], writes=[sl.XT])
            for c in chunks:
                sl, tt, half = info[c]
                bU = next_bank(g, (0, 1, 2, 3))
                for hd in range(4):
                    S.op("tensor", lambda e: e.matmul(out=bU[0:64, hd * 128:(hd + 1) * 128], lhsT=sl.XT[:, hd * 64:(hd + 1) * 64],
                                                      rhs=sl.vb[:, hd * 128:(hd + 1) * 128], start=(hd == 0), stop=True,
                                                      skip_group_check=True), reads=[sl.XT, sl.vb], writes=[bU])
                S.op("scalar", lambda e: e.copy(out=sl.u[:], in_=bU[0:64, 0:512]), reads=[bU], writes=[sl.u])
                bW = next_bank(g, (0, 1, 2, 3))
                for hd in range(4):
                    S.op("tensor", lambda e: e.matmul(out=bW[:, hd * 64:(hd + 1) * 64], lhsT=sl.kbg[:, hd * 128:(hd + 1) * 128],
                                                      rhs=sl.XT[:, hd * 64:(hd + 1) * 64], start=(hd == 0), stop=True,
                                                      skip_group_check=True), reads=[sl.XT, sl.kbg], writes=[bW])
                S.op("vector", lambda e: e.tensor_copy(out=sl.wT[:], in_=bW[:, 0:256]), reads=[bW], writes=[sl.wT])
            return info

        def rec(chunks, info):
            for c in chunks:
                sl, tt, half = info[c]
                rows = slice(c * 64, (c + 1) * 64)
                b1 = next_bank(g, (4, 5, 6, 7))
                for hd in range(4):
                    S.op("tensor", lambda e: e.matmul(out=b1[0:64, hd * 128:(hd + 1) * 128], lhsT=sl.wT[:, hd * 64:(hd + 1) * 64],
                                                      rhs=Sst[:, hd, :], start=(hd == 0), stop=True, skip_group_check=True),
                         reads=[sl.wT, Sst], writes=[b1])
                S.op("vector", lambda e: e.tensor_tensor(out=vnew[:], in0=sl.u[:], in1=b1[0:64, 0:512], op=ALU.subtract),
                     reads=[sl.u, b1], writes=[vnew])
                b2 = next_bank(g, (4, 5, 6, 7))
                for hd in range(4):
                    S.op("tensor", lambda e: e.matmul(out=b2[0:64, hd * 128:(hd + 1) * 128], lhsT=sl.kqT[:, 256 + hd * 64:256 + (hd + 1) * 64],
                                                      rhs=Sst[:, hd, :], start=(hd == 0), stop=True, skip_group_check=True),
                         reads=[sl.kqT, Sst], writes=[b2])
                b3 = next_bank(g, (4, 5, 6, 7))
                for hd in range(4):
                    S.op("tensor", lambda e: e.matmul(out=b3[0:64, hd * 128:(hd + 1) * 128], lhsT=sl.inT[:, hd * 64:(hd + 1) * 64],
                                                      rhs=vnew[:, hd * 128:(hd + 1) * 128], start=(hd == 0), stop=True, skip_group_check=True),
                         reads=[sl.inT, vnew], writes=[b3])
                b4 = next_bank(g, (4, 5, 6, 7))
                for hd in range(4):
                    S.op("tensor", lambda e: e.matmul(out=b4[:, hd * 128:(hd + 1) * 128], lhsT=sl.kdec[:, hd * 128:(hd + 1) * 128],
                                                      rhs=vnew[:, hd * 128:(hd + 1) * 128], start=(hd == 0), stop=True, skip_group_check=True),
                         reads=[sl.kdec, vnew], writes=[b4])
                for hd in range(4):
                    S.op("vector", lambda e: e.scalar_tensor_tensor(out=Sst[:, hd, :], in0=Sst[:, hd, :], scalar=elast[half][:, tt, hd:hd + 1],
                                                                    in1=b4[:, hd * 128:(hd + 1) * 128], op0=ALU.mult, op1=ALU.add),
                         reads=[Sst, elast[half], b4], writes=[Sst])
                S.op("vector", lambda e: e.tensor_tensor(out=v3(otmp[:], 128), in0=v3(b2[0:64, 0:512], 128),
                                                         in1=bc_h(egc[half][:, tt, :], 128), op=ALU.mult),
                     reads=[b2, egc[half]], writes=[otmp])
                S.op("vector", lambda e: e.tensor_tensor(out=otmp[:], in0=otmp[:], in1=b3[0:64, 0:512], op=ALU.add),
                     reads=[otmp, b3], writes=[otmp])
                S.op("scalar", lambda e: e.activation(out=osq[:], in_=otmp[:], func=AF.Square), reads=[otmp], writes=[osq])
                S.op("vector", lambda e: e.tensor_reduce(out=oss[:], in_=v3(osq[:], 128), axis=AX.X, op=ALU.add), reads=[osq], writes=[oss])
                S.op("scalar", lambda e: e.activation(out=osd[:], in_=oss[:], func=AF.Sqrt, bias=g.eps_t[0:64, 0:1], scale=1.0 / 128),
                     reads=[oss, g.eps_t], writes=[osd])
                S.op("vector", lambda e: e.reciprocal(out=ors[:], in_=osd[:]), reads=[osd], writes=[ors])
                S.op("vector", lambda e: e.tensor_tensor(out=v3(otmp[:], 128), in0=v3(otmp[:], 128), in1=bc_h(ors[:, :], 128), op=ALU.mult),
                     reads=[otmp, ors], writes=[otmp])
                S.op("gpsimd", lambda e: e.tensor_tensor(out=v3(otmp[:], 128), in0=v3(otmp[:], 128),
                                                         in1=gno[0:64, :].unsqueeze(1).to_broadcast([64, 4, 128]), op=ALU.mult),
                     reads=[otmp, gno], writes=[otmp])
                yo = yout[c % 2]
                S.op("vector", lambda e: e.tensor_tensor(out=yo[:], in0=otmp[:], in1=sl.dz[:], op=ALU.mult),
                     reads=[otmp, sl.dz], writes=[yo])
                S.dma("gpsimd", g.ydn_s[rows], yo[:], reads=[yo], writes=[g.ydn_s])

        groups = [list(range(c0, min(NCH, c0 + GS))) for c0 in range(0, NCH, GS)]
        S.record()
        inf = par(groups[0])
        S.replay(S.stop())
        for gi_, grp in enumerate(groups):
            S.record()
            rec(grp, inf)
            rr = S.stop()
            rp = None
            if gi_ + 1 < len(groups):
                S.record()
                inf = par(groups[gi_ + 1])
                rp = S.stop()
            S.replay(rr, rp)
        S.barrier()


def phase_p_ops(g, ph):
    nc, S = g.nc, g.S
    S.record()
    if getattr(g, "p_dmacast", 1):
        for ti, src in enumerate((g.peer_u, g.peer_v)):
            dst = g.uv_s
            sv = src.ap().rearrange("(b p j) d -> b p j d", p=128, j=4)
            dv = dst.t.rearrange("(b p j) d -> b p j d", p=128, j=4)
            for b in range(getattr(g, "npb", 32)):
                S.dma("gpsimd", dv[b][:, :, ti * D:(ti + 1) * D], sv[b], writes=[dst])
        return S.stop()
    stg = [sb(g, ph, "pstg%d" % i, [128, 4096], F32) for i in range(2)]
    cst = [sb(g, ph, "pcst%d" % i, [128, 4096], BF16) for i in range(2)]
    k = 0
    for ti, src in enumerate((g.peer_u, g.peer_v)):
        dst = g.uv_s
        sv = src.ap().rearrange("(b p j) d -> b p (j d)", p=128, j=4)
        dv = dst.t.rearrange("(b p j) d -> b p j d", p=128, j=4)
        for b in range(getattr(g, "npb", 32)):
            s_, c_ = stg[k % 2], cst[k % 2]
            pq = "gpsimd" if getattr(g, "p_in_b", False) else "sync"
            S.dma(pq, s_[:], sv[b], writes=[s_])
            S.op("gpsimd", lambda e: e.tensor_copy(out=c_[:], in_=s_[:]), reads=[s_], writes=[c_])
            S.dma(pq, dv[b][:, :, ti * D:(ti + 1) * D], c_[:].rearrange("p (j d) -> p j d", j=4), reads=[c_], writes=[dst])
            k += 1
    return S.stop()


def phase_p(g):
    nc, S = g.nc, g.S
    with contextlib.ExitStack() as ph:
        S.replay(phase_p_ops(g, ph))
        S.barrier()


def phase_de(g):
    nc, S = g.nc, g.S
    g.nrot = 8
    with contextlib.ExitStack() as ph:
        def A(name, shape, dt=F32):
            return sb(g, ph, name, shape, dt)

        wA = A("wA", [128, 4, D], BF16)
        wB = A("wB", [128, 4, D], BF16)
        wo = A("wo", [128, 8, D], BF16)
        wq = A("wq", [128, 8, 2048], BF16)
        cand = A("cand", [128, 8, 256])
        candf = cand[:].rearrange("p a b -> p (a b)")

        class _V:
            def __init__(self, ap, tl):
                self.ap, self.r = ap, tl.r

            def __getitem__(self, idx):
                return self.ap[idx]
        wstg = [_V(candf[:, i * 512:(i + 1) * 512], cand) for i in range(2)]
        k = 0
        for (src, dstt, nk, ncol) in ((g.w_att_branch, wA, 4, D), (g.w_dn_branch, wB, 4, D), (g.w_o, wo, 8, D),
                                      (g.peer_w_query, wq, 8, 2048)):
            sv = src.ap().rearrange("(kc p) n -> p kc n", p=128)
            for kc in range(nk):
                for c0 in range(0, ncol, 512):
                    s_ = wstg[k % 2]
                    S.dma("sync", s_[:], sv[:, kc, c0:c0 + 512], writes=[s_])
                    eng = ("vector", "gpsimd", "scalar")[k % 3]
                    dst = dstt[:, kc, c0:c0 + 512]
                    if eng == "scalar":
                        S.op(eng, lambda e: e.copy(out=dst, in_=s_[:]), reads=[s_], writes=[dstt])
                    else:
                        S.op(eng, lambda e: e.tensor_copy(out=dst, in_=s_[:]), reads=[s_], writes=[dstt])
                    k += 1
        g2_bc = A("g2_bc", [128, D])
        S.dma("sync", g2_bc[:], g.norm2_gain.ap().partition_broadcast(128), writes=[g2_bc])
        skT = A("skT", [128, 16, 128])
        for hp in range(16):
            s_ = wstg[hp % 2]
            S.dma("sync", s_[:, 0:128], g.peer_sub_keys.ap()[hp], writes=[s_])
            b = next_bank(g)
            S.op("tensor", lambda e: e.transpose(out=b[:, 0:128], in_=s_[:, 0:128], identity=g.ident_f[:]),
                 reads=[s_, g.ident_f], writes=[b])
            S.op("vector", lambda e: e.tensor_copy(out=skT[:, hp, :], in_=b[:, 0:128]), reads=[b], writes=[skT])
        iota_i = A("iota_i", [128, 16], I32)
        iota_f = A("iota_f", [128, 16])
        S.op("gpsimd", lambda e: e.iota(out=iota_i[:], pattern=[[1, 16]], base=0, channel_multiplier=0), writes=[iota_i])
        S.op("vector", lambda e: e.tensor_copy(out=iota_f[:], in_=iota_i[:]), reads=[iota_i], writes=[iota_f])

        ya = A("ya", [128, 512], BF16); yd = A("yd", [128, 512], BF16)
        gt = A("gt", [128, 2048], BF16)
        xt = A("xt", [128, D])
        yT = A("yT", [128, 8, 128], BF16)
        mg = A("mg", [128, D], BF16)
        mgT = A("mgT", [128, 8, 128], BF16)
        x1s = [A("x1_%d" % i, [128, D]) for i in range(2)]
        junk = A("junkd", [128, D], BF16)
        ss1 = A("ss1", [128, 1]); sd1 = A("sd1", [128, 1]); rs1 = A("rs1", [128, 1])
        h2s = [A("h2_%d" % i, [128, D], BF16) for i in range(2)]
        class _Alias:
            def __init__(self, tl, name):
                self.t, self.r = tl.t, Res(name)

            def __getitem__(self, idx):
                return self.t[idx]
        junk2 = _Alias(junk, "junk2")
        h2T = A("h2T", [128, 8, 128], BF16)
        qTp = A("qTp", [128, 16, 128])
        s_sb = A("s_sb", [128, 16, 128])
        s2 = A("s2", [128, 256])
        m16 = A("m16", [128, 16, 16])
        i16 = A("i16", [128, 16, 16], U32)
        best = A("best", [128, 8, 16])
        pos = A("pos", [128, 8, 16], U32)
        au = A("au", [128, 128], U32); bu = A("bu", [128, 128], U32)
        af = A("af", [128, 128]); bf = A("bf", [128, 128])
        i16f = A("i16f", [128, 16, 16])
        oh = A("oh", [128, 128, 16])
        ohf = oh[:].rearrange("p a b -> p (a b)")
        e0 = A("e0", [128, 128]); e1 = A("e1", [128, 128])
        eidxs = [A("eidx%d" % i, [128, 128], U32) for i in range(2)]
        gd = A("gd", [128, 8, 16]); gsum = A("gsum", [128, 8]); grec = A("grec", [128, 8])
        gates = [A("gate%d" % i, [128, 128]) for i in range(2)]
        act = A("act", [128, 128]); coef = A("coef", [128, 128])
        NBc = getattr(g, "nbc", 11)
        uv = [A("uv%d" % i, [128, 2 * D], BF16) for i in range(NBc)]
        prod = [A("prod%d" % i, [128, D], BF16) for i in range(2)]
        dgs = [A("dgs%d" % i, [128, 128], BF16) for i in range(4)]
        act1 = [A("act1_%d" % i, [128, 1]) for i in range(8)]
        ag1 = [A("ag1_%d" % i, [128, 1]) for i in range(8)]
        acc = A("acc", [128, D])
        g.nrot = 6
        pacc = g.banks[6:8]
        print("phase DE sbuf remaining", nc.sbuf_bytes_remaining)
        x_v = g.x.ap().rearrange("(n p) d -> n p d", p=128)
        o_v = g.out.ap().rearrange("(n p) d -> n p d", p=128)
        NTD = getattr(g, "ntd", NT)
        def stage_x(tt):
            rows = slice(tt * 128, (tt + 1) * 128)
            x1, h2, eidx, gate = x1s[tt % 2], h2s[tt % 2], eidxs[tt % 2], gates[tt % 2]
            S.dma("sync", ya[:], g.yatt_s[rows], reads=[g.yatt_s], writes=[ya])
            S.dma("sync", yd[:], g.ydn_s[rows], reads=[g.ydn_s], writes=[yd])
            S.dma("sync", gt[:], g.gate_s[rows], reads=[g.gate_s], writes=[gt])
            S.dma("sync", xt[:], x_v[tt], writes=[xt])
            b = next_bank(g)
            bv = b[:, :].bitcast(BF16)
            for j in range(4):
                S.op("tensor", lambda e: e.transpose(out=bv[:, j * 128:(j + 1) * 128], in_=ya[:, j * 128:(j + 1) * 128], identity=g.ident_b[:]),
                     reads=[ya, g.ident_b], writes=[b])
            for j in range(4):
                S.op("tensor", lambda e: e.transpose(out=bv[:, (4 + j) * 128:(5 + j) * 128], in_=yd[:, j * 128:(j + 1) * 128], identity=g.ident_b[:]),
                     reads=[yd, g.ident_b], writes=[b])
            S.op("scalar", lambda e: e.copy(out=yT[:].rearrange("p k t -> p (k t)"), in_=bv[:, 0:1024]), reads=[b], writes=[yT])
            for hf in range(2):
                cs = slice(hf * 512, (hf + 1) * 512)
                bA = next_bank(g)
                for kc in range(4):
                    S.op("tensor", lambda e: e.matmul(out=bA[:, 0:512], lhsT=yT[:, kc, :], rhs=wA[:, kc, cs], start=(kc == 0), stop=(kc == 3)),
                         reads=[yT, wA], writes=[bA])
                bB = next_bank(g)
                for kc in range(4):
                    S.op("tensor", lambda e: e.matmul(out=bB[:, 0:512], lhsT=yT[:, 4 + kc, :], rhs=wB[:, kc, cs], start=(kc == 0), stop=(kc == 3)),
                         reads=[yT, wB], writes=[bB])
                S.op("vector", lambda e: e.tensor_tensor(out=ohf[:, cs], in0=bA[:, 0:512], in1=gt[:, cs], op=ALU.mult),
                     reads=[bA, gt], writes=[oh])
                S.op("vector", lambda e: e.tensor_tensor(out=ohf[:, 1024 + hf * 512:1024 + (hf + 1) * 512], in0=bB[:, 0:512], in1=gt[:, 1024 + hf * 512:1024 + (hf + 1) * 512], op=ALU.mult),
                     reads=[bB, gt], writes=[oh])
            S.op("vector", lambda e: e.tensor_tensor(out=mg[:], in0=ohf[:, 0:1024], in1=ohf[:, 1024:2048], op=ALU.add), reads=[oh], writes=[mg])
            for half in range(2):
                b = next_bank(g)
                bv = b[:, :].bitcast(BF16)
                for j in range(4):
                    kc = half * 4 + j
                    S.op("tensor", lambda e: e.transpose(out=bv[:, j * 128:(j + 1) * 128], in_=mg[:, kc * 128:(kc + 1) * 128], identity=g.ident_b[:]),
                         reads=[mg, g.ident_b], writes=[b])
                S.op("scalar", lambda e: e.copy(out=mgT[:, half * 4:half * 4 + 4, :].rearrange("p k t -> p (k t)"), in_=bv[:, 0:512]),
                     reads=[b], writes=[mgT])
            for hf in range(2):
                cs = slice(hf * 512, (hf + 1) * 512)
                b = next_bank(g)
                for kc in range(8):
                    S.op("tensor", lambda e: e.matmul(out=b[:, 0:512], lhsT=mgT[:, kc, :], rhs=wo[:, kc, cs], start=(kc == 0), stop=(kc == 7)),
                         reads=[mgT, wo], writes=[b])
                S.op("vector", lambda e: e.tensor_tensor(out=x1[:, cs], in0=b[:, 0:512], in1=xt[:, cs], op=ALU.add),
                     reads=[b, xt], writes=[x1])
            S.op("scalar", lambda e: e.activation(out=junk[:], in_=x1[:], func=AF.Square, accum_out=ss1[:, 0:1]),
                 reads=[x1], writes=[junk, ss1])
            rstd_op(g, rs1, ss1, 1.0 / D, 1, sd1)
            S.op("vector", lambda e: e.scalar_tensor_tensor(out=h2[:], in0=x1[:], scalar=rs1[:, 0:1], in1=g2_bc[:], op0=ALU.mult, op1=ALU.mult),
                 reads=[x1, rs1, g2_bc], writes=[h2])
            for half in range(2):
                b = next_bank(g)
                bv = b[:, :].bitcast(BF16)
                for j in range(4):
                    kc = half * 4 + j
                    S.op("tensor", lambda e: e.transpose(out=bv[:, j * 128:(j + 1) * 128], in_=h2[:, kc * 128:(kc + 1) * 128], identity=g.ident_b[:]),
                         reads=[h2, g.ident_b], writes=[b])
                S.op("scalar", lambda e: e.copy(out=h2T[:, half * 4:half * 4 + 4, :].rearrange("p k t -> p (k t)"), in_=bv[:, 0:512]),
                     reads=[b], writes=[h2T])
            for q4 in range(4):
                b = next_bank(g)
                for j in range(4):
                    hp = q4 * 4 + j
                    for kc in range(8):
                        S.op("tensor", lambda e: e.matmul(out=b[:, j * 128:(j + 1) * 128], lhsT=wq[:, kc, hp * 128:(hp + 1) * 128],
                                                          rhs=h2T[:, kc, :], start=(j == 0 and kc == 0), stop=(kc == 7), skip_group_check=True),
                             reads=[wq, h2T], writes=[b])
                dst = qTp[:, q4 * 4:q4 * 4 + 4, :].rearrange("p a t -> p (a t)")
                if q4 % 2 == 0:
                    S.op("scalar", lambda e: e.copy(out=dst, in_=b[:, 0:512]), reads=[b], writes=[qTp])
                else:
                    S.op("vector", lambda e: e.tensor_copy(out=dst, in_=b[:, 0:512]), reads=[b], writes=[qTp])
            for q4 in range(4):
                b = next_bank(g)
                for j in range(4):
                    hp = q4 * 4 + j
                    S.op("tensor", lambda e: e.matmul(out=b[:, j * 128:(j + 1) * 128], lhsT=qTp[:, hp, :], rhs=skT[:, hp, :],
                                                      start=(j == 0), stop=True, skip_group_check=True),
                         reads=[qTp, skT], writes=[b])
                S.op("scalar", lambda e: e.copy(out=s_sb[:, q4 * 4:q4 * 4 + 4, :].rearrange("p a t -> p (a t)"), in_=b[:, 0:512]),
                     reads=[b], writes=[s_sb])
            for hp in range(16):
                sv = s_sb[:, hp, :]
                S.op("vector", lambda e: e.max(out=m16[:, hp, 0:8], in_=sv), reads=[s_sb], writes=[m16])
                S.op("vector", lambda e: e.max_index(out=i16[:, hp, 0:8], in_max=m16[:, hp, 0:8], in_values=sv), reads=[s_sb, m16], writes=[i16])
                S.op("vector", lambda e: e.match_replace(out=s2[:, 0:128], in_to_replace=m16[:, hp, 0:8], in_values=sv, imm_value=-1e30),
                     reads=[s_sb, m16], writes=[s2])
                S.op("vector", lambda e: e.max(out=m16[:, hp, 8:16], in_=s2[:, 0:128]), reads=[s2], writes=[m16])
                S.op("vector", lambda e: e.max_index(out=i16[:, hp, 8:16], in_max=m16[:, hp, 8:16], in_values=s2[:, 0:128]),
                     reads=[s2, m16], writes=[i16])
            m16v = m16[:].rearrange("p (h two) a -> p h two a", two=2)
            S.op("vector", lambda e: e.tensor_tensor(out=cand[:].rearrange("p h (a b) -> p h a b", b=16),
                                                     in0=m16v[:, :, 0, :].unsqueeze(3).to_broadcast([128, 8, 16, 16]),
                                                     in1=m16v[:, :, 1, :].unsqueeze(2).to_broadcast([128, 8, 16, 16]), op=ALU.add),
                 reads=[m16], writes=[cand])
            for h in range(8):
                cv = cand[:, h, :]
                S.op("vector", lambda e: e.max(out=best[:, h, 0:8], in_=cv), reads=[cand], writes=[best])
                S.op("vector", lambda e: e.max_index(out=pos[:, h, 0:8], in_max=best[:, h, 0:8], in_values=cv), reads=[cand, best], writes=[pos])
                S.op("vector", lambda e: e.match_replace(out=s2[:], in_to_replace=best[:, h, 0:8], in_values=cv, imm_value=-1e30),
                     reads=[cand, best], writes=[s2])
                S.op("vector", lambda e: e.max(out=best[:, h, 8:16], in_=s2[:]), reads=[s2], writes=[best])
                S.op("vector", lambda e: e.max_index(out=pos[:, h, 8:16], in_max=best[:, h, 8:16], in_values=s2[:]), reads=[s2, best], writes=[pos])
            posf = pos[:].rearrange("p h k -> p (h k)")
            S.op("vector", lambda e: e.tensor_scalar(out=au[:], in0=posf, scalar1=4, scalar2=None, op0=ALU.logical_shift_right), reads=[pos], writes=[au])
            S.op("vector", lambda e: e.tensor_scalar(out=bu[:], in0=posf, scalar1=15, scalar2=None, op0=ALU.bitwise_and), reads=[pos], writes=[bu])
            S.op("vector", lambda e: e.tensor_copy(out=af[:], in_=au[:]), reads=[au], writes=[af])
            S.op("vector", lambda e: e.tensor_copy(out=bf[:], in_=bu[:]), reads=[bu], writes=[bf])
            S.op("vector", lambda e: e.tensor_copy(out=i16f[:], in_=i16[:]), reads=[i16], writes=[i16f])
            i16fv = i16f[:].rearrange("p (h two) a -> p h two a", two=2)
            for which, (sel, dst) in enumerate(((af, e0), (bf, e1))):
                S.op("vector", lambda e: e.tensor_tensor(out=oh[:], in0=sel[:, :].unsqueeze(2).to_broadcast([128, 128, 16]),
                                                         in1=iota_f[:, :].unsqueeze(1).to_broadcast([128, 128, 16]), op=ALU.is_equal),
                     reads=[sel, iota_f], writes=[oh])
                S.op("vector", lambda e: e.tensor_tensor(out=oh[:].rearrange("p (h k) a -> p h k a", k=16),
                                                         in0=oh[:].rearrange("p (h k) a -> p h k a", k=16),
                                                         in1=i16fv[:, :, which, :].unsqueeze(2).to_broadcast([128, 8, 16, 16]), op=ALU.mult),
                     reads=[oh, i16f], writes=[oh])
                S.op("vector", lambda e: e.tensor_reduce(out=dst[:], in_=oh[:], axis=AX.X, op=ALU.add), reads=[oh], writes=[dst])
            S.op("vector", lambda e: e.scalar_tensor_tensor(out=e0[:], in0=e0[:], scalar=128.0, in1=e1[:], op0=ALU.mult, op1=ALU.add),
                 reads=[e0, e1], writes=[e0])
            S.op("vector", lambda e: e.tensor_copy(out=eidx[:], in_=e0[:]), reads=[e0], writes=[eidx])
            S.op("vector", lambda e: e.tensor_tensor(out=gd[:], in0=best[:], in1=best[:, :, 0:1].to_broadcast([128, 8, 16]), op=ALU.subtract),
                 reads=[best], writes=[gd])
            S.op("scalar", lambda e: e.activation(out=gd[:], in_=gd[:], func=AF.Exp), reads=[gd], writes=[gd])
            S.op("vector", lambda e: e.tensor_reduce(out=gsum[:], in_=gd[:], axis=AX.X, op=ALU.add), reads=[gd], writes=[gsum])
            S.op("vector", lambda e: e.reciprocal(out=grec[:], in_=gsum[:]), reads=[gsum], writes=[grec])
            S.op("vector", lambda e: e.tensor_tensor(out=gate[:].rearrange("p (h k) -> p h k", k=16), in0=gd[:],
                                                     in1=grec[:, :].unsqueeze(2).to_broadcast([128, 8, 16]), op=ALU.mult),
                 reads=[gd, grec], writes=[gate])

        def stage_y(tt):
            x1, h2, eidx, gate = x1s[tt % 2], h2s[tt % 2], eidxs[tt % 2], gates[tt % 2]
            NSL = getattr(g, "ngrp", 16) * 8
            LAG = getattr(g, "lag", 4)

            def front(j):
                ub = uv[j % NBc]
                S.dma("gpsimd", ub[:], g.uv_s[:, :], reads=[eidx, g.uv_s], writes=[ub],
                      indirect=bass.IndirectOffsetOnAxis(ap=eidx[:, j:j + 1], axis=0))
                pr = prod[j % 2]
                a1 = act1[j % 8]
                a2 = ag1[j % 8]
                S.op("vector", lambda e: e.tensor_tensor(out=pr[:], in0=ub[:, 0:D], in1=h2[:], op=ALU.mult), reads=[ub, h2], writes=[pr])
                S.op("scalar", lambda e: e.activation(out=junk2[:], in_=pr[:], func=AF.Identity, accum_out=a1[:, 0:1]),
                     reads=[pr], writes=[junk2, a1])
                S.op("scalar", lambda e: e.activation(out=a2[:], in_=a1[:], func=AF.Gelu), reads=[a1], writes=[a2])

            def back(j):
                ub = uv[j % NBc]
                a2 = ag1[j % 8]
                dg = dgs[j % 4]
                S.op("vector", lambda e: e.scalar_tensor_tensor(out=dg[:], in0=g.ident_b[:], scalar=a2[:, 0:1],
                                                                in1=gate[:, j:j + 1].to_broadcast([128, 128]), op0=ALU.mult, op1=ALU.mult),
                     reads=[g.ident_b, a2, gate], writes=[dg])
                for hf in range(2):
                    S.op("tensor", lambda e: e.matmul(out=pacc[hf][:, 0:512], lhsT=dg[:], rhs=ub[:, D + hf * 512:D + (hf + 1) * 512],
                                                      start=(j == 0), stop=(j == NSL - 1)),
                         reads=[dg, ub], writes=[pacc[hf]])

            for j in range(NSL + LAG):
                if j < NSL:
                    front(j)
                if j >= LAG:
                    back(j - LAG)
            for hf in range(2):
                cs = slice(hf * 512, (hf + 1) * 512)
                S.op("vector", lambda e: e.tensor_tensor(out=acc[:, cs], in0=pacc[hf][:, 0:512], in1=x1[:, cs], op=ALU.add),
                     reads=[pacc[hf], x1], writes=[acc])
            S.dma("sync", o_v[tt], acc[:], reads=[acc])

        S.record()
        stage_x(0)
        S.replay(S.stop())
        for tt in range(NTD):
            S.record()
            stage_y(tt)
            ry = S.stop()
            rx = None
            if tt + 1 < NTD:
                S.record()
                stage_x(tt + 1)
                rx = S.stop()
            S.replay(ry, rx)
        S.barrier()


_CACHE = {}


def kernel(**inputs):
    x = np.asarray(inputs["x"], dtype=np.float32)
    if "nc" not in _CACHE:
        _CACHE["nc"] = build_program()
    nc = _CACHE["nc"]
    shared = {}
    for k in ("norm1_gain", "w_in", "q_norm_gain", "k_norm_gain", "dn_conv_w", "dn_a_log", "dn_dt_bias",
              "dn_out_norm_gain", "w_att_branch", "w_dn_branch", "w_o", "norm2_gain", "peer_w_query",
              "peer_u", "peer_v"):
        a = np.asarray(inputs[k], dtype=np.float32)[0]
        if a.ndim == 1:
            a = a.reshape(1, -1)
        shared[k] = np.ascontiguousarray(a)
    shared["peer_sub_keys"] = np.ascontiguousarray(
        np.asarray(inputs["peer_sub_keys"], dtype=np.float32)[0].reshape(16, 128, 128))
    in_maps = []
    for c in range(N_CORES):
        m = dict(shared)
        m["x"] = np.ascontiguousarray(x[c])
        in_maps.append(m)
    res = run_bass_kernel_spmd(nc, in_maps, core_ids=list(range(N_CORES)))
    out = np.stack([np.asarray(r["out"]) for r in res.results], axis=0)
    return out.astype(np.float32)
```

```python
import contextlib
import numpy as np
import concourse.bass as bass
import concourse.mybir as mybir
from concourse.bass_utils import run_bass_kernel_spmd

F32 = mybir.dt.float32
BF16 = mybir.dt.bfloat16
I32 = mybir.dt.int32
U32 = mybir.dt.uint32
AF = mybir.ActivationFunctionType
ALU = mybir.AluOpType
AX = mybir.AxisListType

T = 4096
D = 1024
NT = T // 128
IN_W = 5456
EPS = 1e-6
N_CORES = 8

C_AQ, C_AK, C_AV, C_IQ, C_IK, C_IW = 0, 512, 640, 768, 1280, 1344
C_DQ, C_DK, C_DV, C_DZ, C_DA, C_DB, C_GA, C_GB = 1352, 1864, 2376, 2888, 3400, 3404, 3408, 4432


class Res:
    __slots__ = ("name", "writer", "readers")

    def __init__(self, name):
        self.name = name
        self.writer = None
        self.readers = {}


class Tl:
    def __init__(self, t, name):
        self.t = t.ap() if hasattr(t, "ap") and "DRam" in type(t).__name__ else t
        self.r = Res(name)

    def __getitem__(self, idx):
        return self.t[idx]


class _RecEng:
    def __getattr__(self, name):
        def f(*args, **kw):
            self.__dict__["name"] = name
            self.__dict__["args"] = args
            self.__dict__["kw"] = kw
            return None
        return f


class Sched:
    CE = ("tensor", "vector", "scalar", "gpsimd")

    def __init__(self, nc, st, n_dma=12):
        self.nc = nc
        self.st = st
        self.eng = {n: getattr(nc, n) for n in self.CE + ("sync",)}
        self.sem = {}
        self.cnt = {}
        for n in self.CE:
            self.sem[n] = st.enter_context(nc.semaphore("s_" + n))
            self.cnt[n] = 0
        self.dq = {}
        for q in ("sync", "gpsimd"):
            sems = [st.enter_context(nc.semaphore("d_%s_%d" % (q, i))) for i in range(n_dma)]
            for i, s in enumerate(sems):
                self.sem[(q, i)] = s
                self.cnt[(q, i)] = 0
            self.dq[q] = [0, n_dma]
        self.seen = {}
        self.ninst = 0
        self.rec = None

    def record(self):
        self.rec = []

    def stop(self):
        r = self.rec
        self.rec = None
        return r

    def replay(self, *lists):
        lists = [l for l in lists if l]
        items = []
        for li, l in enumerate(lists):
            n = len(l)
            for i, it in enumerate(l):
                items.append(((i + 0.5) / n, li, i, it))
        items.sort(key=lambda t: (t[0], t[1], t[2]))
        for _, _, _, it in items:
            if it[0] == "op":
                _, E, name, args, kw, reads, writes = it
                self.op(E, lambda e: getattr(e, name)(*args, **kw), reads, writes)
            else:
                _, q, out, in_, reads, writes, indirect, kw = it
                self.dma(q, out, in_, reads, writes, indirect, **kw)

    def _wait(self, E, key, val):
        if val <= 0:
            return
        if E == "tensor" and key == "tensor":
            return
        k = (E, key)
        if self.seen.get(k, 0) >= val:
            return
        self.eng[E].wait_ge(self.sem[key], val)
        self.seen[k] = val
        self.ninst += 1

    def _deps(self, E, reads, writes):
        for r in reads:
            r = getattr(r, "r", r)
            if r.writer is not None:
                self._wait(E, *r.writer)
        for w in writes:
            w = getattr(w, "r", w)
            if w.writer is not None:
                self._wait(E, *w.writer)
            for key, val in w.readers.items():
                self._wait(E, key, val)

    def _mark(self, ev, reads, writes):
        key, val = ev
        for r in reads:
            r = getattr(r, "r", r)
            r.readers[key] = val
        for w in writes:
            w = getattr(w, "r", w)
            w.writer = ev
            w.readers = {}

    def op(self, E, emit, reads=(), writes=()):
        if self.rec is not None:
            r = _RecEng()
            emit(r)
            self.rec.append(("op", E, r.name, r.args, r.kw, list(reads), list(writes)))
            return
        self._deps(E, reads, writes)
        inst = emit(self.eng[E])
        self.cnt[E] += 1
        inst.then_inc(self.sem[E], 1)
        self.ninst += 1
        self._mark((E, self.cnt[E]), reads, writes)

    def dma(self, q, out, in_, reads=(), writes=(), indirect=None, **kw):
        if self.rec is not None:
            self.rec.append(("dma", q, out, in_, list(reads), list(writes), indirect, kw))
            return
        st = self.dq[q]
        i = st[0]
        st[0] = (i + 1) % st[1]
        key = (q, i)
        self._wait(q, key, self.cnt[key])
        self._deps(q, reads, writes)
        if indirect is not None:
            inst = self.eng[q].indirect_dma_start(out=out, out_offset=None, in_=in_, in_offset=indirect, **kw)
        else:
            inst = self.eng[q].dma_start(out=out, in_=in_, **kw)
        self.cnt[key] += 16
        inst.then_inc(self.sem[key], 16)
        self.ninst += 1
        self._mark((key, self.cnt[key]), reads, writes)

    def barrier(self):
        for E in self.CE + ("sync",):
            for key, val in self.cnt.items():
                if key == E:
                    continue
                if E == "tensor" and key == "tensor":
                    continue
                self._wait(E, key, val)

    def finish(self):
        for E in self.CE + ("sync",):
            for key, val in self.cnt.items():
                if key == E:
                    continue
                k = (E, key)
                if val > 0 and self.seen.get(k, 0) < val:
                    self.eng[E].wait_ge(self.sem[key], val)
                    self.seen[k] = val


class Ctx:
    pass


def build_program(debug=False, phases=("A", "B", "C", "P", "D"), **opts):
    nc = bass.Bass("TRN2", target_bir_lowering=False)
    g = Ctx()
    for k_, v_ in opts.items():
        setattr(g, k_, v_)
    g.nc = nc
    g.debug = debug

    def din(name, shape, dt=F32):
        return nc.dram_tensor(name, list(shape), dt, kind="ExternalInput")

    g.x = din("x", [T, D])
    g.norm1_gain = din("norm1_gain", [1, D])
    g.w_in = din("w_in", [D, IN_W])
    g.q_norm_gain = din("q_norm_gain", [1, 64])
    g.k_norm_gain = din("k_norm_gain", [1, 64])
    g.dn_conv_w = din("dn_conv_w", [4, 1536])
    g.dn_a_log = din("dn_a_log", [1, 4])
    g.dn_dt_bias = din("dn_dt_bias", [1, 4])
    g.dn_out_norm_gain = din("dn_out_norm_gain", [1, 128])
    g.w_att_branch = din("w_att_branch", [512, D])
    g.w_dn_branch = din("w_dn_branch", [512, D])
    g.w_o = din("w_o", [D, D])
    g.norm2_gain = din("norm2_gain", [1, D])
    g.peer_w_query = din("peer_w_query", [D, 2048])
    g.peer_sub_keys = din("peer_sub_keys", [16, 128, 128])
    g.peer_u = din("peer_u", [16384, D])
    g.peer_v = din("peer_v", [16384, D])
    g.out = nc.dram_tensor("out", [T, D], F32, kind="ExternalOutput")

    skind = "ExternalOutput" if debug else "Internal"

    def dscr(name, shape, dt):
        return Tl(nc.dram_tensor(name, list(shape), dt, kind=skind), name)

    g.qT_s = dscr("qT_s", [NT, 64, 8, 128], BF16)
    g.iqT_s = dscr("iqT_s", [NT, 64, 8, 128], BF16)
    g.kT_s = dscr("kT_s", [64, 2, T], BF16)
    g.ikT_s = dscr("ikT_s", [64, T], BF16)
    g.v_s = dscr("v_s", [T, 128], BF16)
    g.iw_s = dscr("iw_s", [128, NT, 8], F32)
    g.gb_s = dscr("gb_s", [128, NT, 8], F32)
    g.dnq_s = dscr("dnq_s", [T, 512], F32)
    g.dnk_s = dscr("dnk_s", [T, 512], F32)
    g.dnv_s = dscr("dnv_s", [T, 512], F32)
    g.dz_s = dscr("dz_s", [T, 512], BF16)
    g.gate_s = dscr("gate_s", [T, 2048], BF16)
    g.yatt_s = dscr("yatt_s", [T, 512], BF16)
    g.ydn_s = dscr("ydn_s", [T, 512], BF16)
    g.uv_s = Tl(nc.dram_tensor("uv_s", [16384, 2 * D], BF16, kind="Internal"), "uv_s")

    with contextlib.ExitStack() as st:
        S = Sched(nc, st)
        g.S = S
        g.st = st
        g.banks = [Tl(st.enter_context(nc.psum_tensor("bank%d" % i, [128, 512], F32)), "bank%d" % i)
                   for i in range(8)]
        g.bank_rr = 0
        g.pool_rr = {}
        setup_consts(g)
        if "A" in phases:
            phase_a(g)
        g.p_in_b = ("P" in phases and "B" in phases and not getattr(g, "p_sep", 0))
        if "B" in phases:
            phase_b(g)
        if "C" in phases:
            phase_c(g)
        if "P" in phases and not g.p_in_b:
            phase_p(g)
        if "D" in phases:
            phase_de(g)
        S.finish()
    return nc


def next_bank(g, pool=None):
    if pool is not None:
        rr = g.pool_rr.get(pool, 0)
        g.pool_rr[pool] = rr + 1
        return g.banks[pool[rr % len(pool)]]
    n = getattr(g, "nrot", 8)
    g.bank_rr = g.bank_rr % n
    b = g.banks[g.bank_rr]
    g.bank_rr = (g.bank_rr + 1) % n
    return b


def sb(g, st, name, shape, dt):
    g.uid = getattr(g, "uid", 0) + 1
    name = "%s_%d" % (name, g.uid)
    return Tl(st.enter_context(g.nc.sbuf_tensor(name, list(shape), dt)), name)


def setup_consts(g):
    nc, S, st = g.nc, g.S, g.st
    g.fill0 = nc.gpsimd.to_reg(0.0)
    g.fillneg = nc.gpsimd.to_reg(-1e30)
    g.ones_f = sb(g, st, "ones_f", [128, 128], F32)
    g.ident_f = sb(g, st, "ident_f", [128, 128], F32)
    g.ident_b = sb(g, st, "ident_b", [128, 128], BF16)
    g.eps_t = sb(g, st, "eps_t", [128, 1], F32)
    S.op("gpsimd", lambda e: e.memset(g.ones_f[:], 1.0), writes=[g.ones_f])
    S.op("gpsimd", lambda e: e.memset(g.eps_t[:], EPS), writes=[g.eps_t])
    S.op("gpsimd", lambda e: e.affine_select(out=g.ident_f[:], in_=g.ones_f[:], pattern=[[-1, 128]],
                                             compare_op=ALU.is_equal, fill=g.fill0, base=0, channel_multiplier=1),
         reads=[g.ones_f], writes=[g.ident_f])
    S.op("vector", lambda e: e.tensor_copy(out=g.ident_b[:], in_=g.ident_f[:]), reads=[g.ident_f], writes=[g.ident_b])


def bcast_row(ap_row, n):
    return ap_row.partition_broadcast(n)


def rstd_op(g, out, ss, scale, n_free, tmp):
    S = g.S
    S.op("scalar", lambda e: e.activation(out=tmp[:, 0:n_free], in_=ss[:, 0:n_free], func=AF.Sqrt,
                                          bias=g.eps_t[:, 0:1], scale=scale),
         reads=[ss, g.eps_t], writes=[tmp])
    S.op("vector", lambda e: e.reciprocal(out=out[:, 0:n_free], in_=tmp[:, 0:n_free]), reads=[tmp], writes=[out])


def phase_a(g):
    nc, S = g.nc, g.S
    with contextlib.ExitStack() as ph:
        def A(name, shape, dt):
            return sb(g, ph, name, shape, dt)

        w_bf = A("w_bf", [128, 8, IN_W], BF16)
        WCH = 1364
        wst = [A("wst%d" % i, [128, WCH], F32) for i in range(2)]
        w_in_v = g.w_in.ap().rearrange("(kc p) n -> p kc n", p=128)
        k = 0
        for kc in range(8):
            for c in range(IN_W // WCH):
                s_ = wst[k % 2]
                S.dma("sync", s_[:], w_in_v[:, kc, c * WCH:(c + 1) * WCH], writes=[s_])
                eng = ("vector", "gpsimd", "scalar")[k % 3]
                dst = w_bf[:, kc, c * WCH:(c + 1) * WCH]
                if eng == "scalar":
                    S.op(eng, lambda e: e.copy(out=dst, in_=s_[:]), reads=[s_], writes=[w_bf])
                else:
                    S.op(eng, lambda e: e.tensor_copy(out=dst, in_=s_[:]), reads=[s_], writes=[w_bf])
                k += 1

        g1_bc = A("g1_bc", [128, D], F32)
        S.dma("sync", g1_bc[:], g.norm1_gain.ap().partition_broadcast(128), writes=[g1_bc])
        gq_bc = A("gq_bc", [128, 64], F32)
        gk_bc = A("gk_bc", [128, 64], F32)
        S.dma("sync", gq_bc[:], g.q_norm_gain.ap().partition_broadcast(128), writes=[gq_bc])
        S.dma("sync", gk_bc[:], g.k_norm_gain.ap().partition_broadcast(128), writes=[gk_bc])
        S.op("vector", lambda e: e.tensor_scalar(out=gq_bc[:], in0=gq_bc[:], scalar1=0.125, scalar2=None, op0=ALU.mult),
             reads=[gq_bc], writes=[gq_bc])
        cw_bc = A("cw_bc", [128, 4, 1536], F32)
        for j in range(4):
            S.dma("sync", cw_bc[:, j, :], g.dn_conv_w.ap()[j:j + 1, :].partition_broadcast(128), writes=[cw_bc])
        dtb_bc = A("dtb_bc", [128, 4], F32)
        nea_bc = A("nea_bc", [128, 4], F32)
        S.dma("sync", dtb_bc[:], g.dn_dt_bias.ap().partition_broadcast(128), writes=[dtb_bc])
        S.dma("sync", nea_bc[:], g.dn_a_log.ap().partition_broadcast(128), writes=[nea_bc])
        S.op("scalar", lambda e: e.activation(out=nea_bc[:], in_=nea_bc[:], func=AF.Exp), reads=[nea_bc], writes=[nea_bc])
        S.op("vector", lambda e: e.tensor_scalar(out=nea_bc[:], in0=nea_bc[:], scalar1=-1.0, scalar2=None, op0=ALU.mult),
             reads=[nea_bc], writes=[nea_bc])

        shf = A("shf", [128, 128], F32)
        Sh = [g.ident_b] + [A("sh%d" % d, [128, 128], BF16) for d in (1, 2, 3)]
        ShP = [None] + [A("shp%d" % d, [128, 128], BF16) for d in (1, 2, 3)]
        for d in (1, 2, 3):
            S.op("gpsimd", lambda e: e.affine_select(out=shf[:], in_=g.ones_f[:], pattern=[[-1, 128]],
                                                     compare_op=ALU.is_equal, fill=g.fill0, base=d, channel_multiplier=1),
                 reads=[g.ones_f], writes=[shf])
            S.op("vector", lambda e: e.tensor_copy(out=Sh[d][:], in_=shf[:]), reads=[shf], writes=[Sh[d]])
            S.op("gpsimd", lambda e: e.affine_select(out=shf[:], in_=g.ones_f[:], pattern=[[-1, 128]],
                                                     compare_op=ALU.is_equal, fill=g.fill0, base=d - 128, channel_multiplier=1),
                 reads=[g.ones_f], writes=[shf])
            S.op("vector", lambda e: e.tensor_copy(out=ShP[d][:], in_=shf[:]), reads=[shf], writes=[ShP[d]])

        xt = [A("xt%d" % i, [128, D], F32) for i in range(2)]
        junk = A("junk", [128, D], BF16)
        h_bf = A("h_bf", [128, D], BF16)
        hT = [A("hT%d" % i, [128, 8, 128], BF16) for i in range(2)]
        ss1 = A("ss1", [128, 1], F32)
        sd1 = A("sd1", [128, 1], F32)
        rs1 = A("rs1", [128, 1], F32)
        sq = A("sq", [128, 512], F32)
        ss8 = A("ss8", [128, 8], F32)
        sd8 = A("sd8", [128, 8], F32)
        rs8 = A("rs8", [128, 8], F32)
        tmpf = A("tmpf", [128, 512], F32)
        qn_bf = A("qn_bf", [128, 512], BF16)
        kn_bf = A("kn_bf", [128, 128], BF16)
        iq_bf = A("iq_bf", [128, 512], BF16)
        ik_bf = A("ik_bf", [128, 64], BF16)
        qT_t = [A("qT_t%d" % i, [64, 8, 128], BF16) for i in range(2)]
        iqT_t = [A("iqT_t%d" % i, [64, 8, 128], BF16) for i in range(2)]
        kT_t = [A("kT_t%d" % i, [64, 2, 128], BF16) for i in range(2)]
        ikT_t = [A("ikT_t%d" % i, [64, 128], BF16) for i in range(2)]
        v_t = [A("v_t%d" % i, [128, 128], BF16) for i in range(2)]
        iw_all = A("iw_all", [128, NT, 8], F32)
        gb_all = A("gb_all", [128, NT, 8], F32)
        ab_tmp = A("ab_tmp", [128, 4], F32)
        xw = [A("xw%d" % i, [128, 4, 1536], BF16) for i in range(2)]
        yc = [A("yc%d" % i, [128, 512], F32) for i in range(3)]
        dz_t = [A("dz_t%d" % i, [128, 512], BF16) for i in range(2)]
        gt_t = [A("gt_t%d" % i, [128, 1024], BF16) for i in range(2)]
        print("phase A sbuf remaining", nc.sbuf_bytes_remaining)

        def proj(cols, lo, hi, hTt):
            b = next_bank(g)
            n = hi - lo
            for kc in range(8):
                S.op("tensor", lambda e: e.matmul(out=b[:, 0:n], lhsT=hTt[:, kc, :], rhs=w_bf[:, kc, lo:hi],
                                                  start=(kc == 0), stop=(kc == 7)),
                     reads=[hTt, w_bf], writes=[b])
            return b

        def headnorm(ps, nh, gain_bc, out_bf):
            n = nh * 64
            S.op("scalar", lambda e: e.activation(out=sq[:, 0:n], in_=ps[:, 0:n], func=AF.Square), reads=[ps], writes=[sq])
            S.op("vector", lambda e: e.tensor_reduce(out=ss8[:, 0:nh], in_=sq[:, 0:n].rearrange("p (h d) -> p h d", d=64),
                                                     axis=AX.X, op=ALU.add), reads=[sq], writes=[ss8])
            rstd_op(g, rs8, ss8, 1.0 / 64, nh, sd8)
            S.op("vector", lambda e: e.tensor_tensor(out=tmpf[:, 0:n].rearrange("p (h d) -> p h d", d=64),
                                                     in0=ps[:, 0:n].rearrange("p (h d) -> p h d", d=64),
                                                     in1=rs8[:, 0:nh].unsqueeze(2).to_broadcast([128, nh, 64]), op=ALU.mult),
                 reads=[ps, rs8], writes=[tmpf])
            S.op("vector", lambda e: e.tensor_tensor(out=out_bf[:, 0:n].rearrange("p (h d) -> p h d", d=64),
                                                     in0=tmpf[:, 0:n].rearrange("p (h d) -> p h d", d=64),
                                                     in1=gain_bc[:, :].unsqueeze(1).to_broadcast([128, nh, 64]), op=ALU.mult),
                 reads=[tmpf, gain_bc], writes=[out_bf])

        def transpose_heads(src_bf, nh, dstT, eng):
            b = next_bank(g)
            bv = b[:, :].bitcast(BF16)
            for h in range(nh):
                S.op("tensor", lambda e: e.transpose(out=bv[0:64, h * 128:(h + 1) * 128], in_=src_bf[:, h * 64:(h + 1) * 64],
                                                     identity=g.ident_b[:]),
                     reads=[src_bf, g.ident_b], writes=[b])
            src = bv[0:64, 0:nh * 128]
            dst = dstT[:, :, :].rearrange("p h t -> p (h t)") if nh > 1 else dstT[:, :]
            if eng == "scalar":
                S.op("scalar", lambda e: e.copy(out=dst, in_=src), reads=[b], writes=[dstT])
            else:
                S.op("vector", lambda e: e.tensor_copy(out=dst, in_=src), reads=[b], writes=[dstT])

        x_v = g.x.ap().rearrange("(n p) d -> n p d", p=128)
        S.dma("sync", xt[0][:], x_v[0], writes=[xt[0]])
        NTR = getattr(g, "ntr", NT)
        SECT = getattr(g, "sect", 99)
        for tt in range(NTR):
            cur = tt % 2
            if tt + 1 < NTR:
                S.dma("sync", xt[1 - cur][:], x_v[tt + 1], writes=[xt[1 - cur]])
            x_t = xt[cur]
            hTt = hT[cur]
            rows = slice(tt * 128, (tt + 1) * 128)
            S.op("scalar", lambda e: e.activation(out=junk[:], in_=x_t[:], func=AF.Square, accum_out=ss1[:, 0:1]),
                 reads=[x_t], writes=[junk, ss1])
            rstd_op(g, rs1, ss1, 1.0 / D, 1, sd1)
            S.op("vector", lambda e: e.scalar_tensor_tensor(out=h_bf[:], in0=x_t[:], scalar=rs1[:, 0:1], in1=g1_bc[:],
                                                            op0=ALU.mult, op1=ALU.mult),
                 reads=[x_t, rs1, g1_bc], writes=[h_bf])
            for half in range(2):
                b = next_bank(g)
                bv = b[:, :].bitcast(BF16)
                for j in range(4):
                    kc = half * 4 + j
                    S.op("tensor", lambda e: e.transpose(out=bv[:, j * 128:(j + 1) * 128], in_=h_bf[:, kc * 128:(kc + 1) * 128],
                                                         identity=g.ident_b[:]),
                         reads=[h_bf, g.ident_b], writes=[b])
                dst = hTt[:, half * 4:half * 4 + 4, :].rearrange("p k t -> p (k t)")
                if half == 0:
                    S.op("scalar", lambda e: e.copy(out=dst, in_=bv[:, 0:512]), reads=[b], writes=[hTt])
                else:
                    S.op("vector", lambda e: e.tensor_copy(out=dst, in_=bv[:, 0:512]), reads=[b], writes=[hTt])

            if SECT < 1:
                continue
            ps = proj("aq", C_AQ, C_AQ + 512, hTt)
            headnorm(ps, 8, gq_bc, qn_bf)
            transpose_heads(qn_bf, 8, qT_t[cur], "scalar")
            S.dma("gpsimd", g.qT_s[tt], qT_t[cur][:], reads=[qT_t[cur]], writes=[g.qT_s])
            if SECT < 2:
                continue
            ps = proj("kv", C_AK, C_AK + 256, hTt)
            headnorm(ps, 2, gk_bc, kn_bf)
            S.op("scalar", lambda e: e.copy(out=v_t[cur][:], in_=ps[:, 128:256]), reads=[ps], writes=[v_t[cur]])
            transpose_heads(kn_bf, 2, kT_t[cur], "vector")
            S.dma("gpsimd", g.kT_s[:, :, rows], kT_t[cur][:], reads=[kT_t[cur]], writes=[g.kT_s])
            S.dma("gpsimd", g.v_s[rows], v_t[cur][:], reads=[v_t[cur]], writes=[g.v_s])
            if SECT < 3:
                continue
            ps = proj("iq", C_IQ, C_IQ + 512, hTt)
            S.op("scalar", lambda e: e.copy(out=iq_bf[:], in_=ps[:, 0:512]), reads=[ps], writes=[iq_bf])
            transpose_heads(iq_bf, 8, iqT_t[cur], "vector")
            S.dma("gpsimd", g.iqT_s[tt], iqT_t[cur][:], reads=[iqT_t[cur]], writes=[g.iqT_s])
            if SECT < 4:
                continue
            ps = proj("ikw", C_IK, C_IK + 72, hTt)
            S.op("vector", lambda e: e.tensor_copy(out=ik_bf[:], in_=ps[:, 0:64]), reads=[ps], writes=[ik_bf])
            S.op("scalar", lambda e: e.copy(out=iw_all[:, tt, :], in_=ps[:, 64:72]), reads=[ps], writes=[iw_all])
            transpose_heads(ik_bf, 1, ikT_t[cur], "scalar")
            if "ikT" not in getattr(g, "skip", ()):
                S.dma("gpsimd", g.ikT_s[:, rows], ikT_t[cur][:], reads=[ikT_t[cur]], writes=[g.ikT_s])
            if SECT < 5:
                continue
            ps = proj("ab", C_DA, C_DA + 8, hTt)
            S.op("vector", lambda e: e.tensor_tensor(out=ab_tmp[:], in0=ps[:, 0:4], in1=dtb_bc[:], op=ALU.add),
                 reads=[ps, dtb_bc], writes=[ab_tmp])
            S.op("scalar", lambda e: e.activation(out=ab_tmp[:], in_=ab_tmp[:], func=AF.Exp), reads=[ab_tmp], writes=[ab_tmp])
            S.op("scalar", lambda e: e.activation(out=ab_tmp[:], in_=ab_tmp[:], func=AF.Ln, bias=g.ones_f[:, 0:1], scale=1.0),
                 reads=[ab_tmp, g.ones_f], writes=[ab_tmp])
            S.op("vector", lambda e: e.tensor_tensor(out=gb_all[:, tt, 0:4], in0=ab_tmp[:], in1=nea_bc[:], op=ALU.mult),
                 reads=[ab_tmp, nea_bc], writes=[gb_all])
            S.op("scalar", lambda e: e.activation(out=gb_all[:, tt, 4:8], in_=ps[:, 4:8], func=AF.Sigmoid),
                 reads=[ps], writes=[gb_all])
            if SECT < 6:
                continue
            for gi, (c0, dst_s) in enumerate(((C_DQ, g.dnq_s), (C_DK, g.dnk_s), (C_DV, g.dnv_s))):
                ps = proj("dn", c0, c0 + 512, hTt)
                cs = slice(gi * 512, (gi + 1) * 512)
                for j in range(4):
                    S.op("vector", lambda e: e.tensor_tensor(out=xw[cur][:, j, cs], in0=ps[:, 0:512], in1=cw_bc[:, j, cs], op=ALU.mult),
                         reads=[ps, cw_bc], writes=[xw[cur]])
                b = next_bank(g)
                mm = []
                for j in range(4):
                    mm.append((Sh[3 - j], xw[cur], j))
                if tt > 0:
                    for j in range(3):
                        mm.append((ShP[3 - j], xw[1 - cur], j))
                for i, (sh, xsrc, j) in enumerate(mm):
                    S.op("tensor", lambda e: e.matmul(out=b[:, 0:512], lhsT=sh[:], rhs=xsrc[:, j, cs],
                                                      start=(i == 0), stop=(i == len(mm) - 1)),
                         reads=[sh, xsrc], writes=[b])
                y = yc[gi]
                S.op("scalar", lambda e: e.activation(out=y[:], in_=b[:, 0:512], func=AF.Silu), reads=[b], writes=[y])
                if gi < 2:
                    S.op("gpsimd", lambda e: e.tensor_tensor(out=sq[:], in0=y[:], in1=y[:], op=ALU.mult), reads=[y], writes=[sq])
                    S.op("vector", lambda e: e.tensor_reduce(out=ss8[:, 0:4], in_=sq[:].rearrange("p (h d) -> p h d", d=128),
                                                             axis=AX.X, op=ALU.add), reads=[sq], writes=[ss8])
                    rstd_op(g, rs8, ss8, 1.0, 4, sd8)
                    if gi == 0:
                        S.op("vector", lambda e: e.tensor_scalar(out=rs8[:, 0:4], in0=rs8[:, 0:4], scalar1=128 ** -0.5, scalar2=None,
                                                                 op0=ALU.mult), reads=[rs8], writes=[rs8])
                    S.op("vector", lambda e: e.tensor_tensor(out=y[:].rearrange("p (h d) -> p h d", d=128),
                                                             in0=y[:].rearrange("p (h d) -> p h d", d=128),
                                                             in1=rs8[:, 0:4].unsqueeze(2).to_broadcast([128, 4, 128]), op=ALU.mult),
                         reads=[y, rs8], writes=[y])
                S.dma("gpsimd", dst_s[rows], y[:], reads=[y], writes=[dst_s])
            if SECT < 7:
                continue
            ps = proj("dz", C_DZ, C_DZ + 512, hTt)
            S.op("scalar", lambda e: e.activation(out=dz_t[cur][:], in_=ps[:, 0:512], func=AF.Silu), reads=[ps], writes=[dz_t[cur]])
            S.dma("gpsimd", g.dz_s[rows], dz_t[cur][:], reads=[dz_t[cur]], writes=[g.dz_s])
            if SECT < 8:
                continue
            for gi, c0 in enumerate((C_GA, C_GB)):
                for hf in range(2):
                    ps = proj("gate", c0 + hf * 512, c0 + (hf + 1) * 512, hTt)
                    S.op("scalar", lambda e: e.activation(out=gt_t[gi][:, hf * 512:(hf + 1) * 512], in_=ps[:, 0:512], func=AF.Sigmoid),
                         reads=[ps], writes=[gt_t[gi]])
                S.dma("gpsimd", g.gate_s[rows, gi * 1024:(gi + 1) * 1024], gt_t[gi][:], reads=[gt_t[gi]], writes=[g.gate_s])
        S.dma("gpsimd", g.iw_s[:, :, :], iw_all[:], reads=[iw_all], writes=[g.iw_s])
        S.dma("gpsimd", g.gb_s[:, :, :], gb_all[:], reads=[gb_all], writes=[g.gb_s])
        S.barrier()


NIT = 15


def phase_b(g):
    nc, S = g.nc, g.S
    g.nrot = 6
    acc = g.banks[6:8]
    with contextlib.ExitStack() as ph:
        def A(name, shape, dt):
            return sb(g, ph, name, shape, dt)

        kT_all = A("kT_all", [128, 2, T], BF16)
        ikT_all = A("ikT_all", [128, T], BF16)
        S.op("gpsimd", lambda e: e.memset(kT_all[64:128, :, :], 0.0), writes=[kT_all])
        S.op("gpsimd", lambda e: e.memset(ikT_all[64:128, :], 0.0), writes=[ikT_all])
        v_raw = A("v_raw", [128, NT, 128], BF16)
        v_all = A("v_all", [128, NT, 2, 65], BF16)
        iw_all = A("iw_all", [128, NT, 8], F32)
        S.dma("sync", kT_all[0:64, :, :], g.kT_s[:, :, :], reads=[g.kT_s], writes=[kT_all])
        S.dma("sync", ikT_all[0:64, :], g.ikT_s[:, :], reads=[g.ikT_s], writes=[ikT_all])
        S.dma("sync", v_raw[:], g.v_s[:, :].rearrange("(n p) c -> p n c", p=128), reads=[g.v_s], writes=[v_raw])
        S.dma("sync", iw_all[:], g.iw_s[:, :, :], reads=[g.iw_s], writes=[iw_all])
        S.op("gpsimd", lambda e: e.memset(v_all[:], 1.0), writes=[v_all])
        S.op("vector", lambda e: e.tensor_copy(out=v_all[:, :, :, 0:64],
                                               in_=v_raw[:].rearrange("p n (g d) -> p n g d", d=64)),
             reads=[v_raw], writes=[v_all])
        thr0 = A("thr0", [128, 1], F32)
        S.op("gpsimd", lambda e: e.memset(thr0[:], -1e29), writes=[thr0])
        cmask = A("cmask", [128, 128], F32)
        S.op("gpsimd", lambda e: e.memset(cmask[:], 0.0), writes=[cmask])
        S.op("gpsimd", lambda e: e.affine_select(out=cmask[:], in_=cmask[:], pattern=[[-1, 128]], compare_op=ALU.is_ge,
                                                 fill=g.fillneg, base=0, channel_multiplier=1), reads=[cmask], writes=[cmask])

        sc = [A("sc%d" % i, [128, T], F32) for i in range(2)]
        Rb = [A("Rb%d" % i, [128, 512], F32) for i in range(2)]
        junk = A("junkb", [128, T], BF16)
        mask = A("mask", [128, T], BF16)
        maskT = [A("maskT%d" % i, [128, NT, 128], BF16) for i in range(2)]
        iqT = [A("iqT%d" % i, [128, 8, 128], BF16) for i in range(2)]
        qT = [A("qT%d" % i, [128, 8, 128], BF16) for i in range(3)]
        for t_ in iqT + qT:
            S.op("gpsimd", lambda e: e.memset(t_[64:128, :, :], 0.0), writes=[t_])
        Eb = [A("Eb%d" % i, [128, 512], BF16) for i in range(2)]
        Pb = [A("Pb%d" % i, [128, 512], BF16) for i in range(2)]
        yat = [A("yat%d" % i, [128, 512], BF16) for i in range(2)]
        rec = A("rec", [128, 4], F32)
        hi = A("hi", [128, 1], F32)
        lo = A("lo", [128, 1], F32)
        rk = A("rk", [128, 1], F32)
        mid = A("mid", [128, 1], F32)
        cnt = A("cnt", [128, 1], F32)
        step = A("step", [128, 1], F32)
        print("phase B sbuf remaining", nc.sbuf_bytes_remaining)

        NQB = getattr(g, "nqb", NT)
        pw = A("pw", [128, NIT + 1], F32)
        for it in range(NIT + 1):
            S.op("gpsimd", lambda e: e.memset(pw[:, it:it + 1], 2.0 ** -(it + 1)), writes=[pw])
        rkall = A("rkall", [128, NIT + 1], F32)
        nmid = A("nmid", [128, 1], F32)
        tq = A("tq", [128, 1], F32)
        cnt2 = A("cnt2", [128, 1], F32)
        thr_t = A("thr_t", [128, 1], F32)
        ctr = {"ke": 0}

        def stage1a(qb):
            cur = qb % 2
            S.dma("sync", iqT[cur][0:64, :, :], g.iqT_s[qb], reads=[g.iqT_s], writes=[iqT[cur]])
            S.dma("sync", qT[qb % 3][0:64, :, :], g.qT_s[qb], reads=[g.qT_s], writes=[qT[qb % 3]])
            NS = qb + 1
            SS = NS * 128
            sct = sc[cur]
            for ci in range((SS + 511) // 512):
                c0 = ci * 512
                n = min(512, SS - c0)
                for h in range(8):
                    b = next_bank(g, (0, 1))
                    S.op("tensor", lambda e: e.matmul(out=b[:, 0:n], lhsT=iqT[cur][:, h, :], rhs=ikT_all[:, c0:c0 + n],
                                                      start=True, stop=True),
                         reads=[iqT[cur], ikT_all], writes=[b])
                    R = Rb[ctr["ke"] % 2]
                    ctr["ke"] += 1
                    S.op("scalar", lambda e: e.activation(out=R[:, 0:n], in_=b[:, 0:n], func=AF.Relu), reads=[b], writes=[R])
                    if h == 0:
                        S.op("vector", lambda e: e.tensor_scalar(out=sct[:, c0:c0 + n], in0=R[:, 0:n], scalar1=iw_all[:, qb, 0:1],
                                                                 scalar2=None, op0=ALU.mult),
                             reads=[R, iw_all], writes=[sct])
                    else:
                        S.op("vector", lambda e: e.scalar_tensor_tensor(out=sct[:, c0:c0 + n], in0=R[:, 0:n],
                                                                        scalar=iw_all[:, qb, h:h + 1], in1=sct[:, c0:c0 + n],
                                                                        op0=ALU.mult, op1=ALU.add),
                             reads=[R, iw_all, sct], writes=[sct])
            dg = sct[:, qb * 128:(qb + 1) * 128]
            S.op("vector", lambda e: e.tensor_tensor(out=dg, in0=dg, in1=cmask[:], op=ALU.add), reads=[sct, cmask], writes=[sct])
        def stage1b(qb):
            cur = qb % 2
            NS = qb + 1
            SS = NS * 128
            sct = sc[cur]
            if qb >= 2:
                S.op("vector", lambda e: e.tensor_reduce(out=hi[:], in_=sct[:, 0:SS], axis=AX.X, op=ALU.max), reads=[sct], writes=[hi])
                S.op("vector", lambda e: e.tensor_reduce(out=lo[:], in_=sct[:, 0:qb * 128], axis=AX.X, op=ALU.min), reads=[sct], writes=[lo])
                S.op("vector", lambda e: e.tensor_tensor(out=rk[:], in0=hi[:], in1=lo[:], op=ALU.subtract), reads=[hi, lo], writes=[rk])
                S.op("vector", lambda e: e.tensor_tensor(out=rkall[:], in0=pw[:], in1=rk[:, 0:1].to_broadcast([128, NIT + 1]), op=ALU.mult),
                     reads=[pw, rk], writes=[rkall])
                S.op("vector", lambda e: e.tensor_scalar(out=nmid[:], in0=lo[:], scalar1=rkall[:, 0:1], scalar2=-1.0, op0=ALU.add, op1=ALU.mult),
                     reads=[lo, rkall], writes=[nmid])
                for it in range(NIT):
                    if getattr(g, "dvecnt", 0):
                        S.op("vector", lambda e: e.tensor_scalar(out=mid[:], in0=nmid[:], scalar1=-1.0, scalar2=None, op0=ALU.mult),
                             reads=[nmid], writes=[mid])
                        S.op("vector", lambda e: e.tensor_scalar(out=junk[:, 0:SS], in0=sct[:, 0:SS], scalar1=mid[:, 0:1], scalar2=None,
                                                                 op0=ALU.is_gt, op1=ALU.add, accum_out=cnt[:, 0:1]),
                             reads=[sct, mid], writes=[junk, cnt])
                        S.op("vector", lambda e: e.tensor_scalar(out=cnt2[:], in0=cnt[:], scalar1=2.0, scalar2=float(SS), op0=ALU.mult, op1=ALU.subtract),
                             reads=[cnt], writes=[cnt2])
                    else:
                        S.op("scalar", lambda e: e.activation(out=junk[:, 0:SS], in_=sct[:, 0:SS], func=AF.Sign, bias=nmid[:, 0:1], scale=1.0,
                                                              accum_out=cnt[:, 0:1]),
                             reads=[sct, nmid], writes=[junk, cnt])
                    cx = cnt2 if getattr(g, "dvecnt", 0) else cnt
                    S.op("vector", lambda e: e.tensor_scalar(out=tq[:], in0=cx[:], scalar1=511.5 - SS, scalar2=0.5, op0=ALU.is_lt, op1=ALU.subtract),
                         reads=[cx], writes=[tq])
                    S.op("vector", lambda e: e.scalar_tensor_tensor(out=nmid[:], in0=tq[:], scalar=rkall[:, it:it + 1], in1=nmid[:],
                                                                    op0=ALU.mult, op1=ALU.add), reads=[tq, rkall, nmid], writes=[nmid])
                S.op("vector", lambda e: e.tensor_scalar(out=thr_t[:], in0=nmid[:], scalar1=-1.0, scalar2=rkall[:, NIT:NIT + 1],
                                                         op0=ALU.mult, op1=ALU.subtract), reads=[nmid, rkall], writes=[thr_t])
                thr = thr_t
            else:
                thr = thr0
            S.op("vector", lambda e: e.tensor_scalar(out=mask[:, 0:SS], in0=sct[:, 0:SS], scalar1=thr[:, 0:1], scalar2=None,
                                                     op0=ALU.is_ge), reads=[sct, thr], writes=[mask])
            mT = maskT[cur]
            for b0 in range(0, NS, 8):
                nb = min(8, NS - b0)
                b = next_bank(g, (2,))
                bv = b[:, :].bitcast(BF16)
                for j in range(nb):
                    S.op("tensor", lambda e: e.transpose(out=bv[:, j * 128:(j + 1) * 128],
                                                         in_=mask[:, (b0 + j) * 128:(b0 + j + 1) * 128], identity=g.ident_b[:]),
                         reads=[mask, g.ident_b], writes=[b])
                dst = mT[:, b0:b0 + nb, :].rearrange("p n t -> p (n t)")
                S.op("gpsimd", lambda e: e.tensor_copy(out=dst, in_=bv[:, 0:nb * 128]), reads=[b], writes=[mT]) if False else \
                    S.op("vector", lambda e: e.tensor_copy(out=dst, in_=bv[:, 0:nb * 128]), reads=[b], writes=[mT])

        def stage2(qb):
            cur = qb % 2
            NS = qb + 1
            mT = maskT[cur]
            yt = yat[cur]
            for gi in range(2):
                po = acc[gi]
                for sbk in range(NS):
                    b = next_bank(g, (3, 4, 5))
                    S.op("tensor", lambda e: e.matmul(out=b[:, 0:512], lhsT=kT_all[:, gi, sbk * 128:(sbk + 1) * 128],
                                                      rhs=qT[qb % 3][:, 4 * gi:4 * gi + 4, :].rearrange("p h t -> p (h t)"),
                                                      start=True, stop=True),
                         reads=[kT_all, qT[qb % 3]], writes=[b])
                    E = Eb[ctr["ke"] % 2]
                    P = Pb[ctr["ke"] % 2]
                    ctr["ke"] += 1
                    S.op("scalar", lambda e: e.activation(out=E[:], in_=b[:, 0:512], func=AF.Exp), reads=[b], writes=[E])
                    S.op("vector", lambda e: e.tensor_tensor(out=P[:].rearrange("p (h t) -> p h t", h=4),
                                                             in0=E[:].rearrange("p (h t) -> p h t", h=4),
                                                             in1=mT[:, sbk, :].unsqueeze(1).to_broadcast([128, 4, 128]), op=ALU.mult),
                         reads=[E, mT], writes=[P])
                    for h in range(4):
                        S.op("tensor", lambda e: e.matmul(out=po[:, h * 65:(h + 1) * 65], lhsT=P[:, h * 128:(h + 1) * 128],
                                                          rhs=v_all[:, sbk, gi, :], start=(sbk == 0 and h == 0),
                                                          stop=(sbk == NS - 1), skip_group_check=True),
                             reads=[P, v_all], writes=[po])
                pov = po[:, 0:260].rearrange("p (h e) -> p h e", e=65)
                S.op("vector", lambda e: e.reciprocal(out=rec[:], in_=pov[:, :, 64]), reads=[po], writes=[rec])
                S.op("vector", lambda e: e.tensor_tensor(out=yt[:, gi * 256:(gi + 1) * 256].rearrange("p (h d) -> p h d", d=64),
                                                         in0=pov[:, :, 0:64],
                                                         in1=rec[:, :].unsqueeze(2).to_broadcast([128, 4, 64]), op=ALU.mult),
                     reads=[po, rec], writes=[yt])
            S.dma("sync", g.yatt_s[qb * 128:(qb + 1) * 128], yt[:], reads=[yt], writes=[g.yatt_s])

        plist = phase_p_ops(g, ph) if getattr(g, "p_in_b", False) else []
        def recd(fn, qb):
            if qb >= NQB:
                return None
            S.record()
            fn(qb)
            return S.stop()
        S.replay(recd(stage1a, 0))
        S.replay(recd(stage1b, 0), recd(stage1a, 1))
        for qb in range(NQB):
            pchunk = plist[(qb * len(plist)) // NQB:((qb + 1) * len(plist)) // NQB]
            r2 = recd(stage2, qb)
            r1b = recd(stage1b, qb + 1)
            r1a = recd(stage1a, qb + 2)
            if getattr(g, "noil", 0):
                for r in (r2, pchunk, r1b, r1a):
                    S.replay(r)
            else:
                S.replay(r2, r1b, r1a, pchunk)
        S.barrier()
    g.nrot = 8


def phase_c(g):
    nc, S = g.nc, g.S
    g.nrot = 8
    GS = 2
    with contextlib.ExitStack() as ph:
        def A(name, shape, dt=F32):
            return sb(g, ph, name, shape, dt)

        utri = A("utri", [64, 64])
        sel63 = A("sel63", [64, 128])
        S.op("gpsimd", lambda e: e.affine_select(out=utri[:], in_=g.ones_f[0:64, 0:64], pattern=[[1, 64]], compare_op=ALU.is_ge,
                                                 fill=g.fill0, base=0, channel_multiplier=-1), reads=[g.ones_f], writes=[utri])
        S.op("gpsimd", lambda e: e.affine_select(out=sel63[:], in_=g.ones_f[0:64, :], pattern=[[0, 128]], compare_op=ALU.is_equal,
                                                 fill=g.fill0, base=-63, channel_multiplier=1), reads=[g.ones_f], writes=[sel63])
        gno = A("gno", [128, 128])
        S.dma("sync", gno[:], g.dn_out_norm_gain.ap().partition_broadcast(128), writes=[gno])
        gbc = [A("gbc%d" % h, [64, NT, 8]) for h in range(2)]
        gn = [A("gn%d" % h, [64, NT, 8]) for h in range(2)]
        egc = [A("egc%d" % h, [64, NT, 4]) for h in range(2)]
        bg = [A("bg%d" % h, [64, NT, 4]) for h in range(2)]
        kd = [A("kd%d" % h, [64, NT, 4]) for h in range(2)]
        elast = [A("elast%d" % h, [128, NT, 4]) for h in range(2)]
        for h in range(2):
            S.dma("sync", gbc[h][:], g.gb_s[h * 64:(h + 1) * 64, :, :], reads=[g.gb_s], writes=[gbc[h]])
            b = next_bank(g)
            S.op("tensor", lambda e: e.matmul(out=b[0:64, 0:128], lhsT=utri[:], rhs=gbc[h][:, :, 0:4], start=True, stop=True),
                 reads=[utri, gbc[h]], writes=[b])
            S.op("vector", lambda e: e.tensor_copy(out=gn[h][:, :, 0:4], in_=b[0:64, 0:128].rearrange("p (n f) -> p n f", f=4)),
                 reads=[b], writes=[gn[h]])
            S.op("vector", lambda e: e.tensor_scalar(out=gn[h][:, :, 4:8], in0=gbc[h][:, :, 4:8], scalar1=-1.0, scalar2=None, op0=ALU.mult),
                 reads=[gbc[h]], writes=[gn[h]])
            S.op("scalar", lambda e: e.activation(out=egc[h][:], in_=gn[h][:, :, 0:4], func=AF.Exp), reads=[gn[h]], writes=[egc[h]])
            S.op("vector", lambda e: e.tensor_tensor(out=bg[h][:], in0=egc[h][:], in1=gbc[h][:, :, 4:8], op=ALU.mult),
                 reads=[egc[h], gbc[h]], writes=[bg[h]])
            b2 = next_bank(g)
            S.op("tensor", lambda e: e.matmul(out=b2[:, 0:128], lhsT=sel63[:], rhs=gn[h][:, :, 0:4], start=True, stop=True),
                 reads=[sel63, gn[h]], writes=[b2])
            S.op("scalar", lambda e: e.activation(out=elast[h][:], in_=b2[:, 0:128].rearrange("p (n f) -> p n f", f=4), func=AF.Exp),
                 reads=[b2], writes=[elast[h]])
            S.op("vector", lambda e: e.tensor_tensor(out=kd[h][:], in0=b2[0:64, 0:128].rearrange("p (n f) -> p n f", f=4),
                                                     in1=gn[h][:, :, 0:4], op=ALU.subtract), reads=[b2, gn[h]], writes=[kd[h]])
            S.op("scalar", lambda e: e.activation(out=kd[h][:], in_=kd[h][:], func=AF.Exp), reads=[kd[h]], writes=[kd[h]])

        Sst = A("Sst", [128, 4, 128])
        S.op("gpsimd", lambda e: e.memset(Sst[:], 0.0), writes=[Sst])

        class Slot:
            pass
        slots = []
        for i in range(2 * GS):
            s_ = Slot()
            s_.q = A("cq%d" % i, [64, 512]); s_.k = A("ck%d" % i, [64, 512]); s_.v = A("cv%d" % i, [64, 512])
            s_.dz = A("cdz%d" % i, [64, 512], BF16)
            s_.dgb = A("dgb%d" % i, [64, 512]); s_.G1 = A("G1%d" % i, [64, 256]); s_.G2 = A("G2%d" % i, [64, 256])
            s_.sel3 = A("sel3%d" % i, [64, 768]); s_.E3 = A("E3%d" % i, [64, 768])
            s_.DATn = A("DATn%d" % i, [64, 256]); s_.DAn = A("DAn%d" % i, [64, 256])
            s_.kqT = A("kqT%d" % i, [128, 512])
            s_.MM = [A("MM%d_%d" % (i, j), [64, 512]) for j in range(2)]
            s_.XT = A("XT%d" % i, [64, 256]); s_.inT = A("inT%d" % i, [64, 256])
            s_.vb = A("vb%d" % i, [64, 512]); s_.kbg = A("kbg%d" % i, [64, 512]); s_.kdec = A("kdec%d" % i, [64, 512])
            s_.u = A("u%d" % i, [64, 512]); s_.wT = A("wT%d" % i, [128, 256])
            slots.append(s_)
        vnew = A("vnew", [64, 512])
        otmp = A("otmp", [64, 512])
        osq = A("osq", [64, 512])
        oss = A("oss", [64, 4]); osd = A("osd", [64, 4]); ors = A("ors", [64, 4])
        yout = [A("yout%d" % i, [64, 512], BF16) for i in range(2)]
        print("phase C sbuf remaining", nc.sbuf_bytes_remaining)
        idb = g.ident_f[0:64, 0:64]
        NCH = getattr(g, "nch", 64)

        def bc_h(ap4, n):
            return ap4.unsqueeze(2).to_broadcast([64, 4, n])

        def v3(ap, n):
            return ap.rearrange("p (h f) -> p h f", h=4)

        def par(chunks):
            info = {}
            for c in chunks:
                sl = slots[c % (2 * GS)]
                tt, half = c // 2, c % 2
                rows = slice(c * 64, (c + 1) * 64)
                S.dma("sync", sl.q[:], g.dnq_s[rows], reads=[g.dnq_s], writes=[sl.q])
                S.dma("sync", sl.k[:], g.dnk_s[rows], reads=[g.dnk_s], writes=[sl.k])
                S.dma("sync", sl.v[:], g.dnv_s[rows], reads=[g.dnv_s], writes=[sl.v])
                S.dma("sync", sl.dz[:], g.dz_s[rows], reads=[g.dz_s], writes=[sl.dz])
                info[c] = (sl, tt, half)
            for c in chunks:
                sl, tt, half = info[c]
                gnc = gn[half][:, tt, :]
                S.op("vector", lambda e: e.tensor_tensor(out=sl.dgb[:].rearrange("p (a f) -> p a f", a=8),
                                                         in0=gnc.unsqueeze(2).to_broadcast([64, 8, 64]),
                                                         in1=idb.unsqueeze(1).to_broadcast([64, 8, 64]), op=ALU.mult),
                     reads=[gn[half], g.ident_f], writes=[sl.dgb])
                bR = next_bank(g, (0, 1, 2, 3))
                S.op("tensor", lambda e: e.matmul(out=bR[0:64, 0:512], lhsT=g.ones_f[0:64, 0:64], rhs=sl.dgb[:], start=True, stop=True),
                     reads=[g.ones_f, sl.dgb], writes=[bR])
                S.op("vector", lambda e: e.tensor_tensor(out=v3(sl.G1[:], 64), in0=v3(bR[0:64, 0:256], 64),
                                                         in1=bc_h(gn[half][:, tt, 0:4], 64), op=ALU.subtract),
                     reads=[bR, gn[half]], writes=[sl.G1])
                S.op("vector", lambda e: e.tensor_scalar(out=sl.G2[:], in0=sl.G1[:], scalar1=-1.0, scalar2=None, op0=ALU.mult),
                     reads=[sl.G1], writes=[sl.G2])
                S.op("gpsimd", lambda e: e.affine_select(out=sl.sel3[:, 0:256], in_=sl.G1[:], pattern=[[0, 4], [1, 64]],
                                                         compare_op=ALU.is_ge, fill=g.fillneg, base=0, channel_multiplier=-1),
                     reads=[sl.G1], writes=[sl.sel3])
                S.op("gpsimd", lambda e: e.affine_select(out=sl.sel3[:, 256:512], in_=sl.G1[:], pattern=[[0, 4], [1, 64]],
                                                         compare_op=ALU.is_ge, fill=g.fillneg, base=-1, channel_multiplier=-1),
                     reads=[sl.G1], writes=[sl.sel3])
                S.op("gpsimd", lambda e: e.affine_select(out=sl.sel3[:, 512:768], in_=sl.G2[:], pattern=[[0, 4], [-1, 64]],
                                                         compare_op=ALU.is_ge, fill=g.fillneg, base=-1, channel_multiplier=1),
                     reads=[sl.G2], writes=[sl.sel3])
                S.op("scalar", lambda e: e.activation(out=sl.E3[:], in_=sl.sel3[:], func=AF.Exp), reads=[sl.sel3], writes=[sl.E3])
                S.op("vector", lambda e: e.tensor_tensor(out=sl.DATn[:], in0=sl.E3[:, 256:512], in1=bR[0:64, 256:512], op=ALU.mult),
                     reads=[sl.E3, bR], writes=[sl.DATn])
                S.op("gpsimd", lambda e: e.tensor_tensor(out=v3(sl.DAn[:], 64), in0=v3(sl.E3[:, 512:768], 64),
                                                         in1=bc_h(gn[half][:, tt, 4:8], 64), op=ALU.mult),
                     reads=[sl.E3, gn[half]], writes=[sl.DAn])
                S.op("vector", lambda e: e.tensor_tensor(out=v3(sl.vb[:], 128), in0=v3(sl.v[:], 128),
                                                         in1=bc_h(gbc[half][:, tt, 4:8], 128), op=ALU.mult),
                     reads=[sl.v, gbc[half]], writes=[sl.vb])
                S.op("gpsimd", lambda e: e.tensor_tensor(out=v3(sl.kbg[:], 128), in0=v3(sl.k[:], 128),
                                                         in1=bc_h(bg[half][:, tt, :], 128), op=ALU.mult),
                     reads=[sl.k, bg[half]], writes=[sl.kbg])
                S.op("gpsimd", lambda e: e.tensor_tensor(out=v3(sl.kdec[:], 128), in0=v3(sl.k[:], 128),
                                                         in1=bc_h(kd[half][:, tt, :], 128), op=ALU.mult),
                     reads=[sl.k, kd[half]], writes=[sl.kdec])
                bT = next_bank(g, (0, 1, 2, 3))
                for hd in range(4):
                    S.op("tensor", lambda e: e.transpose(out=bT[:, hd * 64:(hd + 1) * 64], in_=sl.k[:, hd * 128:(hd + 1) * 128], identity=idb),
                         reads=[sl.k, g.ident_f], writes=[bT])
                for hd in range(4):
                    S.op("tensor", lambda e: e.transpose(out=bT[:, 256 + hd * 64:256 + (hd + 1) * 64], in_=sl.q[:, hd * 128:(hd + 1) * 128],
                                                         identity=idb), reads=[sl.q, g.ident_f], writes=[bT])
                S.op("scalar", lambda e: e.copy(out=sl.kqT[:], in_=bT[:, 0:512]), reads=[bT], writes=[sl.kqT])
                bK = next_bank(g, (0, 1, 2, 3))
                for hd in range(4):
                    kT_h = sl.kqT[:, hd * 64:(hd + 1) * 64]
                    qT_h = sl.kqT[:, 256 + hd * 64:256 + (hd + 1) * 64]
                    S.op("tensor", lambda e: e.matmul(out=bK[0:64, hd * 64:(hd + 1) * 64], lhsT=kT_h, rhs=kT_h, start=(hd == 0), stop=True,
                                                      skip_group_check=True), reads=[sl.kqT], writes=[bK])
                for hd in range(4):
                    kT_h = sl.kqT[:, hd * 64:(hd + 1) * 64]
                    qT_h = sl.kqT[:, 256 + hd * 64:256 + (hd + 1) * 64]
                    S.op("tensor", lambda e: e.matmul(out=bK[0:64, 256 + hd * 64:256 + (hd + 1) * 64], lhsT=kT_h, rhs=qT_h, start=False,
                                                      stop=True, skip_group_check=True), reads=[sl.kqT], writes=[bK])
                MM0 = sl.MM[0]
                S.op("vector", lambda e: e.tensor_tensor(out=MM0[:, 0:256], in0=bK[0:64, 0:256], in1=sl.DAn[:], op=ALU.mult),
                     reads=[bK, sl.DAn], writes=[MM0])
                S.op("vector", lambda e: e.tensor_tensor(out=MM0[:, 256:512], in0=bK[0:64, 0:256], in1=sl.DATn[:], op=ALU.mult),
                     reads=[bK, sl.DATn], writes=[MM0])
                S.op("vector", lambda e: e.tensor_tensor(out=sl.inT[:], in0=bK[0:64, 256:512], in1=sl.E3[:, 0:256], op=ALU.mult),
                     reads=[bK, sl.E3], writes=[sl.inT])
                S.op("gpsimd", lambda e: e.tensor_tensor(out=v3(sl.XT[:], 64), in0=v3(MM0[:, 256:512], 64),
                                                         in1=idb.unsqueeze(1).to_broadcast([64, 4, 64]), op=ALU.add),
                     reads=[MM0, g.ident_f], writes=[sl.XT])
            for lvl in range(1, 6):
                for c in chunks:
                    sl, tt, half = info[c]
                    Mp = sl.MM[(lvl - 1) % 2]
                    Mn = sl.MM[lvl % 2]
                    bM = next_bank(g, (0, 1, 2, 3))
                    for hd in range(4):
                        M_h = Mp[:, hd * 64:(hd + 1) * 64]
                        MT_h = Mp[:, 256 + hd * 64:256 + (hd + 1) * 64]
                        S.op("tensor", lambda e: e.matmul(out=bM[0:64, hd * 64:(hd + 1) * 64], lhsT=MT_h, rhs=M_h, start=(hd == 0), stop=True,
                                                          skip_group_check=True), reads=[Mp], writes=[bM])
                    nw = 256
                    if lvl < 5:
                        nw = 512
                        for hd in range(4):
                            M_h = Mp[:, hd * 64:(hd + 1) * 64]
                            MT_h = Mp[:, 256 + hd * 64:256 + (hd + 1) * 64]
                            S.op("tensor", lambda e: e.matmul(out=bM[0:64, 256 + hd * 64:256 + (hd + 1) * 64], lhsT=M_h, rhs=MT_h, start=False,
                                                              stop=True, skip_group_check=True), reads=[Mp], writes=[bM])
                    S.op("scalar", lambda e: e.copy(out=Mn[:, 0:nw], in_=bM[0:64, 0:nw]), reads=[bM], writes=[Mn])
                    bX = next_bank(g, (0, 1, 2, 3))
                    for hd in range(4):
                        S.op("tensor", lambda e: e.matmul(out=bX[0:64, hd * 64:(hd + 1) * 64], lhsT=Mn[:, hd * 64:(hd + 1) * 64],
                                                          rhs=sl.XT[:, hd * 64:(hd + 1) * 64], start=(hd == 0), stop=True,
                                                          skip_group_check=True), reads=[Mn, sl.XT], writes=[bX])
                    S.op("vector", lambda e: e.tensor_tensor(out=sl.XT[:], in0=bX[0:64, 0:256], in1=sl.XT[:], op=ALU.add),
                         reads=[bX, sl.XT], writes=[sl.XT])
            for c in chunks:
                sl, tt, half = info[c]
                bU = next_bank(g, (0, 1, 2, 3))
                for hd in range(4):
                    S.op("tensor", lambda e: e.matmul(out=bU[0:64, hd * 128:(hd + 1) * 128], lhsT=sl.XT[:, hd * 64:(hd + 1) * 64],
                                                      rhs=sl.vb[:, hd * 128:(hd + 1) * 128], start=(hd == 0), stop=True,
                                                      skip_group_check=True), reads=[sl.XT, sl.vb], writes=[bU])
                S.op("scalar", lambda e: e.copy(out=sl.u[:], in_=bU[0:64, 0:512]), reads=[bU], writes=[sl.u])
                bW = next_bank(g, (0, 1, 2, 3))
                for hd in range(4):
                    S.op("tensor", lambda e: e.matmul(out=bW[:, hd * 64:(hd + 1) * 64], lhsT=sl.kbg[:, hd * 128:(hd + 1) * 128],
                                                      rhs=sl.XT[:, hd * 64:(hd + 1) * 64], start=(hd == 0), stop=True,
                                                      skip_group_check=True), reads=[sl.XT, sl.kbg], writes=[bW])
                S.op("vector", lambda e: e.tensor_copy(out=sl.wT[:], in_=bW[:, 0:256]), reads=[bW], writes=[sl.wT])
            return info

        def rec(chunks, info):
            for c in chunks:
                sl, tt, half = info[c]
                rows = slice(c * 64, (c + 1) * 64)
                b1 = next_bank(g, (4, 5, 6, 7))
                for hd in range(4):
                    S.op("tensor", lambda e: e.matmul(out=b1[0:64, hd * 128:(hd + 1) * 128], lhsT=sl.wT[:, hd * 64:(hd + 1) * 64],
                                                      rhs=Sst[:, hd, :], start=(hd == 0), stop=True, skip_group_check=True),
                         reads=[sl.wT, Sst], writes=[b1])
                S.op("vector", lambda e: e.tensor_tensor(out=vnew[:], in0=sl.u[:], in1=b1[0:64, 0:512], op=ALU.subtract),
                     reads=[sl.u, b1], writes=[vnew])
                b2 = next_bank(g, (4, 5, 6, 7))
                for hd in range(4):
                    S.op("tensor", lambda e: e.matmul(out=b2[0:64, hd * 128:(hd + 1) * 128], lhsT=sl.kqT[:, 256 + hd * 64:256 + (hd + 1) * 64],
                                                      rhs=Sst[:, hd, :], start=(hd == 0), stop=True, skip_group_check=True),
                         reads=[sl.kqT, Sst], writes=[b2])
                b3 = next_bank(g, (4, 5, 6, 7))
                for hd in range(4):
                    S.op("tensor", lambda e: e.matmul(out=b3[0:64, hd * 128:(hd + 1) * 128], lhsT=sl.inT[:, hd * 64:(hd + 1) * 64],
                                                      rhs=vnew[:, hd * 128:(hd + 1) * 128], start=(hd == 0), stop=True, skip_group_check=True),
                         reads=[sl.inT, vnew], writes=[b3])
                b4 = next_bank(g, (4, 5, 6, 7))
                for hd in range(4):
                    S.op("tensor", lambda e: e.matmul(out=b4[:, hd * 128:(hd + 1) * 128], lhsT=sl.kdec[:, hd * 128:(hd + 1) * 128],
                                                      rhs=vnew[:, hd * 128:(hd + 1) * 128], start=(hd == 0), stop=True, skip_group_check=True),
                         reads=[sl.kdec, vnew], writes=[b4])
                for hd in range(4):
                    S.op("vector", lambda e: e.scalar_tensor_tensor(out=Sst[:, hd, :], in0=Sst[:, hd, :], scalar=elast[half][:, tt, hd:hd + 1],
                                                                    in1=b4[:, hd * 128:(hd + 1) * 128], op0=ALU.mult, op1=ALU.add),
                         reads=[Sst, elast[half], b4], writes=[Sst])
                S.op("vector", lambda e: e.tensor_tensor(out=v3(otmp[:], 128), in0=v3(b2[0:64, 0:512], 128),
                                                         in1=bc_h(egc[half][:, tt, :], 128), op=ALU.mult),
                     reads=[b2, egc[half]], writes=[otmp])
                S.op("vector", lambda e: e.tensor_tensor(out=otmp[:], in0=otmp[:], in1=b3[0:64, 0:512], op=ALU.add),
                     reads=[otmp, b3], writes=[otmp])
                S.op("scalar", lambda e: e.activation(out=osq[:], in_=otmp[:], func=AF.Square), reads=[otmp], writes=[osq])
                S.op("vector", lambda e: e.tensor_reduce(out=oss[:], in_=v3(osq[:], 128), axis=AX.X, op=ALU.add), reads=[osq], writes=[oss])
                S.op("scalar", lambda e: e.activation(out=osd[:], in_=oss[:], func=AF.Sqrt, bias=g.eps_t[0:64, 0:1], scale=1.0 / 128),
                     reads=[oss, g.eps_t], writes=[osd])
                S.op("vector", lambda e: e.reciprocal(out=ors[:], in_=osd[:]), reads=[osd], writes=[ors])
                S.op("vector", lambda e: e.tensor_tensor(out=v3(otmp[:], 128), in0=v3(otmp[:], 128), in1=bc_h(ors[:, :], 128), op=ALU.mult),
                     reads=[otmp, ors], writes=[otmp])
                S.op("gpsimd", lambda e: e.tensor_tensor(out=v3(otmp[:], 128), in0=v3(otmp[:], 128),
                                                         in1=gno[0:64, :].unsqueeze(1).to_broadcast([64, 4, 128]), op=ALU.mult),
                     reads=[otmp, gno], writes=[otmp])
                yo = yout[c % 2]
                S.op("vector", lambda e: e.tensor_tensor(out=yo[:], in0=otmp[:], in1=sl.dz[:], op=ALU.mult),
                     reads=[otmp, sl.dz], writes=[yo])
                S.dma("gpsimd", g.ydn_s[rows], yo[:], reads=[yo], writes=[g.ydn_s])

        groups = [list(range(c0, min(NCH, c0 + GS))) for c0 in range(0, NCH, GS)]
        S.record()
        inf = par(groups[0])
        S.replay(S.stop())
        for gi_, grp in enumerate(groups):
            S.record()
            rec(grp, inf)
            rr = S.stop()
            rp = None
            if gi_ + 1 < len(groups):
                S.record()
                inf = par(groups[gi_ + 1])
                rp = S.stop()
            S.replay(rr, rp)
        S.barrier()


def phase_p_ops(g, ph):
    nc, S = g.nc, g.S
    S.record()
    if getattr(g, "p_dmacast", 1):
        for ti, src in enumerate((g.peer_u, g.peer_v)):
            dst = g.uv_s
            sv = src.ap().rearrange("(b p j) d -> b p j d", p=128, j=4)
            dv = dst.t.rearrange("(b p j) d -> b p j d", p=128, j=4)
            for b in range(getattr(g, "npb", 32)):
                S.dma("gpsimd", dv[b][:, :, ti * D:(ti + 1) * D], sv[b], writes=[dst])
        return S.stop()
    stg = [sb(g, ph, "pstg%d" % i, [128, 4096], F32) for i in range(2)]
    cst = [sb(g, ph, "pcst%d" % i, [128, 4096], BF16) for i in range(2)]
    k = 0
    for ti, src in enumerate((g.peer_u, g.peer_v)):
        dst = g.uv_s
        sv = src.ap().rearrange("(b p j) d -> b p (j d)", p=128, j=4)
        dv = dst.t.rearrange("(b p j) d -> b p j d", p=128, j=4)
        for b in range(getattr(g, "npb", 32)):
            s_, c_ = stg[k % 2], cst[k % 2]
            pq = "gpsimd" if getattr(g, "p_in_b", False) else "sync"
            S.dma(pq, s_[:], sv[b], writes=[s_])
            S.op("gpsimd", lambda e: e.tensor_copy(out=c_[:], in_=s_[:]), reads=[s_], writes=[c_])
            S.dma(pq, dv[b][:, :, ti * D:(ti + 1) * D], c_[:].rearrange("p (j d) -> p j d", j=4), reads=[c_], writes=[dst])
            k += 1
    return S.stop()


def phase_p(g):
    nc, S = g.nc, g.S
    with contextlib.ExitStack() as ph:
        S.replay(phase_p_ops(g, ph))
        S.barrier()


def phase_de(g):
    nc, S = g.nc, g.S
    g.nrot = 8
    with contextlib.ExitStack() as ph:
        def A(name, shape, dt=F32):
            return sb(g, ph, name, shape, dt)

        wA = A("wA", [128, 4, D], BF16)
        wB = A("wB", [128, 4, D], BF16)
        wo = A("wo", [128, 8, D], BF16)
        wq = A("wq", [128, 8, 2048], BF16)
        cand = A("cand", [128, 8, 256])
        candf = cand[:].rearrange("p a b -> p (a b)")

        class _V:
            def __init__(self, ap, tl):
                self.ap, self.r = ap, tl.r

            def __getitem__(self, idx):
                return self.ap[idx]
        wstg = [_V(candf[:, i * 512:(i + 1) * 512], cand) for i in range(2)]
        k = 0
        for (src, dstt, nk, ncol) in ((g.w_att_branch, wA, 4, D), (g.w_dn_branch, wB, 4, D), (g.w_o, wo, 8, D),
                                      (g.peer_w_query, wq, 8, 2048)):
            sv = src.ap().rearrange("(kc p) n -> p kc n", p=128)
            for kc in range(nk):
                for c0 in range(0, ncol, 512):
                    s_ = wstg[k % 2]
                    S.dma("sync", s_[:], sv[:, kc, c0:c0 + 512], writes=[s_])
                    eng = ("vector", "gpsimd", "scalar")[k % 3]
                    dst = dstt[:, kc, c0:c0 + 512]
                    if eng == "scalar":
                        S.op(eng, lambda e: e.copy(out=dst, in_=s_[:]), reads=[s_], writes=[dstt])
                    else:
                        S.op(eng, lambda e: e.tensor_copy(out=dst, in_=s_[:]), reads=[s_], writes=[dstt])
                    k += 1
        g2_bc = A("g2_bc", [128, D])
        S.dma("sync", g2_bc[:], g.norm2_gain.ap().partition_broadcast(128), writes=[g2_bc])
        skT = A("skT", [128, 16, 128])
        for hp in range(16):
            s_ = wstg[hp % 2]
            S.dma("sync", s_[:, 0:128], g.peer_sub_keys.ap()[hp], writes=[s_])
            b = next_bank(g)
            S.op("tensor", lambda e: e.transpose(out=b[:, 0:128], in_=s_[:, 0:128], identity=g.ident_f[:]),
                 reads=[s_, g.ident_f], writes=[b])
            S.op("vector", lambda e: e.tensor_copy(out=skT[:, hp, :], in_=b[:, 0:128]), reads=[b], writes=[skT])
        iota_i = A("iota_i", [128, 16], I32)
        iota_f = A("iota_f", [128, 16])
        S.op("gpsimd", lambda e: e.iota(out=iota_i[:], pattern=[[1, 16]], base=0, channel_multiplier=0), writes=[iota_i])
        S.op("vector", lambda e: e.tensor_copy(out=iota_f[:], in_=iota_i[:]), reads=[iota_i], writes=[iota_f])

        ya = A("ya", [128, 512], BF16); yd = A("yd", [128, 512], BF16)
        gt = A("gt", [128, 2048], BF16)
        xt = A("xt", [128, D])
        yT = A("yT", [128, 8, 128], BF16)
        mg = A("mg", [128, D], BF16)
        mgT = A("mgT", [128, 8, 128], BF16)
        x1s = [A("x1_%d" % i, [128, D]) for i in range(2)]
        junk = A("junkd", [128, D], BF16)
        ss1 = A("ss1", [128, 1]); sd1 = A("sd1", [128, 1]); rs1 = A("rs1", [128, 1])
        h2s = [A("h2_%d" % i, [128, D], BF16) for i in range(2)]
        class _Alias:
            def __init__(self, tl, name):
                self.t, self.r = tl.t, Res(name)

            def __getitem__(self, idx):
                return self.t[idx]
        junk2 = _Alias(junk, "junk2")
        h2T = A("h2T", [128, 8, 128], BF16)
        qTp = A("qTp", [128, 16, 128])
        s_sb = A("s_sb", [128, 16, 128])
        s2 = A("s2", [128, 256])
        m16 = A("m16", [128, 16, 16])
        i16 = A("i16", [128, 16, 16], U32)
        best = A("best", [128, 8, 16])
        pos = A("pos", [128, 8, 16], U32)
        au = A("au", [128, 128], U32); bu = A("bu", [128, 128], U32)
        af = A("af", [128, 128]); bf = A("bf", [128, 128])
        i16f = A("i16f", [128, 16, 16])
        oh = A("oh", [128, 128, 16])
        ohf = oh[:].rearrange("p a b -> p (a b)")
        e0 = A("e0", [128, 128]); e1 = A("e1", [128, 128])
        eidxs = [A("eidx%d" % i, [128, 128], U32) for i in range(2)]
        gd = A("gd", [128, 8, 16]); gsum = A("gsum", [128, 8]); grec = A("grec", [128, 8])
        gates = [A("gate%d" % i, [128, 128]) for i in range(2)]
        act = A("act", [128, 128]); coef = A("coef", [128, 128])
        NBc = getattr(g, "nbc", 11)
        uv = [A("uv%d" % i, [128, 2 * D], BF16) for i in range(NBc)]
        prod = [A("prod%d" % i, [128, D], BF16) for i in range(2)]
        dgs = [A("dgs%d" % i, [128, 128], BF16) for i in range(4)]
        act1 = [A("act1_%d" % i, [128, 1]) for i in range(8)]
        ag1 = [A("ag1_%d" % i, [128, 1]) for i in range(8)]
        acc = A("acc", [128, D])
        g.nrot = 6
        pacc = g.banks[6:8]
        print("phase DE sbuf remaining", nc.sbuf_bytes_remaining)
        x_v = g.x.ap().rearrange("(n p) d -> n p d", p=128)
        o_v = g.out.ap().rearrange("(n p) d -> n p d", p=128)
        NTD = getattr(g, "ntd", NT)
        def stage_x(tt):
            rows = slice(tt * 128, (tt + 1) * 128)
            x1, h2, eidx, gate = x1s[tt % 2], h2s[tt % 2], eidxs[tt % 2], gates[tt % 2]
            S.dma("sync", ya[:], g.yatt_s[rows], reads=[g.yatt_s], writes=[ya])
            S.dma("sync", yd[:], g.ydn_s[rows], reads=[g.ydn_s], writes=[yd])
            S.dma("sync", gt[:], g.gate_s[rows], reads=[g.gate_s], writes=[gt])
            S.dma("sync", xt[:], x_v[tt], writes=[xt])
            b = next_bank(g)
            bv = b[:, :].bitcast(BF16)
            for j in range(4):
                S.op("tensor", lambda e: e.transpose(out=bv[:, j * 128:(j + 1) * 128], in_=ya[:, j * 128:(j + 1) * 128], identity=g.ident_b[:]),
                     reads=[ya, g.ident_b], writes=[b])
            for j in range(4):
                S.op("tensor", lambda e: e.transpose(out=bv[:, (4 + j) * 128:(5 + j) * 128], in_=yd[:, j * 128:(j + 1) * 128], identity=g.ident_b[:]),
                     reads=[yd, g.ident_b], writes=[b])
            S.op("scalar", lambda e: e.copy(out=yT[:].rearrange("p k t -> p (k t)"), in_=bv[:, 0:1024]), reads=[b], writes=[yT])
            for hf in range(2):
                cs = slice(hf * 512, (hf + 1) * 512)
                bA = next_bank(g)
                for kc in range(4):
                    S.op("tensor", lambda e: e.matmul(out=bA[:, 0:512], lhsT=yT[:, kc, :], rhs=wA[:, kc, cs], start=(kc == 0), stop=(kc == 3)),
                         reads=[yT, wA], writes=[bA])
                bB = next_bank(g)
                for kc in range(4):
                    S.op("tensor", lambda e: e.matmul(out=bB[:, 0:512], lhsT=yT[:, 4 + kc, :], rhs=wB[:, kc, cs], start=(kc == 0), stop=(kc == 3)),
                         reads=[yT, wB], writes=[bB])
                S.op("vector", lambda e: e.tensor_tensor(out=ohf[:, cs], in0=bA[:, 0:512], in1=gt[:, cs], op=ALU.mult),
                     reads=[bA, gt], writes=[oh])
                S.op("vector", lambda e: e.tensor_tensor(out=ohf[:, 1024 + hf * 512:1024 + (hf + 1) * 512], in0=bB[:, 0:512], in1=gt[:, 1024 + hf * 512:1024 + (hf + 1) * 512], op=ALU.mult),
                     reads=[bB, gt], writes=[oh])
            S.op("vector", lambda e: e.tensor_tensor(out=mg[:], in0=ohf[:, 0:1024], in1=ohf[:, 1024:2048], op=ALU.add), reads=[oh], writes=[mg])
            for half in range(2):
                b = next_bank(g)
                bv = b[:, :].bitcast(BF16)
                for j in range(4):
                    kc = half * 4 + j
                    S.op("tensor", lambda e: e.transpose(out=bv[:, j * 128:(j + 1) * 128], in_=mg[:, kc * 128:(kc + 1) * 128], identity=g.ident_b[:]),
                         reads=[mg, g.ident_b], writes=[b])
                S.op("scalar", lambda e: e.copy(out=mgT[:, half * 4:half * 4 + 4, :].rearrange("p k t -> p (k t)"), in_=bv[:, 0:512]),
                     reads=[b], writes=[mgT])
            for hf in range(2):
                cs = slice(hf * 512, (hf + 1) * 512)
                b = next_bank(g)
                for kc in range(8):
                    S.op("tensor", lambda e: e.matmul(out=b[:, 0:512], lhsT=mgT[:, kc, :], rhs=wo[:, kc, cs], start=(kc == 0), stop=(kc == 7)),
                         reads=[mgT, wo], writes=[b])
                S.op("vector", lambda e: e.tensor_tensor(out=x1[:, cs], in0=b[:, 0:512], in1=xt[:, cs], op=ALU.add),
                     reads=[b, xt], writes=[x1])
            S.op("scalar", lambda e: e.activation(out=junk[:], in_=x1[:], func=AF.Square, accum_out=ss1[:, 0:1]),
                 reads=[x1], writes=[junk, ss1])
            rstd_op(g, rs1, ss1, 1.0 / D, 1, sd1)
            S.op("vector", lambda e: e.scalar_tensor_tensor(out=h2[:], in0=x1[:], scalar=rs1[:, 0:1], in1=g2_bc[:], op0=ALU.mult, op1=ALU.mult),
                 reads=[x1, rs1, g2_bc], writes=[h2])
            for half in range(2):
                b = next_bank(g)
                bv = b[:, :].bitcast(BF16)
                for j in range(4):
                    kc = half * 4 + j
                    S.op("tensor", lambda e: e.transpose(out=bv[:, j * 128:(j + 1) * 128], in_=h2[:, kc * 128:(kc + 1) * 128], identity=g.ident_b[:]),
                         reads=[h2, g.ident_b], writes=[b])
                S.op("scalar", lambda e: e.copy(out=h2T[:, half * 4:half * 4 + 4, :].rearrange("p k t -> p (k t)"), in_=bv[:, 0:512]),
                     reads=[b], writes=[h2T])
            for q4 in range(4):
                b = next_bank(g)
                for j in range(4):
                    hp = q4 * 4 + j
                    for kc in range(8):
                        S.op("tensor", lambda e: e.matmul(out=b[:, j * 128:(j + 1) * 128], lhsT=wq[:, kc, hp * 128:(hp + 1) * 128],
                                                          rhs=h2T[:, kc, :], start=(j == 0 and kc == 0), stop=(kc == 7), skip_group_check=True),
                             reads=[wq, h2T], writes=[b])
                dst = qTp[:, q4 * 4:q4 * 4 + 4, :].rearrange("p a t -> p (a t)")
                if q4 % 2 == 0:
                    S.op("scalar", lambda e: e.copy(out=dst, in_=b[:, 0:512]), reads=[b], writes=[qTp])
                else:
                    S.op("vector", lambda e: e.tensor_copy(out=dst, in_=b[:, 0:512]), reads=[b], writes=[qTp])
            for q4 in range(4):
                b = next_bank(g)
                for j in range(4):
                    hp = q4 * 4 + j
                    S.op("tensor", lambda e: e.matmul(out=b[:, j * 128:(j + 1) * 128], lhsT=qTp[:, hp, :], rhs=skT[:, hp, :],
                                                      start=(j == 0), stop=True, skip_group_check=True),
                         reads=[qTp, skT], writes=[b])
                S.op("scalar", lambda e: e.copy(out=s_sb[:, q4 * 4:q4 * 4 + 4, :].rearrange("p a t -> p (a t)"), in_=b[:, 0:512]),
                     reads=[b], writes=[s_sb])
            for hp in range(16):
                sv = s_sb[:, hp, :]
                S.op("vector", lambda e: e.max(out=m16[:, hp, 0:8], in_=sv), reads=[s_sb], writes=[m16])
                S.op("vector", lambda e: e.max_index(out=i16[:, hp, 0:8], in_max=m16[:, hp, 0:8], in_values=sv), reads=[s_sb, m16], writes=[i16])
                S.op("vector", lambda e: e.match_replace(out=s2[:, 0:128], in_to_replace=m16[:, hp, 0:8], in_values=sv, imm_value=-1e30),
                     reads=[s_sb, m16], writes=[s2])
                S.op("vector", lambda e: e.max(out=m16[:, hp, 8:16], in_=s2[:, 0:128]), reads=[s2], writes=[m16])
                S.op("vector", lambda e: e.max_index(out=i16[:, hp, 8:16], in_max=m16[:, hp, 8:16], in_values=s2[:, 0:128]),
                     reads=[s2, m16], writes=[i16])
            m16v = m16[:].rearrange("p (h two) a -> p h two a", two=2)
            S.op("vector", lambda e: e.tensor_tensor(out=cand[:].rearrange("p h (a b) -> p h a b", b=16),
                                                     in0=m16v[:, :, 0, :].unsqueeze(3).to_broadcast([128, 8, 16, 16]),
                                                     in1=m16v[:, :, 1, :].unsqueeze(2).to_broadcast([128, 8, 16, 16]), op=ALU.add),
                 reads=[m16], writes=[cand])
            for h in range(8):
                cv = cand[:, h, :]
                S.op("vector", lambda e: e.max(out=best[:, h, 0:8], in_=cv), reads=[cand], writes=[best])
                S.op("vector", lambda e: e.max_index(out=pos[:, h, 0:8], in_max=best[:, h, 0:8], in_values=cv), reads=[cand, best], writes=[pos])
                S.op("vector", lambda e: e.match_replace(out=s2[:], in_to_replace=best[:, h, 0:8], in_values=cv, imm_value=-1e30),
                     reads=[cand, best], writes=[s2])
                S.op("vector", lambda e: e.max(out=best[:, h, 8:16], in_=s2[:]), reads=[s2], writes=[best])
                S.op("vector", lambda e: e.max_index(out=pos[:, h, 8:16], in_max=best[:, h, 8:16], in_values=s2[:]), reads=[s2, best], writes=[pos])
            posf = pos[:].rearrange("p h k -> p (h k)")
            S.op("vector", lambda e: e.tensor_scalar(out=au[:], in0=posf, scalar1=4, scalar2=None, op0=ALU.logical_shift_right), reads=[pos], writes=[au])
            S.op("vector", lambda e: e.tensor_scalar(out=bu[:], in0=posf, scalar1=15, scalar2=None, op0=ALU.bitwise_and), reads=[pos], writes=[bu])
            S.op("vector", lambda e: e.tensor_copy(out=af[:], in_=au[:]), reads=[au], writes=[af])
            S.op("vector", lambda e: e.tensor_copy(out=bf[:], in_=bu[:]), reads=[bu], writes=[bf])
            S.op("vector", lambda e: e.tensor_copy(out=i16f[:], in_=i16[:]), reads=[i16], writes=[i16f])
            i16fv = i16f[:].rearrange("p (h two) a -> p h two a", two=2)
            for which, (sel, dst) in enumerate(((af, e0), (bf, e1))):
                S.op("vector", lambda e: e.tensor_tensor(out=oh[:], in0=sel[:, :].unsqueeze(2).to_broadcast([128, 128, 16]),
                                                         in1=iota_f[:, :].unsqueeze(1).to_broadcast([128, 128, 16]), op=ALU.is_equal),
                     reads=[sel, iota_f], writes=[oh])
                S.op("vector", lambda e: e.tensor_tensor(out=oh[:].rearrange("p (h k) a -> p h k a", k=16),
                                                         in0=oh[:].rearrange("p (h k) a -> p h k a", k=16),
                                                         in1=i16fv[:, :, which, :].unsqueeze(2).to_broadcast([128, 8, 16, 16]), op=ALU.mult),
                     reads=[oh, i16f], writes=[oh])
                S.op("vector", lambda e: e.tensor_reduce(out=dst[:], in_=oh[:], axis=AX.X, op=ALU.add), reads=[oh], writes=[dst])
            S.op("vector", lambda e: e.scalar_tensor_tensor(out=e0[:], in0=e0[:], scalar=128.0, in1=e1[:], op0=ALU.mult, op1=ALU.add),
                 reads=[e0, e1], writes=[e0])
            S.op("vector", lambda e: e.tensor_copy(out=eidx[:], in_=e0[:]), reads=[e0], writes=[eidx])
            S.op("vector", lambda e: e.tensor_tensor(out=gd[:], in0=best[:], in1=best[:, :, 0:1].to_broadcast([128, 8, 16]), op=ALU.subtract),
                 reads=[best], writes=[gd])
            S.op("scalar", lambda e: e.activation(out=gd[:], in_=gd[:], func=AF.Exp), reads=[gd], writes=[gd])
            S.op("vector", lambda e: e.tensor_reduce(out=gsum[:], in_=gd[:], axis=AX.X, op=ALU.add), reads=[gd], writes=[gsum])
            S.op("vector", lambda e: e.reciprocal(out=grec[:], in_=gsum[:]), reads=[gsum], writes=[grec])
            S.op("vector", lambda e: e.tensor_tensor(out=gate[:].rearrange("p (h k) -> p h k", k=16), in0=gd[:],
                                                     in1=grec[:, :].unsqueeze(2).to_broadcast([128, 8, 16]), op=ALU.mult),
                 reads=[gd, grec], writes=[gate])

        def stage_y(tt):
            x1, h2, eidx, gate = x1s[tt % 2], h2s[tt % 2], eidxs[tt % 2], gates[tt % 2]
            NSL = getattr(g, "ngrp", 16) * 8
            LAG = getattr(g, "lag", 4)

            def front(j):
                ub = uv[j % NBc]
                S.dma("gpsimd", ub[:], g.uv_s[:, :], reads=[eidx, g.uv_s], writes=[ub],
                      indirect=bass.IndirectOffsetOnAxis(ap=eidx[:, j:j + 1], axis=0))
                pr = prod[j % 2]
                a1 = act1[j % 8]
                a2 = ag1[j % 8]
                S.op("vector", lambda e: e.tensor_tensor(out=pr[:], in0=ub[:, 0:D], in1=h2[:], op=ALU.mult), reads=[ub, h2], writes=[pr])
                S.op("scalar", lambda e: e.activation(out=junk2[:], in_=pr[:], func=AF.Identity, accum_out=a1[:, 0:1]),
                     reads=[pr], writes=[junk2, a1])
                S.op("scalar", lambda e: e.activation(out=a2[:], in_=a1[:], func=AF.Gelu), reads=[a1], writes=[a2])

            def back(j):
                ub = uv[j % NBc]
                a2 = ag1[j % 8]
                dg = dgs[j % 4]
                S.op("vector", lambda e: e.scalar_tensor_tensor(out=dg[:], in0=g.ident_b[:], scalar=a2[:, 0:1],
                                                                in1=gate[:, j:j + 1].to_broadcast([128, 128]), op0=ALU.mult, op1=ALU.mult),
                     reads=[g.ident_b, a2, gate], writes=[dg])
                for hf in range(2):
                    S.op("tensor", lambda e: e.matmul(out=pacc[hf][:, 0:512], lhsT=dg[:], rhs=ub[:, D + hf * 512:D + (hf + 1) * 512],
                                                      start=(j == 0), stop=(j == NSL - 1)),
                         reads=[dg, ub], writes=[pacc[hf]])

            for j in range(NSL + LAG):
                if j < NSL:
                    front(j)
                if j >= LAG:
                    back(j - LAG)
            for hf in range(2):
                cs = slice(hf * 512, (hf + 1) * 512)
                S.op("vector", lambda e: e.tensor_tensor(out=acc[:, cs], in0=pacc[hf][:, 0:512], in1=x1[:, cs], op=ALU.add),
                     reads=[pacc[hf], x1], writes=[acc])
            S.dma("sync", o_v[tt], acc[:], reads=[acc])

        S.record()
        stage_x(0)
        S.replay(S.stop())
        for tt in range(NTD):
            S.record()
            stage_y(tt)
            ry = S.stop()
            rx = None
            if tt + 1 < NTD:
                S.record()
                stage_x(tt + 1)
                rx = S.stop()
            S.replay(ry, rx)
        S.barrier()


_CACHE = {}


def kernel(**inputs):
    x = np.asarray(inputs["x"], dtype=np.float32)
    if "nc" not in _CACHE:
        _CACHE["nc"] = build_program()
    nc = _CACHE["nc"]
    shared = {}
    for k in ("norm1_gain", "w_in", "q_norm_gain", "k_norm_gain", "dn_conv_w", "dn_a_log", "dn_dt_bias",
              "dn_out_norm_gain", "w_att_branch", "w_dn_branch", "w_o", "norm2_gain", "peer_w_query",
              "peer_u", "peer_v"):
        a = np.asarray(inputs[k], dtype=np.float32)[0]
        if a.ndim == 1:
            a = a.reshape(1, -1)
        shared[k] = np.ascontiguousarray(a)
    shared["peer_sub_keys"] = np.ascontiguousarray(
        np.asarray(inputs["peer_sub_keys"], dtype=np.float32)[0].reshape(16, 128, 128))
    in_maps = []
    for c in range(N_CORES):
        m = dict(shared)
        m["x"] = np.ascontiguousarray(x[c])
        in_maps.append(m)
    res = run_bass_kernel_spmd(nc, in_maps, core_ids=list(range(N_CORES)))
    out = np.stack([np.asarray(r["out"]) for r in res.results], axis=0)
    return out.astype(np.float32)
```
